# Optimizing a Trainium2 kernel written in Bass

```python
import math
import jax
import jax.numpy as jnp
from jax import lax
import numpy as np

D_MODEL = 1024
BATCH = 4
SEQ = 4096
DEPTH = 2

RWKV_HEADS = 8
RWKV_HEAD_DIM = 64
RWKV_WIDTH = RWKV_HEADS * RWKV_HEAD_DIM
DECAY_LORA = 64
ICLR_LORA = 64
VRES_LORA = 32
GATE_LORA = 128
RWKV_GN_EPS = 64e-5
MLA_HEADS = 8
QK_NOPE_DIM = 64
QK_ROPE_DIM = 32
MLA_QK_DIM = QK_NOPE_DIM + QK_ROPE_DIM
MLA_V_DIM = 64
MLA_WIDTH = MLA_HEADS * MLA_V_DIM
Q_LORA_RANK = 256
KV_LORA_RANK = 128
ROPE_THETA = 10000.0
Q_BLOCK = 128
GDN_HEADS = 8
GDN_K_DIM = 64
GDN_V_DIM = 64
GDN_WIDTH = GDN_HEADS * GDN_V_DIM
GDN_QKV = 2 * GDN_HEADS * GDN_K_DIM + GDN_HEADS * GDN_V_DIM
GDN_CONV = 4
GDN_CHUNK = 64
N_BRANCH = 3
FF_DENSE = 2816
N_EXPERTS = 8
TOP_K = 2
FF_EXPERT = 3584
MOE_BLOCK = 128
PLE_DIM = 256
NORM_EPS = 1e-6

RWKV_IN_SIZES = (RWKV_WIDTH, RWKV_WIDTH, RWKV_WIDTH, DECAY_LORA, ICLR_LORA, GATE_LORA)
MLA_IN_SIZES = (Q_LORA_RANK, KV_LORA_RANK, QK_ROPE_DIM)
GDN_IN_SIZES = (GDN_QKV, GDN_HEADS, GDN_HEADS, GDN_WIDTH)
RWKV_IN = sum(RWKV_IN_SIZES)
MLA_IN = sum(MLA_IN_SIZES)
GDN_IN = sum(GDN_IN_SIZES)
GATE_IN = N_BRANCH * D_MODEL
N_IN = RWKV_IN + MLA_IN + GDN_IN + GATE_IN

kernel_name = 'hybrid_rwkv7_mla_gdn_moe_block'


def rms_norm(x, g, eps=NORM_EPS):
    xf = x.astype(jnp.float32)
    y = xf * lax.rsqrt(jnp.mean(jnp.square(xf), axis=-1, keepdims=True) + eps)
    return (y * g.astype(jnp.float32)).astype(x.dtype)


def l2_normalize(x, eps=1e-12):
    xf = x.astype(jnp.float32)
    return xf * lax.rsqrt(jnp.sum(jnp.square(xf), axis=-1, keepdims=True) + eps)


def split_cols(z, sizes):
    return jnp.split(z, [int(c) for c in np.cumsum(sizes)[:-1]], axis=-1)


def heads(t, n):
    return t.reshape(t.shape[:-1] + (n, t.shape[-1] // n))


def token_shift(z):
    return jnp.pad(z, ((0, 0), (1, 0), (0, 0)))[:, :-1]


def rope_tables(positions):
    inv_freq = 1.0 / (ROPE_THETA ** (jnp.arange(0, QK_ROPE_DIM, 2, dtype=jnp.float32) / QK_ROPE_DIM))
    ang = positions.astype(jnp.float32)[..., None] * inv_freq
    return jnp.cos(ang)[:, :, None, :], jnp.sin(ang)[:, :, None, :]


def apply_rope(x, cos, sin):
    x1, x2 = jnp.split(x.astype(jnp.float32), 2, axis=-1)
    return jnp.concatenate([x1 * cos - x2 * sin, x2 * cos + x1 * sin], axis=-1).astype(x.dtype)


def rwkv7_recurrence(r, w, k, v, a, b):
    bsz, _, h, n = r.shape

    def step(state, xs):
        r_t, w_t, k_t, v_t, a_t, b_t = xs
        sa = jnp.einsum('bhij,bhj->bhi', state, a_t)
        state = (state * w_t[:, :, None, :] + sa[..., None] * b_t[:, :, None, :]
                 + v_t[..., None] * k_t[:, :, None, :])
        return state, jnp.einsum('bhij,bhj->bhi', state, r_t)

    xs = tuple(jnp.moveaxis(t, 1, 0) for t in (r, w, k, v, a, b))
    _, y = lax.scan(step, jnp.zeros((bsz, h, n, n), jnp.float32), xs)
    return jnp.moveaxis(y, 0, 1)


def rwkv7_branch(z, u, v_first, vres, mu, w0, w_up, a0, a_up, g_up, k_k, k_a, r_k, ln_g, ln_b):
    b, s, _ = z.shape
    z = z + (token_shift(z) - z) * mu
    r, k, v, w_lo, a_lo, g_lo = split_cols(z, RWKV_IN_SIZES)
    log_w = -jax.nn.softplus(-(w0 + jnp.tanh(w_lo) @ w_up)) - 0.5
    decay = jnp.exp(-jnp.exp(log_w.astype(jnp.float32)))
    iclr = jax.nn.sigmoid(a0 + a_lo @ a_up)
    gate = jax.nn.sigmoid(g_lo) @ g_up
    if vres is None:
        v_first = v
    else:
        v_mu, v_down, v_up, v_bias = vres
        xv = u + (token_shift(u) - u) * v_mu
        v = v + (v_first - v) * jax.nn.sigmoid(v_bias + (xv @ v_down) @ v_up)
    kk = l2_normalize(heads(k * k_k, RWKV_HEADS))
    k = k * (1.0 + (iclr - 1.0) * k_a)
    rh, kh, vh, ah, wh = (heads(t, RWKV_HEADS).astype(jnp.float32) for t in (r, k, v, iclr, decay))
    y = rwkv7_recurrence(rh, wh, kh, vh, -kk, kk * ah)
    mean = jnp.mean(y, axis=-1, keepdims=True)
    var = jnp.mean(jnp.square(y - mean), axis=-1, keepdims=True)
    y = ((y - mean) * lax.rsqrt(var + RWKV_GN_EPS)).reshape(b, s, RWKV_WIDTH) * ln_g + ln_b
    bonus = jnp.sum(rh * kh * r_k, axis=-1, keepdims=True) * vh
    y = y + bonus.reshape(b, s, RWKV_WIDTH)
    return (y * gate).astype(z.dtype), v_first


def causal_block_attention(q, k, v, scale):
    seq = q.shape[1]
    outs = []
    for start in range(0, seq, Q_BLOCK):
        stop = start + Q_BLOCK
        s = jnp.einsum('bqhd,bkhd->bhqk', q[:, start:stop], k[:, :stop]).astype(jnp.float32) * scale
        mask = (start + jnp.arange(Q_BLOCK))[:, None] >= jnp.arange(stop)[None, :]
        prob = jax.nn.softmax(jnp.where(mask, s, -jnp.inf), axis=-1)
        outs.append(jnp.einsum('bhqk,bkhd->bqhd', prob.astype(v.dtype), v[:, :stop]))
    return jnp.concatenate(outs, axis=1)


def mla_branch(z, cos, sin, q_norm_g, kv_norm_g, w_uq, w_ukv, qk_g_q, qk_g_k):
    b, s, _ = z.shape
    c_q, c_kv, k_pe = split_cols(z, MLA_IN_SIZES)
    q = heads(rms_norm(c_q, q_norm_g) @ w_uq, MLA_HEADS)
    kv = heads(rms_norm(c_kv, kv_norm_g) @ w_ukv, MLA_HEADS)
    k_nope, v = kv[..., :QK_NOPE_DIM], kv[..., QK_NOPE_DIM:]
    k_pe = jnp.broadcast_to(k_pe[:, :, None, :], k_nope.shape[:-1] + (QK_ROPE_DIM,))
    k = jnp.concatenate([k_nope, k_pe], axis=-1)
    q = rms_norm(q, qk_g_q)
    k = rms_norm(k, qk_g_k)
    q = jnp.concatenate([q[..., :QK_NOPE_DIM], apply_rope(q[..., QK_NOPE_DIM:], cos, sin)], axis=-1)
    k = jnp.concatenate([k[..., :QK_NOPE_DIM], apply_rope(k[..., QK_NOPE_DIM:], cos, sin)], axis=-1)
    o = causal_block_attention(q, k, v, MLA_QK_DIM ** -0.5)
    return o.reshape(b, s, MLA_WIDTH)


def causal_depthwise_conv(x, w):
    xp = jnp.pad(x, ((0, 0), (GDN_CONV - 1, 0), (0, 0)))
    return lax.conv_general_dilated(xp, w[:, None, :].astype(x.dtype), (1,), 'VALID',
                                    dimension_numbers=('NWC', 'WIO', 'NWC'),
                                    feature_group_count=x.shape[-1])


def chunk_gated_delta_rule(q, k, v, g, beta):
    b, s, h, dk = q.shape
    dv = v.shape[-1]
    c = GDN_CHUNK
    n = s // c

    def chunks(t):
        return jnp.moveaxis(t.reshape((b, n, c, h) + t.shape[3:]), 3, 2)

    q, k, v, g, beta = (chunks(t) for t in (q, k, v, g, beta))
    gc = jnp.cumsum(g, axis=-1)
    causal = jnp.tril(jnp.ones((c, c), dtype=bool))
    strict = jnp.tril(jnp.ones((c, c), dtype=bool), -1)
    gamma = jnp.exp(jnp.where(causal, gc[..., :, None] - gc[..., None, :], -jnp.inf))
    kb = k * beta[..., None]
    m = jnp.where(strict, jnp.einsum('bnhid,bnhjd->bnhij', kb, k) * gamma, 0.0) + jnp.eye(c, dtype=jnp.float32)
    rhs = jnp.concatenate([v * beta[..., None], kb * jnp.exp(gc)[..., None]], axis=-1)
    sol = lax.linalg.triangular_solve(m, rhs, left_side=True, lower=True, unit_diagonal=True)
    u_c, w_c = sol[..., :dv], sol[..., dv:]
    a_in = jnp.einsum('bnhid,bnhjd->bnhij', q, k) * gamma
    q_dec = q * jnp.exp(gc)[..., None]
    k_dec = k * jnp.exp(gc[..., -1:] - gc)[..., None]
    chunk_dec = jnp.exp(gc[..., -1])

    def step(state, xs):
        u_i, w_i, a_i, qd_i, kd_i, d_i = xs
        v_new = u_i - jnp.einsum('bhck,bhkv->bhcv', w_i, state)
        o_i = jnp.einsum('bhck,bhkv->bhcv', qd_i, state) + jnp.einsum('bhcj,bhjv->bhcv', a_i, v_new)
        state = state * d_i[..., None, None] + jnp.einsum('bhck,bhcv->bhkv', kd_i, v_new)
        return state, o_i

    xs = tuple(jnp.moveaxis(t, 1, 0) for t in (u_c, w_c, a_in, q_dec, k_dec, chunk_dec))
    _, o = lax.scan(step, jnp.zeros((b, h, dk, dv), jnp.float32), xs)
    return jnp.moveaxis(o, 0, 1).swapaxes(2, 3).reshape(b, s, h, dv)


def gdn_branch(z, conv_w, a_log, dt_bias, norm_g):
    b, s, _ = z.shape
    qkv, b_logit, a_logit, gate = split_cols(z, GDN_IN_SIZES)
    qkv = jax.nn.silu(causal_depthwise_conv(qkv, conv_w))
    q, k, v = split_cols(qkv, (GDN_HEADS * GDN_K_DIM, GDN_HEADS * GDN_K_DIM, GDN_HEADS * GDN_V_DIM))
    q = l2_normalize(heads(q, GDN_HEADS)) * (GDN_K_DIM ** -0.5)
    k = l2_normalize(heads(k, GDN_HEADS))
    v = heads(v, GDN_HEADS).astype(jnp.float32)
    beta = jax.nn.sigmoid(b_logit.astype(jnp.float32))
    g = -jnp.exp(a_log.astype(jnp.float32)) * jax.nn.softplus(a_logit.astype(jnp.float32) + dt_bias)
    o = chunk_gated_delta_rule(q, k, v, g, beta)
    o = rms_norm(o, norm_g) * jax.nn.silu(heads(gate, GDN_HEADS).astype(jnp.float32))
    return o.reshape(b, s, GDN_WIDTH).astype(z.dtype)


def swiglu(x, w_gate, w_up, w_down):
    return (jax.nn.silu(x @ w_gate) * (x @ w_up)) @ w_down


def moe_swiglu(x2d, w_router, w_gate, w_up, w_down):
    n_tok, d = x2d.shape
    n_assign = n_tok * TOP_K
    n_blocks = -(-(n_assign + N_EXPERTS * (MOE_BLOCK - 1)) // MOE_BLOCK)
    logits = jnp.dot(x2d, w_router).astype(jnp.float32)
    top_logits, top_e = lax.top_k(logits, TOP_K)
    top_w = jax.nn.softmax(top_logits, axis=-1).reshape(-1)
    flat_e = top_e.reshape(-1)
    order = jnp.argsort(flat_e)
    e_sorted = flat_e[order]
    tok_sorted = order // TOP_K
    w_sorted = top_w[order]
    counts = jnp.bincount(flat_e, length=N_EXPERTS)
    padded = (counts + MOE_BLOCK - 1) // MOE_BLOCK * MOE_BLOCK
    pad_end = jnp.cumsum(padded)
    rank = jnp.arange(n_assign) - (jnp.cumsum(counts) - counts)[e_sorted]
    dest = (pad_end - padded)[e_sorted] + rank
    rows = jnp.zeros((n_blocks * MOE_BLOCK, d), x2d.dtype).at[dest].set(x2d[tok_sorted])
    block_e = jnp.minimum(jnp.searchsorted(pad_end, jnp.arange(n_blocks) * MOE_BLOCK, side='right'), N_EXPERTS - 1)

    def expert_block(args):
        xb, e = args
        return (jax.nn.silu(xb @ w_gate[e]) * (xb @ w_up[e])) @ w_down[e]

    y_rows = lax.map(expert_block, (rows.reshape(n_blocks, MOE_BLOCK, d), block_e)).reshape(n_blocks * MOE_BLOCK, d)
    y = jax.ops.segment_sum(y_rows[dest] * w_sorted[:, None], tok_sorted, num_segments=n_tok)
    return y.astype(x2d.dtype)


def setup_inputs(seed: int = 0) -> dict:
    key = jax.random.key(seed)
    ks = iter(jax.random.split(key, 64))
    f32 = jnp.float32
    L = DEPTH
    LD = (DEPTH + 1) // 2
    LM = DEPTH // 2
    LV = DEPTH - 1

    def nrm(shape, scale):
        return jax.random.normal(next(ks), shape, f32) * scale

    def unif(shape, lo, hi):
        return jax.random.uniform(next(ks), shape, f32, lo, hi)

    def gain(shape):
        return 1.0 + nrm(shape, 0.02)

    x = nrm((BATCH, SEQ, D_MODEL), 1.0)
    p = nrm((DEPTH, BATCH, SEQ, PLE_DIM), 1.0)
    positions = (jnp.arange(SEQ, dtype=jnp.int32)[None, :]
                 + jax.random.randint(next(ks), (BATCH, 1), 0, 1024, dtype=jnp.int32))
    dt = jnp.exp(unif((L, GDN_HEADS), math.log(1e-3), math.log(1e-1)))
    gdn_dt_bias = dt + jnp.log(-jnp.expm1(-dt))
    return {
        'x': x,
        'p': p,
        'positions': positions,
        'norm_mix_g': gain((L, D_MODEL)),
        'w_in': nrm((L, D_MODEL, N_IN), D_MODEL ** -0.5),
        'rwkv_mu': unif((L, RWKV_IN), 0.0, 1.0),
        'rwkv_w0': unif((L, RWKV_WIDTH), -6.0, 1.0),
        'rwkv_w_up': nrm((L, DECAY_LORA, RWKV_WIDTH), DECAY_LORA ** -0.5),
        'rwkv_a0': unif((L, RWKV_WIDTH), -1.0, 1.0),
        'rwkv_a_up': nrm((L, ICLR_LORA, RWKV_WIDTH), ICLR_LORA ** -0.5),
        'rwkv_g_up': nrm((L, GATE_LORA, RWKV_WIDTH), GATE_LORA ** -0.5),
        'rwkv_k_k': 0.85 + nrm((L, RWKV_WIDTH), 0.05),
        'rwkv_k_a': 1.0 + nrm((L, RWKV_WIDTH), 0.05),
        'rwkv_r_k': nrm((L, RWKV_HEADS, RWKV_HEAD_DIM), 0.1),
        'rwkv_ln_g': gain((L, RWKV_WIDTH)),
        'rwkv_ln_b': nrm((L, RWKV_WIDTH), 0.02),
        'vres_mu': unif((LV, D_MODEL), 0.0, 1.0),
        'vres_down': nrm((LV, D_MODEL, VRES_LORA), D_MODEL ** -0.5),
        'vres_up': nrm((LV, VRES_LORA, RWKV_WIDTH), VRES_LORA ** -0.5),
        'vres_b': 1.0 + nrm((LV, RWKV_WIDTH), 0.1),
        'mla_q_norm_g': gain((L, Q_LORA_RANK)),
        'mla_kv_norm_g': gain((L, KV_LORA_RANK)),
        'mla_w_uq': nrm((L, Q_LORA_RANK, MLA_HEADS * MLA_QK_DIM), Q_LORA_RANK ** -0.5),
        'mla_w_ukv': nrm((L, KV_LORA_RANK, MLA_HEADS * (QK_NOPE_DIM + MLA_V_DIM)), KV_LORA_RANK ** -0.5),
        'mla_qk_norm_q': gain((L, MLA_QK_DIM)),
        'mla_qk_norm_k': gain((L, MLA_QK_DIM)),
        'gdn_conv_w': nrm((L, GDN_CONV, GDN_QKV), GDN_CONV ** -0.5),
        'gdn_a_log': jnp.log(unif((L, GDN_HEADS), 1.0, 16.0)),
        'gdn_dt_bias': gdn_dt_bias,
        'gdn_norm_g': gain((L, GDN_V_DIM)),
        'w_br_rwkv': nrm((L, RWKV_WIDTH, D_MODEL), RWKV_WIDTH ** -0.5),
        'w_br_mla': nrm((L, MLA_WIDTH, D_MODEL), MLA_WIDTH ** -0.5),
        'w_br_gdn': nrm((L, GDN_WIDTH, D_MODEL), GDN_WIDTH ** -0.5),
        'w_out': nrm((L, D_MODEL, D_MODEL), D_MODEL ** -0.5),
        'norm_ffn_g': gain((L, D_MODEL)),
        'ffn_wg': nrm((LD, D_MODEL, FF_DENSE), D_MODEL ** -0.5),
        'ffn_wu': nrm((LD, D_MODEL, FF_DENSE), D_MODEL ** -0.5),
        'ffn_wd': nrm((LD, FF_DENSE, D_MODEL), FF_DENSE ** -0.5),
        'moe_router': nrm((LM, D_MODEL, N_EXPERTS), D_MODEL ** -0.5),
        'moe_wg': nrm((LM, N_EXPERTS, D_MODEL, FF_EXPERT), D_MODEL ** -0.5),
        'moe_wu': nrm((LM, N_EXPERTS, D_MODEL, FF_EXPERT), D_MODEL ** -0.5),
        'moe_wd': nrm((LM, N_EXPERTS, FF_EXPERT, D_MODEL), FF_EXPERT ** -0.5),
        'ple_proj': nrm((L, PLE_DIM, D_MODEL), PLE_DIM ** -0.5),
        'ple_gate': nrm((L, D_MODEL, D_MODEL), D_MODEL ** -0.5),
        'ple_norm_g': gain((L, D_MODEL)),
    }


def reference(x, p, positions, norm_mix_g, w_in, rwkv_mu, rwkv_w0, rwkv_w_up, rwkv_a0, rwkv_a_up,
              rwkv_g_up, rwkv_k_k, rwkv_k_a, rwkv_r_k, rwkv_ln_g, rwkv_ln_b, vres_mu, vres_down,
              vres_up, vres_b, mla_q_norm_g, mla_kv_norm_g, mla_w_uq, mla_w_ukv, mla_qk_norm_q,
              mla_qk_norm_k, gdn_conv_w, gdn_a_log, gdn_dt_bias, gdn_norm_g, w_br_rwkv, w_br_mla,
              w_br_gdn, w_out, norm_ffn_g, ffn_wg, ffn_wu, ffn_wd, moe_router, moe_wg, moe_wu, moe_wd,
              ple_proj, ple_gate, ple_norm_g):
    b, s, d = x.shape
    cos, sin = rope_tables(positions)
    h = x
    v_first = None
    for i in range(DEPTH):
        u = rms_norm(h, norm_mix_g[i])
        z = u @ w_in[i]
        z_rwkv, z_mla, z_gdn, z_gate = split_cols(z, (RWKV_IN, MLA_IN, GDN_IN, GATE_IN))
        vres = None if i == 0 else (vres_mu[i - 1], vres_down[i - 1], vres_up[i - 1], vres_b[i - 1])
        o_a, v_first = rwkv7_branch(z_rwkv, u, v_first, vres, rwkv_mu[i], rwkv_w0[i], rwkv_w_up[i],
                                    rwkv_a0[i], rwkv_a_up[i], rwkv_g_up[i], rwkv_k_k[i], rwkv_k_a[i],
                                    rwkv_r_k[i], rwkv_ln_g[i], rwkv_ln_b[i])
        o_b = mla_branch(z_mla, cos, sin, mla_q_norm_g[i], mla_kv_norm_g[i], mla_w_uq[i], mla_w_ukv[i],
                         mla_qk_norm_q[i], mla_qk_norm_k[i])
        o_c = gdn_branch(z_gdn, gdn_conv_w[i], gdn_a_log[i], gdn_dt_bias[i], gdn_norm_g[i])
        g_a, g_b, g_c = jnp.split(jax.nn.sigmoid(z_gate), N_BRANCH, axis=-1)
        merged = (g_a * (o_a @ w_br_rwkv[i]) + g_b * (o_b @ w_br_mla[i])
                  + g_c * (o_c @ w_br_gdn[i]))
        h = h + merged @ w_out[i]
        u2 = rms_norm(h, norm_ffn_g[i])
        if i % 2 == 0:
            f = swiglu(u2, ffn_wg[i // 2], ffn_wu[i // 2], ffn_wd[i // 2])
        else:
            f = moe_swiglu(u2.reshape(b * s, d), moe_router[i // 2], moe_wg[i // 2],
                           moe_wu[i // 2], moe_wd[i // 2]).reshape(b, s, d)
        h = h + f
        e = rms_norm(p[i] @ ple_proj[i], ple_norm_g[i])
        h = h + jax.nn.sigmoid(h @ ple_gate[i]) * e
    return h
```

```python
import numpy as np
from contextlib import ExitStack
import concourse.bass as bass
import concourse.mybir as mybir

F32 = mybir.dt.float32
BF16 = mybir.dt.bfloat16
I32 = mybir.dt.int32
ALU = mybir.AluOpType
AF = mybir.ActivationFunctionType
AX = mybir.AxisListType

ENGS = ("pe", "act", "dve", "pool", "sp")
N_DMA_SEMS = 24


class Op:
    __slots__ = ("eng", "fn", "deps", "is_dma", "idx", "sig", "slot", "slot_target", "slot_prev", "epoch")

    def __init__(self, eng, fn, is_dma):
        self.eng = eng
        self.fn = fn
        self.is_dma = is_dma
        self.deps = set()
        self.sig = 0
        self.slot = None
        self.slot_target = 0
        self.slot_prev = None


class V:
    __slots__ = ("ap", "key")

    def __init__(self, ap, key):
        self.ap = ap
        self.key = key

    def __getitem__(self, idx):
        return V(self.ap[idx], self.key)

    def sub(self, k):
        base = self.key[0] if isinstance(self.key, tuple) else self.key
        return V(self.ap, (base, k))

    def re(self, pat, **kw):
        return V(self.ap.rearrange(pat, **kw), self.key)

    def bc(self, shape):
        return V(self.ap.to_broadcast(list(shape)), self.key)

    @property
    def shape(self):
        return self.ap.shape


def _ap(x):
    return x.ap if isinstance(x, V) else x


ARENA_F32 = 53000


class Prog:
    def __init__(self, name="k"):
        self.nc = bass.Bass("TRN2", target_bir_lowering=False)
        self.st = ExitStack()
        self.arena = None
        self.aoff = 0
        self.amax = 0
        self.psum = None
        self.bar_deps = None
        self.since_bar = []
        self.bar_seen = {}
        self.epoch = 0
        self.ep_cnt = {}
        self.ops = []
        self.track = {}
        self.n_dma = 0
        self.slot_last = [None] * N_DMA_SEMS
        self.slot_count = [0] * N_DMA_SEMS
        self.uid = 0

    def sb(self, shape, dtype=F32, name=None):
        self.uid += 1
        return self.st.enter_context(self.nc.sbuf_tensor(name or f"sb{self.uid}", list(shape), dtype))

    def ps(self, shape, dtype=F32, name=None):
        self.uid += 1
        return self.st.enter_context(self.nc.psum_tensor(name or f"ps{self.uid}", list(shape), dtype))

    def dram(self, name, shape, dtype=F32, kind="Internal"):
        return self.nc.dram_tensor(name, list(shape), dtype, kind=kind)

    def tile(self, shape, name=None, dtype=None):
        if self.arena is None:
            self.arena = self.st.enter_context(self.nc.sbuf_tensor("arena", [128, ARENA_F32], F32))
        self.uid += 1
        p = shape[0]
        n = int(np.prod(shape[1:]))
        if dtype == BF16:
            nw = (n + 1) // 2
            assert self.aoff + nw <= ARENA_F32, f"arena overflow {self.aoff}+{nw}"
            ap = self.arena[0:p, self.aoff:self.aoff + nw].bitcast(BF16)[:, 0:n]
            self.aoff += nw
        else:
            assert self.aoff + n <= ARENA_F32, f"arena overflow {self.aoff}+{n}"
            ap = self.arena[0:p, self.aoff:self.aoff + n]
            self.aoff += n
        self.amax = max(self.amax, self.aoff)
        if len(shape) == 3:
            ap = ap.rearrange("p (a b) -> p a b", a=shape[1])
        elif len(shape) == 4:
            ap = ap.rearrange("p (a b c) -> p a b c", a=shape[1], b=shape[2])
        return V(ap, name or f"t{self.uid}")

    def mark(self):
        return self.aoff

    def release(self, mark):
        self.barrier()
        self.aoff = mark

    def pbank(self, i):
        if self.psum is None:
            self.psum = [self.st.enter_context(self.nc.psum_tensor(f"psb{j}", [128, 512], F32)) for j in range(8)]
        return V(self.psum[i][:], f"psb{i}")

    def pslot(self, bank, half):
        self.pbank(0)
        return V(self.psum[bank][:, half * 256:(half + 1) * 256], f"psb{bank}_{half}")

    def run_interleaved(self, fns):
        import threading
        il = {"turn": 0, "alive": [True] * len(fns), "cv": threading.Condition(), "tl": threading.local()}
        errs = []

        def nxt(i):
            n = len(fns)
            for d in range(1, n + 1):
                j = (i + d) % n
                if il["alive"][j]:
                    return j
            return i

        def runner(i, fn):
            with il["cv"]:
                while il["turn"] != i:
                    il["cv"].wait()
            il["tl"].i = i
            try:
                fn()
            except BaseException as e:
                errs.append(e)
            finally:
                with il["cv"]:
                    il["alive"][i] = False
                    il["turn"] = nxt(i)
                    il["cv"].notify_all()

        def yield_turn():
            i = getattr(il["tl"], "i", None)
            if i is None:
                return
            with il["cv"]:
                j = nxt(i)
                if j == i:
                    return
                il["turn"] = j
                il["cv"].notify_all()
                while il["turn"] != i:
                    il["cv"].wait()

        self._yield = yield_turn
        ths = [threading.Thread(target=runner, args=(i, f)) for i, f in enumerate(fns)]
        for t in ths:
            t.start()
        for t in ths:
            t.join()
        self._yield = None
        if errs:
            raise errs[0]

    def dview(self, t, name=None):
        ap = t.ap() if hasattr(t, "ap") and callable(t.ap) else t
        return V(ap, name or ap.tensor.name)

    def barrier(self):
        self.bar_deps = list(self.since_bar) if self.bar_deps is None else self.bar_deps + self.since_bar
        last = {}
        dm = []
        for o in self.bar_deps:
            if o.is_dma:
                dm.append(o)
            else:
                last[o.eng] = o
        self.bar_deps = list(last.values()) + dm[-2 * N_DMA_SEMS:]
        self.since_bar = []
        self.bar_seen = {}
        if max(self.ep_cnt.values(), default=0) > 20000:
            self.epoch += 1
            self.ep_cnt = {}

    @staticmethod
    def _key(x):
        if isinstance(x, V):
            x = x.key
        if isinstance(x, tuple):
            t, sub = x
        else:
            t, sub = x, None
        nm = t if isinstance(t, str) else (t.name if hasattr(t, "name") else t.tensor.name)
        return nm, sub

    def _conf(self, nm, sub):
        ent = self.track.setdefault(nm, {})
        if sub is None:
            return list(ent.keys())
        ks = [k for k in ent.keys() if k is None or k == sub]
        return ks

    def op(self, eng, fn, reads=(), writes=(), is_dma=False):
        o = Op(eng, fn, is_dma)
        o.epoch = self.epoch
        if not is_dma:
            self.ep_cnt[eng] = self.ep_cnt.get(eng, 0) + 1
        for r in reads:
            nm, sub = self._key(r)
            ent = self.track.setdefault(nm, {})
            for k in self._conf(nm, sub):
                w = ent[k][0]
                if w is not None:
                    o.deps.add(w)
        for wv in writes:
            nm, sub = self._key(wv)
            ent = self.track.setdefault(nm, {})
            for k in self._conf(nm, sub):
                w, rs = ent[k]
                if w is not None:
                    o.deps.add(w)
                for r in rs:
                    o.deps.add(r)
        for r in reads:
            nm, sub = self._key(r)
            ent = self.track[nm]
            if sub not in ent:
                ent[sub] = [None, []]
            ent[sub][1].append(o)
        for wv in writes:
            nm, sub = self._key(wv)
            ent = self.track[nm]
            if sub is None:
                for k in list(ent.keys()):
                    del ent[k]
            ent[sub] = [o, []]
        o.deps.discard(o)
        if self.bar_deps is not None and not self.bar_seen.get(eng):
            self.bar_seen[eng] = True
            o.deps.update(self.bar_deps)
        self.since_bar.append(o)
        if is_dma:
            s = self.n_dma % N_DMA_SEMS
            self.n_dma += 1
            o.slot = s
            o.slot_prev = self.slot_last[s]
            self.slot_count[s] += 1
            o.slot_target = 16 * self.slot_count[s]
            self.slot_last[s] = o
        o.idx = len(self.ops)
        self.ops.append(o)
        if getattr(self, "_yield", None) is not None:
            self._yield()
        return o

    def dma(self, out, in_, reads=None, writes=None, q="sp", in_fn=None, **kw):
        if in_fn is not None:
            return self.op(q, lambda e: e.dma_start(out=_ap(out), in_=in_fn(e), **kw),
                           reads if reads is not None else [in_], writes if writes is not None else [out], is_dma=True)
        return self.op(q, lambda e: e.dma_start(out=_ap(out), in_=_ap(in_), **kw),
                       reads if reads is not None else [in_], writes if writes is not None else [out], is_dma=True)

    def mm(self, out, lhsT, rhs, start=True, stop=True, reads=None, writes=None, **kw):
        return self.op("pe", lambda e: e.matmul(_ap(out), _ap(lhsT), _ap(rhs), start=start, stop=stop, **kw),
                       reads if reads is not None else [lhsT, rhs], writes if writes is not None else [out])

    def transpose(self, out, in_, ident, reads=None, writes=None):
        return self.op("pe", lambda e: e.transpose(_ap(out), _ap(in_), _ap(ident)),
                       reads if reads is not None else [in_, ident], writes if writes is not None else [out])

    def act(self, out, in_, func, bias=None, scale=1.0, reads=None, writes=None, accum_out=None, eng="act"):
        kw = {}
        rd = [in_]
        if bias is not None:
            kw["bias"] = _ap(bias)
            if not isinstance(bias, (int, float)):
                rd.append(bias)
        if not isinstance(scale, (int, float)):
            rd.append(scale)
        wr = [out]
        if accum_out is not None:
            kw["accum_out"] = _ap(accum_out)
            wr.append(accum_out)
        return self.op(eng, lambda e: e.activation(_ap(out), _ap(in_), func, scale=_ap(scale), **kw),
                       reads if reads is not None else rd, writes if writes is not None else wr)

    def tt(self, out, in0, in1, op, eng="dve", reads=None, writes=None):
        return self.op(eng, lambda e: e.tensor_tensor(_ap(out), _ap(in0), _ap(in1), op),
                       reads if reads is not None else [in0, in1], writes if writes is not None else [out])

    def ts(self, out, in0, s1, op0, s2=None, op1=None, eng="dve", reads=None, writes=None):
        rd = [in0] + [s for s in (s1, s2) if s is not None and not isinstance(s, (int, float))]
        if op1 is None:
            f = lambda e: e.tensor_scalar(_ap(out), _ap(in0), _ap(s1), None, op0)
        else:
            f = lambda e: e.tensor_scalar(_ap(out), _ap(in0), _ap(s1), _ap(s2), op0, op1)
        return self.op(eng, f, reads if reads is not None else rd, writes if writes is not None else [out])

    def stt(self, out, in0, scalar, in1, op0, op1, eng="dve", reads=None, writes=None):
        rd = [in0, in1] + ([] if isinstance(scalar, (int, float)) else [scalar])
        return self.op(eng, lambda e: e.scalar_tensor_tensor(_ap(out), _ap(in0), _ap(scalar), _ap(in1), op0, op1),
                       reads if reads is not None else rd, writes if writes is not None else [out])

    def copy(self, out, in_, eng="dve", reads=None, writes=None):
        if eng == "act":
            f = lambda e: e.copy(_ap(out), _ap(in_))
        else:
            f = lambda e: e.tensor_copy(_ap(out), _ap(in_))
        return self.op(eng, f, reads if reads is not None else [in_], writes if writes is not None else [out])

    def memset(self, ap, val, eng="pool", writes=None):
        return self.op(eng, lambda e: e.memset(_ap(ap), val), [], writes if writes is not None else [ap])

    def recip(self, out, in_, reads=None, writes=None):
        return self.op("dve", lambda e: e.reciprocal(_ap(out), _ap(in_)),
                       reads if reads is not None else [in_], writes if writes is not None else [out])

    def finalize(self, final_waits=()):
        nc = self.nc
        ops = self.ops
        needed = set()
        for o in ops:
            for d in o.deps:
                if not d.is_dma:
                    needed.add(d.idx)
        cnt = {}
        for o in ops:
            if not o.is_dma and (o.idx in needed):
                k_ = (o.eng, o.epoch)
                cnt[k_] = cnt.get(k_, 0) + 1
                o.sig = cnt[k_]
            else:
                o.sig = 0
        sems = {k_: self.st.enter_context(nc.semaphore(f"s_{k_[0]}_{k_[1]}")) for k_ in cnt}
        dsems = [self.st.enter_context(nc.semaphore(f"s_d{i}")) for i in range(N_DMA_SEMS)]
        per_eng = {e: [o for o in ops if o.eng == e] for e in ENGS}
        final = list(final_waits)

        def body(eng_name):
            def _b(e):
                waited = {}
                def wait(key, sem, val):
                    if waited.get(key, 0) >= val:
                        return
                    e.wait_ge(sem, val)
                    waited[key] = val
                for o in per_eng[eng_name]:
                    for d in sorted(o.deps, key=lambda x: x.idx):
                        if d.is_dma:
                            wait(("d", d.slot), dsems[d.slot], d.slot_target)
                        else:
                            if d.eng == eng_name and eng_name == "pe":
                                continue
                            wait(("c", d.eng, d.epoch), sems[(d.eng, d.epoch)], d.sig)
                    if o.is_dma and o.slot_prev is not None:
                        wait(("d", o.slot), dsems[o.slot], o.slot_prev.slot_target)
                    ins = o.fn(e)
                    if o.is_dma:
                        ins.then_inc(dsems[o.slot], 16)
                    elif o.sig:
                        ins.then_inc(sems[(eng_name, o.epoch)], 1)
                if eng_name == "sp":
                    for s in range(N_DMA_SEMS):
                        if self.slot_last[s] is not None:
                            wait(("d", s), dsems[s], self.slot_last[s].slot_target)
            return _b

        with nc.Block() as block:
            block.tensor(body("pe"))
            block.scalar(body("act"))
            block.vector(body("dve"))
            block.gpsimd(body("pool"))
            block.sync(body("sp"))
        self.st.close()
        return nc

D = 1024
TT = 512
ZG = [64] * 12 + [64, 64, 128] + [128, 128, 128, 32] + [64] * 16 + [4, 4]
NG = len(ZG)
G_R, G_K, G_V, G_WLO, G_ALO, G_GLO = 0, 4, 8, 12, 13, 14
G_CQ, G_CKV, G_KPE = 15, 17, 18
G_GQ, G_GK, G_GV, G_GG, G_GB, G_GA = 19, 23, 27, 31, 35, 36
ZOFF = np.concatenate([[0], np.cumsum(ZG)]).astype(int)
NZ = int(ZOFF[-1])

CV = {}
_n = 0
for nm, k in [("nmg", 8), ("mu", 15), ("w0", 4), ("a0", 4), ("kk", 4), ("ka", 4), ("rk", 4), ("lng", 4), ("lnb", 4),
              ("vmu", 8), ("vb", 4), ("qng", 2), ("kvng", 1), ("qkq", 1), ("qkk", 1), ("invf", 1),
              ("conv", 48), ("alog", 1), ("dtb", 1), ("gng", 1), ("ropec", 1), ("nfg", 8), ("png", 8), ("m0", 1), ("m1", 1)]:
    CV[nm] = _n
    _n += k
NCV = _n


def consts_np():
    c = {}
    c["ident"] = np.eye(128, dtype=np.float32)
    c["ones"] = np.ones((128, 128), np.float32)
    bd = np.zeros((128, 128), np.float32)
    bd[:64, :64] = 1
    bd[64:, 64:] = 1
    c["bd64"] = bd
    s = np.arange(64)[:, None]
    t = np.arange(64)[None, :]
    incl = (t >= s).astype(np.float32)
    strict = (t > s).astype(np.float32)
    c["m_incl"] = np.concatenate([incl, incl], 0)
    c["m_strict"] = np.concatenate([strict, strict], 0)
    c["m_incl_T"] = np.concatenate([incl.T, incl.T], 0)
    c["m_strict_T"] = np.concatenate([strict.T, strict.T], 0)
    c["neg_incl"] = (1.0 - c["m_incl"]) * -1e4
    c["neg_incl_T"] = (1.0 - c["m_incl_T"]) * -1e4
    c["id64x2"] = np.concatenate([np.eye(64, dtype=np.float32)] * 2, 0)
    kk = np.arange(128)[:, None]
    qq = np.arange(128)[None, :]
    c["att_mask"] = (qq >= kk).astype(np.float32)
    R = np.zeros((96, 96), np.float32)
    for m in range(16):
        R[64 + m, 64 + m + 16] = -1.0
        R[64 + 16 + m, 64 + m] = 1.0
    c["ropeRT"] = np.ascontiguousarray(R.T)
    sel = np.zeros((4, 4, 64), np.float32)
    for h in range(4):
        sel[h, h, :] = 1.0
    c["sel4"] = sel.reshape(4, 256)
    sel8 = np.zeros((8, 8, 128), np.float32)
    for e in range(8):
        sel8[e, e, :] = 1.0
    c["sel8"] = sel8.reshape(8, 1024)
    c["id4"] = np.tile(np.eye(64, dtype=np.float32)[:, None, :], (1, 4, 1)).reshape(64, 256)
    c["neg4"] = np.tile(c["neg_incl"][:64][:, None, :], (1, 4, 1)).reshape(64, 256)
    c["neg4T"] = np.tile(c["neg_incl_T"][:64][:, None, :], (1, 4, 1)).reshape(64, 256)
    c["ms4"] = np.tile(c["m_strict"][:64][:, None, :], (1, 4, 1)).reshape(64, 256)
    c["ms4T"] = np.tile(c["m_strict_T"][:64][:, None, :], (1, 4, 1)).reshape(64, 256)
    c["mi4"] = np.tile(c["m_incl"][:64][:, None, :], (1, 4, 1)).reshape(64, 256)
    return c


CONST_SHAPES = {k: v.shape for k, v in consts_np().items()}


class Ctx:
    pass


def load_consts(P, names):
    out = {}
    for nm in names:
        shp = CONST_SHAPES[nm]
        d = P.dview(P.dram("c_" + nm, shp, F32, kind="ExternalInput"))
        t = P.tile(list(shp), name="c_" + nm)
        P.dma(t, d)
        out[nm] = t
    return out


def rstd_from_ps(P, out, ps, n, eps):
    P.act(out, ps, AF.Ln, scale=float(1.0 / n), bias=float(eps))
    P.act(out, out, AF.Exp, scale=-0.5)


def phase_proj(P, C, S, hT, w_d, ncols, groups, zT, gcol, uT=None, func=None, nbuf=2, zflat=False, dyn=None):
    mk = P.mark()
    cv = C.cv
    w = P.tile([128, 8, ncols], name="w_in", dtype=BF16)
    wst = [P.tile([128, 8, 512], name=f"wst{i}") for i in range(2)]
    wv = w_d.re("(c p) n -> p c n", p=128)
    step = 512
    for i_, c0 in enumerate(range(0, ncols, step)):
        c1 = min(ncols, c0 + step)
        st_ = wst[i_ % 2]
        P.dma(st_[:, :, 0:c1 - c0], wv[:, :, c0:c1])
        P.copy(w[:, :, c0:c1].sub(c0), st_[:, :, 0:c1 - c0], eng=("pool" if i_ % 2 == 0 else "act"))
    hb = [P.tile([128, 8, TT], name=f"hb{i}") for i in range(nbuf)]
    sq = P.tile([128, 8, TT], name="sq")
    ub = [P.tile([128, 8, TT], name=f"ub{i}", dtype=BF16) for i in range(nbuf)]
    rs = P.tile([128, TT], name="rs")
    stg = [P.tile([128, TT], name=f"stg{i}") for i in range(4)]
    hv = hT.re("(c p) t -> p c t", p=128)
    ps_ss = P.pbank(0)
    pz = [P.pbank(1 + i) for i in range(4)]
    nt = S // TT
    k = 0
    for ti in range(nt):
        tsl = slice(ti * TT, (ti + 1) * TT)
        h = hb[ti % nbuf]
        u = ub[ti % nbuf]
        if dyn is not None:
            P.dma(h, hv[:, :, tsl])
            P.dma(sq, hv[:, :, dyn + ti * TT:dyn + (ti + 1) * TT])
            P.ts(h, h, cv[:, CV["m0"]:CV["m0"] + 1], ALU.mult)
            P.stt(h, sq, cv[:, CV["m1"]:CV["m1"] + 1], h, ALU.mult, ALU.add)
        else:
            P.dma(h, hv[:, :, tsl])
        P.act(sq, h, AF.Square)
        for c in range(8):
            P.mm(ps_ss, C.k["ones"], sq[:, c, :], start=(c == 0), stop=(c == 7))
        rstd_from_ps(P, rs, ps_ss, D, 1e-6)
        if uT is not None:
            for c in range(8):
                P.stt(sq[:, c, :], h[:, c, :], cv[:, gcol + c:gcol + c + 1], rs, ALU.mult, ALU.mult)
            P.dma(uT.re("(c p) t -> p c t", p=128)[:, :, tsl], sq, q="pool")
            P.copy(u, sq, eng="pool")
        else:
            for c in range(8):
                P.stt(u[:, c, :], h[:, c, :], cv[:, gcol + c:gcol + c + 1], rs, ALU.mult, ALU.mult)
        for gi, (co, n) in enumerate(groups):
            pp = pz[k % 4]
            st = stg[k % 4]
            for c in range(8):
                P.mm(pp[0:n, :], w[:, c, co:co + n], u[:, c, :], start=(c == 0), stop=(c == 7))
            if func is not None:
                P.act(st[0:n, :], pp[0:n, :], func)
            else:
                P.copy(st[0:n, :], pp[0:n, :], eng=("act" if k % 2 == 0 else "dve"))
            if zflat:
                P.dma(zT[co:co + n, tsl].sub(gi), st[0:n, :], q="pool")
            else:
                P.dma(zT[gi, 0:n, tsl].sub(gi), st[0:n, :], q="pool")
            k += 1
    P.release(mk)


def phase_mla(P, C, S, zT, pos_d, w_uq_d, w_uk_d, w_uv_d, oT):
    mk = P.mark()
    cv = C.cv
    K = C.k
    nt = S // TT
    wuq_f = P.tile([128, 2, 384], name="wuq_f")
    P.dma(wuq_f, w_uq_d.re("(c p) n -> p c n", p=128))
    wuk_f = P.tile([128, 256], name="wuk_f")
    P.dma(wuk_f, w_uk_d)
    wuv_f = P.tile([128, 256], name="wuv_f")
    P.dma(wuv_f, w_uv_d)
    wuq = P.tile([128, 2, 384], name="wuq", dtype=BF16)
    wuk = P.tile([128, 256], name="wuk", dtype=BF16)
    wuv = P.tile([128, 256], name="wuv", dtype=BF16)
    P.copy(wuq, wuq_f, eng="pool")
    P.copy(wuk, wuk_f, eng="pool")
    P.copy(wuv, wuv_f, eng="pool")
    ones_b = P.tile([128, 64], name="ones_b", dtype=BF16)
    P.copy(ones_b, K["ones"][:, 0:64], eng="pool")
    KT = P.tile([96, 4, S], name="KT", dtype=BF16)
    VT = P.tile([128, S // 128, 256], name="Vtm", dtype=BF16)
    rc = P.tile([96, TT], name="rope_c")
    rsn = P.tile([96, TT], name="rope_s")
    posi = P.tile([96, TT], name="posi")
    posf = P.tile([96, TT], name="posf")
    ang = P.tile([96, TT], name="ang")
    tmp = P.tile([96, TT], name="ropetmp")
    tmp2 = P.tile([96, TT], name="ropetmp2")
    P.memset(rc[0:64, :], 1.0, writes=[rc.sub("lo")])
    P.memset(rsn[0:64, :], 0.0, writes=[rsn.sub("lo")])
    pi_ap = V(posi.ap.bitcast(I32), posi.key)
    invf = cv[64:96, CV["invf"]:CV["invf"] + 1]
    negpi = cv[64:96, CV["ropec"]:CV["ropec"] + 1]
    TWO_PI = float(2 * np.pi)

    def rope_tile(ti):
        tsl = slice(ti * TT, (ti + 1) * TT)
        P.dma(pi_ap[64:96, :], V(pos_d.ap[:, tsl].partition_broadcast(32), pos_d.key))
        P.copy(posf[64:96, :], pi_ap[64:96, :])
        P.ts(ang[64:96, :], posf[64:96, :], invf, ALU.mult)
        for dst, shift in ((rsn, 0.0), (rc, float(np.pi / 2))):
            a_ = ang[64:96, :]
            if shift:
                P.ts(tmp2[64:96, :], ang[64:96, :], shift, ALU.add)
                a_ = tmp2[64:96, :]
            P.ts(tmp[64:96, :], a_, float(1.0 / TWO_PI), ALU.mult)
            P.copy(pi_ap[64:96, :], tmp[64:96, :])
            P.copy(tmp[64:96, :], pi_ap[64:96, :])
            P.stt(tmp[64:96, :], tmp[64:96, :], -TWO_PI, a_, ALU.mult, ALU.add)
            P.ts(posf[64:96, :], tmp[64:96, :], float(np.pi), ALU.is_gt, TWO_PI, ALU.mult)
            P.tt(tmp[64:96, :], tmp[64:96, :], posf[64:96, :], ALU.subtract)
            P.act(dst[64:96, :], tmp[64:96, :], AF.Sin, writes=[dst.sub("hi")])

    ones = K["ones"]
    cq = [P.tile([128, 2, TT], name=f"cq{i}") for i in range(2)]
    ckv = [P.tile([128, TT], name=f"ckv{i}") for i in range(2)]
    cqb = [P.tile([128, 2, TT], name=f"cqb{i}", dtype=BF16) for i in range(2)]
    ckvb = [P.tile([128, TT], name=f"ckvb{i}", dtype=BF16) for i in range(2)]
    kpe = [P.tile([96, TT], name=f"kpe{i}") for i in range(2)]
    sq = P.tile([128, 2, TT], name="msq")
    rs = P.tile([128, TT], name="mrs")
    raw = P.tile([96, TT], name="raw")
    nrm = P.tile([96, TT], name="nrm")
    rot = P.tile([96, TT], name="rot")
    QT = P.tile([96, 4, TT], name="QT", dtype=BF16)
    ps_a = P.pbank(0)
    ps_b = P.pbank(1)
    ps_c = P.pbank(2)

    def qk_finish(src_raw, gcolname, dst, tsl):
        P.act(sq[0:96, 0, :], src_raw, AF.Square)
        P.mm(ps_b[0:96, :], ones[0:96, 0:96], sq[0:96, 0, :])
        rstd_from_ps(P, rs[0:96, :], ps_b[0:96, :], 96, 1e-6)
        g = cv[0:96, CV[gcolname]:CV[gcolname] + 1]
        P.stt(nrm, src_raw, g, rs[0:96, :], ALU.mult, ALU.mult)
        P.mm(ps_c[0:96, :], K["ropeRT"], nrm)
        P.tt(rot, ps_c[0:96, :], rsn, ALU.mult)
        P.tt(nrm, nrm, rc, ALU.mult, eng="pool")
        P.tt(dst, nrm, rot, ALU.add)

    def load_norm(ti, want_q):
        tsl = slice(ti * TT, (ti + 1) * TT)
        i2 = ti % 2
        if want_q:
            P.dma(cq[i2], zT[G_CQ:G_CQ + 2, :, tsl].re("g p t -> p g t"))
            P.act(sq, cq[i2], AF.Square)
            P.mm(ps_a, ones, sq[:, 0, :], start=True, stop=False)
            P.mm(ps_a, ones, sq[:, 1, :], start=False, stop=True)
            rstd_from_ps(P, rs, ps_a, 256, 1e-6)
            for c in range(2):
                P.stt(cqb[i2][:, c, :], cq[i2][:, c, :], cv[:, CV["qng"] + c:CV["qng"] + c + 1], rs, ALU.mult, ALU.mult)
        else:
            P.dma(ckv[i2], zT[G_CKV, :, tsl])
            P.dma(kpe[i2][64:96, :], zT[G_KPE, 0:32, tsl])
            P.act(sq[:, 0, :], ckv[i2], AF.Square)
            P.mm(ps_a, ones, sq[:, 0, :])
            rstd_from_ps(P, rs, ps_a, 128, 1e-6)
            P.stt(ckvb[i2], ckv[i2], cv[:, CV["kvng"]:CV["kvng"] + 1], rs, ALU.mult, ALU.mult)
        return tsl, i2

    for ti in range(nt):
        rope_tile(ti)
        tsl, i2 = load_norm(ti, False)
        for h in range(4):
            P.mm(ps_b[0:64, :], wuk[:, h * 64:(h + 1) * 64], ckvb[i2])
            P.copy(raw[0:64, :], ps_b[0:64, :], eng="act", writes=[raw.sub("lo")])
            P.copy(raw[64:96, :], kpe[i2][64:96, :], eng="pool", writes=[raw.sub("hi")])
            qk_finish(raw, "qkk", KT[:, h, tsl].sub(h), tsl)
        for j in range(TT // 128):
            P.mm(ps_c[:, 0:256], ckvb[i2][:, j * 128:(j + 1) * 128], wuv)
            P.copy(VT[:, ti * (TT // 128) + j, :], ps_c[:, 0:256], eng="act")

    pt = [P.tile([128, TT], name=f"pt{i}", dtype=BF16) for i in range(3)]
    osb = P.tile([64, TT], name="osb")
    lsb = P.tile([64, TT], name="lsb")
    ps_s = [P.pbank(3), P.pbank(4)]
    ps_o = P.pbank(5)
    ps_l = P.pbank(6)
    scale = float(96 ** -0.5)
    for ti in range(nt):
        rope_tile(ti)
        tsl, i2 = load_norm(ti, True)
        for h in range(4):
            for c in range(2):
                P.mm(ps_b[0:96, :], wuq[:, c, h * 96:(h + 1) * 96], cqb[i2][:, c, :], start=(c == 0), stop=(c == 1))
            P.copy(raw, ps_b[0:96, :], eng="act")
            qk_finish(raw, "qkq", QT[:, h, :].sub(h), tsl)
        for h in range(4):
            nkc = 4 * (ti + 1)

            def c0_of(kc):
                j = kc - 4 * ti
                return 0 if j <= 0 else j * 128

            def score(kc):
                c0 = c0_of(kc)
                P.mm(ps_s[kc % 2][:, c0:TT], KT[:, h, kc * 128:(kc + 1) * 128].sub(h), QT[:, h, c0:TT].sub(h))
            score(0)
            for kc in range(nkc):
                if kc + 1 < nkc:
                    score(kc + 1)
                j = kc - 4 * ti
                c0 = c0_of(kc)
                pss = ps_s[kc % 2]
                p_t = pt[kc % 3]
                P.act(p_t[:, c0:TT], pss[:, c0:TT], AF.Exp, scale=scale)
                if j >= 0:
                    P.tt(p_t[:, c0:c0 + 128], p_t[:, c0:c0 + 128], K["att_mask"], ALU.mult, eng="pool")
                P.mm(ps_o[0:64, c0:TT], VT[:, kc, h * 64:(h + 1) * 64], p_t[:, c0:TT], start=(kc == 0), stop=(kc == nkc - 1))
                P.mm(ps_l[0:64, c0:TT], ones_b, p_t[:, c0:TT], start=(kc == 0), stop=(kc == nkc - 1))
            P.act(lsb, ps_l[0:64, :], AF.Ln)
            P.act(lsb, lsb, AF.Exp, scale=-1.0)
            P.tt(osb, ps_o[0:64, :], lsb, ALU.mult)
            P.dma(oT[h * 64:(h + 1) * 64, tsl].sub(h), osb, q="pool")
    P.release(mk)


def neumann_inv(P, C, A0, B0, bufs, ps1, ps2, ps3):
    id4 = C.k["id4"]
    TTt = bufs["TT"]
    P.tt(TTt, B0, id4, ALU.add)
    A = [A0, bufs["A1"]]
    B = [B0, bufs["B1"]]
    for k in range(1, 6):
        a_prev, a_new = A[(k - 1) % 2], A[k % 2]
        b_prev, b_new = B[(k - 1) % 2], B[k % 2]
        for h in range(4):
            hs = slice(h * 64, (h + 1) * 64)
            P.mm(ps1[0:64, hs], b_prev[:, hs], a_prev[:, hs])
        if k < 5:
            for h in range(4):
                hs = slice(h * 64, (h + 1) * 64)
                P.mm(ps2[0:64, hs], a_prev[:, hs], b_prev[:, hs])
        P.copy(a_new, ps1[0:64, 0:256], eng="act")
        if k < 5:
            P.copy(b_new, ps2[0:64, 0:256], eng="dve")
        for h in range(4):
            hs = slice(h * 64, (h + 1) * 64)
            P.mm(ps3[0:64, hs], a_new[:, hs], TTt[:, hs])
        P.tt(TTt, TTt, ps3[0:64, 0:256], ALU.add)
    return TTt


def phase_gdn(P, C, S, zT, oT, ttl=512, psbase=None, release=True):
    mk = P.mark()
    TT = ttl
    cv = C.cv
    K = C.k
    nt = S // TT
    NCH = TT // 64
    ones = K["ones"]
    ident = K["ident"]
    cvw = lambda seg, h, j: cv[0:64, CV["conv"] + (seg * 4 + h) * 4 + j:CV["conv"] + (seg * 4 + h) * 4 + j + 1]
    St = P.tile([64, 4, 64], name="gS")
    P.memset(St, 0.0)
    Sb = P.tile([64, 4, 64], name="gSb", dtype=BF16)
    P.copy(Sb, St, eng="pool")
    nA = P.tile([4, 1], name="nA")
    P.act(nA, cv[0:4, CV["alog"]:CV["alog"] + 1], AF.Exp)
    P.ts(nA, nA, -1.0, ALU.mult)
    def mkbuf(i):
        b = {}
        for nm in ["q", "k", "kb", "qd"]:
            b[nm] = P.tile([64, 4, TT], name=f"g{nm}{i}", dtype=BF16)
        b["k32"] = P.tile([64, 4, TT], name=f"gk32{i}")
        b["q32"] = P.tile([64, 4, TT], name=f"gq32{i}")
        b["ktm"] = P.tile([64, NCH, 4, 64], name=f"gktm{i}", dtype=BF16)
        b["bv"] = P.tile([64, NCH, 4, 64], name=f"gbv{i}")
        b["bg"] = P.tile([64, NCH, 12], name=f"gbg{i}")
        b["c2"] = P.tile([64, NCH, 4], name=f"gc2{i}")
        b["ngc"] = P.tile([64, NCH, 4], name=f"gngc{i}")
        b["dl"] = P.tile([64, 4, NCH], name=f"gdl{i}")
        return b
    TB = [mkbuf(0), mkbuf(1)]
    xin = [P.tile([64, TT + 3], name=f"gxin{i}") for i in range(3)]
    acc = [P.tile([64, TT], name=f"gacc{i}") for i in range(2)]
    vfm = P.tile([64, 4, TT], name="gvfm")
    sq = P.tile([64, TT], name="gsq")
    rs = P.tile([64, TT], name="grs")
    bfm = P.tile([4, TT], name="gbfm")
    gfm = [P.tile([4, TT], name=f"ggfm{i}") for i in range(2)]
    efm = P.tile([4, TT], name="gefm")
    kdf = P.tile([4, TT], name="gkdf")
    gl4 = P.tile([4, NCH], name="ggl4")
    ob = [P.tile([64, 4, TT], name=f"gob{i}") for i in range(2)]
    gate = P.tile([64, TT], name="ggate")
    def mkch(i):
        d_ = {nm: P.tile([64, 256], name=f"gc_{nm}{i}") for nm in ["E", "F", "G1", "G2", "Gs"]}
        d_.update({nm: P.tile([64, 256], name=f"gc_{nm}{i}", dtype=BF16) for nm in ["A0", "B0", "A1", "B1", "TT", "Ain", "X", "vn"]})
        return d_
    CB = [mkch(0), mkch(1)]
    if psbase is None:
        psA, psB, psC, psD, psE, psF, psG, psH = [P.pbank(i) for i in range(8)]
    else:
        psA, psB, psC, psD, psE, psF, psG, psH = [P.pbank(psbase + j % 4) for j in range(8)]
    xk = 0
    for ti in range(nt):
        tb = TB[ti % 2]
        tsl = slice(ti * TT, (ti + 1) * TT)
        for seg, (g0, dst) in enumerate([(G_GQ, tb["q32"]), (G_GK, tb["k32"]), (G_GV, vfm)]):
            for h in range(4):
                x = xin[xk % 3]
                a = acc[xk % 2]
                xk += 1
                if ti == 0:
                    P.memset(x[:, 0:3], 0.0, writes=[x.sub("halo")])
                    P.dma(x[:, 3:TT + 3].sub("body"), zT[g0 + h, 0:64, 0:TT])
                else:
                    P.dma(x, zT[g0 + h, 0:64, ti * TT - 3:(ti + 1) * TT])
                P.ts(a, x[:, 0:TT], cvw(seg, h, 0), ALU.mult)
                for j in range(1, 4):
                    P.stt(a, x[:, j:TT + j], cvw(seg, h, j), a, ALU.mult, ALU.add)
                if seg == 2:
                    P.act(dst[:, h, :].sub(h), a, AF.Silu)
                else:
                    P.act(a, a, AF.Silu)
                    P.act(sq, a, AF.Square)
                    P.mm(psA[0:64, 0:TT], ones[0:64, 0:64], sq)
                    rstd_from_ps(P, rs, psA[0:64, 0:TT], 1.0, 1e-12)
                    P.stt(dst[:, h, :].sub(h), a, (0.125 if seg == 0 else 1.0), rs, ALU.mult, ALU.mult)
        P.dma(bfm, zT[G_GB, 0:4, tsl])
        P.act(bfm, bfm, AF.Sigmoid)
        g0t = gfm[0]
        P.dma(g0t, zT[G_GA, 0:4, tsl])
        P.act(g0t, g0t, AF.Exp, bias=cv[0:4, CV["dtb"]:CV["dtb"] + 1])
        P.act(g0t, g0t, AF.Ln, bias=1.0)
        P.ts(g0t, g0t, nA[:, 0:1], ALU.mult)
        cur = 0
        for sh in (1, 2, 4, 8, 16, 32):
            src = gfm[cur].re("h (n c) -> h n c", c=64)
            dstt = gfm[1 - cur].re("h (n c) -> h n c", c=64)
            P.copy(dstt[:, :, 0:sh], src[:, :, 0:sh], eng="pool", writes=[gfm[1 - cur].sub("a")])
            P.tt(dstt[:, :, sh:64], src[:, :, sh:64], src[:, :, 0:64 - sh], ALU.add, writes=[gfm[1 - cur].sub("b")])
            cur = 1 - cur
        gc = gfm[cur]
        P.act(efm, gc, AF.Exp)
        gc3 = gc.re("h (n c) -> h n c", c=64)
        P.copy(gl4, gc3[:, :, 63])
        P.tt(kdf.re("h (n c) -> h n c", c=64), V(gl4.ap.unsqueeze(2).to_broadcast([4, NCH, 64]), gl4.key), gc3, ALU.subtract)
        P.act(kdf, kdf, AF.Exp)
        for h in range(4):
            P.mm(psA[0:64, h * NCH:(h + 1) * NCH], K["sel4"][:, h * 64:(h + 1) * 64], gl4)
        P.act(tb["dl"].re("p h n -> p (h n)"), psA[0:64, 0:4 * NCH], AF.Exp)
        P.copy(tb["k"], tb["k32"], eng="pool")
        P.copy(tb["q"], tb["q32"], eng="pool")
        for h in range(4):
            P.mm(psB[0:64, 0:TT], K["sel4"][:, h * 64:(h + 1) * 64], bfm)
            P.tt(tb["kb"][:, h, :].sub(h), tb["k32"][:, h, :].sub(h), psB[0:64, 0:TT], ALU.mult)
            P.mm(psC[0:64, 0:TT], K["sel4"][:, h * 64:(h + 1) * 64], efm)
            P.tt(tb["qd"][:, h, :].sub(h), tb["q32"][:, h, :].sub(h), psC[0:64, 0:TT], ALU.mult)
        for n in range(NCH):
            cs = slice(n * 64, (n + 1) * 64)
            for h in range(4):
                P.transpose(psD[0:64, h * 64:(h + 1) * 64], tb["k32"][:, h, cs].sub(h), ident[0:64, 0:64])
            P.copy(tb["ktm"][:, n, :, :].re("p h d -> p (h d)"), psD[0:64, 0:256], eng="act")
            for h in range(4):
                P.transpose(psE[0:64, h * 64:(h + 1) * 64], vfm[:, h, cs].sub(h), ident[0:64, 0:64])
            P.copy(tb["bv"][:, n, :, :].re("p h d -> p (h d)"), psE[0:64, 0:256], eng="dve")
            P.transpose(psF[0:64, 0:4], bfm[:, cs], ident[0:4, 0:4])
            P.transpose(psF[0:64, 4:8], gc[:, cs], ident[0:4, 0:4])
            P.transpose(psF[0:64, 8:12], kdf[:, cs], ident[0:4, 0:4])
            P.copy(tb["bg"][:, n, :], psF[0:64, 0:12], eng="act")
        bg = tb["bg"]
        P.ts(tb["ngc"], bg[:, :, 4:8], -1.0, ALU.mult)
        P.act(tb["c2"], bg[:, :, 4:8], AF.Exp)
        P.stt(tb["c2"], tb["c2"], -1.0, bg[:, :, 0:4], ALU.mult, ALU.mult)
        P.tt(tb["ktm"], tb["ktm"], V(bg.ap[:, :, 8:12].unsqueeze(3).to_broadcast([64, NCH, 4, 64]), bg.key), ALU.mult)
        P.tt(tb["bv"], tb["bv"], V(bg.ap[:, :, 0:4].unsqueeze(3).to_broadcast([64, NCH, 4, 64]), bg.key), ALU.mult)
        o_t = ob[ti % 2]
        for n in range(NCH):
            cb = CB[n % 2]
            cs = slice(n * 64, (n + 1) * 64)
            gcn = V(bg.ap[:, n, 4:8].unsqueeze(2).to_broadcast([64, 4, 64]), bg.key)
            ngcn = V(tb["ngc"].ap[:, n, :].unsqueeze(2).to_broadcast([64, 4, 64]), tb["ngc"].key)
            E3 = cb["E"].re("p (h c) -> p h c", h=4)
            F3 = cb["F"].re("p (h c) -> p h c", h=4)
            P.tt(E3, K["id4"].re("p (h c) -> p h c", h=4), gcn, ALU.mult)
            P.tt(F3, K["neg4"].re("p (h c) -> p h c", h=4), ngcn, ALU.add)
            P.mm(psG[0:64, 0:256], ones[0:64, 0:64], cb["E"], start=True, stop=False)
            P.mm(psG[0:64, 0:256], ident[0:64, 0:64], cb["F"], start=False, stop=True)
            P.act(cb["G1"], psG[0:64, 0:256], AF.Exp)
            P.ts(cb["E"], cb["E"], -1.0, ALU.mult)
            P.tt(F3, K["neg4T"].re("p (h c) -> p h c", h=4), gcn, ALU.add)
            P.mm(psH[0:64, 0:256], ones[0:64, 0:64], cb["E"], start=True, stop=False)
            P.mm(psH[0:64, 0:256], ident[0:64, 0:64], cb["F"], start=False, stop=True)
            P.act(cb["G2"], psH[0:64, 0:256], AF.Exp)
            P.tt(cb["Gs"], cb["G1"], K["ms4"], ALU.mult, eng="pool")
            P.tt(cb["G2"], cb["G2"], K["ms4T"], ALU.mult, eng="pool")
            for h in range(4):
                hs = slice(h * 64, (h + 1) * 64)
                P.mm(psA[0:64, hs], tb["k"][:, h, cs].sub(h), tb["kb"][:, h, cs].sub(h))
                P.mm(psB[0:64, hs], tb["kb"][:, h, cs].sub(h), tb["k"][:, h, cs].sub(h))
                P.mm(psC[0:64, hs], tb["k"][:, h, cs].sub(h), tb["q"][:, h, cs].sub(h))
            P.stt(cb["B0"], psA[0:64, 0:256], -1.0, cb["Gs"], ALU.mult, ALU.mult)
            P.stt(cb["A0"], psB[0:64, 0:256], -1.0, cb["G2"], ALU.mult, ALU.mult)
            P.tt(cb["Ain"], psC[0:64, 0:256], cb["G1"], ALU.mult)
            TTm = neumann_inv(P, C, cb["A0"], cb["B0"], cb, psA, psB, psC)
            for h in range(4):
                hs = slice(h * 64, (h + 1) * 64)
                P.mm(psD[0:64, hs], tb["k"][:, h, cs].sub(h), Sb[:, h, :])
            X3 = cb["X"].re("p (h v) -> p h v", h=4)
            c2n = V(tb["c2"].ap[:, n, :].unsqueeze(2).to_broadcast([64, 4, 64]), tb["c2"].key)
            P.tt(X3, psD[0:64, 0:256].re("p (h v) -> p h v", h=4), c2n, ALU.mult)
            P.tt(X3, X3, tb["bv"][:, n, :, :], ALU.add)
            for h in range(4):
                hs = slice(h * 64, (h + 1) * 64)
                P.mm(psE[0:64, hs], TTm[:, hs], cb["X"][:, hs])
            P.copy(cb["vn"], psE[0:64, 0:256], eng="act")
            for h in range(4):
                hs = slice(h * 64, (h + 1) * 64)
                P.mm(psF[0:64, hs], Sb[:, h, :], tb["qd"][:, h, cs].sub(h), start=True, stop=False)
                P.mm(psF[0:64, hs], cb["vn"][:, hs], cb["Ain"][:, hs], start=False, stop=True)
            P.copy(o_t[:, :, cs], psF[0:64, 0:256].re("p (h c) -> p h c", h=4), eng="act")
            for h in range(4):
                hs = slice(h * 64, (h + 1) * 64)
                P.mm(psG[0:64, hs], tb["ktm"][:, n, h, :], cb["vn"][:, hs])
            dln = V(tb["dl"].ap[:, :, n].unsqueeze(2).to_broadcast([64, 4, 64]), tb["dl"].key)
            P.tt(St, St, dln, ALU.mult)
            P.tt(St, St, psG[0:64, 0:256].re("p (h v) -> p h v", h=4), ALU.add)
            P.copy(Sb, St, eng="pool")
        for h in range(4):
            P.dma(gate, zT[G_GG + h, 0:64, tsl])
            P.act(gate, gate, AF.Silu)
            P.act(sq, o_t[:, h, :], AF.Square)
            P.mm(psH[0:64, 0:TT], ones[0:64, 0:64], sq)
            rstd_from_ps(P, rs, psH[0:64, 0:TT], 64.0, 1e-6)
            P.stt(rs, rs, cv[0:64, CV["gng"]:CV["gng"] + 1], gate, ALU.mult, ALU.mult)
            P.tt(sq, o_t[:, h, :], rs, ALU.mult)
            P.dma(oT[h * 64:(h + 1) * 64, tsl].sub(h), sq, q="pool")
    if release:
        P.release(mk)


def phase_rwkv(P, C, S, L, zT, oT, w_up_d, a_up_d, g_up_d, vfT, uT, v_down_d, v_up_d, ttl=512, psbase=None, release=True):
    mk = P.mark()
    TT = ttl
    cv = C.cv
    K = C.k
    nt = S // TT
    NCH = TT // 64
    ones = K["ones"]
    ident = K["ident"]
    col = lambda nm, h: cv[0:64, CV[nm] + h:CV[nm] + h + 1]
    w_up = P.tile([64, 256], name="r_wup"); P.dma(w_up, w_up_d)
    a_up = P.tile([64, 256], name="r_aup"); P.dma(a_up, a_up_d)
    g_up = P.tile([128, 256], name="r_gup"); P.dma(g_up, g_up_d)
    if L > 0:
        v_dn = P.tile([128, 8, 32], name="r_vdn"); P.dma(v_dn, v_down_d.re("(c p) n -> p c n", p=128))
        v_upt = P.tile([32, 256], name="r_vup"); P.dma(v_upt, v_up_d)
    oma = P.tile([64, 4], name="r_oma")
    P.ts(oma, cv[0:64, CV["ka"]:CV["ka"] + 4], -1.0, ALU.mult, 1.0, ALU.add)
    ST = P.tile([64, 4, 64], name="rST")
    P.memset(ST, 0.0)
    STb = P.tile([64, 4, 64], name="rSTb", dtype=BF16)
    P.copy(STb, ST, eng="pool")
    T4 = lambda nm: P.tile([64, 4, TT], name=nm)
    T4b = lambda nm: P.tile([64, 4, TT], name=nm, dtype=BF16)
    at, bt, kt, rt = T4b("r_at"), T4b("r_bt"), T4b("r_kt"), T4b("r_rt")
    bon, gate4 = T4("r_bon"), T4("r_gate")
    t0, t1, t2, t3, t4_, t5 = [T4(f"r_t{i}") for i in range(6)]
    y4 = t0
    bh_tm = P.tile([64, NCH, 4, 64], name="r_bhtm", dtype=BF16)
    kh_tm = P.tile([64, NCH, 4, 64], name="r_khtm", dtype=BF16)
    v_tm = P.tile([64, NCH, 4, 64], name="r_vtm", dtype=BF16)
    WC = P.tile([64, 4, NCH], name="r_WC")
    xin = [P.tile([128, TT + 1], name=f"r_xin{i}") for i in range(2)]
    dd = P.tile([128, TT], name="r_dd")
    lo_w = P.tile([64, TT], name="r_low")
    lo_a = P.tile([64, TT], name="r_loa")
    lo_g = P.tile([128, TT], name="r_log")
    sq = P.tile([64, TT], name="r_sq")
    rs = P.tile([64, TT], name="r_rs")
    if L > 0:
        uxb = [P.tile([128, TT + 1], name=f"r_ux{i}") for i in range(2)]
        xvb = [P.tile([128, TT], name=f"r_xv{i}") for i in range(2)]
        vl = P.tile([32, TT], name="r_vl")
        vf = rs

    def mkch(i):
        d_ = {nm: P.tile([64, 256], name=f"rc_{nm}{i}", dtype=BF16) for nm in ["A0", "B0", "A1", "B1", "TT", "Bak", "Brb", "Brk"]}
        d_["X"] = d_["A1"]
        d_["U"] = d_["B1"]
        return d_
    CB = [mkch(0), mkch(1)]
    if psbase is None:
        psA, psB, psC, psD, psE, psF, psG, psH = [P.pbank(i) for i in range(8)]
    else:
        psA, psB, psC, psD, psE, psF, psG, psH = [P.pbank(psbase + j % 4) for j in range(8)]
    xk = 0

    def shifted(g, rows, ti, dst):
        nonlocal xk
        x = xin[xk % 2]
        xk += 1
        if ti == 0:
            P.memset(x[0:rows, 0:1], 0.0, writes=[x.sub("halo")])
            P.dma(x[0:rows, 1:TT + 1].sub("body"), zT[g, 0:rows, 0:TT])
        else:
            P.dma(x[0:rows, :], zT[g, 0:rows, ti * TT - 1:(ti + 1) * TT])
        P.tt(dd[0:rows, :], x[0:rows, 0:TT], x[0:rows, 1:TT + 1], ALU.subtract)
        P.stt(dst, dd[0:rows, :], cv[0:rows, CV["mu"] + g:CV["mu"] + g + 1], x[0:rows, 1:TT + 1], ALU.mult, ALU.add)

    for ti in range(nt):
        tsl = slice(ti * TT, (ti + 1) * TT)
        r4, k4, v4, kk4, ic4, lw4 = t0, t1, t2, t3, t4_, t5
        shifted(G_WLO, 64, ti, lo_w)
        P.act(lo_w, lo_w, AF.Tanh)
        shifted(G_ALO, 64, ti, lo_a)
        shifted(G_GLO, 128, ti, lo_g)
        P.act(lo_g, lo_g, AF.Sigmoid)
        if L > 0:
            uv = uT.re("(c p) t -> p c t", p=128)
            for c in range(8):
                ux = uxb[c % 2]
                xv = xvb[c % 2]
                if ti == 0:
                    P.memset(ux[:, 0:1], 0.0, writes=[ux.sub("halo")])
                    P.dma(ux[:, 1:TT + 1].sub("body"), uv[:, c, 0:TT])
                else:
                    P.dma(ux, uv[:, c, ti * TT - 1:(ti + 1) * TT])
                P.tt(xv, ux[:, 0:TT], ux[:, 1:TT + 1], ALU.subtract)
                P.stt(xv, xv, cv[:, CV["vmu"] + c:CV["vmu"] + c + 1], ux[:, 1:TT + 1], ALU.mult, ALU.add)
                P.mm(psH[0:32, 0:TT], v_dn[:, c, :], xv, start=(c == 0), stop=(c == 7))
            P.copy(vl, psH[0:32, 0:TT], eng="act")
        for h in range(4):
            hs = slice(h * 64, (h + 1) * 64)
            shifted(G_R + h, 64, ti, r4[:, h, :].sub(h))
            shifted(G_K + h, 64, ti, k4[:, h, :].sub(h))
            shifted(G_V + h, 64, ti, v4[:, h, :].sub(h))
            P.mm(psA[0:64, 0:TT], w_up[:, hs], lo_w)
            P.act(lw4[:, h, :].sub(h), psA[0:64, 0:TT], AF.Sigmoid, bias=col("w0", h))
            P.mm(psB[0:64, 0:TT], a_up[:, hs], lo_a)
            P.act(ic4[:, h, :].sub(h), psB[0:64, 0:TT], AF.Sigmoid, bias=col("a0", h))
            P.mm(psC[0:64, 0:TT], g_up[:, hs], lo_g)
            P.copy(gate4[:, h, :].sub(h), psC[0:64, 0:TT], eng="act")
            if L == 0:
                P.dma(vfT[h * 64:(h + 1) * 64, tsl].sub(h), v4[:, h, :].sub(h), q="pool")
            else:
                P.dma(vf, vfT[h * 64:(h + 1) * 64, tsl])
                P.mm(psD[0:64, 0:TT], v_upt[:, hs], vl)
                P.act(sq, psD[0:64, 0:TT], AF.Sigmoid, bias=col("vb", h))
                P.tt(vf, vf, v4[:, h, :].sub(h), ALU.subtract)
                P.tt(vf, vf, sq, ALU.mult)
                P.tt(v4[:, h, :].sub(h), v4[:, h, :].sub(h), vf, ALU.add)
            P.ts(kk4[:, h, :].sub(h), k4[:, h, :].sub(h), col("kk", h), ALU.mult)
            P.act(sq, kk4[:, h, :].sub(h), AF.Square)
            P.mm(psE[0:64, 0:TT], ones[0:64, 0:64], sq)
            rstd_from_ps(P, rs, psE[0:64, 0:TT], 1.0, 1e-12)
            P.tt(kk4[:, h, :].sub(h), kk4[:, h, :].sub(h), rs, ALU.mult)
            P.ts(sq, ic4[:, h, :].sub(h), col("ka", h), ALU.mult, oma[:, h:h + 1], ALU.add)
            P.tt(k4[:, h, :].sub(h), k4[:, h, :].sub(h), sq, ALU.mult)
            P.stt(sq, r4[:, h, :].sub(h), col("rk", h), k4[:, h, :].sub(h), ALU.mult, ALU.mult)
            P.mm(psF[0:64, 0:TT], ones[0:64, 0:64], sq)
            P.tt(bon[:, h, :].sub(h), psF[0:64, 0:TT], v4[:, h, :].sub(h), ALU.mult)
        P.ts(lw4, lw4, float(-np.exp(-0.5)), ALU.mult)
        for n in range(NCH):
            cs = slice(n * 64, (n + 1) * 64)
            for h in range(4):
                P.transpose(psG[0:64, h * 64:(h + 1) * 64], v4[:, h, cs], ident[0:64, 0:64])
            P.copy(v_tm[:, n, :, :].re("p h d -> p (h d)"), psG[0:64, 0:256], eng="act")
        P.tt(ic4, ic4, kk4, ALU.mult)
        cb_ = [lw4, v4]
        cur = 0
        for sh in (1, 2, 4, 8, 16, 32):
            src = cb_[cur].re("p h (n c) -> p (h n) c", c=64)
            dstt = cb_[1 - cur].re("p h (n c) -> p (h n) c", c=64)
            P.copy(dstt[:, :, 0:sh], src[:, :, 0:sh], eng="pool", writes=[cb_[1 - cur].sub("a")])
            P.tt(dstt[:, :, sh:64], src[:, :, sh:64], src[:, :, 0:64 - sh], ALU.add, writes=[cb_[1 - cur].sub("b")])
            cur = 1 - cur
        assert cur == 0
        cl = lw4
        cl3 = cl.re("p h (n c) -> p (h n) c", c=64)
        e = v4
        e3 = e.re("p h (n c) -> p (h n) c", c=64)
        P.act(e, cl, AF.Exp)
        P.tt(rt, r4, e, ALU.mult)
        P.memset(at.re("p h (n c) -> p (h n) c", c=64)[:, :, 0:1], 1.0, writes=[at.sub("a")])
        P.copy(at.re("p h (n c) -> p (h n) c", c=64)[:, :, 1:64], e3[:, :, 0:63], eng="pool", writes=[at.sub("b")])
        P.stt(at, at, -1.0, kk4, ALU.mult, ALU.mult)
        P.act(e, cl, AF.Exp, scale=-1.0)
        P.tt(bt, ic4, e, ALU.mult)
        P.tt(kt, k4, e, ALU.mult)
        cl4 = cl.re("p h (n c) -> p h n c", c=64)
        P.copy(WC, cl4[:, :, :, 63])
        P.tt(e.re("p h (n c) -> p h n c", c=64), V(WC.ap.unsqueeze(3).to_broadcast([64, 4, NCH, 64]), WC.key), cl4, ALU.subtract)
        P.act(e, e, AF.Exp)
        P.act(WC, WC, AF.Exp)
        P.tt(ic4, ic4, e, ALU.mult)
        P.tt(k4, k4, e, ALU.mult)
        for n in range(NCH):
            cs = slice(n * 64, (n + 1) * 64)
            for h in range(4):
                P.transpose(psG[0:64, h * 64:(h + 1) * 64], ic4[:, h, cs], ident[0:64, 0:64])
            P.copy(bh_tm[:, n, :, :].re("p h d -> p (h d)"), psG[0:64, 0:256], eng="act")
            for h in range(4):
                P.transpose(psH[0:64, h * 64:(h + 1) * 64], k4[:, h, cs], ident[0:64, 0:64])
            P.copy(kh_tm[:, n, :, :].re("p h d -> p (h d)"), psH[0:64, 0:256], eng="dve")
        for n in range(NCH):
            cb = CB[n % 2]
            cs = slice(n * 64, (n + 1) * 64)
            for h in range(4):
                hs = slice(h * 64, (h + 1) * 64)
                P.mm(psA[0:64, hs], bt[:, h, cs], at[:, h, cs])
                P.mm(psB[0:64, hs], at[:, h, cs], bt[:, h, cs])
                P.mm(psC[0:64, hs], kt[:, h, cs], at[:, h, cs])
                P.mm(psD[0:64, hs], bt[:, h, cs], rt[:, h, cs])
            P.tt(cb["B0"], psA[0:64, 0:256], K["ms4"], ALU.mult)
            P.tt(cb["A0"], psB[0:64, 0:256], K["ms4T"], ALU.mult)
            P.tt(cb["Bak"], psC[0:64, 0:256], K["ms4"], ALU.mult)
            P.tt(cb["Brb"], psD[0:64, 0:256], K["mi4"], ALU.mult)
            for h in range(4):
                hs = slice(h * 64, (h + 1) * 64)
                P.mm(psE[0:64, hs], kt[:, h, cs], rt[:, h, cs])
            P.tt(cb["Brk"], psE[0:64, 0:256], K["mi4"], ALU.mult)
            TTm = neumann_inv(P, C, cb["A0"], cb["B0"], cb, psA, psB, psC)
            for h in range(4):
                hs = slice(h * 64, (h + 1) * 64)
                P.mm(psD[0:64, hs], at[:, h, cs], STb[:, h, :], start=True, stop=False)
                P.mm(psD[0:64, hs], cb["Bak"][:, hs], v_tm[:, n, h, :], start=False, stop=True)
            P.copy(cb["X"], psD[0:64, 0:256], eng="act")
            for h in range(4):
                hs = slice(h * 64, (h + 1) * 64)
                P.mm(psE[0:64, hs], TTm[:, hs], cb["X"][:, hs])
            P.copy(cb["U"], psE[0:64, 0:256], eng="act")
            for h in range(4):
                hs = slice(h * 64, (h + 1) * 64)
                P.mm(psF[0:64, hs], STb[:, h, :], rt[:, h, cs], start=True, stop=False)
                P.mm(psF[0:64, hs], cb["U"][:, hs], cb["Brb"][:, hs], start=False, stop=False)
                P.mm(psF[0:64, hs], v_tm[:, n, h, :], cb["Brk"][:, hs], start=False, stop=True)
            P.copy(y4[:, :, cs], psF[0:64, 0:256].re("p (h c) -> p h c", h=4), eng="act")
            for h in range(4):
                hs = slice(h * 64, (h + 1) * 64)
                P.mm(psG[0:64, hs], bh_tm[:, n, h, :], cb["U"][:, hs], start=True, stop=False)
                P.mm(psG[0:64, hs], kh_tm[:, n, h, :], v_tm[:, n, h, :], start=False, stop=True)
            wcn = V(WC.ap[:, :, n].unsqueeze(2).to_broadcast([64, 4, 64]), WC.key)
            P.tt(ST, ST, wcn, ALU.mult)
            P.tt(ST, ST, psG[0:64, 0:256].re("p (h v) -> p h v", h=4), ALU.add)
            P.copy(STb, ST, eng="pool")
        for h in range(4):
            yh = y4[:, h, :]
            P.mm(psH[0:64, 0:TT], ones[0:64, 0:64], yh)
            P.stt(yh, psH[0:64, 0:TT], float(-1.0 / 64), yh, ALU.mult, ALU.add)
            P.act(sq, yh, AF.Square)
            P.mm(psH[0:64, 0:TT], ones[0:64, 0:64], sq)
            rstd_from_ps(P, rs, psH[0:64, 0:TT], 64.0, 64e-5)
            P.stt(yh, yh, col("lng", h), rs, ALU.mult, ALU.mult)
            P.stt(yh, yh, col("lnb", h), bon[:, h, :].sub(h), ALU.add, ALU.add)
            P.tt(sq, yh, gate4[:, h, :].sub(h), ALU.mult)
            P.dma(oT[h * 64:(h + 1) * 64, tsl].sub(h), sq, q="pool")
    if release:
        P.release(mk)


def phase_merge(P, C, NT, hT, oT, gT, wbr_d, wout_d, h1T, dyn=None):
    mk = P.mark()
    nt = NT // TT
    wstg = [P.tile([128, 4, 1024], name=f"f_wstg{i}") for i in range(2)]
    wbr = []
    for br in range(3):
        t = P.tile([128, 4, 1024], name=f"wbr{br}", dtype=BF16)
        P.dma(wstg[br % 2], wbr_d[br].re("(c p) n -> p c n", p=128))
        P.copy(t, wstg[br % 2], eng=("pool" if br % 2 == 0 else "act"))
        wbr.append(t)
    wout = P.tile([128, 8, 1024], name="wout", dtype=BF16)
    wov = wout_d.re("(c p) n -> p c n", p=128)
    for hh in range(2):
        P.dma(wstg[(hh + 1) % 2], wov[:, hh * 4:(hh + 1) * 4, :])
        P.copy(wout[:, hh * 4:(hh + 1) * 4, :].sub(hh), wstg[(hh + 1) % 2], eng=("act" if hh == 0 else "pool"))
    h = P.tile([128, 8, TT], name="f_h")
    o = P.tile([128, 12, TT], name="f_o")
    ob_ = P.tile([128, 12, TT], name="f_ob", dtype=BF16)
    mg = P.tile([128, 8, TT], name="f_mg32") if dyn is not None else None
    mgb = P.tile([128, 8, TT], name="f_mg", dtype=BF16)
    o2 = P.tile([128, 6, TT], name="f_o2") if dyn is not None else None
    g3 = [P.tile([128, 3, TT], name=f"f_g3{i}") for i in range(2)]
    tmp = [P.tile([128, TT], name=f"f_tmp{i}") for i in range(2)]
    tmp2 = [P.tile([128, TT], name=f"f_tmpb{i}") for i in range(2)]
    ps = [P.pbank(i) for i in range(8)]
    hv = hT.re("(c p) t -> p c t", p=128)
    ov = oT.re("(c p) t -> p c t", p=128)
    gv = gT.re("(b c p) t -> p b c t", b=3, p=128)
    h1v = h1T.re("(c p) t -> p c t", p=128)
    for ti in range(nt):
        tsl = slice(ti * TT, (ti + 1) * TT)
        if dyn is not None:
            m0 = C.cv[:, CV["m0"]:CV["m0"] + 1]
            m1 = C.cv[:, CV["m1"]:CV["m1"] + 1]
            tsl2 = slice(dyn + ti * TT, dyn + (ti + 1) * TT)
            P.dma(h, hv[:, :, tsl])
            P.dma(mg, hv[:, :, tsl2])
            P.ts(h, h, m0, ALU.mult)
            P.stt(h, mg, m1, h, ALU.mult, ALU.add)
            for part in range(2):
                cs_ = slice(part * 6, (part + 1) * 6)
                P.dma(o[:, cs_, :].sub(part), ov[:, cs_, tsl])
                P.dma(o2, ov[:, cs_, tsl2])
                P.ts(o[:, cs_, :].sub(part), o[:, cs_, :].sub(part), m0, ALU.mult)
                P.stt(o[:, cs_, :].sub(part), o2, m1, o[:, cs_, :].sub(part), ALU.mult, ALU.add)
        else:
            P.dma(h, hv[:, :, tsl])
            P.dma(o, ov[:, :, tsl])
        P.copy(ob_[:, 0:6, :].sub(0), o[:, 0:6, :], eng="pool")
        P.copy(ob_[:, 6:12, :].sub(1), o[:, 6:12, :], eng="act")
        for n in range(8):
            ns = slice(n * 128, (n + 1) * 128)
            g = g3[n % 2]
            P.dma(g, gv[:, :, n, tsl])
            for br in range(3):
                pp = ps[(n % 2) * 3 + br]
                for k in range(4):
                    P.mm(pp, wbr[br][:, k, ns], ob_[:, br * 4 + k, :], start=(k == 0), stop=(k == 3))
            tm_ = tmp[n % 2]
            P.tt(tm_, ps[(n % 2) * 3 + 0], g[:, 0, :], ALU.mult)
            P.tt(tmp2[n % 2], ps[(n % 2) * 3 + 1], g[:, 1, :], ALU.mult)
            P.tt(tm_, tm_, tmp2[n % 2], ALU.add, eng="pool")
            P.tt(tmp2[n % 2], ps[(n % 2) * 3 + 2], g[:, 2, :], ALU.mult)
            P.tt(mgb[:, n, :].sub(n), tm_, tmp2[n % 2], ALU.add, eng="pool")
        for n in range(8):
            ns = slice(n * 128, (n + 1) * 128)
            pp = ps[6 + n % 2]
            for k in range(8):
                P.mm(pp, wout[:, k, ns], mgb[:, k, :], start=(k == 0), stop=(k == 7))
            P.tt(h[:, n, :].sub(n), h[:, n, :].sub(n), pp, ALU.add)
        P.dma(h1v[:, :, tsl], h, q="pool")
    P.release(mk)


def phase_ffn(P, C, NT, h1T, h2T, gcol, experts, FF, router_d=None):
    mk = P.mark()
    cv = C.cv
    K = C.k
    ones = K["ones"]
    ident = K["ident"]
    nt = NT // TT
    NF = FF // 128
    CB = 256
    h = P.tile([128, 8, TT], name="m_h")
    u32 = P.tile([128, 8, TT], name="m_u32")
    u = P.tile([128, 8, TT], name="m_u", dtype=BF16)
    rs = P.tile([128, TT], name="m_rs")
    hid = P.tile([128, NF, TT], name="m_hid", dtype=BF16)
    sg = [P.tile([128, TT], name=f"m_sg{i}") for i in range(2)]
    wgb = [P.tile([128, 8, CB], name=f"m_wg{i}") for i in range(2)]
    wub = [P.tile([128, 8, CB], name=f"m_wu{i}") for i in range(2)]
    wgc = [P.tile([128, 8, CB], name=f"m_wgc{i}", dtype=BF16) for i in range(2)]
    wuc = [P.tile([128, 8, CB], name=f"m_wuc{i}", dtype=BF16) for i in range(2)]
    wdb = [P.tile([128, 512], name=f"m_wd{i}") for i in range(3)]
    wdc = [P.tile([128, 512], name=f"m_wdc{i}", dtype=BF16) for i in range(3)]
    ps = [P.pbank(i) for i in range(8)]
    hv = h1T.re("(c p) t -> p c t", p=128)
    h2v = h2T.re("(c p) t -> p c t", p=128)
    ne = len(experts)
    if router_d is not None:
        sel8 = P.tile([8, 1024], name="c_sel8")
        P.dma(sel8, C.sel8_d)
        rt_w = P.tile([128, 8, 8], name="m_rw")
        P.dma(rt_w, router_d.re("(c p) e -> p c e", p=128))
        lg = P.tile([8, TT], name="m_lg")
        ltm = P.tile([128, 4, 8], name="m_ltm")
        l2 = P.tile([128, 4, 8], name="m_l2")
        eq1 = P.tile([128, 4, 8], name="m_eq1")
        eq2 = P.tile([128, 4, 8], name="m_eq2")
        m1 = P.tile([128, 4], name="m_m1")
        m2 = P.tile([128, 4], name="m_m2")
        w1 = P.tile([128, 4], name="m_w1")
        w2 = P.tile([128, 4], name="m_w2")
        gwf = P.tile([8, TT], name="m_gwf")
        gwb = P.tile([128, ne, TT], name="m_gwb")
    wk = 0
    dk = 0
    for ti in range(nt):
        tsl = slice(ti * TT, (ti + 1) * TT)
        P.dma(h, hv[:, :, tsl])
        P.act(u32, h, AF.Square)
        for c in range(8):
            P.mm(ps[7], ones, u32[:, c, :], start=(c == 0), stop=(c == 7))
        rstd_from_ps(P, rs, ps[7], D, 1e-6)
        if router_d is not None:
            for c in range(8):
                P.stt(u32[:, c, :], h[:, c, :], cv[:, gcol + c:gcol + c + 1], rs, ALU.mult, ALU.mult)
            P.copy(u, u32, eng="pool")
            for c in range(8):
                P.mm(ps[6][0:8, :], rt_w[:, c, :], u32[:, c, :], start=(c == 0), stop=(c == 7))
        else:
            for c in range(8):
                P.stt(u[:, c, :], h[:, c, :], cv[:, gcol + c:gcol + c + 1], rs, ALU.mult, ALU.mult)
        if router_d is not None:
            P.copy(lg, ps[6][0:8, :], eng="act")
            for j in range(4):
                P.transpose(ps[5][:, j * 8:(j + 1) * 8], lg[:, j * 128:(j + 1) * 128], ident[0:8, 0:8])
            P.copy(ltm.re("p j e -> p (j e)"), ps[5][:, 0:32])
            bc = lambda t: V(t.ap.unsqueeze(2).to_broadcast([128, 4, 8]), t.key)
            P.op("dve", lambda e: e.reduce_max(_ap(m1), _ap(ltm), AX.X), [ltm], [m1])
            P.tt(eq1, ltm, bc(m1), ALU.is_equal)
            P.stt(l2, eq1, -1e30, ltm, ALU.mult, ALU.add)
            P.op("dve", lambda e: e.reduce_max(_ap(m2), _ap(l2), AX.X), [l2], [m2])
            P.tt(eq2, l2, bc(m2), ALU.is_equal)
            P.tt(w2, m2, m1, ALU.subtract)
            P.act(w2, w2, AF.Exp)
            P.ts(w1, w2, 1.0, ALU.add)
            P.recip(w1, w1)
            P.tt(w2, w2, w1, ALU.mult)
            P.tt(eq1, eq1, bc(w1), ALU.mult)
            P.tt(eq2, eq2, bc(w2), ALU.mult)
            P.tt(eq1, eq1, eq2, ALU.add)
            for j in range(4):
                P.transpose(ps[5][0:8, j * 128:(j + 1) * 128], eq1[:, j, :], ident)
            P.copy(gwf, ps[5][0:8, :], eng="act")
            for e_ in range(ne):
                P.mm(ps[6], sel8[:, e_ * 128:(e_ + 1) * 128], gwf)
                P.copy(gwb[:, e_, :].sub(e_), ps[6], eng="act")
        for e_, (wg_d, wu_d, wd_d) in enumerate(experts):
            wgv = wg_d.re("(c p) f -> p c f", p=128)
            wuv = wu_d.re("(c p) f -> p c f", p=128)
            wdv = wd_d.re("(f p) n -> p f n", p=128)
            for fb in range(FF // CB):
                wg_t = wgb[wk % 2]
                wu_t = wub[wk % 2]
                wk += 1
                P.dma(wg_t, wgv[:, :, fb * CB:(fb + 1) * CB])
                P.dma(wu_t, wuv[:, :, fb * CB:(fb + 1) * CB])
                wg_c = wgc[(wk - 1) % 2]
                wu_c = wuc[(wk - 1) % 2]
                P.copy(wg_c, wg_t, eng="pool")
                P.copy(wu_c, wu_t, eng="act")
                wg_t, wu_t = wg_c, wu_c
                for j in range(CB // 128):
                    f = fb * (CB // 128) + j
                    pg = ps[4 + f % 2]
                    pu = ps[6 + f % 2]
                    for c in range(8):
                        P.mm(pg, wg_t[:, c, j * 128:(j + 1) * 128], u[:, c, :], start=(c == 0), stop=(c == 7))
                    for c in range(8):
                        P.mm(pu, wu_t[:, c, j * 128:(j + 1) * 128], u[:, c, :], start=(c == 0), stop=(c == 7))
                    s_ = sg[f % 2]
                    P.act(s_, pg, AF.Silu)
                    if router_d is not None:
                        P.tt(s_, s_, gwb[:, e_, :].sub(e_), ALU.mult, eng="pool")
                    P.tt(hid[:, f, :].sub(f), s_, pu, ALU.mult)
            for half in range(2):
                for f in range(NF):
                    wd_s = wdb[dk % 3]
                    wd_t = wdc[dk % 3]
                    dk += 1
                    P.dma(wd_s, wdv[:, f, half * 512:(half + 1) * 512])
                    P.copy(wd_t, wd_s, eng=("dve" if dk % 2 == 0 else "pool"))
                    for n4 in range(4):
                        P.mm(ps[n4], wd_t[:, n4 * 128:(n4 + 1) * 128], hid[:, f, :].sub(f), start=(f == 0), stop=(f == NF - 1))
                for n4 in range(4):
                    n = half * 4 + n4
                    P.tt(h[:, n, :].sub(n), h[:, n, :].sub(n), ps[n4], ALU.add)
        P.dma(h2v[:, :, tsl], h, q="pool")
    P.release(mk)


def phase_ple(P, C, NT, h2T, pT, proj_d, pgate_d, gcol, h3T):
    mk = P.mark()
    cv = C.cv
    ones = C.k["ones"]
    nt = NT // TT
    pstg = [P.tile([128, 4, 1024], name=f"p_stg{i}") for i in range(2)]
    proj = P.tile([128, 2, 1024], name="p_proj", dtype=BF16)
    P.dma(pstg[0][:, 0:2, :], proj_d.re("(c p) n -> p c n", p=128))
    P.copy(proj, pstg[0][:, 0:2, :], eng="pool")
    pg = P.tile([128, 8, 1024], name="p_gate", dtype=BF16)
    pgv = pgate_d.re("(c p) n -> p c n", p=128)
    for hh in range(2):
        P.dma(pstg[(hh + 1) % 2], pgv[:, hh * 4:(hh + 1) * 4, :])
        P.copy(pg[:, hh * 4:(hh + 1) * 4, :].sub(hh), pstg[(hh + 1) % 2], eng=("act" if hh == 0 else "pool"))
    h = P.tile([128, 8, TT], name="p_h")
    hb_ = P.tile([128, 8, TT], name="p_hb", dtype=BF16)
    pt = P.tile([128, 2, TT], name="p_p")
    ptb = P.tile([128, 2, TT], name="p_pb", dtype=BF16)
    er = P.tile([128, 8, TT], name="p_er")
    ho = P.tile([128, 8, TT], name="p_ho")
    sq = P.tile([128, TT], name="p_sq")
    rs = P.tile([128, TT], name="p_rs")
    gp = [P.tile([128, TT], name=f"p_gp{i}") for i in range(2)]
    ps = [P.pbank(i) for i in range(8)]
    hv = h2T.re("(c p) t -> p c t", p=128)
    pv = pT.re("(c p) t -> p c t", p=128)
    h3v = h3T.re("(c p) t -> p c t", p=128)
    for ti in range(nt):
        tsl = slice(ti * TT, (ti + 1) * TT)
        P.dma(h, hv[:, :, tsl])
        P.dma(pt, pv[:, :, tsl])
        P.copy(ptb, pt, eng="pool")
        P.copy(hb_, h, eng="pool")
        for n in range(8):
            ns = slice(n * 128, (n + 1) * 128)
            pp = ps[n % 2]
            P.mm(pp, proj[:, 0, ns], ptb[:, 0, :], start=True, stop=False)
            P.mm(pp, proj[:, 1, ns], ptb[:, 1, :], start=False, stop=True)
            P.copy(er[:, n, :].sub(n), pp, eng="act")
            P.act(sq, pp, AF.Square)
            P.mm(ps[2], ones, sq, start=(n == 0), stop=(n == 7))
        rstd_from_ps(P, rs, ps[2], D, 1e-6)
        for n in range(8):
            ns = slice(n * 128, (n + 1) * 128)
            pp = ps[3 + n % 2]
            for k in range(8):
                P.mm(pp, pg[:, k, ns], hb_[:, k, :], start=(k == 0), stop=(k == 7))
            g = gp[n % 2]
            P.act(g, pp, AF.Sigmoid)
            P.stt(er[:, n, :].sub(n), er[:, n, :].sub(n), cv[:, gcol + n:gcol + n + 1], rs, ALU.mult, ALU.mult)
            P.tt(g, g, er[:, n, :].sub(n), ALU.mult)
            P.tt(ho[:, n, :].sub(n), h[:, n, :], g, ALU.add)
        P.dma(h3v[:, :, tsl], ho, q="pool")
    P.release(mk)


def own(hg, width=64):
    return slice(hg * 4 * width, (hg + 1) * 4 * width)


def col4(v):
    return np.ascontiguousarray(v.reshape(4, 64).T)


def col2(v):
    return np.ascontiguousarray(v.reshape(2, 128).T)


def mixer_host_inputs(inp, L, b, hg):
    f = np.float32
    w_in = inp["w_in"][L]
    o = own(hg)
    cols = np.concatenate([
        np.arange(0, 512)[o], np.arange(512, 1024)[o], np.arange(1024, 1536)[o],
        np.arange(1536, 1600), np.arange(1600, 1664), np.arange(1664, 1792),
        np.arange(1792, 2048), np.arange(2048, 2176), np.arange(2176, 2208),
        np.arange(2208, 2720)[o], np.arange(2720, 3232)[o], np.arange(3232, 3744)[o],
        np.arange(3760, 4272)[o],
        np.arange(3744, 3752)[hg * 4:(hg + 1) * 4], np.arange(3752, 3760)[hg * 4:(hg + 1) * 4]])
    assert len(cols) == NZ
    d = {}
    d["w_in_m"] = np.ascontiguousarray(w_in[:, cols])
    cv = np.zeros((128, NCV), f)

    def put(nm, arr):
        arr = np.asarray(arr, f)
        cv[:arr.shape[0], CV[nm]:CV[nm] + arr.shape[1]] = arr
    put("nmg", inp["norm_mix_g"][L].reshape(8, 128).T)
    mu = inp["rwkv_mu"][L]
    mucols = np.zeros((128, 15), f)
    rcols = cols[:1024]
    for g in range(15):
        seg = mu[rcols[ZOFF[g]:ZOFF[g + 1]]]
        mucols[:len(seg), g] = seg
    put("mu", mucols)
    put("w0", col4(inp["rwkv_w0"][L][o]))
    put("a0", col4(inp["rwkv_a0"][L][o]))
    put("kk", col4(inp["rwkv_k_k"][L][o]))
    put("ka", col4(inp["rwkv_k_a"][L][o]))
    put("rk", col4(inp["rwkv_r_k"][L].reshape(512)[o]))
    put("lng", col4(inp["rwkv_ln_g"][L][o]))
    put("lnb", col4(inp["rwkv_ln_b"][L][o]))
    if L > 0:
        put("vmu", inp["vres_mu"][L - 1].reshape(8, 128).T)
        put("vb", col4(inp["vres_b"][L - 1][o]))
    put("qng", inp["mla_q_norm_g"][L].reshape(2, 128).T)
    put("kvng", inp["mla_kv_norm_g"][L].reshape(128, 1))
    put("qkq", inp["mla_qk_norm_q"][L].reshape(96, 1))
    put("qkk", inp["mla_qk_norm_k"][L].reshape(96, 1))
    invf = (1.0 / (10000.0 ** (np.arange(0, 32, 2, dtype=f) / f(32)))).astype(f)
    iv = np.zeros((96, 1), f)
    iv[64:80, 0] = invf
    iv[80:96, 0] = invf
    put("invf", iv)
    put("ropec", np.full((128, 1), -np.pi, f))
    cw = inp["gdn_conv_w"][L]
    convc = np.zeros((128, 48), f)
    for seg in range(3):
        cc = cw[:, seg * 512:(seg + 1) * 512][:, o]
        for hh in range(4):
            for j in range(4):
                convc[:64, (seg * 4 + hh) * 4 + j] = cc[j, hh * 64:(hh + 1) * 64]
    put("conv", convc)
    put("alog", inp["gdn_a_log"][L][hg * 4:(hg + 1) * 4].reshape(4, 1))
    put("dtb", inp["gdn_dt_bias"][L][hg * 4:(hg + 1) * 4].reshape(4, 1))
    put("gng", inp["gdn_norm_g"][L].reshape(64, 1))
    d["cv"] = cv
    d["w_up"] = np.ascontiguousarray(inp["rwkv_w_up"][L][:, o])
    d["a_up"] = np.ascontiguousarray(inp["rwkv_a_up"][L][:, o])
    d["g_up"] = np.ascontiguousarray(inp["rwkv_g_up"][L][:, o])
    if L > 0:
        d["v_down"] = np.ascontiguousarray(inp["vres_down"][L - 1])
        d["v_up"] = np.ascontiguousarray(inp["vres_up"][L - 1][:, o])
    d["w_uq"] = np.ascontiguousarray(inp["mla_w_uq"][L][:, hg * 384:(hg + 1) * 384])
    ukv = inp["mla_w_ukv"][L].reshape(128, 8, 128)[:, hg * 4:(hg + 1) * 4, :]
    d["w_uk"] = np.ascontiguousarray(ukv[:, :, :64].reshape(128, 256))
    d["w_uv"] = np.ascontiguousarray(ukv[:, :, 64:].reshape(128, 256))
    d["pos"] = np.ascontiguousarray(inp["positions"][b:b + 1].astype(np.int32))
    for k_, v_ in consts_np().items():
        d["c_" + k_] = v_
    return d
from concourse.bass_utils import run_bass_kernel_spmd

B_, S_, NCORE = 4, 4096, 8
M_CONSTS = ["ident", "ones", "att_mask", "ropeRT", "sel4", "id4", "neg4", "neg4T", "ms4", "ms4T", "mi4"]
F_CONSTS = ["ident", "ones"]
_PROG_CACHE = {}


def build_mixer(S, L):
    P = Prog()
    C = Ctx()
    names = []

    def din(name, shape, dt=F32):
        names.append(name)
        return P.dview(P.dram(name, shape, dt, kind="ExternalInput"))
    hT = din("hT", [1024, S])
    w_d = din("w_in_m", [1024, NZ])
    cv_d = din("cv", [128, NCV])
    pos_d = din("pos", [1, S], I32)
    w_uq, w_uk, w_uv = din("w_uq", [256, 384]), din("w_uk", [128, 256]), din("w_uv", [128, 256])
    w_up, a_up, g_up = din("w_up", [64, 256]), din("a_up", [64, 256]), din("g_up", [128, 256])
    zT = P.dview(P.dram("zT", [NG, 128, S], F32, kind="Internal"))
    oT = P.dview(P.dram("oT", [768, S], F32, kind="ExternalOutput"))
    uT = v_down = v_up = None
    if L == 0:
        vfT = P.dview(P.dram("vfT_out", [256, S], F32, kind="ExternalOutput"))
    else:
        vfT = din("vfT_in", [256, S])
        uT = P.dview(P.dram("uT", [1024, S], F32, kind="Internal"))
        v_down, v_up = din("v_down", [1024, 32]), din("v_up", [32, 256])
    C.k = load_consts(P, M_CONSTS)
    names.extend(["c_" + c for c in M_CONSTS])
    C.cv = P.tile([128, NCV], name="cv")
    P.dma(C.cv, cv_d)
    groups = [(int(ZOFF[g]), int(ZG[g])) for g in range(NG)]
    phase_proj(P, C, S, hT, w_d, NZ, groups, zT, CV["nmg"], uT=uT)
    phase_rwkv(P, C, S, L, zT, V(oT.ap[0:256, :], "oT_r"), w_up, a_up, g_up, vfT, uT, v_down, v_up)
    phase_mla(P, C, S, zT, pos_d, w_uq, w_uk, w_uv, V(oT.ap[256:512, :], "oT_m"))
    phase_gdn(P, C, S, zT, V(oT.ap[512:768, :], "oT_g"))
    P.finalize()
    return P.nc, names


def build_token(NT, L):
    P = Prog()
    C = Ctx()
    names = []

    def din(name, shape, dt=F32):
        names.append(name)
        return P.dview(P.dram(name, shape, dt, kind="ExternalInput"))
    hT = din("hT", [1024, NT])
    oT = din("oT_all", [1536, NT])
    pT = din("pT", [256, NT])
    cv_d = din("cv", [128, NCV])
    w_g = din("w_gate", [1024, 3072])
    wbr = [din(f"w_br{i}", [512, 1024]) for i in range(3)]
    wout = din("w_out", [1024, 1024])
    proj = din("ple_proj", [256, 1024])
    pgate = din("ple_gate", [1024, 1024])
    if L % 2 == 0:
        experts = [(din("ffn_wg", [1024, 2816]), din("ffn_wu", [1024, 2816]), din("ffn_wd", [2816, 1024]))]
        FF = 2816
        router = None
    else:
        wg_all = din("moe_wg", [8, 1024, 3584])
        wu_all = din("moe_wu", [8, 1024, 3584])
        wd_all = din("moe_wd", [8, 3584, 1024])
        experts = [(V(wg_all.ap[e], wg_all.key), V(wu_all.ap[e], wu_all.key), V(wd_all.ap[e], wd_all.key)) for e in range(8)]
        FF = 3584
        router = din("moe_router", [1024, 8])
    gT = P.dview(P.dram("gT", [3072, NT], F32, kind="Internal"))
    h1T = P.dview(P.dram("h1T", [1024, NT], F32, kind="Internal"))
    h2T = P.dview(P.dram("h2T", [1024, NT], F32, kind="Internal"))
    h3T = P.dview(P.dram("h3T", [1024, NT], F32, kind="ExternalOutput"))
    C.k = load_consts(P, F_CONSTS)
    names.extend(["c_" + c for c in F_CONSTS])
    C.sel8_d = din("c_sel8", [8, 1024])
    C.cv = P.tile([128, NCV], name="cv")
    P.dma(C.cv, cv_d)
    groups = [(g * 128, 128) for g in range(24)]
    phase_proj(P, C, NT, hT, w_g, 3072, groups, gT, CV["nmg"], func=AF.Sigmoid, nbuf=1, zflat=True)
    phase_merge(P, C, NT, hT, oT, gT, wbr, wout, h1T)
    phase_ffn(P, C, NT, h1T, h2T, CV["nfg"], experts, FF, router)
    phase_ple(P, C, NT, h2T, pT, proj, pgate, CV["png"], h3T)
    P.finalize()
    return P.nc, names


def token_host_inputs(inp, L, half=0):
    f = np.float32
    d = {}
    cv = np.zeros((128, NCV), f)
    cv[:, CV["m0"]] = 1.0 if half == 0 else 0.0
    cv[:, CV["m1"]] = 1.0 if half == 1 else 0.0
    cv[:, CV["nmg"]:CV["nmg"] + 8] = inp["norm_mix_g"][L].reshape(8, 128).T
    cv[:, CV["nfg"]:CV["nfg"] + 8] = inp["norm_ffn_g"][L].reshape(8, 128).T
    cv[:, CV["png"]:CV["png"] + 8] = inp["ple_norm_g"][L].reshape(8, 128).T
    d["cv"] = cv
    d["w_gate"] = np.ascontiguousarray(inp["w_in"][L][:, 4272:7344])
    d["w_br0"] = inp["w_br_rwkv"][L]
    d["w_br1"] = inp["w_br_mla"][L]
    d["w_br2"] = inp["w_br_gdn"][L]
    d["w_out"] = inp["w_out"][L]
    d["ple_proj"] = inp["ple_proj"][L]
    d["ple_gate"] = inp["ple_gate"][L]
    if L % 2 == 0:
        d["ffn_wg"], d["ffn_wu"], d["ffn_wd"] = inp["ffn_wg"][L // 2], inp["ffn_wu"][L // 2], inp["ffn_wd"][L // 2]
    else:
        d["moe_wg"], d["moe_wu"], d["moe_wd"] = inp["moe_wg"][L // 2], inp["moe_wu"][L // 2], inp["moe_wd"][L // 2]
        d["moe_router"] = inp["moe_router"][L // 2]
    cs = consts_np()
    for c in F_CONSTS + ["sel8"]:
        d["c_" + c] = cs[c]
    return d


def kernel(**inputs):
    inp = {k: np.asarray(v) for k, v in inputs.items()}
    x = inp["x"].astype(np.float32)
    Bn, S, Dm = x.shape
    NT = S // 2
    hT = [np.ascontiguousarray(x[b].T) for b in range(Bn)]
    vf = [None] * NCORE
    for L in range(2):
        key = ("M", S, L)
        if key not in _PROG_CACHE:
            _PROG_CACHE[key] = build_mixer(S, L)
        nc, names = _PROG_CACHE[key]
        in_maps = []
        for core in range(NCORE):
            b, hg = core // 2, core % 2
            d = mixer_host_inputs(inp, L, b, hg)
            d["hT"] = hT[b]
            if L > 0:
                d["vfT_in"] = vf[core]
            in_maps.append({n: np.ascontiguousarray(d[n]) for n in names})
        res = run_bass_kernel_spmd(nc, in_maps, core_ids=list(range(NCORE)))
        oTs = [r["oT"] for r in res.results]
        if L == 0:
            vf = [r["vfT_out"] for r in res.results]
        key = ("F", NT, L)
        if key not in _PROG_CACHE:
            _PROG_CACHE[key] = build_token(NT, L)
        nc, names = _PROG_CACHE[key]
        th = token_host_inputs(inp, L)
        in_maps = []
        for core in range(NCORE):
            b, half = core // 2, core % 2
            tsl = slice(half * NT, (half + 1) * NT)
            d = dict(th)
            d["hT"] = hT[b][:, tsl]
            o0, o1 = oTs[2 * b], oTs[2 * b + 1]
            d["oT_all"] = np.concatenate([o0[0:256, tsl], o1[0:256, tsl], o0[256:512, tsl], o1[256:512, tsl],
                                          o0[512:768, tsl], o1[512:768, tsl]], axis=0)
            d["pT"] = inp["p"][L, b, tsl, :].T
            in_maps.append({n: np.ascontiguousarray(d[n]) for n in names})
        res = run_bass_kernel_spmd(nc, in_maps, core_ids=list(range(NCORE)))
        for b in range(Bn):
            hT[b] = np.concatenate([res.results[2 * b]["h3T"], res.results[2 * b + 1]["h3T"]], axis=1)
    out = np.stack([hT[b].T for b in range(Bn)], axis=0)
    return np.ascontiguousarray(out.astype(np.float32))


ALL_M_KEYS = ["w_in_m", "cv", "w_uq", "w_uk", "w_uv", "w_up", "a_up", "g_up"]


def build_fused(S):
    NTH = S // 2
    P = Prog()
    C = Ctx()
    names = []

    def din(name, shape, dt=F32):
        names.append(name)
        return P.dview(P.dram(name, shape, dt, kind="ExternalInput"))
    hT0 = din("hT0", [1024, S])
    pos_d = din("pos", [1, S], I32)
    pT = [din("pT0", [256, S]), din("pT1", [256, NTH])]
    C.sel8_d = din("c_sel8", [8, 1024])
    mi = {}
    for L in range(2):
        for hg in range(2):
            pre = f"m{L}{hg}_"
            d = {"w_in_m": din(pre + "w_in_m", [1024, NZ]), "cv": din(pre + "cv", [128, NCV]),
                 "w_uq": din(pre + "w_uq", [256, 384]), "w_uk": din(pre + "w_uk", [128, 256]), "w_uv": din(pre + "w_uv", [128, 256]),
                 "w_up": din(pre + "w_up", [64, 256]), "a_up": din(pre + "a_up", [64, 256]), "g_up": din(pre + "g_up", [128, 256])}
            if L > 0:
                d["v_down"] = din(pre + "v_down", [1024, 32])
                d["v_up"] = din(pre + "v_up", [32, 256])
            mi[(L, hg)] = d
    ti_ = {}
    for L in range(2):
        pre = f"t{L}_"
        d = {"cv": din(pre + "cv", [128, NCV]), "w_gate": din(pre + "w_gate", [1024, 3072]),
             "wbr": [din(pre + f"w_br{i}", [512, 1024]) for i in range(3)], "w_out": din(pre + "w_out", [1024, 1024]),
             "ple_proj": din(pre + "ple_proj", [256, 1024]), "ple_gate": din(pre + "ple_gate", [1024, 1024])}
        if L % 2 == 0:
            d["experts"] = [(din(pre + "ffn_wg", [1024, 2816]), din(pre + "ffn_wu", [1024, 2816]), din(pre + "ffn_wd", [2816, 1024]))]
            d["FF"] = 2816
            d["router"] = None
        else:
            wg_all = din(pre + "moe_wg", [8, 1024, 3584])
            wu_all = din(pre + "moe_wu", [8, 1024, 3584])
            wd_all = din(pre + "moe_wd", [8, 3584, 1024])
            d["experts"] = [(V(wg_all.ap[e], wg_all.key), V(wu_all.ap[e], wu_all.key), V(wd_all.ap[e], wd_all.key)) for e in range(8)]
            d["FF"] = 3584
            d["router"] = din(pre + "moe_router", [1024, 8])
        ti_[L] = d
    zT = P.dview(P.dram("zT", [NG, 128, S], F32, kind="Internal"))
    uT = P.dview(P.dram("uT", [1024, S], F32, kind="Internal"))
    oTa = P.dview(P.dram("oT_all", [1536, S], F32, kind="Internal"))
    vfT = P.dview(P.dram("vfT", [512, S], F32, kind="Internal"))
    gT = P.dview(P.dram("gT", [3072, S], F32, kind="Internal"))
    h1T = P.dview(P.dram("h1T", [1024, S], F32, kind="Internal"))
    h2T = P.dview(P.dram("h2T", [1024, S], F32, kind="Internal"))
    hT1 = P.dview(P.dram("hT1", [1024, S], F32, kind="Internal"))
    h3T = P.dview(P.dram("h3T", [1024, NTH], F32, kind="ExternalOutput"))
    C.k = load_consts(P, M_CONSTS)
    names.extend(["c_" + c for c in M_CONSTS])
    C.cv = P.tile([128, NCV], name="cv")
    groups = [(int(ZOFF[g]), int(ZG[g])) for g in range(NG)]
    ggroups = [(g * 128, 128) for g in range(24)]
    hin = hT0
    for L in range(2):
        for hg in range(2):
            d = mi[(L, hg)]
            P.dma(C.cv, d["cv"])
            phase_proj(P, C, S, hin, d["w_in_m"], NZ, groups, zT, CV["nmg"], uT=(uT if L > 0 else None))
            sub = lambda br: V(oTa.ap[br * 512 + hg * 256:br * 512 + (hg + 1) * 256, :], f"oT_{br}_{hg}")
            vfv = V(vfT.ap[hg * 256:(hg + 1) * 256, :], f"vfT_{hg}")
            mk_ = P.mark()
            o_r, o_g = sub(0), sub(2)
            P.run_interleaved([
                lambda: phase_rwkv(P, C, S, L, zT, o_r, d["w_up"], d["a_up"], d["g_up"], vfv,
                                   (uT if L > 0 else None), d.get("v_down"), d.get("v_up"), ttl=256, psbase=0, release=False),
                lambda: phase_gdn(P, C, S, zT, o_g, ttl=256, psbase=4, release=False)])
            P.release(mk_)
            phase_mla(P, C, S, zT, pos_d, d["w_uq"], d["w_uk"], d["w_uv"], sub(1))
        t = ti_[L]
        P.dma(C.cv, t["cv"])
        if L == 0:
            NT, dyn, hout = S, None, hT1
        else:
            NT, dyn, hout = NTH, NTH, h3T
        gv = V(gT.ap[:, 0:NT], gT.key)
        h1v = V(h1T.ap[:, 0:NT], h1T.key)
        h2v = V(h2T.ap[:, 0:NT], h2T.key)
        phase_proj(P, C, NT, hin, t["w_gate"], 3072, ggroups, gv, CV["nmg"], func=AF.Sigmoid, nbuf=1, zflat=True, dyn=dyn)
        phase_merge(P, C, NT, hin, oTa, gv, t["wbr"], t["w_out"], h1v, dyn=dyn)
        phase_ffn(P, C, NT, h1v, h2v, CV["nfg"], t["experts"], t["FF"], t["router"])
        phase_ple(P, C, NT, h2v, pT[L], t["ple_proj"], t["ple_gate"], CV["png"], hout)
        hin = hT1
    P.finalize()
    return P.nc, names


def kernel_unfused(**inputs):
    return _kernel_unfused(**inputs)


_kernel_unfused = kernel


def kernel(**inputs):
    inp = {k: np.asarray(v) for k, v in inputs.items()}
    x = inp["x"].astype(np.float32)
    Bn, S, Dm = x.shape
    NTH = S // 2
    key = ("FUSED", S)
    if key not in _PROG_CACHE:
        _PROG_CACHE[key] = build_fused(S)
    nc, names = _PROG_CACHE[key]
    cs = consts_np()
    shared = {"c_" + k: v for k, v in cs.items()}
    tok = []
    for L in range(2):
        th = token_host_inputs(inp, L)
        tok.append({f"t{L}_" + k: v for k, v in th.items() if not k.startswith("c_")})
    in_maps = []
    for core in range(NCORE):
        b, half = core // 2, core % 2
        d = dict(shared)
        d["hT0"] = x[b].T
        d["pos"] = inp["positions"][b:b + 1].astype(np.int32)
        d["pT0"] = inp["p"][0, b].T
        d["pT1"] = inp["p"][1, b, half * NTH:(half + 1) * NTH, :].T
        for L in range(2):
            for hg in range(2):
                md = mixer_host_inputs(inp, L, b, hg)
                for k, v in md.items():
                    if not k.startswith("c_") and k != "pos":
                        d[f"m{L}{hg}_" + k] = v
            d.update(tok[L])
            cvt = tok[L][f"t{L}_cv"].copy()
            cvt[:, CV["m0"]] = 1.0 if half == 0 else 0.0
            cvt[:, CV["m1"]] = 1.0 if half == 1 else 0.0
            d[f"t{L}_cv"] = cvt
        in_maps.append({n: np.ascontiguousarray(d[n]) for n in names})
    res = run_bass_kernel_spmd(nc, in_maps, core_ids=list(range(NCORE)))
    out = np.empty((Bn, S, Dm), np.float32)
    for core in range(NCORE):
        b, half = core // 2, core % 2
        out[b, half * NTH:(half + 1) * NTH, :] = res.results[core]["h3T"].T
    return out
```

```python
import numpy as np
from contextlib import ExitStack
import concourse.bass as bass
import concourse.mybir as mybir

F32 = mybir.dt.float32
BF16 = mybir.dt.bfloat16
I32 = mybir.dt.int32
ALU = mybir.AluOpType
AF = mybir.ActivationFunctionType
AX = mybir.AxisListType

ENGS = ("pe", "act", "dve", "pool", "sp")
N_DMA_SEMS = 24


class Op:
    __slots__ = ("eng", "fn", "deps", "is_dma", "idx", "sig", "slot", "slot_target", "slot_prev", "epoch")

    def __init__(self, eng, fn, is_dma):
        self.eng = eng
        self.fn = fn
        self.is_dma = is_dma
        self.deps = set()
        self.sig = 0
        self.slot = None
        self.slot_target = 0
        self.slot_prev = None


class V:
    __slots__ = ("ap", "key")

    def __init__(self, ap, key):
        self.ap = ap
        self.key = key

    def __getitem__(self, idx):
        return V(self.ap[idx], self.key)

    def sub(self, k):
        base = self.key[0] if isinstance(self.key, tuple) else self.key
        return V(self.ap, (base, k))

    def re(self, pat, **kw):
        return V(self.ap.rearrange(pat, **kw), self.key)

    def bc(self, shape):
        return V(self.ap.to_broadcast(list(shape)), self.key)

    @property
    def shape(self):
        return self.ap.shape


def _ap(x):
    return x.ap if isinstance(x, V) else x


ARENA_F32 = 53000


class Prog:
    def __init__(self, name="k"):
        self.nc = bass.Bass("TRN2", target_bir_lowering=False)
        self.st = ExitStack()
        self.arena = None
        self.aoff = 0
        self.amax = 0
        self.psum = None
        self.bar_deps = None
        self.since_bar = []
        self.bar_seen = {}
        self.epoch = 0
        self.ep_cnt = {}
        self.ops = []
        self.track = {}
        self.n_dma = 0
        self.slot_last = [None] * N_DMA_SEMS
        self.slot_count = [0] * N_DMA_SEMS
        self.uid = 0

    def sb(self, shape, dtype=F32, name=None):
        self.uid += 1
        return self.st.enter_context(self.nc.sbuf_tensor(name or f"sb{self.uid}", list(shape), dtype))

    def ps(self, shape, dtype=F32, name=None):
        self.uid += 1
        return self.st.enter_context(self.nc.psum_tensor(name or f"ps{self.uid}", list(shape), dtype))

    def dram(self, name, shape, dtype=F32, kind="Internal"):
        return self.nc.dram_tensor(name, list(shape), dtype, kind=kind)

    def tile(self, shape, name=None, dtype=None):
        if self.arena is None:
            self.arena = self.st.enter_context(self.nc.sbuf_tensor("arena", [128, ARENA_F32], F32))
        self.uid += 1
        p = shape[0]
        n = int(np.prod(shape[1:]))
        if dtype == BF16:
            nw = (n + 1) // 2
            assert self.aoff + nw <= ARENA_F32, f"arena overflow {self.aoff}+{nw}"
            ap = self.arena[0:p, self.aoff:self.aoff + nw].bitcast(BF16)[:, 0:n]
            self.aoff += nw
        else:
            assert self.aoff + n <= ARENA_F32, f"arena overflow {self.aoff}+{n}"
            ap = self.arena[0:p, self.aoff:self.aoff + n]
            self.aoff += n
        self.amax = max(self.amax, self.aoff)
        if len(shape) == 3:
            ap = ap.rearrange("p (a b) -> p a b", a=shape[1])
        elif len(shape) == 4:
            ap = ap.rearrange("p (a b c) -> p a b c", a=shape[1], b=shape[2])
        return V(ap, name or f"t{self.uid}")

    def mark(self):
        return self.aoff

    def release(self, mark):
        self.barrier()
        self.aoff = mark

    def pbank(self, i):
        if self.psum is None:
            self.psum = [self.st.enter_context(self.nc.psum_tensor(f"psb{j}", [128, 512], F32)) for j in range(8)]
        return V(self.psum[i][:], f"psb{i}")

    def pslot(self, bank, half):
        self.pbank(0)
        return V(self.psum[bank][:, half * 256:(half + 1) * 256], f"psb{bank}_{half}")

    def run_interleaved(self, fns):
        import threading
        il = {"turn": 0, "alive": [True] * len(fns), "cv": threading.Condition(), "tl": threading.local()}
        errs = []

        def nxt(i):
            n = len(fns)
            for d in range(1, n + 1):
                j = (i + d) % n
                if il["alive"][j]:
                    return j
            return i

        def runner(i, fn):
            with il["cv"]:
                while il["turn"] != i:
                    il["cv"].wait()
            il["tl"].i = i
            try:
                fn()
            except BaseException as e:
                errs.append(e)
            finally:
                with il["cv"]:
                    il["alive"][i] = False
                    il["turn"] = nxt(i)
                    il["cv"].notify_all()

        def yield_turn():
            i = getattr(il["tl"], "i", None)
            if i is None:
                return
            with il["cv"]:
                j = nxt(i)
                if j == i:
                    return
                il["turn"] = j
                il["cv"].notify_all()
                while il["turn"] != i:
                    il["cv"].wait()

        self._yield = yield_turn
        ths = [threading.Thread(target=runner, args=(i, f)) for i, f in enumerate(fns)]
        for t in ths:
            t.start()
        for t in ths:
            t.join()
        self._yield = None
        if errs:
            raise errs[0]

    def dview(self, t, name=None):
        ap = t.ap() if hasattr(t, "ap") and callable(t.ap) else t
        return V(ap, name or ap.tensor.name)

    def barrier(self):
        self.bar_deps = list(self.since_bar) if self.bar_deps is None else self.bar_deps + self.since_bar
        last = {}
        dm = []
        for o in self.bar_deps:
            if o.is_dma:
                dm.append(o)
            else:
                last[o.eng] = o
        self.bar_deps = list(last.values()) + dm[-2 * N_DMA_SEMS:]
        self.since_bar = []
        self.bar_seen = {}
        if max(self.ep_cnt.values(), default=0) > 20000:
            self.epoch += 1
            self.ep_cnt = {}

    @staticmethod
    def _key(x):
        if isinstance(x, V):
            x = x.key
        if isinstance(x, tuple):
            t, sub = x
        else:
            t, sub = x, None
        nm = t if isinstance(t, str) else (t.name if hasattr(t, "name") else t.tensor.name)
        return nm, sub

    def _conf(self, nm, sub):
        ent = self.track.setdefault(nm, {})
        if sub is None:
            return list(ent.keys())
        ks = [k for k in ent.keys() if k is None or k == sub]
        return ks

    def op(self, eng, fn, reads=(), writes=(), is_dma=False):
        o = Op(eng, fn, is_dma)
        o.epoch = self.epoch
        if not is_dma:
            self.ep_cnt[eng] = self.ep_cnt.get(eng, 0) + 1
        for r in reads:
            nm, sub = self._key(r)
            ent = self.track.setdefault(nm, {})
            for k in self._conf(nm, sub):
                w = ent[k][0]
                if w is not None:
                    o.deps.add(w)
        for wv in writes:
            nm, sub = self._key(wv)
            ent = self.track.setdefault(nm, {})
            for k in self._conf(nm, sub):
                w, rs = ent[k]
                if w is not None:
                    o.deps.add(w)
                for r in rs:
                    o.deps.add(r)
        for r in reads:
            nm, sub = self._key(r)
            ent = self.track[nm]
            if sub not in ent:
                ent[sub] = [None, []]
            ent[sub][1].append(o)
        for wv in writes:
            nm, sub = self._key(wv)
            ent = self.track[nm]
            if sub is None:
                for k in list(ent.keys()):
                    del ent[k]
            ent[sub] = [o, []]
        o.deps.discard(o)
        if self.bar_deps is not None and not self.bar_seen.get(eng):
            self.bar_seen[eng] = True
            o.deps.update(self.bar_deps)
        self.since_bar.append(o)
        if is_dma:
            s = self.n_dma % N_DMA_SEMS
            self.n_dma += 1
            o.slot = s
            o.slot_prev = self.slot_last[s]
            self.slot_count[s] += 1
            o.slot_target = 16 * self.slot_count[s]
            self.slot_last[s] = o
        o.idx = len(self.ops)
        self.ops.append(o)
        if getattr(self, "_yield", None) is not None:
            self._yield()
        return o

    def dma(self, out, in_, reads=None, writes=None, q="sp", in_fn=None, **kw):
        if in_fn is not None:
            return self.op(q, lambda e: e.dma_start(out=_ap(out), in_=in_fn(e), **kw),
                           reads if reads is not None else [in_], writes if writes is not None else [out], is_dma=True)
        return self.op(q, lambda e: e.dma_start(out=_ap(out), in_=_ap(in_), **kw),
                       reads if reads is not None else [in_], writes if writes is not None else [out], is_dma=True)

    def mm(self, out, lhsT, rhs, start=True, stop=True, reads=None, writes=None, **kw):
        return self.op("pe", lambda e: e.matmul(_ap(out), _ap(lhsT), _ap(rhs), start=start, stop=stop, **kw),
                       reads if reads is not None else [lhsT, rhs], writes if writes is not None else [out])

    def transpose(self, out, in_, ident, reads=None, writes=None):
        return self.op("pe", lambda e: e.transpose(_ap(out), _ap(in_), _ap(ident)),
                       reads if reads is not None else [in_, ident], writes if writes is not None else [out])

    def act(self, out, in_, func, bias=None, scale=1.0, reads=None, writes=None, accum_out=None, eng="act"):
        kw = {}
        rd = [in_]
        if bias is not None:
            kw["bias"] = _ap(bias)
            if not isinstance(bias, (int, float)):
                rd.append(bias)
        if not isinstance(scale, (int, float)):
            rd.append(scale)
        wr = [out]
        if accum_out is not None:
            kw["accum_out"] = _ap(accum_out)
            wr.append(accum_out)
        return self.op(eng, lambda e: e.activation(_ap(out), _ap(in_), func, scale=_ap(scale), **kw),
                       reads if reads is not None else rd, writes if writes is not None else wr)

    def tt(self, out, in0, in1, op, eng="dve", reads=None, writes=None):
        return self.op(eng, lambda e: e.tensor_tensor(_ap(out), _ap(in0), _ap(in1), op),
                       reads if reads is not None else [in0, in1], writes if writes is not None else [out])

    def ts(self, out, in0, s1, op0, s2=None, op1=None, eng="dve", reads=None, writes=None):
        rd = [in0] + [s for s in (s1, s2) if s is not None and not isinstance(s, (int, float))]
        if op1 is None:
            f = lambda e: e.tensor_scalar(_ap(out), _ap(in0), _ap(s1), None, op0)
        else:
            f = lambda e: e.tensor_scalar(_ap(out), _ap(in0), _ap(s1), _ap(s2), op0, op1)
        return self.op(eng, f, reads if reads is not None else rd, writes if writes is not None else [out])

    def stt(self, out, in0, scalar, in1, op0, op1, eng="dve", reads=None, writes=None):
        rd = [in0, in1] + ([] if isinstance(scalar, (int, float)) else [scalar])
        return self.op(eng, lambda e: e.scalar_tensor_tensor(_ap(out), _ap(in0), _ap(scalar), _ap(in1), op0, op1),
                       reads if reads is not None else rd, writes if writes is not None else [out])

    def copy(self, out, in_, eng="dve", reads=None, writes=None):
        if eng == "act":
            f = lambda e: e.copy(_ap(out), _ap(in_))
        else:
            f = lambda e: e.tensor_copy(_ap(out), _ap(in_))
        return self.op(eng, f, reads if reads is not None else [in_], writes if writes is not None else [out])

    def memset(self, ap, val, eng="pool", writes=None):
        return self.op(eng, lambda e: e.memset(_ap(ap), val), [], writes if writes is not None else [ap])

    def recip(self, out, in_, reads=None, writes=None):
        return self.op("dve", lambda e: e.reciprocal(_ap(out), _ap(in_)),
                       reads if reads is not None else [in_], writes if writes is not None else [out])

    def finalize(self, final_waits=()):
        nc = self.nc
        ops = self.ops
        needed = set()
        for o in ops:
            for d in o.deps:
                if not d.is_dma:
                    needed.add(d.idx)
        cnt = {}
        for o in ops:
            if not o.is_dma and (o.idx in needed):
                k_ = (o.eng, o.epoch)
                cnt[k_] = cnt.get(k_, 0) + 1
                o.sig = cnt[k_]
            else:
                o.sig = 0
        sems = {k_: self.st.enter_context(nc.semaphore(f"s_{k_[0]}_{k_[1]}")) for k_ in cnt}
        dsems = [self.st.enter_context(nc.semaphore(f"s_d{i}")) for i in range(N_DMA_SEMS)]
        per_eng = {e: [o for o in ops if o.eng == e] for e in ENGS}
        final = list(final_waits)

        def body(eng_name):
            def _b(e):
                waited = {}
                def wait(key, sem, val):
                    if waited.get(key, 0) >= val:
                        return
                    e.wait_ge(sem, val)
                    waited[key] = val
                for o in per_eng[eng_name]:
                    for d in sorted(o.deps, key=lambda x: x.idx):
                        if d.is_dma:
                            wait(("d", d.slot), dsems[d.slot], d.slot_target)
                        else:
                            if d.eng == eng_name and eng_name == "pe":
                                continue
                            wait(("c", d.eng, d.epoch), sems[(d.eng, d.epoch)], d.sig)
                    if o.is_dma and o.slot_prev is not None:
                        wait(("d", o.slot), dsems[o.slot], o.slot_prev.slot_target)
                    ins = o.fn(e)
                    if o.is_dma:
                        ins.then_inc(dsems[o.slot], 16)
                    elif o.sig:
                        ins.then_inc(sems[(eng_name, o.epoch)], 1)
                if eng_name == "sp":
                    for s in range(N_DMA_SEMS):
                        if self.slot_last[s] is not None:
                            wait(("d", s), dsems[s], self.slot_last[s].slot_target)
            return _b

        with nc.Block() as block:
            block.tensor(body("pe"))
            block.scalar(body("act"))
            block.vector(body("dve"))
            block.gpsimd(body("pool"))
            block.sync(body("sp"))
        self.st.close()
        return nc

D = 1024
TT = 512
ZG = [64] * 12 + [64, 64, 128] + [128, 128, 128, 32] + [64] * 16 + [4, 4]
NG = len(ZG)
G_R, G_K, G_V, G_WLO, G_ALO, G_GLO = 0, 4, 8, 12, 13, 14
G_CQ, G_CKV, G_KPE = 15, 17, 18
G_GQ, G_GK, G_GV, G_GG, G_GB, G_GA = 19, 23, 27, 31, 35, 36
ZOFF = np.concatenate([[0], np.cumsum(ZG)]).astype(int)
NZ = int(ZOFF[-1])

CV = {}
_n = 0
for nm, k in [("nmg", 8), ("mu", 15), ("w0", 4), ("a0", 4), ("kk", 4), ("ka", 4), ("rk", 4), ("lng", 4), ("lnb", 4),
              ("vmu", 8), ("vb", 4), ("qng", 2), ("kvng", 1), ("qkq", 1), ("qkk", 1), ("invf", 1),
              ("conv", 48), ("alog", 1), ("dtb", 1), ("gng", 1), ("ropec", 1), ("nfg", 8), ("png", 8), ("m0", 1), ("m1", 1)]:
    CV[nm] = _n
    _n += k
NCV = _n


def consts_np():
    c = {}
    c["ident"] = np.eye(128, dtype=np.float32)
    c["ones"] = np.ones((128, 128), np.float32)
    bd = np.zeros((128, 128), np.float32)
    bd[:64, :64] = 1
    bd[64:, 64:] = 1
    c["bd64"] = bd
    s = np.arange(64)[:, None]
    t = np.arange(64)[None, :]
    incl = (t >= s).astype(np.float32)
    strict = (t > s).astype(np.float32)
    c["m_incl"] = np.concatenate([incl, incl], 0)
    c["m_strict"] = np.concatenate([strict, strict], 0)
    c["m_incl_T"] = np.concatenate([incl.T, incl.T], 0)
    c["m_strict_T"] = np.concatenate([strict.T, strict.T], 0)
    c["neg_incl"] = (1.0 - c["m_incl"]) * -1e4
    c["neg_incl_T"] = (1.0 - c["m_incl_T"]) * -1e4
    c["id64x2"] = np.concatenate([np.eye(64, dtype=np.float32)] * 2, 0)
    kk = np.arange(128)[:, None]
    qq = np.arange(128)[None, :]
    c["att_mask"] = (qq >= kk).astype(np.float32)
    R = np.zeros((96, 96), np.float32)
    for m in range(16):
        R[64 + m, 64 + m + 16] = -1.0
        R[64 + 16 + m, 64 + m] = 1.0
    c["ropeRT"] = np.ascontiguousarray(R.T)
    sel = np.zeros((4, 4, 64), np.float32)
    for h in range(4):
        sel[h, h, :] = 1.0
    c["sel4"] = sel.reshape(4, 256)
    sel8 = np.zeros((8, 8, 128), np.float32)
    for e in range(8):
        sel8[e, e, :] = 1.0
    c["sel8"] = sel8.reshape(8, 1024)
    c["id4"] = np.tile(np.eye(64, dtype=np.float32)[:, None, :], (1, 4, 1)).reshape(64, 256)
    c["neg4"] = np.tile(c["neg_incl"][:64][:, None, :], (1, 4, 1)).reshape(64, 256)
    c["neg4T"] = np.tile(c["neg_incl_T"][:64][:, None, :], (1, 4, 1)).reshape(64, 256)
    c["ms4"] = np.tile(c["m_strict"][:64][:, None, :], (1, 4, 1)).reshape(64, 256)
    c["ms4T"] = np.tile(c["m_strict_T"][:64][:, None, :], (1, 4, 1)).reshape(64, 256)
    c["mi4"] = np.tile(c["m_incl"][:64][:, None, :], (1, 4, 1)).reshape(64, 256)
    return c


CONST_SHAPES = {k: v.shape for k, v in consts_np().items()}


class Ctx:
    pass


def load_consts(P, names):
    out = {}
    for nm in names:
        shp = CONST_SHAPES[nm]
        d = P.dview(P.dram("c_" + nm, shp, F32, kind="ExternalInput"))
        t = P.tile(list(shp), name="c_" + nm)
        P.dma(t, d)
        out[nm] = t
    return out


def rstd_from_ps(P, out, ps, n, eps):
    P.act(out, ps, AF.Ln, scale=float(1.0 / n), bias=float(eps))
    P.act(out, out, AF.Exp, scale=-0.5)


def phase_proj(P, C, S, hT, w_d, ncols, groups, zT, gcol, uT=None, func=None, nbuf=2, zflat=False, dyn=None):
    mk = P.mark()
    cv = C.cv
    w = P.tile([128, 8, ncols], name="w_in", dtype=BF16)
    wst = [P.tile([128, 8, 512], name=f"wst{i}") for i in range(2)]
    wv = w_d.re("(c p) n -> p c n", p=128)
    step = 512
    for i_, c0 in enumerate(range(0, ncols, step)):
        c1 = min(ncols, c0 + step)
        st_ = wst[i_ % 2]
        P.dma(st_[:, :, 0:c1 - c0], wv[:, :, c0:c1])
        P.copy(w[:, :, c0:c1].sub(c0), st_[:, :, 0:c1 - c0], eng=("pool" if i_ % 2 == 0 else "act"))
    hb = [P.tile([128, 8, TT], name=f"hb{i}") for i in range(nbuf)]
    sq = P.tile([128, 8, TT], name="sq")
    ub = [P.tile([128, 8, TT], name=f"ub{i}", dtype=BF16) for i in range(nbuf)]
    rs = P.tile([128, TT], name="rs")
    stg = [P.tile([128, TT], name=f"stg{i}") for i in range(4)]
    hv = hT.re("(c p) t -> p c t", p=128)
    ps_ss = P.pbank(0)
    pz = [P.pbank(1 + i) for i in range(4)]
    nt = S // TT
    k = 0
    for ti in range(nt):
        tsl = slice(ti * TT, (ti + 1) * TT)
        h = hb[ti % nbuf]
        u = ub[ti % nbuf]
        if dyn is not None:
            P.dma(h, hv[:, :, tsl])
            P.dma(sq, hv[:, :, dyn + ti * TT:dyn + (ti + 1) * TT])
            P.ts(h, h, cv[:, CV["m0"]:CV["m0"] + 1], ALU.mult)
            P.stt(h, sq, cv[:, CV["m1"]:CV["m1"] + 1], h, ALU.mult, ALU.add)
        else:
            P.dma(h, hv[:, :, tsl])
        P.act(sq, h, AF.Square)
        for c in range(8):
            P.mm(ps_ss, C.k["ones"], sq[:, c, :], start=(c == 0), stop=(c == 7))
        rstd_from_ps(P, rs, ps_ss, D, 1e-6)
        if uT is not None:
            for c in range(8):
                P.stt(sq[:, c, :], h[:, c, :], cv[:, gcol + c:gcol + c + 1], rs, ALU.mult, ALU.mult)
            P.dma(uT.re("(c p) t -> p c t", p=128)[:, :, tsl], sq, q="pool")
            P.copy(u, sq, eng="pool")
        else:
            for c in range(8):
                P.stt(u[:, c, :], h[:, c, :], cv[:, gcol + c:gcol + c + 1], rs, ALU.mult, ALU.mult)
        for gi, (co, n) in enumerate(groups):
            pp = pz[k % 4]
            st = stg[k % 4]
            for c in range(8):
                P.mm(pp[0:n, :], w[:, c, co:co + n], u[:, c, :], start=(c == 0), stop=(c == 7))
            if func is not None:
                P.act(st[0:n, :], pp[0:n, :], func)
            else:
                P.copy(st[0:n, :], pp[0:n, :], eng=("act" if k % 2 == 0 else "dve"))
            if zflat:
                P.dma(zT[co:co + n, tsl].sub(gi), st[0:n, :], q="pool")
            else:
                P.dma(zT[gi, 0:n, tsl].sub(gi), st[0:n, :], q="pool")
            k += 1
    P.release(mk)


def phase_mla(P, C, S, zT, pos_d, w_uq_d, w_uk_d, w_uv_d, oT):
    mk = P.mark()
    cv = C.cv
    K = C.k
    nt = S // TT
    wuq_f = P.tile([128, 2, 384], name="wuq_f")
    P.dma(wuq_f, w_uq_d.re("(c p) n -> p c n", p=128))
    wuk_f = P.tile([128, 256], name="wuk_f")
    P.dma(wuk_f, w_uk_d)
    wuv_f = P.tile([128, 256], name="wuv_f")
    P.dma(wuv_f, w_uv_d)
    wuq = P.tile([128, 2, 384], name="wuq", dtype=BF16)
    wuk = P.tile([128, 256], name="wuk", dtype=BF16)
    wuv = P.tile([128, 256], name="wuv", dtype=BF16)
    P.copy(wuq, wuq_f, eng="pool")
    P.copy(wuk, wuk_f, eng="pool")
    P.copy(wuv, wuv_f, eng="pool")
    ones_b = P.tile([128, 64], name="ones_b", dtype=BF16)
    P.copy(ones_b, K["ones"][:, 0:64], eng="pool")
    KT = P.tile([96, 4, S], name="KT", dtype=BF16)
    VT = P.tile([128, S // 128, 256], name="Vtm", dtype=BF16)
    rc = P.tile([96, TT], name="rope_c")
    rsn = P.tile([96, TT], name="rope_s")
    posi = P.tile([96, TT], name="posi")
    posf = P.tile([96, TT], name="posf")
    ang = P.tile([96, TT], name="ang")
    tmp = P.tile([96, TT], name="ropetmp")
    tmp2 = P.tile([96, TT], name="ropetmp2")
    P.memset(rc[0:64, :], 1.0, writes=[rc.sub("lo")])
    P.memset(rsn[0:64, :], 0.0, writes=[rsn.sub("lo")])
    pi_ap = V(posi.ap.bitcast(I32), posi.key)
    invf = cv[64:96, CV["invf"]:CV["invf"] + 1]
    negpi = cv[64:96, CV["ropec"]:CV["ropec"] + 1]
    TWO_PI = float(2 * np.pi)

    def rope_tile(ti):
        tsl = slice(ti * TT, (ti + 1) * TT)
        P.dma(pi_ap[64:96, :], V(pos_d.ap[:, tsl].partition_broadcast(32), pos_d.key))
        P.copy(posf[64:96, :], pi_ap[64:96, :])
        P.ts(ang[64:96, :], posf[64:96, :], invf, ALU.mult)
        for dst, shift in ((rsn, 0.0), (rc, float(np.pi / 2))):
            a_ = ang[64:96, :]
            if shift:
                P.ts(tmp2[64:96, :], ang[64:96, :], shift, ALU.add)
                a_ = tmp2[64:96, :]
            P.ts(tmp[64:96, :], a_, float(1.0 / TWO_PI), ALU.mult)
            P.copy(pi_ap[64:96, :], tmp[64:96, :])
            P.copy(tmp[64:96, :], pi_ap[64:96, :])
            P.stt(tmp[64:96, :], tmp[64:96, :], -TWO_PI, a_, ALU.mult, ALU.add)
            P.ts(posf[64:96, :], tmp[64:96, :], float(np.pi), ALU.is_gt, TWO_PI, ALU.mult)
            P.tt(tmp[64:96, :], tmp[64:96, :], posf[64:96, :], ALU.subtract)
            P.act(dst[64:96, :], tmp[64:96, :], AF.Sin, writes=[dst.sub("hi")])

    ones = K["ones"]
    cq = [P.tile([128, 2, TT], name=f"cq{i}") for i in range(2)]
    ckv = [P.tile([128, TT], name=f"ckv{i}") for i in range(2)]
    cqb = [P.tile([128, 2, TT], name=f"cqb{i}", dtype=BF16) for i in range(2)]
    ckvb = [P.tile([128, TT], name=f"ckvb{i}", dtype=BF16) for i in range(2)]
    kpe = [P.tile([96, TT], name=f"kpe{i}") for i in range(2)]
    sq = P.tile([128, 2, TT], name="msq")
    rs = P.tile([128, TT], name="mrs")
    raw = P.tile([96, TT], name="raw")
    nrm = P.tile([96, TT], name="nrm")
    rot = P.tile([96, TT], name="rot")
    QT = P.tile([96, 4, TT], name="QT", dtype=BF16)
    ps_a = P.pbank(0)
    ps_b = P.pbank(1)
    ps_c = P.pbank(2)

    def qk_finish(src_raw, gcolname, dst, tsl):
        P.act(sq[0:96, 0, :], src_raw, AF.Square)
        P.mm(ps_b[0:96, :], ones[0:96, 0:96], sq[0:96, 0, :])
        rstd_from_ps(P, rs[0:96, :], ps_b[0:96, :], 96, 1e-6)
        g = cv[0:96, CV[gcolname]:CV[gcolname] + 1]
        P.stt(nrm, src_raw, g, rs[0:96, :], ALU.mult, ALU.mult)
        P.mm(ps_c[0:96, :], K["ropeRT"], nrm)
        P.tt(rot, ps_c[0:96, :], rsn, ALU.mult)
        P.tt(nrm, nrm, rc, ALU.mult, eng="pool")
        P.tt(dst, nrm, rot, ALU.add)

    def load_norm(ti, want_q):
        tsl = slice(ti * TT, (ti + 1) * TT)
        i2 = ti % 2
        if want_q:
            P.dma(cq[i2], zT[G_CQ:G_CQ + 2, :, tsl].re("g p t -> p g t"))
            P.act(sq, cq[i2], AF.Square)
            P.mm(ps_a, ones, sq[:, 0, :], start=True, stop=False)
            P.mm(ps_a, ones, sq[:, 1, :], start=False, stop=True)
            rstd_from_ps(P, rs, ps_a, 256, 1e-6)
            for c in range(2):
                P.stt(cqb[i2][:, c, :], cq[i2][:, c, :], cv[:, CV["qng"] + c:CV["qng"] + c + 1], rs, ALU.mult, ALU.mult)
        else:
            P.dma(ckv[i2], zT[G_CKV, :, tsl])
            P.dma(kpe[i2][64:96, :], zT[G_KPE, 0:32, tsl])
            P.act(sq[:, 0, :], ckv[i2], AF.Square)
            P.mm(ps_a, ones, sq[:, 0, :])
            rstd_from_ps(P, rs, ps_a, 128, 1e-6)
            P.stt(ckvb[i2], ckv[i2], cv[:, CV["kvng"]:CV["kvng"] + 1], rs, ALU.mult, ALU.mult)
        return tsl, i2

    for ti in range(nt):
        rope_tile(ti)
        tsl, i2 = load_norm(ti, False)
        for h in range(4):
            P.mm(ps_b[0:64, :], wuk[:, h * 64:(h + 1) * 64], ckvb[i2])
            P.copy(raw[0:64, :], ps_b[0:64, :], eng="act", writes=[raw.sub("lo")])
            P.copy(raw[64:96, :], kpe[i2][64:96, :], eng="pool", writes=[raw.sub("hi")])
            qk_finish(raw, "qkk", KT[:, h, tsl].sub(h), tsl)
        for j in range(TT // 128):
            P.mm(ps_c[:, 0:256], ckvb[i2][:, j * 128:(j + 1) * 128], wuv)
            P.copy(VT[:, ti * (TT // 128) + j, :], ps_c[:, 0:256], eng="act")

    pt = [P.tile([128, TT], name=f"pt{i}", dtype=BF16) for i in range(3)]
    osb = P.tile([64, TT], name="osb")
    lsb = P.tile([64, TT], name="lsb")
    ps_s = [P.pbank(3), P.pbank(4)]
    ps_o = P.pbank(5)
    ps_l = P.pbank(6)
    scale = float(96 ** -0.5)
    for ti in range(nt):
        rope_tile(ti)
        tsl, i2 = load_norm(ti, True)
        for h in range(4):
            for c in range(2):
                P.mm(ps_b[0:96, :], wuq[:, c, h * 96:(h + 1) * 96], cqb[i2][:, c, :], start=(c == 0), stop=(c == 1))
            P.copy(raw, ps_b[0:96, :], eng="act")
            qk_finish(raw, "qkq", QT[:, h, :].sub(h), tsl)
        for h in range(4):
            nkc = 4 * (ti + 1)

            def c0_of(kc):
                j = kc - 4 * ti
                return 0 if j <= 0 else j * 128

            def score(kc):
                c0 = c0_of(kc)
                P.mm(ps_s[kc % 2][:, c0:TT], KT[:, h, kc * 128:(kc + 1) * 128].sub(h), QT[:, h, c0:TT].sub(h))
            score(0)
            for kc in range(nkc):
                if kc + 1 < nkc:
                    score(kc + 1)
                j = kc - 4 * ti
                c0 = c0_of(kc)
                pss = ps_s[kc % 2]
                p_t = pt[kc % 3]
                P.act(p_t[:, c0:TT], pss[:, c0:TT], AF.Exp, scale=scale)
                if j >= 0:
                    P.tt(p_t[:, c0:c0 + 128], p_t[:, c0:c0 + 128], K["att_mask"], ALU.mult, eng="pool")
                P.mm(ps_o[0:64, c0:TT], VT[:, kc, h * 64:(h + 1) * 64], p_t[:, c0:TT], start=(kc == 0), stop=(kc == nkc - 1))
                P.mm(ps_l[0:64, c0:TT], ones_b, p_t[:, c0:TT], start=(kc == 0), stop=(kc == nkc - 1))
            P.act(lsb, ps_l[0:64, :], AF.Ln)
            P.act(lsb, lsb, AF.Exp, scale=-1.0)
            P.tt(osb, ps_o[0:64, :], lsb, ALU.mult)
            P.dma(oT[h * 64:(h + 1) * 64, tsl].sub(h), osb, q="pool")
    P.release(mk)


def neumann_inv(P, C, A0, B0, bufs, ps1, ps2, ps3):
    id4 = C.k["id4"]
    TTt = bufs["TT"]
    P.tt(TTt, B0, id4, ALU.add)
    A = [A0, bufs["A1"]]
    B = [B0, bufs["B1"]]
    for k in range(1, 6):
        a_prev, a_new = A[(k - 1) % 2], A[k % 2]
        b_prev, b_new = B[(k - 1) % 2], B[k % 2]
        for h in range(4):
            hs = slice(h * 64, (h + 1) * 64)
            P.mm(ps1[0:64, hs], b_prev[:, hs], a_prev[:, hs])
        if k < 5:
            for h in range(4):
                hs = slice(h * 64, (h + 1) * 64)
                P.mm(ps2[0:64, hs], a_prev[:, hs], b_prev[:, hs])
        P.copy(a_new, ps1[0:64, 0:256], eng="act")
        if k < 5:
            P.copy(b_new, ps2[0:64, 0:256], eng="dve")
        for h in range(4):
            hs = slice(h * 64, (h + 1) * 64)
            P.mm(ps3[0:64, hs], a_new[:, hs], TTt[:, hs])
        P.tt(TTt, TTt, ps3[0:64, 0:256], ALU.add)
    return TTt


def phase_gdn(P, C, S, zT, oT, ttl=512, psbase=None, release=True):
    mk = P.mark()
    TT = ttl
    cv = C.cv
    K = C.k
    nt = S // TT
    NCH = TT // 64
    ones = K["ones"]
    ident = K["ident"]
    cvw = lambda seg, h, j: cv[0:64, CV["conv"] + (seg * 4 + h) * 4 + j:CV["conv"] + (seg * 4 + h) * 4 + j + 1]
    St = P.tile([64, 4, 64], name="gS")
    P.memset(St, 0.0)
    Sb = P.tile([64, 4, 64], name="gSb", dtype=BF16)
    P.copy(Sb, St, eng="pool")
    nA = P.tile([4, 1], name="nA")
    P.act(nA, cv[0:4, CV["alog"]:CV["alog"] + 1], AF.Exp)
    P.ts(nA, nA, -1.0, ALU.mult)
    def mkbuf(i):
        b = {}
        for nm in ["q", "k", "kb", "qd"]:
            b[nm] = P.tile([64, 4, TT], name=f"g{nm}{i}", dtype=BF16)
        b["k32"] = P.tile([64, 4, TT], name=f"gk32{i}")
        b["q32"] = P.tile([64, 4, TT], name=f"gq32{i}")
        b["ktm"] = P.tile([64, NCH, 4, 64], name=f"gktm{i}", dtype=BF16)
        b["bv"] = P.tile([64, NCH, 4, 64], name=f"gbv{i}")
        b["bg"] = P.tile([64, NCH, 12], name=f"gbg{i}")
        b["c2"] = P.tile([64, NCH, 4], name=f"gc2{i}")
        b["ngc"] = P.tile([64, NCH, 4], name=f"gngc{i}")
        b["dl"] = P.tile([64, 4, NCH], name=f"gdl{i}")
        return b
    TB = [mkbuf(0), mkbuf(1)]
    xin = [P.tile([64, TT + 3], name=f"gxin{i}") for i in range(3)]
    acc = [P.tile([64, TT], name=f"gacc{i}") for i in range(2)]
    vfm = P.tile([64, 4, TT], name="gvfm")
    sq = P.tile([64, TT], name="gsq")
    rs = P.tile([64, TT], name="grs")
    bfm = P.tile([4, TT], name="gbfm")
    gfm = [P.tile([4, TT], name=f"ggfm{i}") for i in range(2)]
    efm = P.tile([4, TT], name="gefm")
    kdf = P.tile([4, TT], name="gkdf")
    gl4 = P.tile([4, NCH], name="ggl4")
    ob = [P.tile([64, 4, TT], name=f"gob{i}") for i in range(2)]
    gate = P.tile([64, TT], name="ggate")
    def mkch(i):
        d_ = {nm: P.tile([64, 256], name=f"gc_{nm}{i}") for nm in ["E", "F", "G1", "G2", "Gs"]}
        d_.update({nm: P.tile([64, 256], name=f"gc_{nm}{i}", dtype=BF16) for nm in ["A0", "B0", "A1", "B1", "TT", "Ain", "X", "vn"]})
        return d_
    CB = [mkch(0), mkch(1)]
    if psbase is None:
        psA, psB, psC, psD, psE, psF, psG, psH = [P.pbank(i) for i in range(8)]
    else:
        psA, psB, psC, psD, psE, psF, psG, psH = [P.pbank(psbase + j % 4) for j in range(8)]
    xk = 0
    for ti in range(nt):
        tb = TB[ti % 2]
        tsl = slice(ti * TT, (ti + 1) * TT)
        for seg, (g0, dst) in enumerate([(G_GQ, tb["q32"]), (G_GK, tb["k32"]), (G_GV, vfm)]):
            for h in range(4):
                x = xin[xk % 3]
                a = acc[xk % 2]
                xk += 1
                if ti == 0:
                    P.memset(x[:, 0:3], 0.0, writes=[x.sub("halo")])
                    P.dma(x[:, 3:TT + 3].sub("body"), zT[g0 + h, 0:64, 0:TT])
                else:
                    P.dma(x, zT[g0 + h, 0:64, ti * TT - 3:(ti + 1) * TT])
                P.ts(a, x[:, 0:TT], cvw(seg, h, 0), ALU.mult)
                for j in range(1, 4):
                    P.stt(a, x[:, j:TT + j], cvw(seg, h, j), a, ALU.mult, ALU.add)
                if seg == 2:
                    P.act(dst[:, h, :].sub(h), a, AF.Silu)
                else:
                    P.act(a, a, AF.Silu)
                    P.act(sq, a, AF.Square)
                    P.mm(psA[0:64, 0:TT], ones[0:64, 0:64], sq)
                    rstd_from_ps(P, rs, psA[0:64, 0:TT], 1.0, 1e-12)
                    P.stt(dst[:, h, :].sub(h), a, (0.125 if seg == 0 else 1.0), rs, ALU.mult, ALU.mult)
        P.dma(bfm, zT[G_GB, 0:4, tsl])
        P.act(bfm, bfm, AF.Sigmoid)
        g0t = gfm[0]
        P.dma(g0t, zT[G_GA, 0:4, tsl])
        P.act(g0t, g0t, AF.Exp, bias=cv[0:4, CV["dtb"]:CV["dtb"] + 1])
        P.act(g0t, g0t, AF.Ln, bias=1.0)
        P.ts(g0t, g0t, nA[:, 0:1], ALU.mult)
        cur = 0
        for sh in (1, 2, 4, 8, 16, 32):
            src = gfm[cur].re("h (n c) -> h n c", c=64)
            dstt = gfm[1 - cur].re("h (n c) -> h n c", c=64)
            P.copy(dstt[:, :, 0:sh], src[:, :, 0:sh], eng="pool", writes=[gfm[1 - cur].sub("a")])
            P.tt(dstt[:, :, sh:64], src[:, :, sh:64], src[:, :, 0:64 - sh], ALU.add, writes=[gfm[1 - cur].sub("b")])
            cur = 1 - cur
        gc = gfm[cur]
        P.act(efm, gc, AF.Exp)
        gc3 = gc.re("h (n c) -> h n c", c=64)
        P.copy(gl4, gc3[:, :, 63])
        P.tt(kdf.re("h (n c) -> h n c", c=64), V(gl4.ap.unsqueeze(2).to_broadcast([4, NCH, 64]), gl4.key), gc3, ALU.subtract)
        P.act(kdf, kdf, AF.Exp)
        for h in range(4):
            P.mm(psA[0:64, h * NCH:(h + 1) * NCH], K["sel4"][:, h * 64:(h + 1) * 64], gl4)
        P.act(tb["dl"].re("p h n -> p (h n)"), psA[0:64, 0:4 * NCH], AF.Exp)
        P.copy(tb["k"], tb["k32"], eng="pool")
        P.copy(tb["q"], tb["q32"], eng="pool")
        for h in range(4):
            P.mm(psB[0:64, 0:TT], K["sel4"][:, h * 64:(h + 1) * 64], bfm)
            P.tt(tb["kb"][:, h, :].sub(h), tb["k32"][:, h, :].sub(h), psB[0:64, 0:TT], ALU.mult)
            P.mm(psC[0:64, 0:TT], K["sel4"][:, h * 64:(h + 1) * 64], efm)
            P.tt(tb["qd"][:, h, :].sub(h), tb["q32"][:, h, :].sub(h), psC[0:64, 0:TT], ALU.mult)
        for n in range(NCH):
            cs = slice(n * 64, (n + 1) * 64)
            for h in range(4):
                P.transpose(psD[0:64, h * 64:(h + 1) * 64], tb["k32"][:, h, cs].sub(h), ident[0:64, 0:64])
            P.copy(tb["ktm"][:, n, :, :].re("p h d -> p (h d)"), psD[0:64, 0:256], eng="act")
            for h in range(4):
                P.transpose(psE[0:64, h * 64:(h + 1) * 64], vfm[:, h, cs].sub(h), ident[0:64, 0:64])
            P.copy(tb["bv"][:, n, :, :].re("p h d -> p (h d)"), psE[0:64, 0:256], eng="dve")
            P.transpose(psF[0:64, 0:4], bfm[:, cs], ident[0:4, 0:4])
            P.transpose(psF[0:64, 4:8], gc[:, cs], ident[0:4, 0:4])
            P.transpose(psF[0:64, 8:12], kdf[:, cs], ident[0:4, 0:4])
            P.copy(tb["bg"][:, n, :], psF[0:64, 0:12], eng="act")
        bg = tb["bg"]
        P.ts(tb["ngc"], bg[:, :, 4:8], -1.0, ALU.mult)
        P.act(tb["c2"], bg[:, :, 4:8], AF.Exp)
        P.stt(tb["c2"], tb["c2"], -1.0, bg[:, :, 0:4], ALU.mult, ALU.mult)
        P.tt(tb["ktm"], tb["ktm"], V(bg.ap[:, :, 8:12].unsqueeze(3).to_broadcast([64, NCH, 4, 64]), bg.key), ALU.mult)
        P.tt(tb["bv"], tb["bv"], V(bg.ap[:, :, 0:4].unsqueeze(3).to_broadcast([64, NCH, 4, 64]), bg.key), ALU.mult)
        o_t = ob[ti % 2]
        for n in range(NCH):
            cb = CB[n % 2]
            cs = slice(n * 64, (n + 1) * 64)
            gcn = V(bg.ap[:, n, 4:8].unsqueeze(2).to_broadcast([64, 4, 64]), bg.key)
            ngcn = V(tb["ngc"].ap[:, n, :].unsqueeze(2).to_broadcast([64, 4, 64]), tb["ngc"].key)
            E3 = cb["E"].re("p (h c) -> p h c", h=4)
            F3 = cb["F"].re("p (h c) -> p h c", h=4)
            P.tt(E3, K["id4"].re("p (h c) -> p h c", h=4), gcn, ALU.mult)
            P.tt(F3, K["neg4"].re("p (h c) -> p h c", h=4), ngcn, ALU.add)
            P.mm(psG[0:64, 0:256], ones[0:64, 0:64], cb["E"], start=True, stop=False)
            P.mm(psG[0:64, 0:256], ident[0:64, 0:64], cb["F"], start=False, stop=True)
            P.act(cb["G1"], psG[0:64, 0:256], AF.Exp)
            P.ts(cb["E"], cb["E"], -1.0, ALU.mult)
            P.tt(F3, K["neg4T"].re("p (h c) -> p h c", h=4), gcn, ALU.add)
            P.mm(psH[0:64, 0:256], ones[0:64, 0:64], cb["E"], start=True, stop=False)
            P.mm(psH[0:64, 0:256], ident[0:64, 0:64], cb["F"], start=False, stop=True)
            P.act(cb["G2"], psH[0:64, 0:256], AF.Exp)
            P.tt(cb["Gs"], cb["G1"], K["ms4"], ALU.mult, eng="pool")
            P.tt(cb["G2"], cb["G2"], K["ms4T"], ALU.mult, eng="pool")
            for h in range(4):
                hs = slice(h * 64, (h + 1) * 64)
                P.mm(psA[0:64, hs], tb["k"][:, h, cs].sub(h), tb["kb"][:, h, cs].sub(h))
                P.mm(psB[0:64, hs], tb["kb"][:, h, cs].sub(h), tb["k"][:, h, cs].sub(h))
                P.mm(psC[0:64, hs], tb["k"][:, h, cs].sub(h), tb["q"][:, h, cs].sub(h))
            P.stt(cb["B0"], psA[0:64, 0:256], -1.0, cb["Gs"], ALU.mult, ALU.mult)
            P.stt(cb["A0"], psB[0:64, 0:256], -1.0, cb["G2"], ALU.mult, ALU.mult)
            P.tt(cb["Ain"], psC[0:64, 0:256], cb["G1"], ALU.mult)
            TTm = neumann_inv(P, C, cb["A0"], cb["B0"], cb, psA, psB, psC)
            for h in range(4):
                hs = slice(h * 64, (h + 1) * 64)
                P.mm(psD[0:64, hs], tb["k"][:, h, cs].sub(h), Sb[:, h, :])
            X3 = cb["X"].re("p (h v) -> p h v", h=4)
            c2n = V(tb["c2"].ap[:, n, :].unsqueeze(2).to_broadcast([64, 4, 64]), tb["c2"].key)
            P.tt(X3, psD[0:64, 0:256].re("p (h v) -> p h v", h=4), c2n, ALU.mult)
            P.tt(X3, X3, tb["bv"][:, n, :, :], ALU.add)
            for h in range(4):
                hs = slice(h * 64, (h + 1) * 64)
                P.mm(psE[0:64, hs], TTm[:, hs], cb["X"][:, hs])
            P.copy(cb["vn"], psE[0:64, 0:256], eng="act")
            for h in range(4):
                hs = slice(h * 64, (h + 1) * 64)
                P.mm(psF[0:64, hs], Sb[:, h, :], tb["qd"][:, h, cs].sub(h), start=True, stop=False)
                P.mm(psF[0:64, hs], cb["vn"][:, hs], cb["Ain"][:, hs], start=False, stop=True)
            P.copy(o_t[:, :, cs], psF[0:64, 0:256].re("p (h c) -> p h c", h=4), eng="act")
            for h in range(4):
                hs = slice(h * 64, (h + 1) * 64)
                P.mm(psG[0:64, hs], tb["ktm"][:, n, h, :], cb["vn"][:, hs])
            dln = V(tb["dl"].ap[:, :, n].unsqueeze(2).to_broadcast([64, 4, 64]), tb["dl"].key)
            P.tt(St, St, dln, ALU.mult)
            P.tt(St, St, psG[0:64, 0:256].re("p (h v) -> p h v", h=4), ALU.add)
            P.copy(Sb, St, eng="pool")
        for h in range(4):
            P.dma(gate, zT[G_GG + h, 0:64, tsl])
            P.act(gate, gate, AF.Silu)
            P.act(sq, o_t[:, h, :], AF.Square)
            P.mm(psH[0:64, 0:TT], ones[0:64, 0:64], sq)
            rstd_from_ps(P, rs, psH[0:64, 0:TT], 64.0, 1e-6)
            P.stt(rs, rs, cv[0:64, CV["gng"]:CV["gng"] + 1], gate, ALU.mult, ALU.mult)
            P.tt(sq, o_t[:, h, :], rs, ALU.mult)
            P.dma(oT[h * 64:(h + 1) * 64, tsl].sub(h), sq, q="pool")
    if release:
        P.release(mk)


def phase_rwkv(P, C, S, L, zT, oT, w_up_d, a_up_d, g_up_d, vfT, uT, v_down_d, v_up_d, ttl=512, psbase=None, release=True):
    mk = P.mark()
    TT = ttl
    cv = C.cv
    K = C.k
    nt = S // TT
    NCH = TT // 64
    ones = K["ones"]
    ident = K["ident"]
    col = lambda nm, h: cv[0:64, CV[nm] + h:CV[nm] + h + 1]
    w_up = P.tile([64, 256], name="r_wup"); P.dma(w_up, w_up_d)
    a_up = P.tile([64, 256], name="r_aup"); P.dma(a_up, a_up_d)
    g_up = P.tile([128, 256], name="r_gup"); P.dma(g_up, g_up_d)
    if L > 0:
        v_dn = P.tile([128, 8, 32], name="r_vdn"); P.dma(v_dn, v_down_d.re("(c p) n -> p c n", p=128))
        v_upt = P.tile([32, 256], name="r_vup"); P.dma(v_upt, v_up_d)
    oma = P.tile([64, 4], name="r_oma")
    P.ts(oma, cv[0:64, CV["ka"]:CV["ka"] + 4], -1.0, ALU.mult, 1.0, ALU.add)
    ST = P.tile([64, 4, 64], name="rST")
    P.memset(ST, 0.0)
    STb = P.tile([64, 4, 64], name="rSTb", dtype=BF16)
    P.copy(STb, ST, eng="pool")
    T4 = lambda nm: P.tile([64, 4, TT], name=nm)
    T4b = lambda nm: P.tile([64, 4, TT], name=nm, dtype=BF16)
    at, bt, kt, rt = T4b("r_at"), T4b("r_bt"), T4b("r_kt"), T4b("r_rt")
    bon, gate4 = T4("r_bon"), T4("r_gate")
    t0, t1, t2, t3, t4_, t5 = [T4(f"r_t{i}") for i in range(6)]
    y4 = t0
    bh_tm = P.tile([64, NCH, 4, 64], name="r_bhtm", dtype=BF16)
    kh_tm = P.tile([64, NCH, 4, 64], name="r_khtm", dtype=BF16)
    v_tm = P.tile([64, NCH, 4, 64], name="r_vtm", dtype=BF16)
    WC = P.tile([64, 4, NCH], name="r_WC")
    xin = [P.tile([128, TT + 1], name=f"r_xin{i}") for i in range(2)]
    dd = P.tile([128, TT], name="r_dd")
    lo_w = P.tile([64, TT], name="r_low")
    lo_a = P.tile([64, TT], name="r_loa")
    lo_g = P.tile([128, TT], name="r_log")
    sq = P.tile([64, TT], name="r_sq")
    rs = P.tile([64, TT], name="r_rs")
    if L > 0:
        uxb = [P.tile([128, TT + 1], name=f"r_ux{i}") for i in range(2)]
        xvb = [P.tile([128, TT], name=f"r_xv{i}") for i in range(2)]
        vl = P.tile([32, TT], name="r_vl")
        vf = rs

    def mkch(i):
        d_ = {nm: P.tile([64, 256], name=f"rc_{nm}{i}", dtype=BF16) for nm in ["A0", "B0", "A1", "B1", "TT", "Bak", "Brb", "Brk"]}
        d_["X"] = d_["A1"]
        d_["U"] = d_["B1"]
        return d_
    CB = [mkch(0), mkch(1)]
    if psbase is None:
        psA, psB, psC, psD, psE, psF, psG, psH = [P.pbank(i) for i in range(8)]
    else:
        psA, psB, psC, psD, psE, psF, psG, psH = [P.pbank(psbase + j % 4) for j in range(8)]
    xk = 0

    def shifted(g, rows, ti, dst):
        nonlocal xk
        x = xin[xk % 2]
        xk += 1
        if ti == 0:
            P.memset(x[0:rows, 0:1], 0.0, writes=[x.sub("halo")])
            P.dma(x[0:rows, 1:TT + 1].sub("body"), zT[g, 0:rows, 0:TT])
        else:
            P.dma(x[0:rows, :], zT[g, 0:rows, ti * TT - 1:(ti + 1) * TT])
        P.tt(dd[0:rows, :], x[0:rows, 0:TT], x[0:rows, 1:TT + 1], ALU.subtract)
        P.stt(dst, dd[0:rows, :], cv[0:rows, CV["mu"] + g:CV["mu"] + g + 1], x[0:rows, 1:TT + 1], ALU.mult, ALU.add)

    for ti in range(nt):
        tsl = slice(ti * TT, (ti + 1) * TT)
        r4, k4, v4, kk4, ic4, lw4 = t0, t1, t2, t3, t4_, t5
        shifted(G_WLO, 64, ti, lo_w)
        P.act(lo_w, lo_w, AF.Tanh)
        shifted(G_ALO, 64, ti, lo_a)
        shifted(G_GLO, 128, ti, lo_g)
        P.act(lo_g, lo_g, AF.Sigmoid)
        if L > 0:
            uv = uT.re("(c p) t -> p c t", p=128)
            for c in range(8):
                ux = uxb[c % 2]
                xv = xvb[c % 2]
                if ti == 0:
                    P.memset(ux[:, 0:1], 0.0, writes=[ux.sub("halo")])
                    P.dma(ux[:, 1:TT + 1].sub("body"), uv[:, c, 0:TT])
                else:
                    P.dma(ux, uv[:, c, ti * TT - 1:(ti + 1) * TT])
                P.tt(xv, ux[:, 0:TT], ux[:, 1:TT + 1], ALU.subtract)
                P.stt(xv, xv, cv[:, CV["vmu"] + c:CV["vmu"] + c + 1], ux[:, 1:TT + 1], ALU.mult, ALU.add)
                P.mm(psH[0:32, 0:TT], v_dn[:, c, :], xv, start=(c == 0), stop=(c == 7))
            P.copy(vl, psH[0:32, 0:TT], eng="act")
        for h in range(4):
            hs = slice(h * 64, (h + 1) * 64)
            shifted(G_R + h, 64, ti, r4[:, h, :].sub(h))
            shifted(G_K + h, 64, ti, k4[:, h, :].sub(h))
            shifted(G_V + h, 64, ti, v4[:, h, :].sub(h))
            P.mm(psA[0:64, 0:TT], w_up[:, hs], lo_w)
            P.act(lw4[:, h, :].sub(h), psA[0:64, 0:TT], AF.Sigmoid, bias=col("w0", h))
            P.mm(psB[0:64, 0:TT], a_up[:, hs], lo_a)
            P.act(ic4[:, h, :].sub(h), psB[0:64, 0:TT], AF.Sigmoid, bias=col("a0", h))
            P.mm(psC[0:64, 0:TT], g_up[:, hs], lo_g)
            P.copy(gate4[:, h, :].sub(h), psC[0:64, 0:TT], eng="act")
            if L == 0:
                P.dma(vfT[h * 64:(h + 1) * 64, tsl].sub(h), v4[:, h, :].sub(h), q="pool")
            else:
                P.dma(vf, vfT[h * 64:(h + 1) * 64, tsl])
                P.mm(psD[0:64, 0:TT], v_upt[:, hs], vl)
                P.act(sq, psD[0:64, 0:TT], AF.Sigmoid, bias=col("vb", h))
                P.tt(vf, vf, v4[:, h, :].sub(h), ALU.subtract)
                P.tt(vf, vf, sq, ALU.mult)
                P.tt(v4[:, h, :].sub(h), v4[:, h, :].sub(h), vf, ALU.add)
            P.ts(kk4[:, h, :].sub(h), k4[:, h, :].sub(h), col("kk", h), ALU.mult)
            P.act(sq, kk4[:, h, :].sub(h), AF.Square)
            P.mm(psE[0:64, 0:TT], ones[0:64, 0:64], sq)
            rstd_from_ps(P, rs, psE[0:64, 0:TT], 1.0, 1e-12)
            P.tt(kk4[:, h, :].sub(h), kk4[:, h, :].sub(h), rs, ALU.mult)
            P.ts(sq, ic4[:, h, :].sub(h), col("ka", h), ALU.mult, oma[:, h:h + 1], ALU.add)
            P.tt(k4[:, h, :].sub(h), k4[:, h, :].sub(h), sq, ALU.mult)
            P.stt(sq, r4[:, h, :].sub(h), col("rk", h), k4[:, h, :].sub(h), ALU.mult, ALU.mult)
            P.mm(psF[0:64, 0:TT], ones[0:64, 0:64], sq)
            P.tt(bon[:, h, :].sub(h), psF[0:64, 0:TT], v4[:, h, :].sub(h), ALU.mult)
        P.ts(lw4, lw4, float(-np.exp(-0.5)), ALU.mult)
        for n in range(NCH):
            cs = slice(n * 64, (n + 1) * 64)
            for h in range(4):
                P.transpose(psG[0:64, h * 64:(h + 1) * 64], v4[:, h, cs], ident[0:64, 0:64])
            P.copy(v_tm[:, n, :, :].re("p h d -> p (h d)"), psG[0:64, 0:256], eng="act")
        P.tt(ic4, ic4, kk4, ALU.mult)
        cb_ = [lw4, v4]
        cur = 0
        for sh in (1, 2, 4, 8, 16, 32):
            src = cb_[cur].re("p h (n c) -> p (h n) c", c=64)
            dstt = cb_[1 - cur].re("p h (n c) -> p (h n) c", c=64)
            P.copy(dstt[:, :, 0:sh], src[:, :, 0:sh], eng="pool", writes=[cb_[1 - cur].sub("a")])
            P.tt(dstt[:, :, sh:64], src[:, :, sh:64], src[:, :, 0:64 - sh], ALU.add, writes=[cb_[1 - cur].sub("b")])
            cur = 1 - cur
        assert cur == 0
        cl = lw4
        cl3 = cl.re("p h (n c) -> p (h n) c", c=64)
        e = v4
        e3 = e.re("p h (n c) -> p (h n) c", c=64)
        P.act(e, cl, AF.Exp)
        P.tt(rt, r4, e, ALU.mult)
        P.memset(at.re("p h (n c) -> p (h n) c", c=64)[:, :, 0:1], 1.0, writes=[at.sub("a")])
        P.copy(at.re("p h (n c) -> p (h n) c", c=64)[:, :, 1:64], e3[:, :, 0:63], eng="pool", writes=[at.sub("b")])
        P.stt(at, at, -1.0, kk4, ALU.mult, ALU.mult)
        P.act(e, cl, AF.Exp, scale=-1.0)
        P.tt(bt, ic4, e, ALU.mult)
        P.tt(kt, k4, e, ALU.mult)
        cl4 = cl.re("p h (n c) -> p h n c", c=64)
        P.copy(WC, cl4[:, :, :, 63])
        P.tt(e.re("p h (n c) -> p h n c", c=64), V(WC.ap.unsqueeze(3).to_broadcast([64, 4, NCH, 64]), WC.key), cl4, ALU.subtract)
        P.act(e, e, AF.Exp)
        P.act(WC, WC, AF.Exp)
        P.tt(ic4, ic4, e, ALU.mult)
        P.tt(k4, k4, e, ALU.mult)
        for n in range(NCH):
            cs = slice(n * 64, (n + 1) * 64)
            for h in range(4):
                P.transpose(psG[0:64, h * 64:(h + 1) * 64], ic4[:, h, cs], ident[0:64, 0:64])
            P.copy(bh_tm[:, n, :, :].re("p h d -> p (h d)"), psG[0:64, 0:256], eng="act")
            for h in range(4):
                P.transpose(psH[0:64, h * 64:(h + 1) * 64], k4[:, h, cs], ident[0:64, 0:64])
            P.copy(kh_tm[:, n, :, :].re("p h d -> p (h d)"), psH[0:64, 0:256], eng="dve")
        for n in range(NCH):
            cb = CB[n % 2]
            cs = slice(n * 64, (n + 1) * 64)
            for h in range(4):
                hs = slice(h * 64, (h + 1) * 64)
                P.mm(psA[0:64, hs], bt[:, h, cs], at[:, h, cs])
                P.mm(psB[0:64, hs], at[:, h, cs], bt[:, h, cs])
                P.mm(psC[0:64, hs], kt[:, h, cs], at[:, h, cs])
                P.mm(psD[0:64, hs], bt[:, h, cs], rt[:, h, cs])
            P.tt(cb["B0"], psA[0:64, 0:256], K["ms4"], ALU.mult)
            P.tt(cb["A0"], psB[0:64, 0:256], K["ms4T"], ALU.mult)
            P.tt(cb["Bak"], psC[0:64, 0:256], K["ms4"], ALU.mult)
            P.tt(cb["Brb"], psD[0:64, 0:256], K["mi4"], ALU.mult)
            for h in range(4):
                hs = slice(h * 64, (h + 1) * 64)
                P.mm(psE[0:64, hs], kt[:, h, cs], rt[:, h, cs])
            P.tt(cb["Brk"], psE[0:64, 0:256], K["mi4"], ALU.mult)
            TTm = neumann_inv(P, C, cb["A0"], cb["B0"], cb, psA, psB, psC)
            for h in range(4):
                hs = slice(h * 64, (h + 1) * 64)
                P.mm(psD[0:64, hs], at[:, h, cs], STb[:, h, :], start=True, stop=False)
                P.mm(psD[0:64, hs], cb["Bak"][:, hs], v_tm[:, n, h, :], start=False, stop=True)
            P.copy(cb["X"], psD[0:64, 0:256], eng="act")
            for h in range(4):
                hs = slice(h * 64, (h + 1) * 64)
                P.mm(psE[0:64, hs], TTm[:, hs], cb["X"][:, hs])
            P.copy(cb["U"], psE[0:64, 0:256], eng="act")
            for h in range(4):
                hs = slice(h * 64, (h + 1) * 64)
                P.mm(psF[0:64, hs], STb[:, h, :], rt[:, h, cs], start=True, stop=False)
                P.mm(psF[0:64, hs], cb["U"][:, hs], cb["Brb"][:, hs], start=False, stop=False)
                P.mm(psF[0:64, hs], v_tm[:, n, h, :], cb["Brk"][:, hs], start=False, stop=True)
            P.copy(y4[:, :, cs], psF[0:64, 0:256].re("p (h c) -> p h c", h=4), eng="act")
            for h in range(4):
                hs = slice(h * 64, (h + 1) * 64)
                P.mm(psG[0:64, hs], bh_tm[:, n, h, :], cb["U"][:, hs], start=True, stop=False)
                P.mm(psG[0:64, hs], kh_tm[:, n, h, :], v_tm[:, n, h, :], start=False, stop=True)
            wcn = V(WC.ap[:, :, n].unsqueeze(2).to_broadcast([64, 4, 64]), WC.key)
            P.tt(ST, ST, wcn, ALU.mult)
            P.tt(ST, ST, psG[0:64, 0:256].re("p (h v) -> p h v", h=4), ALU.add)
            P.copy(STb, ST, eng="pool")
        for h in range(4):
            yh = y4[:, h, :]
            P.mm(psH[0:64, 0:TT], ones[0:64, 0:64], yh)
            P.stt(yh, psH[0:64, 0:TT], float(-1.0 / 64), yh, ALU.mult, ALU.add)
            P.act(sq, yh, AF.Square)
            P.mm(psH[0:64, 0:TT], ones[0:64, 0:64], sq)
            rstd_from_ps(P, rs, psH[0:64, 0:TT], 64.0, 64e-5)
            P.stt(yh, yh, col("lng", h), rs, ALU.mult, ALU.mult)
            P.stt(yh, yh, col("lnb", h), bon[:, h, :].sub(h), ALU.add, ALU.add)
            P.tt(sq, yh, gate4[:, h, :].sub(h), ALU.mult)
            P.dma(oT[h * 64:(h + 1) * 64, tsl].sub(h), sq, q="pool")
    if release:
        P.release(mk)


def phase_merge(P, C, NT, hT, oT, gT, wbr_d, wout_d, h1T, dyn=None):
    mk = P.mark()
    nt = NT // TT
    wstg = [P.tile([128, 4, 1024], name=f"f_wstg{i}") for i in range(2)]
    wbr = []
    for br in range(3):
        t = P.tile([128, 4, 1024], name=f"wbr{br}", dtype=BF16)
        P.dma(wstg[br % 2], wbr_d[br].re("(c p) n -> p c n", p=128))
        P.copy(t, wstg[br % 2], eng=("pool" if br % 2 == 0 else "act"))
        wbr.append(t)
    wout = P.tile([128, 8, 1024], name="wout", dtype=BF16)
    wov = wout_d.re("(c p) n -> p c n", p=128)
    for hh in range(2):
        P.dma(wstg[(hh + 1) % 2], wov[:, hh * 4:(hh + 1) * 4, :])
        P.copy(wout[:, hh * 4:(hh + 1) * 4, :].sub(hh), wstg[(hh + 1) % 2], eng=("act" if hh == 0 else "pool"))
    h = P.tile([128, 8, TT], name="f_h")
    o = P.tile([128, 12, TT], name="f_o")
    ob_ = P.tile([128, 12, TT], name="f_ob", dtype=BF16)
    mg = P.tile([128, 8, TT], name="f_mg32") if dyn is not None else None
    mgb = P.tile([128, 8, TT], name="f_mg", dtype=BF16)
    o2 = P.tile([128, 6, TT], name="f_o2") if dyn is not None else None
    g3 = [P.tile([128, 3, TT], name=f"f_g3{i}") for i in range(2)]
    tmp = [P.tile([128, TT], name=f"f_tmp{i}") for i in range(2)]
    tmp2 = [P.tile([128, TT], name=f"f_tmpb{i}") for i in range(2)]
    ps = [P.pbank(i) for i in range(8)]
    hv = hT.re("(c p) t -> p c t", p=128)
    ov = oT.re("(c p) t -> p c t", p=128)
    gv = gT.re("(b c p) t -> p b c t", b=3, p=128)
    h1v = h1T.re("(c p) t -> p c t", p=128)
    for ti in range(nt):
        tsl = slice(ti * TT, (ti + 1) * TT)
        if dyn is not None:
            m0 = C.cv[:, CV["m0"]:CV["m0"] + 1]
            m1 = C.cv[:, CV["m1"]:CV["m1"] + 1]
            tsl2 = slice(dyn + ti * TT, dyn + (ti + 1) * TT)
            P.dma(h, hv[:, :, tsl])
            P.dma(mg, hv[:, :, tsl2])
            P.ts(h, h, m0, ALU.mult)
            P.stt(h, mg, m1, h, ALU.mult, ALU.add)
            for part in range(2):
                cs_ = slice(part * 6, (part + 1) * 6)
                P.dma(o[:, cs_, :].sub(part), ov[:, cs_, tsl])
                P.dma(o2, ov[:, cs_, tsl2])
                P.ts(o[:, cs_, :].sub(part), o[:, cs_, :].sub(part), m0, ALU.mult)
                P.stt(o[:, cs_, :].sub(part), o2, m1, o[:, cs_, :].sub(part), ALU.mult, ALU.add)
        else:
            P.dma(h, hv[:, :, tsl])
            P.dma(o, ov[:, :, tsl])
        P.copy(ob_[:, 0:6, :].sub(0), o[:, 0:6, :], eng="pool")
        P.copy(ob_[:, 6:12, :].sub(1), o[:, 6:12, :], eng="act")
        for n in range(8):
            ns = slice(n * 128, (n + 1) * 128)
            g = g3[n % 2]
            P.dma(g, gv[:, :, n, tsl])
            for br in range(3):
                pp = ps[(n % 2) * 3 + br]
                for k in range(4):
                    P.mm(pp, wbr[br][:, k, ns], ob_[:, br * 4 + k, :], start=(k == 0), stop=(k == 3))
            tm_ = tmp[n % 2]
            P.tt(tm_, ps[(n % 2) * 3 + 0], g[:, 0, :], ALU.mult)
            P.tt(tmp2[n % 2], ps[(n % 2) * 3 + 1], g[:, 1, :], ALU.mult)
            P.tt(tm_, tm_, tmp2[n % 2], ALU.add, eng="pool")
            P.tt(tmp2[n % 2], ps[(n % 2) * 3 + 2], g[:, 2, :], ALU.mult)
            P.tt(mgb[:, n, :].sub(n), tm_, tmp2[n % 2], ALU.add, eng="pool")
        for n in range(8):
            ns = slice(n * 128, (n + 1) * 128)
            pp = ps[6 + n % 2]
            for k in range(8):
                P.mm(pp, wout[:, k, ns], mgb[:, k, :], start=(k == 0), stop=(k == 7))
            P.tt(h[:, n, :].sub(n), h[:, n, :].sub(n), pp, ALU.add)
        P.dma(h1v[:, :, tsl], h, q="pool")
    P.release(mk)


def phase_ffn(P, C, NT, h1T, h2T, gcol, experts, FF, router_d=None):
    mk = P.mark()
    cv = C.cv
    K = C.k
    ones = K["ones"]
    ident = K["ident"]
    nt = NT // TT
    NF = FF // 128
    CB = 512
    blocks = [(c0, min(CB, FF - c0)) for c0 in range(0, FF, CB)]
    h = P.tile([128, 8, TT], name="m_h")
    u = P.tile([128, 8, TT], name="m_u", dtype=BF16)
    rs = P.tile([128, TT], name="m_rs")
    hid_raw = P.tile([128, NF * TT // 2], name="m_hid")
    hid = V(hid_raw.ap.bitcast(BF16).rearrange("p (f t) -> p f t", f=NF), hid_raw.key)
    u32 = V(hid_raw.ap[:, 0:8 * TT].rearrange("p (c t) -> p c t", c=8), hid_raw.key)
    sg = [P.tile([128, TT], name=f"m_sg{i}") for i in range(2)]
    wgb = [P.tile([128, 8, CB], name=f"m_wg{i}") for i in range(2)]
    wub = [P.tile([128, 8, CB], name=f"m_wu{i}") for i in range(2)]
    wgc = [P.tile([128, 8, CB], name=f"m_wgc{i}", dtype=BF16) for i in range(2)]
    wuc = [P.tile([128, 8, CB], name=f"m_wuc{i}", dtype=BF16) for i in range(2)]
    wdb = [P.tile([128, 512], name=f"m_wd{i}") for i in range(3)]
    wdc = [P.tile([128, 512], name=f"m_wdc{i}", dtype=BF16) for i in range(3)]
    ps = [P.pbank(i) for i in range(8)]
    hv = h1T.re("(c p) t -> p c t", p=128)
    h2v = h2T.re("(c p) t -> p c t", p=128)
    ne = len(experts)
    if router_d is not None:
        sel8 = P.tile([8, 1024], name="c_sel8")
        P.dma(sel8, C.sel8_d)
        rt_w = P.tile([128, 8, 8], name="m_rw")
        P.dma(rt_w, router_d.re("(c p) e -> p c e", p=128))
        lg = P.tile([8, TT], name="m_lg")
        ltm = P.tile([128, 4, 8], name="m_ltm")
        l2 = P.tile([128, 4, 8], name="m_l2")
        eq1 = P.tile([128, 4, 8], name="m_eq1")
        eq2 = P.tile([128, 4, 8], name="m_eq2")
        m1 = P.tile([128, 4], name="m_m1")
        m2 = P.tile([128, 4], name="m_m2")
        w1 = P.tile([128, 4], name="m_w1")
        w2 = P.tile([128, 4], name="m_w2")
        gwf = P.tile([8, TT], name="m_gwf")
        gwe = [P.tile([128, TT], name=f"m_gwe{i}") for i in range(2)]
    wk = 0
    dk = 0
    for ti in range(nt):
        tsl = slice(ti * TT, (ti + 1) * TT)
        P.dma(h, hv[:, :, tsl])
        P.act(u32, h, AF.Square)
        for c in range(8):
            P.mm(ps[7], ones, u32[:, c, :], start=(c == 0), stop=(c == 7))
        rstd_from_ps(P, rs, ps[7], D, 1e-6)
        if router_d is not None:
            for c in range(8):
                P.stt(u32[:, c, :], h[:, c, :], cv[:, gcol + c:gcol + c + 1], rs, ALU.mult, ALU.mult)
            P.copy(u, u32, eng="pool")
            for c in range(8):
                P.mm(ps[6][0:8, :], rt_w[:, c, :], u32[:, c, :], start=(c == 0), stop=(c == 7))
        else:
            for c in range(8):
                P.stt(u[:, c, :], h[:, c, :], cv[:, gcol + c:gcol + c + 1], rs, ALU.mult, ALU.mult)
        if router_d is not None:
            P.copy(lg, ps[6][0:8, :], eng="act")
            for j in range(4):
                P.transpose(ps[5][:, j * 8:(j + 1) * 8], lg[:, j * 128:(j + 1) * 128], ident[0:8, 0:8])
            P.copy(ltm.re("p j e -> p (j e)"), ps[5][:, 0:32])
            bc = lambda t: V(t.ap.unsqueeze(2).to_broadcast([128, 4, 8]), t.key)
            P.op("dve", lambda e: e.reduce_max(_ap(m1), _ap(ltm), AX.X), [ltm], [m1])
            P.tt(eq1, ltm, bc(m1), ALU.is_equal)
            P.stt(l2, eq1, -1e30, ltm, ALU.mult, ALU.add)
            P.op("dve", lambda e: e.reduce_max(_ap(m2), _ap(l2), AX.X), [l2], [m2])
            P.tt(eq2, l2, bc(m2), ALU.is_equal)
            P.tt(w2, m2, m1, ALU.subtract)
            P.act(w2, w2, AF.Exp)
            P.ts(w1, w2, 1.0, ALU.add)
            P.recip(w1, w1)
            P.tt(w2, w2, w1, ALU.mult)
            P.tt(eq1, eq1, bc(w1), ALU.mult)
            P.tt(eq2, eq2, bc(w2), ALU.mult)
            P.tt(eq1, eq1, eq2, ALU.add)
            for j in range(4):
                P.transpose(ps[5][0:8, j * 128:(j + 1) * 128], eq1[:, j, :], ident)
            P.copy(gwf, ps[5][0:8, :], eng="act")
        for e_, (wg_d, wu_d, wd_d) in enumerate(experts):
            wgv = wg_d.re("(c p) f -> p c f", p=128)
            wuv = wu_d.re("(c p) f -> p c f", p=128)
            wdv = wd_d.re("(f p) n -> p f n", p=128)
            if router_d is not None:
                gw_e = gwe[e_ % 2]
                P.mm(ps[3], sel8[:, e_ * 128:(e_ + 1) * 128], gwf)
                P.copy(gw_e, ps[3], eng="act")
            for (c0_, wdt) in blocks:
                wg_s = wgb[wk % 2]
                wu_s = wub[wk % 2]
                wg_t = wgc[wk % 2]
                wu_t = wuc[wk % 2]
                wk += 1
                P.dma(wg_s[:, :, 0:wdt], wgv[:, :, c0_:c0_ + wdt])
                P.dma(wu_s[:, :, 0:wdt], wuv[:, :, c0_:c0_ + wdt], q="act")
                P.copy(wg_t[:, :, 0:wdt], wg_s[:, :, 0:wdt], eng="pool")
                P.copy(wu_t[:, :, 0:wdt], wu_s[:, :, 0:wdt], eng="act")
                for j in range(wdt // 128):
                    f = c0_ // 128 + j
                    pg = ps[4 + f % 2]
                    pu = ps[6 + f % 2]
                    for c in range(8):
                        P.mm(pg, wg_t[:, c, j * 128:(j + 1) * 128], u[:, c, :], start=(c == 0), stop=(c == 7))
                    for c in range(8):
                        P.mm(pu, wu_t[:, c, j * 128:(j + 1) * 128], u[:, c, :], start=(c == 0), stop=(c == 7))
                    s_ = sg[f % 2]
                    P.act(s_, pg, AF.Silu)
                    if router_d is not None:
                        P.tt(s_, s_, gw_e, ALU.mult, eng="pool")
                    P.tt(hid[:, f, :].sub(f), s_, pu, ALU.mult)
            for half in range(2):
                for f in range(NF):
                    wd_s = wdb[dk % 3]
                    wd_t = wdc[dk % 3]
                    dk += 1
                    P.dma(wd_s, wdv[:, f, half * 512:(half + 1) * 512])
                    P.copy(wd_t, wd_s, eng=("dve" if dk % 2 == 0 else "pool"))
                    for n4 in range(4):
                        P.mm(ps[n4], wd_t[:, n4 * 128:(n4 + 1) * 128], hid[:, f, :].sub(f), start=(f == 0), stop=(f == NF - 1))
                for n4 in range(4):
                    n = half * 4 + n4
                    P.tt(h[:, n, :].sub(n), h[:, n, :].sub(n), ps[n4], ALU.add)
        P.dma(h2v[:, :, tsl], h, q="pool")
    P.release(mk)


def phase_ple(P, C, NT, h2T, pT, proj_d, pgate_d, gcol, h3T):
    mk = P.mark()
    cv = C.cv
    ones = C.k["ones"]
    nt = NT // TT
    pstg = [P.tile([128, 4, 1024], name=f"p_stg{i}") for i in range(2)]
    proj = P.tile([128, 2, 1024], name="p_proj", dtype=BF16)
    P.dma(pstg[0][:, 0:2, :], proj_d.re("(c p) n -> p c n", p=128))
    P.copy(proj, pstg[0][:, 0:2, :], eng="pool")
    pg = P.tile([128, 8, 1024], name="p_gate", dtype=BF16)
    pgv = pgate_d.re("(c p) n -> p c n", p=128)
    for hh in range(2):
        P.dma(pstg[(hh + 1) % 2], pgv[:, hh * 4:(hh + 1) * 4, :])
        P.copy(pg[:, hh * 4:(hh + 1) * 4, :].sub(hh), pstg[(hh + 1) % 2], eng=("act" if hh == 0 else "pool"))
    h = P.tile([128, 8, TT], name="p_h")
    hb_ = P.tile([128, 8, TT], name="p_hb", dtype=BF16)
    pt = P.tile([128, 2, TT], name="p_p")
    ptb = P.tile([128, 2, TT], name="p_pb", dtype=BF16)
    er = P.tile([128, 8, TT], name="p_er")
    ho = P.tile([128, 8, TT], name="p_ho")
    sq = P.tile([128, TT], name="p_sq")
    rs = P.tile([128, TT], name="p_rs")
    gp = [P.tile([128, TT], name=f"p_gp{i}") for i in range(2)]
    ps = [P.pbank(i) for i in range(8)]
    hv = h2T.re("(c p) t -> p c t", p=128)
    pv = pT.re("(c p) t -> p c t", p=128)
    h3v = h3T.re("(c p) t -> p c t", p=128)
    for ti in range(nt):
        tsl = slice(ti * TT, (ti + 1) * TT)
        P.dma(h, hv[:, :, tsl])
        P.dma(pt, pv[:, :, tsl])
        P.copy(ptb, pt, eng="pool")
        P.copy(hb_, h, eng="pool")
        for n in range(8):
            ns = slice(n * 128, (n + 1) * 128)
            pp = ps[n % 2]
            P.mm(pp, proj[:, 0, ns], ptb[:, 0, :], start=True, stop=False)
            P.mm(pp, proj[:, 1, ns], ptb[:, 1, :], start=False, stop=True)
            P.copy(er[:, n, :].sub(n), pp, eng="act")
            P.act(sq, pp, AF.Square)
            P.mm(ps[2], ones, sq, start=(n == 0), stop=(n == 7))
        rstd_from_ps(P, rs, ps[2], D, 1e-6)
        for n in range(8):
            ns = slice(n * 128, (n + 1) * 128)
            pp = ps[3 + n % 2]
            for k in range(8):
                P.mm(pp, pg[:, k, ns], hb_[:, k, :], start=(k == 0), stop=(k == 7))
            g = gp[n % 2]
            P.act(g, pp, AF.Sigmoid)
            P.stt(er[:, n, :].sub(n), er[:, n, :].sub(n), cv[:, gcol + n:gcol + n + 1], rs, ALU.mult, ALU.mult)
            P.tt(g, g, er[:, n, :].sub(n), ALU.mult)
            P.tt(ho[:, n, :].sub(n), h[:, n, :], g, ALU.add)
        P.dma(h3v[:, :, tsl], ho, q="pool")
    P.release(mk)


def own(hg, width=64):
    return slice(hg * 4 * width, (hg + 1) * 4 * width)


def col4(v):
    return np.ascontiguousarray(v.reshape(4, 64).T)


def col2(v):
    return np.ascontiguousarray(v.reshape(2, 128).T)


def mixer_host_inputs(inp, L, b, hg):
    f = np.float32
    w_in = inp["w_in"][L]
    o = own(hg)
    cols = np.concatenate([
        np.arange(0, 512)[o], np.arange(512, 1024)[o], np.arange(1024, 1536)[o],
        np.arange(1536, 1600), np.arange(1600, 1664), np.arange(1664, 1792),
        np.arange(1792, 2048), np.arange(2048, 2176), np.arange(2176, 2208),
        np.arange(2208, 2720)[o], np.arange(2720, 3232)[o], np.arange(3232, 3744)[o],
        np.arange(3760, 4272)[o],
        np.arange(3744, 3752)[hg * 4:(hg + 1) * 4], np.arange(3752, 3760)[hg * 4:(hg + 1) * 4]])
    assert len(cols) == NZ
    d = {}
    d["w_in_m"] = np.ascontiguousarray(w_in[:, cols])
    cv = np.zeros((128, NCV), f)

    def put(nm, arr):
        arr = np.asarray(arr, f)
        cv[:arr.shape[0], CV[nm]:CV[nm] + arr.shape[1]] = arr
    put("nmg", inp["norm_mix_g"][L].reshape(8, 128).T)
    mu = inp["rwkv_mu"][L]
    mucols = np.zeros((128, 15), f)
    rcols = cols[:1024]
    for g in range(15):
        seg = mu[rcols[ZOFF[g]:ZOFF[g + 1]]]
        mucols[:len(seg), g] = seg
    put("mu", mucols)
    put("w0", col4(inp["rwkv_w0"][L][o]))
    put("a0", col4(inp["rwkv_a0"][L][o]))
    put("kk", col4(inp["rwkv_k_k"][L][o]))
    put("ka", col4(inp["rwkv_k_a"][L][o]))
    put("rk", col4(inp["rwkv_r_k"][L].reshape(512)[o]))
    put("lng", col4(inp["rwkv_ln_g"][L][o]))
    put("lnb", col4(inp["rwkv_ln_b"][L][o]))
    if L > 0:
        put("vmu", inp["vres_mu"][L - 1].reshape(8, 128).T)
        put("vb", col4(inp["vres_b"][L - 1][o]))
    put("qng", inp["mla_q_norm_g"][L].reshape(2, 128).T)
    put("kvng", inp["mla_kv_norm_g"][L].reshape(128, 1))
    put("qkq", inp["mla_qk_norm_q"][L].reshape(96, 1))
    put("qkk", inp["mla_qk_norm_k"][L].reshape(96, 1))
    invf = (1.0 / (10000.0 ** (np.arange(0, 32, 2, dtype=f) / f(32)))).astype(f)
    iv = np.zeros((96, 1), f)
    iv[64:80, 0] = invf
    iv[80:96, 0] = invf
    put("invf", iv)
    put("ropec", np.full((128, 1), -np.pi, f))
    cw = inp["gdn_conv_w"][L]
    convc = np.zeros((128, 48), f)
    for seg in range(3):
        cc = cw[:, seg * 512:(seg + 1) * 512][:, o]
        for hh in range(4):
            for j in range(4):
                convc[:64, (seg * 4 + hh) * 4 + j] = cc[j, hh * 64:(hh + 1) * 64]
    put("conv", convc)
    put("alog", inp["gdn_a_log"][L][hg * 4:(hg + 1) * 4].reshape(4, 1))
    put("dtb", inp["gdn_dt_bias"][L][hg * 4:(hg + 1) * 4].reshape(4, 1))
    put("gng", inp["gdn_norm_g"][L].reshape(64, 1))
    d["cv"] = cv
    d["w_up"] = np.ascontiguousarray(inp["rwkv_w_up"][L][:, o])
    d["a_up"] = np.ascontiguousarray(inp["rwkv_a_up"][L][:, o])
    d["g_up"] = np.ascontiguousarray(inp["rwkv_g_up"][L][:, o])
    if L > 0:
        d["v_down"] = np.ascontiguousarray(inp["vres_down"][L - 1])
        d["v_up"] = np.ascontiguousarray(inp["vres_up"][L - 1][:, o])
    d["w_uq"] = np.ascontiguousarray(inp["mla_w_uq"][L][:, hg * 384:(hg + 1) * 384])
    ukv = inp["mla_w_ukv"][L].reshape(128, 8, 128)[:, hg * 4:(hg + 1) * 4, :]
    d["w_uk"] = np.ascontiguousarray(ukv[:, :, :64].reshape(128, 256))
    d["w_uv"] = np.ascontiguousarray(ukv[:, :, 64:].reshape(128, 256))
    d["pos"] = np.ascontiguousarray(inp["positions"][b:b + 1].astype(np.int32))
    for k_, v_ in consts_np().items():
        d["c_" + k_] = v_
    return d
from concourse.bass_utils import run_bass_kernel_spmd

B_, S_, NCORE = 4, 4096, 8
M_CONSTS = ["ident", "ones", "att_mask", "ropeRT", "sel4", "id4", "neg4", "neg4T", "ms4", "ms4T", "mi4"]
F_CONSTS = ["ident", "ones"]
_PROG_CACHE = {}


def build_mixer(S, L):
    P = Prog()
    C = Ctx()
    names = []

    def din(name, shape, dt=F32):
        names.append(name)
        return P.dview(P.dram(name, shape, dt, kind="ExternalInput"))
    hT = din("hT", [1024, S])
    w_d = din("w_in_m", [1024, NZ])
    cv_d = din("cv", [128, NCV])
    pos_d = din("pos", [1, S], I32)
    w_uq, w_uk, w_uv = din("w_uq", [256, 384]), din("w_uk", [128, 256]), din("w_uv", [128, 256])
    w_up, a_up, g_up = din("w_up", [64, 256]), din("a_up", [64, 256]), din("g_up", [128, 256])
    zT = P.dview(P.dram("zT", [NG, 128, S], F32, kind="Internal"))
    oT = P.dview(P.dram("oT", [768, S], F32, kind="ExternalOutput"))
    uT = v_down = v_up = None
    if L == 0:
        vfT = P.dview(P.dram("vfT_out", [256, S], F32, kind="ExternalOutput"))
    else:
        vfT = din("vfT_in", [256, S])
        uT = P.dview(P.dram("uT", [1024, S], F32, kind="Internal"))
        v_down, v_up = din("v_down", [1024, 32]), din("v_up", [32, 256])
    C.k = load_consts(P, M_CONSTS)
    names.extend(["c_" + c for c in M_CONSTS])
    C.cv = P.tile([128, NCV], name="cv")
    P.dma(C.cv, cv_d)
    groups = [(int(ZOFF[g]), int(ZG[g])) for g in range(NG)]
    phase_proj(P, C, S, hT, w_d, NZ, groups, zT, CV["nmg"], uT=uT)
    phase_rwkv(P, C, S, L, zT, V(oT.ap[0:256, :], "oT_r"), w_up, a_up, g_up, vfT, uT, v_down, v_up)
    phase_mla(P, C, S, zT, pos_d, w_uq, w_uk, w_uv, V(oT.ap[256:512, :], "oT_m"))
    phase_gdn(P, C, S, zT, V(oT.ap[512:768, :], "oT_g"))
    P.finalize()
    return P.nc, names


def build_token(NT, L):
    P = Prog()
    C = Ctx()
    names = []

    def din(name, shape, dt=F32):
        names.append(name)
        return P.dview(P.dram(name, shape, dt, kind="ExternalInput"))
    hT = din("hT", [1024, NT])
    oT = din("oT_all", [1536, NT])
    pT = din("pT", [256, NT])
    cv_d = din("cv", [128, NCV])
    w_g = din("w_gate", [1024, 3072])
    wbr = [din(f"w_br{i}", [512, 1024]) for i in range(3)]
    wout = din("w_out", [1024, 1024])
    proj = din("ple_proj", [256, 1024])
    pgate = din("ple_gate", [1024, 1024])
    if L % 2 == 0:
        experts = [(din("ffn_wg", [1024, 2816]), din("ffn_wu", [1024, 2816]), din("ffn_wd", [2816, 1024]))]
        FF = 2816
        router = None
    else:
        wg_all = din("moe_wg", [8, 1024, 3584])
        wu_all = din("moe_wu", [8, 1024, 3584])
        wd_all = din("moe_wd", [8, 3584, 1024])
        experts = [(V(wg_all.ap[e], wg_all.key), V(wu_all.ap[e], wu_all.key), V(wd_all.ap[e], wd_all.key)) for e in range(8)]
        FF = 3584
        router = din("moe_router", [1024, 8])
    gT = P.dview(P.dram("gT", [3072, NT], F32, kind="Internal"))
    h1T = P.dview(P.dram("h1T", [1024, NT], F32, kind="Internal"))
    h2T = P.dview(P.dram("h2T", [1024, NT], F32, kind="Internal"))
    h3T = P.dview(P.dram("h3T", [1024, NT], F32, kind="ExternalOutput"))
    C.k = load_consts(P, F_CONSTS)
    names.extend(["c_" + c for c in F_CONSTS])
    C.sel8_d = din("c_sel8", [8, 1024])
    C.cv = P.tile([128, NCV], name="cv")
    P.dma(C.cv, cv_d)
    groups = [(g * 128, 128) for g in range(24)]
    phase_proj(P, C, NT, hT, w_g, 3072, groups, gT, CV["nmg"], func=AF.Sigmoid, nbuf=1, zflat=True)
    phase_merge(P, C, NT, hT, oT, gT, wbr, wout, h1T)
    phase_ffn(P, C, NT, h1T, h2T, CV["nfg"], experts, FF, router)
    phase_ple(P, C, NT, h2T, pT, proj, pgate, CV["png"], h3T)
    P.finalize()
    return P.nc, names


def token_host_inputs(inp, L, half=0):
    f = np.float32
    d = {}
    cv = np.zeros((128, NCV), f)
    cv[:, CV["m0"]] = 1.0 if half == 0 else 0.0
    cv[:, CV["m1"]] = 1.0 if half == 1 else 0.0
    cv[:, CV["nmg"]:CV["nmg"] + 8] = inp["norm_mix_g"][L].reshape(8, 128).T
    cv[:, CV["nfg"]:CV["nfg"] + 8] = inp["norm_ffn_g"][L].reshape(8, 128).T
    cv[:, CV["png"]:CV["png"] + 8] = inp["ple_norm_g"][L].reshape(8, 128).T
    d["cv"] = cv
    d["w_gate"] = np.ascontiguousarray(inp["w_in"][L][:, 4272:7344])
    d["w_br0"] = inp["w_br_rwkv"][L]
    d["w_br1"] = inp["w_br_mla"][L]
    d["w_br2"] = inp["w_br_gdn"][L]
    d["w_out"] = inp["w_out"][L]
    d["ple_proj"] = inp["ple_proj"][L]
    d["ple_gate"] = inp["ple_gate"][L]
    if L % 2 == 0:
        d["ffn_wg"], d["ffn_wu"], d["ffn_wd"] = inp["ffn_wg"][L // 2], inp["ffn_wu"][L // 2], inp["ffn_wd"][L // 2]
    else:
        d["moe_wg"], d["moe_wu"], d["moe_wd"] = inp["moe_wg"][L // 2], inp["moe_wu"][L // 2], inp["moe_wd"][L // 2]
        d["moe_router"] = inp["moe_router"][L // 2]
    cs = consts_np()
    for c in F_CONSTS + ["sel8"]:
        d["c_" + c] = cs[c]
    return d


def kernel(**inputs):
    inp = {k: np.asarray(v) for k, v in inputs.items()}
    x = inp["x"].astype(np.float32)
    Bn, S, Dm = x.shape
    NT = S // 2
    hT = [np.ascontiguousarray(x[b].T) for b in range(Bn)]
    vf = [None] * NCORE
    for L in range(2):
        key = ("M", S, L)
        if key not in _PROG_CACHE:
            _PROG_CACHE[key] = build_mixer(S, L)
        nc, names = _PROG_CACHE[key]
        in_maps = []
        for core in range(NCORE):
            b, hg = core // 2, core % 2
            d = mixer_host_inputs(inp, L, b, hg)
            d["hT"] = hT[b]
            if L > 0:
                d["vfT_in"] = vf[core]
            in_maps.append({n: np.ascontiguousarray(d[n]) for n in names})
        res = run_bass_kernel_spmd(nc, in_maps, core_ids=list(range(NCORE)))
        oTs = [r["oT"] for r in res.results]
        if L == 0:
            vf = [r["vfT_out"] for r in res.results]
        key = ("F", NT, L)
        if key not in _PROG_CACHE:
            _PROG_CACHE[key] = build_token(NT, L)
        nc, names = _PROG_CACHE[key]
        th = token_host_inputs(inp, L)
        in_maps = []
        for core in range(NCORE):
            b, half = core // 2, core % 2
            tsl = slice(half * NT, (half + 1) * NT)
            d = dict(th)
            d["hT"] = hT[b][:, tsl]
            o0, o1 = oTs[2 * b], oTs[2 * b + 1]
            d["oT_all"] = np.concatenate([o0[0:256, tsl], o1[0:256, tsl], o0[256:512, tsl], o1[256:512, tsl],
                                          o0[512:768, tsl], o1[512:768, tsl]], axis=0)
            d["pT"] = inp["p"][L, b, tsl, :].T
            in_maps.append({n: np.ascontiguousarray(d[n]) for n in names})
        res = run_bass_kernel_spmd(nc, in_maps, core_ids=list(range(NCORE)))
        for b in range(Bn):
            hT[b] = np.concatenate([res.results[2 * b]["h3T"], res.results[2 * b + 1]["h3T"]], axis=1)
    out = np.stack([hT[b].T for b in range(Bn)], axis=0)
    return np.ascontiguousarray(out.astype(np.float32))


ALL_M_KEYS = ["w_in_m", "cv", "w_uq", "w_uk", "w_uv", "w_up", "a_up", "g_up"]


def build_fused(S):
    NTH = S // 2
    P = Prog()
    C = Ctx()
    names = []

    def din(name, shape, dt=F32):
        names.append(name)
        return P.dview(P.dram(name, shape, dt, kind="ExternalInput"))
    hT0 = din("hT0", [1024, S])
    pos_d = din("pos", [1, S], I32)
    pT = [din("pT0", [256, S]), din("pT1", [256, NTH])]
    C.sel8_d = din("c_sel8", [8, 1024])
    mi = {}
    for L in range(2):
        for hg in range(2):
            pre = f"m{L}{hg}_"
            d = {"w_in_m": din(pre + "w_in_m", [1024, NZ]), "cv": din(pre + "cv", [128, NCV]),
                 "w_uq": din(pre + "w_uq", [256, 384]), "w_uk": din(pre + "w_uk", [128, 256]), "w_uv": din(pre + "w_uv", [128, 256]),
                 "w_up": din(pre + "w_up", [64, 256]), "a_up": din(pre + "a_up", [64, 256]), "g_up": din(pre + "g_up", [128, 256])}
            if L > 0:
                d["v_down"] = din(pre + "v_down", [1024, 32])
                d["v_up"] = din(pre + "v_up", [32, 256])
            mi[(L, hg)] = d
    ti_ = {}
    for L in range(2):
        pre = f"t{L}_"
        d = {"cv": din(pre + "cv", [128, NCV]), "w_gate": din(pre + "w_gate", [1024, 3072]),
             "wbr": [din(pre + f"w_br{i}", [512, 1024]) for i in range(3)], "w_out": din(pre + "w_out", [1024, 1024]),
             "ple_proj": din(pre + "ple_proj", [256, 1024]), "ple_gate": din(pre + "ple_gate", [1024, 1024])}
        if L % 2 == 0:
            d["experts"] = [(din(pre + "ffn_wg", [1024, 2816]), din(pre + "ffn_wu", [1024, 2816]), din(pre + "ffn_wd", [2816, 1024]))]
            d["FF"] = 2816
            d["router"] = None
        else:
            wg_all = din(pre + "moe_wg", [8, 1024, 3584])
            wu_all = din(pre + "moe_wu", [8, 1024, 3584])
            wd_all = din(pre + "moe_wd", [8, 3584, 1024])
            d["experts"] = [(V(wg_all.ap[e], wg_all.key), V(wu_all.ap[e], wu_all.key), V(wd_all.ap[e], wd_all.key)) for e in range(8)]
            d["FF"] = 3584
            d["router"] = din(pre + "moe_router", [1024, 8])
        ti_[L] = d
    zT = P.dview(P.dram("zT", [NG, 128, S], F32, kind="Internal"))
    uT = P.dview(P.dram("uT", [1024, S], F32, kind="Internal"))
    oTa = P.dview(P.dram("oT_all", [1536, S], F32, kind="Internal"))
    vfT = P.dview(P.dram("vfT", [512, S], F32, kind="Internal"))
    gT = P.dview(P.dram("gT", [3072, S], F32, kind="Internal"))
    h1T = P.dview(P.dram("h1T", [1024, S], F32, kind="Internal"))
    h2T = P.dview(P.dram("h2T", [1024, S], F32, kind="Internal"))
    hT1 = P.dview(P.dram("hT1", [1024, S], F32, kind="Internal"))
    h3T = P.dview(P.dram("h3T", [1024, NTH], F32, kind="ExternalOutput"))
    C.k = load_consts(P, M_CONSTS)
    names.extend(["c_" + c for c in M_CONSTS])
    C.cv = P.tile([128, NCV], name="cv")
    groups = [(int(ZOFF[g]), int(ZG[g])) for g in range(NG)]
    ggroups = [(g * 128, 128) for g in range(24)]
    hin = hT0
    for L in range(2):
        for hg in range(2):
            d = mi[(L, hg)]
            P.dma(C.cv, d["cv"])
            phase_proj(P, C, S, hin, d["w_in_m"], NZ, groups, zT, CV["nmg"], uT=(uT if L > 0 else None))
            sub = lambda br: V(oTa.ap[br * 512 + hg * 256:br * 512 + (hg + 1) * 256, :], f"oT_{br}_{hg}")
            vfv = V(vfT.ap[hg * 256:(hg + 1) * 256, :], f"vfT_{hg}")
            mk_ = P.mark()
            o_r, o_g = sub(0), sub(2)
            P.run_interleaved([
                lambda: phase_rwkv(P, C, S, L, zT, o_r, d["w_up"], d["a_up"], d["g_up"], vfv,
                                   (uT if L > 0 else None), d.get("v_down"), d.get("v_up"), ttl=256, psbase=0, release=False),
                lambda: phase_gdn(P, C, S, zT, o_g, ttl=256, psbase=4, release=False)])
            P.release(mk_)
            phase_mla(P, C, S, zT, pos_d, d["w_uq"], d["w_uk"], d["w_uv"], sub(1))
        t = ti_[L]
        P.dma(C.cv, t["cv"])
        if L == 0:
            NT, dyn, hout = S, None, hT1
        else:
            NT, dyn, hout = NTH, NTH, h3T
        gv = V(gT.ap[:, 0:NT], gT.key)
        h1v = V(h1T.ap[:, 0:NT], h1T.key)
        h2v = V(h2T.ap[:, 0:NT], h2T.key)
        phase_proj(P, C, NT, hin, t["w_gate"], 3072, ggroups, gv, CV["nmg"], func=AF.Sigmoid, nbuf=1, zflat=True, dyn=dyn)
        phase_merge(P, C, NT, hin, oTa, gv, t["wbr"], t["w_out"], h1v, dyn=dyn)
        phase_ffn(P, C, NT, h1v, h2v, CV["nfg"], t["experts"], t["FF"], t["router"])
        phase_ple(P, C, NT, h2v, pT[L], t["ple_proj"], t["ple_gate"], CV["png"], hout)
        hin = hT1
    P.finalize()
    return P.nc, names


def kernel_unfused(**inputs):
    return _kernel_unfused(**inputs)


_kernel_unfused = kernel


def kernel(**inputs):
    inp = {k: np.asarray(v) for k, v in inputs.items()}
    x = inp["x"].astype(np.float32)
    Bn, S, Dm = x.shape
    NTH = S // 2
    key = ("FUSED", S)
    if key not in _PROG_CACHE:
        _PROG_CACHE[key] = build_fused(S)
    nc, names = _PROG_CACHE[key]
    cs = consts_np()
    shared = {"c_" + k: v for k, v in cs.items()}
    tok = []
    for L in range(2):
        th = token_host_inputs(inp, L)
        tok.append({f"t{L}_" + k: v for k, v in th.items() if not k.startswith("c_")})
    in_maps = []
    for core in range(NCORE):
        b, half = core // 2, core % 2
        d = dict(shared)
        d["hT0"] = x[b].T
        d["pos"] = inp["positions"][b:b + 1].astype(np.int32)
        d["pT0"] = inp["p"][0, b].T
        d["pT1"] = inp["p"][1, b, half * NTH:(half + 1) * NTH, :].T
        for L in range(2):
            for hg in range(2):
                md = mixer_host_inputs(inp, L, b, hg)
                for k, v in md.items():
                    if not k.startswith("c_") and k != "pos":
                        d[f"m{L}{hg}_" + k] = v
            d.update(tok[L])
            cvt = tok[L][f"t{L}_cv"].copy()
            cvt[:, CV["m0"]] = 1.0 if half == 0 else 0.0
            cvt[:, CV["m1"]] = 1.0 if half == 1 else 0.0
            d[f"t{L}_cv"] = cvt
        in_maps.append({n: np.ascontiguousarray(d[n]) for n in names})
    res = run_bass_kernel_spmd(nc, in_maps, core_ids=list(range(NCORE)))
    out = np.empty((Bn, S, Dm), np.float32)
    for core in range(NCORE):
        b, half = core // 2, core % 2
        out[b, half * NTH:(half + 1) * NTH, :] = res.results[core]["h3T"].T
    return out
```

```python
import numpy as np
from contextlib import ExitStack
import concourse.bass as bass
import concourse.mybir as mybir

F32 = mybir.dt.float32
BF16 = mybir.dt.bfloat16
I32 = mybir.dt.int32
ALU = mybir.AluOpType
AF = mybir.ActivationFunctionType
AX = mybir.AxisListType

ENGS = ("pe", "act", "dve", "pool", "sp")
N_DMA_SEMS = 24


class Op:
    __slots__ = ("eng", "fn", "deps", "is_dma", "idx", "sig", "slot", "slot_target", "slot_prev", "epoch")

    def __init__(self, eng, fn, is_dma):
        self.eng = eng
        self.fn = fn
        self.is_dma = is_dma
        self.deps = set()
        self.sig = 0
        self.slot = None
        self.slot_target = 0
        self.slot_prev = None


class V:
    __slots__ = ("ap", "key")

    def __init__(self, ap, key):
        self.ap = ap
        self.key = key

    def __getitem__(self, idx):
        return V(self.ap[idx], self.key)

    def sub(self, k):
        base = self.key[0] if isinstance(self.key, tuple) else self.key
        return V(self.ap, (base, k))

    def re(self, pat, **kw):
        return V(self.ap.rearrange(pat, **kw), self.key)

    def bc(self, shape):
        return V(self.ap.to_broadcast(list(shape)), self.key)

    @property
    def shape(self):
        return self.ap.shape


def _ap(x):
    return x.ap if isinstance(x, V) else x


ARENA_F32 = 53000


class Prog:
    def __init__(self, name="k"):
        self.nc = bass.Bass("TRN2", target_bir_lowering=False)
        self.st = ExitStack()
        self.arena = None
        self.aoff = 0
        self.amax = 0
        self.psum = None
        self.bar_deps = None
        self.since_bar = []
        self.bar_seen = {}
        self.epoch = 0
        self.ep_cnt = {}
        self.ops = []
        self.track = {}
        self.n_dma = 0
        self.slot_last = [None] * N_DMA_SEMS
        self.slot_count = [0] * N_DMA_SEMS
        self.uid = 0

    def sb(self, shape, dtype=F32, name=None):
        self.uid += 1
        return self.st.enter_context(self.nc.sbuf_tensor(name or f"sb{self.uid}", list(shape), dtype))

    def ps(self, shape, dtype=F32, name=None):
        self.uid += 1
        return self.st.enter_context(self.nc.psum_tensor(name or f"ps{self.uid}", list(shape), dtype))

    def dram(self, name, shape, dtype=F32, kind="Internal"):
        return self.nc.dram_tensor(name, list(shape), dtype, kind=kind)

    def tile(self, shape, name=None, dtype=None):
        if self.arena is None:
            self.arena = self.st.enter_context(self.nc.sbuf_tensor("arena", [128, ARENA_F32], F32))
        self.uid += 1
        p = shape[0]
        n = int(np.prod(shape[1:]))
        if dtype == BF16:
            nw = (n + 1) // 2
            assert self.aoff + nw <= ARENA_F32, f"arena overflow {self.aoff}+{nw}"
            ap = self.arena[0:p, self.aoff:self.aoff + nw].bitcast(BF16)[:, 0:n]
            self.aoff += nw
        else:
            assert self.aoff + n <= ARENA_F32, f"arena overflow {self.aoff}+{n}"
            ap = self.arena[0:p, self.aoff:self.aoff + n]
            self.aoff += n
        self.amax = max(self.amax, self.aoff)
        if len(shape) == 3:
            ap = ap.rearrange("p (a b) -> p a b", a=shape[1])
        elif len(shape) == 4:
            ap = ap.rearrange("p (a b c) -> p a b c", a=shape[1], b=shape[2])
        return V(ap, name or f"t{self.uid}")

    def mark(self):
        return self.aoff

    def release(self, mark):
        self.barrier()
        self.aoff = mark

    def pbank(self, i):
        if self.psum is None:
            self.psum = [self.st.enter_context(self.nc.psum_tensor(f"psb{j}", [128, 512], F32)) for j in range(8)]
        return V(self.psum[i][:], f"psb{i}")

    def pslot(self, bank, half):
        self.pbank(0)
        return V(self.psum[bank][:, half * 256:(half + 1) * 256], f"psb{bank}_{half}")

    def run_interleaved(self, fns):
        import threading
        il = {"turn": 0, "alive": [True] * len(fns), "cv": threading.Condition(), "tl": threading.local()}
        errs = []

        def nxt(i):
            n = len(fns)
            for d in range(1, n + 1):
                j = (i + d) % n
                if il["alive"][j]:
                    return j
            return i

        def runner(i, fn):
            with il["cv"]:
                while il["turn"] != i:
                    il["cv"].wait()
            il["tl"].i = i
            try:
                fn()
            except BaseException as e:
                errs.append(e)
            finally:
                with il["cv"]:
                    il["alive"][i] = False
                    il["turn"] = nxt(i)
                    il["cv"].notify_all()

        def yield_turn():
            i = getattr(il["tl"], "i", None)
            if i is None:
                return
            with il["cv"]:
                j = nxt(i)
                if j == i:
                    return
                il["turn"] = j
                il["cv"].notify_all()
                while il["turn"] != i:
                    il["cv"].wait()

        self._yield = yield_turn
        ths = [threading.Thread(target=runner, args=(i, f)) for i, f in enumerate(fns)]
        for t in ths:
            t.start()
        for t in ths:
            t.join()
        self._yield = None
        if errs:
            raise errs[0]

    def dview(self, t, name=None):
        ap = t.ap() if hasattr(t, "ap") and callable(t.ap) else t
        return V(ap, name or ap.tensor.name)

    def barrier(self):
        self.bar_deps = list(self.since_bar) if self.bar_deps is None else self.bar_deps + self.since_bar
        last = {}
        dm = []
        for o in self.bar_deps:
            if o.is_dma:
                dm.append(o)
            else:
                last[o.eng] = o
        self.bar_deps = list(last.values()) + dm[-2 * N_DMA_SEMS:]
        self.since_bar = []
        self.bar_seen = {}
        if max(self.ep_cnt.values(), default=0) > 20000:
            self.epoch += 1
            self.ep_cnt = {}

    @staticmethod
    def _key(x):
        if isinstance(x, V):
            x = x.key
        if isinstance(x, tuple):
            t, sub = x
        else:
            t, sub = x, None
        nm = t if isinstance(t, str) else (t.name if hasattr(t, "name") else t.tensor.name)
        return nm, sub

    def _conf(self, nm, sub):
        ent = self.track.setdefault(nm, {})
        if sub is None:
            return list(ent.keys())
        ks = [k for k in ent.keys() if k is None or k == sub]
        return ks

    def op(self, eng, fn, reads=(), writes=(), is_dma=False):
        o = Op(eng, fn, is_dma)
        o.epoch = self.epoch
        if not is_dma:
            self.ep_cnt[eng] = self.ep_cnt.get(eng, 0) + 1
        for r in reads:
            nm, sub = self._key(r)
            ent = self.track.setdefault(nm, {})
            for k in self._conf(nm, sub):
                w = ent[k][0]
                if w is not None:
                    o.deps.add(w)
        for wv in writes:
            nm, sub = self._key(wv)
            ent = self.track.setdefault(nm, {})
            for k in self._conf(nm, sub):
                w, rs = ent[k]
                if w is not None:
                    o.deps.add(w)
                for r in rs:
                    o.deps.add(r)
        for r in reads:
            nm, sub = self._key(r)
            ent = self.track[nm]
            if sub not in ent:
                ent[sub] = [None, []]
            ent[sub][1].append(o)
        for wv in writes:
            nm, sub = self._key(wv)
            ent = self.track[nm]
            if sub is None:
                for k in list(ent.keys()):
                    del ent[k]
            ent[sub] = [o, []]
        o.deps.discard(o)
        if self.bar_deps is not None and not self.bar_seen.get(eng):
            self.bar_seen[eng] = True
            o.deps.update(self.bar_deps)
        self.since_bar.append(o)
        if is_dma:
            s = self.n_dma % N_DMA_SEMS
            self.n_dma += 1
            o.slot = s
            o.slot_prev = self.slot_last[s]
            self.slot_count[s] += 1
            o.slot_target = 16 * self.slot_count[s]
            self.slot_last[s] = o
        o.idx = len(self.ops)
        self.ops.append(o)
        if getattr(self, "_yield", None) is not None:
            self._yield()
        return o

    def dma(self, out, in_, reads=None, writes=None, q="sp", in_fn=None, **kw):
        if in_fn is not None:
            return self.op(q, lambda e: e.dma_start(out=_ap(out), in_=in_fn(e), **kw),
                           reads if reads is not None else [in_], writes if writes is not None else [out], is_dma=True)
        return self.op(q, lambda e: e.dma_start(out=_ap(out), in_=_ap(in_), **kw),
                       reads if reads is not None else [in_], writes if writes is not None else [out], is_dma=True)

    def mm(self, out, lhsT, rhs, start=True, stop=True, reads=None, writes=None, **kw):
        return self.op("pe", lambda e: e.matmul(_ap(out), _ap(lhsT), _ap(rhs), start=start, stop=stop, **kw),
                       reads if reads is not None else [lhsT, rhs], writes if writes is not None else [out])

    def transpose(self, out, in_, ident, reads=None, writes=None):
        return self.op("pe", lambda e: e.transpose(_ap(out), _ap(in_), _ap(ident)),
                       reads if reads is not None else [in_, ident], writes if writes is not None else [out])

    def act(self, out, in_, func, bias=None, scale=1.0, reads=None, writes=None, accum_out=None, eng="act"):
        kw = {}
        rd = [in_]
        if bias is not None:
            kw["bias"] = _ap(bias)
            if not isinstance(bias, (int, float)):
                rd.append(bias)
        if not isinstance(scale, (int, float)):
            rd.append(scale)
        wr = [out]
        if accum_out is not None:
            kw["accum_out"] = _ap(accum_out)
            wr.append(accum_out)
        return self.op(eng, lambda e: e.activation(_ap(out), _ap(in_), func, scale=_ap(scale), **kw),
                       reads if reads is not None else rd, writes if writes is not None else wr)

    def tt(self, out, in0, in1, op, eng="dve", reads=None, writes=None):
        return self.op(eng, lambda e: e.tensor_tensor(_ap(out), _ap(in0), _ap(in1), op),
                       reads if reads is not None else [in0, in1], writes if writes is not None else [out])

    def ts(self, out, in0, s1, op0, s2=None, op1=None, eng="dve", reads=None, writes=None):
        rd = [in0] + [s for s in (s1, s2) if s is not None and not isinstance(s, (int, float))]
        if op1 is None:
            f = lambda e: e.tensor_scalar(_ap(out), _ap(in0), _ap(s1), None, op0)
        else:
            f = lambda e: e.tensor_scalar(_ap(out), _ap(in0), _ap(s1), _ap(s2), op0, op1)
        return self.op(eng, f, reads if reads is not None else rd, writes if writes is not None else [out])

    def stt(self, out, in0, scalar, in1, op0, op1, eng="dve", reads=None, writes=None):
        rd = [in0, in1] + ([] if isinstance(scalar, (int, float)) else [scalar])
        return self.op(eng, lambda e: e.scalar_tensor_tensor(_ap(out), _ap(in0), _ap(scalar), _ap(in1), op0, op1),
                       reads if reads is not None else rd, writes if writes is not None else [out])

    def copy(self, out, in_, eng="dve", reads=None, writes=None):
        if eng == "act":
            f = lambda e: e.copy(_ap(out), _ap(in_))
        else:
            f = lambda e: e.tensor_copy(_ap(out), _ap(in_))
        return self.op(eng, f, reads if reads is not None else [in_], writes if writes is not None else [out])

    def memset(self, ap, val, eng="pool", writes=None):
        return self.op(eng, lambda e: e.memset(_ap(ap), val), [], writes if writes is not None else [ap])

    def recip(self, out, in_, reads=None, writes=None):
        return self.op("dve", lambda e: e.reciprocal(_ap(out), _ap(in_)),
                       reads if reads is not None else [in_], writes if writes is not None else [out])

    def finalize(self, final_waits=()):
        nc = self.nc
        ops = self.ops
        needed = set()
        for o in ops:
            for d in o.deps:
                if not d.is_dma:
                    needed.add(d.idx)
        cnt = {}
        for o in ops:
            if not o.is_dma and (o.idx in needed):
                k_ = (o.eng, o.epoch)
                cnt[k_] = cnt.get(k_, 0) + 1
                o.sig = cnt[k_]
            else:
                o.sig = 0
        sems = {k_: self.st.enter_context(nc.semaphore(f"s_{k_[0]}_{k_[1]}")) for k_ in cnt}
        dsems = [self.st.enter_context(nc.semaphore(f"s_d{i}")) for i in range(N_DMA_SEMS)]
        per_eng = {e: [o for o in ops if o.eng == e] for e in ENGS}
        final = list(final_waits)

        def body(eng_name):
            def _b(e):
                waited = {}
                def wait(key, sem, val):
                    if waited.get(key, 0) >= val:
                        return
                    e.wait_ge(sem, val)
                    waited[key] = val
                for o in per_eng[eng_name]:
                    for d in sorted(o.deps, key=lambda x: x.idx):
                        if d.is_dma:
                            wait(("d", d.slot), dsems[d.slot], d.slot_target)
                        else:
                            if d.eng == eng_name and eng_name == "pe":
                                continue
                            wait(("c", d.eng, d.epoch), sems[(d.eng, d.epoch)], d.sig)
                    if o.is_dma and o.slot_prev is not None:
                        wait(("d", o.slot), dsems[o.slot], o.slot_prev.slot_target)
                    ins = o.fn(e)
                    if o.is_dma:
                        ins.then_inc(dsems[o.slot], 16)
                    elif o.sig:
                        ins.then_inc(sems[(eng_name, o.epoch)], 1)
                if eng_name == "sp":
                    for s in range(N_DMA_SEMS):
                        if self.slot_last[s] is not None:
                            wait(("d", s), dsems[s], self.slot_last[s].slot_target)
            return _b

        with nc.Block() as block:
            block.tensor(body("pe"))
            block.scalar(body("act"))
            block.vector(body("dve"))
            block.gpsimd(body("pool"))
            block.sync(body("sp"))
        self.st.close()
        return nc

D = 1024
TT = 512
ZG = [64] * 12 + [64, 64, 128] + [128, 128, 128, 32] + [64] * 16 + [4, 4]
NG = len(ZG)
G_R, G_K, G_V, G_WLO, G_ALO, G_GLO = 0, 4, 8, 12, 13, 14
G_CQ, G_CKV, G_KPE = 15, 17, 18
G_GQ, G_GK, G_GV, G_GG, G_GB, G_GA = 19, 23, 27, 31, 35, 36
ZOFF = np.concatenate([[0], np.cumsum(ZG)]).astype(int)
NZ = int(ZOFF[-1])

CV = {}
_n = 0
for nm, k in [("nmg", 8), ("mu", 15), ("w0", 4), ("a0", 4), ("kk", 4), ("ka", 4), ("rk", 4), ("lng", 4), ("lnb", 4),
              ("vmu", 8), ("vb", 4), ("qng", 2), ("kvng", 1), ("qkq", 1), ("qkk", 1), ("invf", 1),
              ("conv", 48), ("alog", 1), ("dtb", 1), ("gng", 1), ("ropec", 1), ("nfg", 8), ("png", 8), ("m0", 1), ("m1", 1)]:
    CV[nm] = _n
    _n += k
NCV = _n


def consts_np():
    c = {}
    c["ident"] = np.eye(128, dtype=np.float32)
    c["ones"] = np.ones((128, 128), np.float32)
    bd = np.zeros((128, 128), np.float32)
    bd[:64, :64] = 1
    bd[64:, 64:] = 1
    c["bd64"] = bd
    s = np.arange(64)[:, None]
    t = np.arange(64)[None, :]
    incl = (t >= s).astype(np.float32)
    strict = (t > s).astype(np.float32)
    c["m_incl"] = np.concatenate([incl, incl], 0)
    c["m_strict"] = np.concatenate([strict, strict], 0)
    c["m_incl_T"] = np.concatenate([incl.T, incl.T], 0)
    c["m_strict_T"] = np.concatenate([strict.T, strict.T], 0)
    c["neg_incl"] = (1.0 - c["m_incl"]) * -1e4
    c["neg_incl_T"] = (1.0 - c["m_incl_T"]) * -1e4
    c["id64x2"] = np.concatenate([np.eye(64, dtype=np.float32)] * 2, 0)
    kk = np.arange(128)[:, None]
    qq = np.arange(128)[None, :]
    c["att_mask"] = (qq >= kk).astype(np.float32)
    R = np.zeros((96, 96), np.float32)
    for m in range(16):
        R[64 + m, 64 + m + 16] = -1.0
        R[64 + 16 + m, 64 + m] = 1.0
    c["ropeRT"] = np.ascontiguousarray(R.T)
    sel = np.zeros((4, 4, 64), np.float32)
    for h in range(4):
        sel[h, h, :] = 1.0
    c["sel4"] = sel.reshape(4, 256)
    sel8 = np.zeros((8, 8, 128), np.float32)
    for e in range(8):
        sel8[e, e, :] = 1.0
    c["sel8"] = sel8.reshape(8, 1024)
    c["id4"] = np.tile(np.eye(64, dtype=np.float32)[:, None, :], (1, 4, 1)).reshape(64, 256)
    c["neg4"] = np.tile(c["neg_incl"][:64][:, None, :], (1, 4, 1)).reshape(64, 256)
    c["neg4T"] = np.tile(c["neg_incl_T"][:64][:, None, :], (1, 4, 1)).reshape(64, 256)
    c["ms4"] = np.tile(c["m_strict"][:64][:, None, :], (1, 4, 1)).reshape(64, 256)
    c["ms4T"] = np.tile(c["m_strict_T"][:64][:, None, :], (1, 4, 1)).reshape(64, 256)
    c["mi4"] = np.tile(c["m_incl"][:64][:, None, :], (1, 4, 1)).reshape(64, 256)
    return c


CONST_SHAPES = {k: v.shape for k, v in consts_np().items()}


class Ctx:
    pass


def load_consts(P, names):
    out = {}
    for nm in names:
        shp = CONST_SHAPES[nm]
        d = P.dview(P.dram("c_" + nm, shp, F32, kind="ExternalInput"))
        t = P.tile(list(shp), name="c_" + nm)
        P.dma(t, d)
        out[nm] = t
    return out


def rstd_from_ps(P, out, ps, n, eps):
    P.act(out, ps, AF.Ln, scale=float(1.0 / n), bias=float(eps))
    P.act(out, out, AF.Exp, scale=-0.5)


def phase_proj(P, C, S, hT, w_d, ncols, groups, zT, gcol, uT=None, func=None, nbuf=2, zflat=False, dyn=None):
    mk = P.mark()
    cv = C.cv
    w = P.tile([128, 8, ncols], name="w_in", dtype=BF16)
    wst = [P.tile([128, 8, 512], name=f"wst{i}") for i in range(2)]
    wv = w_d.re("(c p) n -> p c n", p=128)
    step = 512
    for i_, c0 in enumerate(range(0, ncols, step)):
        c1 = min(ncols, c0 + step)
        st_ = wst[i_ % 2]
        P.dma(st_[:, :, 0:c1 - c0], wv[:, :, c0:c1])
        P.copy(w[:, :, c0:c1].sub(c0), st_[:, :, 0:c1 - c0], eng=("pool" if i_ % 2 == 0 else "act"))
    hb = [P.tile([128, 8, TT], name=f"hb{i}") for i in range(nbuf)]
    sq = P.tile([128, 8, TT], name="sq")
    ub = [P.tile([128, 8, TT], name=f"ub{i}", dtype=BF16) for i in range(nbuf)]
    rs = P.tile([128, TT], name="rs")
    stg = [P.tile([128, TT], name=f"stg{i}") for i in range(4)]
    hv = hT.re("(c p) t -> p c t", p=128)
    ps_ss = P.pbank(0)
    pz = [P.pbank(1 + i) for i in range(4)]
    nt = S // TT
    k = 0
    for ti in range(nt):
        tsl = slice(ti * TT, (ti + 1) * TT)
        h = hb[ti % nbuf]
        u = ub[ti % nbuf]
        if dyn is not None:
            P.dma(h, hv[:, :, tsl])
            P.dma(sq, hv[:, :, dyn + ti * TT:dyn + (ti + 1) * TT])
            P.ts(h, h, cv[:, CV["m0"]:CV["m0"] + 1], ALU.mult)
            P.stt(h, sq, cv[:, CV["m1"]:CV["m1"] + 1], h, ALU.mult, ALU.add)
        else:
            P.dma(h, hv[:, :, tsl])
        P.act(sq, h, AF.Square)
        for c in range(8):
            P.mm(ps_ss, C.k["ones"], sq[:, c, :], start=(c == 0), stop=(c == 7))
        rstd_from_ps(P, rs, ps_ss, D, 1e-6)
        if uT is not None:
            for c in range(8):
                P.stt(sq[:, c, :], h[:, c, :], cv[:, gcol + c:gcol + c + 1], rs, ALU.mult, ALU.mult)
            P.dma(uT.re("(c p) t -> p c t", p=128)[:, :, tsl], sq, q="pool")
            P.copy(u, sq, eng="act")
        else:
            for c in range(8):
                P.stt(u[:, c, :], h[:, c, :], cv[:, gcol + c:gcol + c + 1], rs, ALU.mult, ALU.mult)
        for gi, (co, n) in enumerate(groups):
            pp = pz[k % 4]
            st = stg[k % 4]
            for c in range(8):
                P.mm(pp[0:n, :], w[:, c, co:co + n], u[:, c, :], start=(c == 0), stop=(c == 7))
            if func is not None:
                P.act(st[0:n, :], pp[0:n, :], func)
            else:
                P.copy(st[0:n, :], pp[0:n, :], eng=("act" if k % 2 == 0 else "dve"))
            if zflat:
                P.dma(zT[co:co + n, tsl].sub(gi), st[0:n, :], q="pool")
            else:
                P.dma(zT[gi, 0:n, tsl].sub(gi), st[0:n, :], q="pool")
            k += 1
    P.release(mk)


def phase_mla(P, C, S, zT, pos_d, w_uq_d, w_uk_d, w_uv_d, oT):
    mk = P.mark()
    cv = C.cv
    K = C.k
    nt = S // TT
    wuq_f = P.tile([128, 2, 384], name="wuq_f")
    P.dma(wuq_f, w_uq_d.re("(c p) n -> p c n", p=128))
    wuk_f = P.tile([128, 256], name="wuk_f")
    P.dma(wuk_f, w_uk_d)
    wuv_f = P.tile([128, 256], name="wuv_f")
    P.dma(wuv_f, w_uv_d)
    wuq = P.tile([128, 2, 384], name="wuq", dtype=BF16)
    wuk = P.tile([128, 256], name="wuk", dtype=BF16)
    wuv = P.tile([128, 256], name="wuv", dtype=BF16)
    P.copy(wuq, wuq_f, eng="pool")
    P.copy(wuk, wuk_f, eng="pool")
    P.copy(wuv, wuv_f, eng="pool")
    ones_b = P.tile([128, 64], name="ones_b", dtype=BF16)
    P.copy(ones_b, K["ones"][:, 0:64], eng="pool")
    KT = P.tile([96, 4, S], name="KT", dtype=BF16)
    VT = P.tile([128, S // 128, 256], name="Vtm", dtype=BF16)
    rc = P.tile([96, TT], name="rope_c")
    rsn = P.tile([96, TT], name="rope_s")
    posi = P.tile([96, TT], name="posi")
    posf = P.tile([96, TT], name="posf")
    ang = P.tile([96, TT], name="ang")
    tmp = P.tile([96, TT], name="ropetmp")
    tmp2 = P.tile([96, TT], name="ropetmp2")
    P.memset(rc[0:64, :], 1.0, writes=[rc.sub("lo")])
    P.memset(rsn[0:64, :], 0.0, writes=[rsn.sub("lo")])
    pi_ap = V(posi.ap.bitcast(I32), posi.key)
    invf = cv[64:96, CV["invf"]:CV["invf"] + 1]
    negpi = cv[64:96, CV["ropec"]:CV["ropec"] + 1]
    TWO_PI = float(2 * np.pi)

    def rope_tile(ti):
        tsl = slice(ti * TT, (ti + 1) * TT)
        P.dma(pi_ap[64:96, :], V(pos_d.ap[:, tsl].partition_broadcast(32), pos_d.key))
        P.copy(posf[64:96, :], pi_ap[64:96, :])
        P.ts(ang[64:96, :], posf[64:96, :], invf, ALU.mult)
        for dst, shift in ((rsn, 0.0), (rc, float(np.pi / 2))):
            a_ = ang[64:96, :]
            if shift:
                P.ts(tmp2[64:96, :], ang[64:96, :], shift, ALU.add)
                a_ = tmp2[64:96, :]
            P.ts(tmp[64:96, :], a_, float(1.0 / TWO_PI), ALU.mult)
            P.copy(pi_ap[64:96, :], tmp[64:96, :])
            P.copy(tmp[64:96, :], pi_ap[64:96, :])
            P.stt(tmp[64:96, :], tmp[64:96, :], -TWO_PI, a_, ALU.mult, ALU.add)
            P.ts(posf[64:96, :], tmp[64:96, :], float(np.pi), ALU.is_gt, TWO_PI, ALU.mult)
            P.tt(tmp[64:96, :], tmp[64:96, :], posf[64:96, :], ALU.subtract)
            P.act(dst[64:96, :], tmp[64:96, :], AF.Sin, writes=[dst.sub("hi")])

    ones = K["ones"]
    cq = [P.tile([128, 2, TT], name=f"cq{i}") for i in range(2)]
    ckv = [P.tile([128, TT], name=f"ckv{i}") for i in range(2)]
    cqb = [P.tile([128, 2, TT], name=f"cqb{i}", dtype=BF16) for i in range(2)]
    ckvb = [P.tile([128, TT], name=f"ckvb{i}", dtype=BF16) for i in range(2)]
    kpe = [P.tile([96, TT], name=f"kpe{i}") for i in range(2)]
    sq = P.tile([128, 2, TT], name="msq")
    rs = P.tile([128, TT], name="mrs")
    raw = P.tile([96, TT], name="raw")
    nrm = P.tile([96, TT], name="nrm")
    rot = P.tile([96, TT], name="rot")
    QT = P.tile([96, 4, TT], name="QT", dtype=BF16)
    ps_a = P.pbank(0)
    ps_b = P.pbank(1)
    ps_c = P.pbank(2)

    def qk_finish(src_raw, gcolname, dst, tsl):
        P.act(sq[0:96, 0, :], src_raw, AF.Square)
        P.mm(ps_b[0:96, :], ones[0:96, 0:96], sq[0:96, 0, :])
        rstd_from_ps(P, rs[0:96, :], ps_b[0:96, :], 96, 1e-6)
        g = cv[0:96, CV[gcolname]:CV[gcolname] + 1]
        P.stt(nrm, src_raw, g, rs[0:96, :], ALU.mult, ALU.mult)
        P.mm(ps_c[0:96, :], K["ropeRT"], nrm)
        P.tt(rot, ps_c[0:96, :], rsn, ALU.mult)
        P.tt(nrm, nrm, rc, ALU.mult, eng="pool")
        P.tt(dst, nrm, rot, ALU.add)

    def load_norm(ti, want_q):
        tsl = slice(ti * TT, (ti + 1) * TT)
        i2 = ti % 2
        if want_q:
            P.dma(cq[i2], zT[G_CQ:G_CQ + 2, :, tsl].re("g p t -> p g t"))
            P.act(sq, cq[i2], AF.Square)
            P.mm(ps_a, ones, sq[:, 0, :], start=True, stop=False)
            P.mm(ps_a, ones, sq[:, 1, :], start=False, stop=True)
            rstd_from_ps(P, rs, ps_a, 256, 1e-6)
            for c in range(2):
                P.stt(cqb[i2][:, c, :], cq[i2][:, c, :], cv[:, CV["qng"] + c:CV["qng"] + c + 1], rs, ALU.mult, ALU.mult)
        else:
            P.dma(ckv[i2], zT[G_CKV, :, tsl])
            P.dma(kpe[i2][64:96, :], zT[G_KPE, 0:32, tsl])
            P.act(sq[:, 0, :], ckv[i2], AF.Square)
            P.mm(ps_a, ones, sq[:, 0, :])
            rstd_from_ps(P, rs, ps_a, 128, 1e-6)
            P.stt(ckvb[i2], ckv[i2], cv[:, CV["kvng"]:CV["kvng"] + 1], rs, ALU.mult, ALU.mult)
        return tsl, i2

    for ti in range(nt):
        rope_tile(ti)
        tsl, i2 = load_norm(ti, False)
        for h in range(4):
            P.mm(ps_b[0:64, :], wuk[:, h * 64:(h + 1) * 64], ckvb[i2])
            P.copy(raw[0:64, :], ps_b[0:64, :], eng="act", writes=[raw.sub("lo")])
            P.copy(raw[64:96, :], kpe[i2][64:96, :], eng="pool", writes=[raw.sub("hi")])
            qk_finish(raw, "qkk", KT[:, h, tsl].sub(h), tsl)
        for j in range(TT // 128):
            P.mm(ps_c[:, 0:256], ckvb[i2][:, j * 128:(j + 1) * 128], wuv)
            P.copy(VT[:, ti * (TT // 128) + j, :], ps_c[:, 0:256], eng="act")

    pt = [P.tile([128, TT], name=f"pt{i}", dtype=BF16) for i in range(3)]
    osb = P.tile([64, TT], name="osb")
    lsb = P.tile([64, TT], name="lsb")
    ps_s = [P.pbank(3), P.pbank(4)]
    ps_o = P.pbank(5)
    ps_l = P.pbank(6)
    scale = float(96 ** -0.5)
    for ti in range(nt):
        rope_tile(ti)
        tsl, i2 = load_norm(ti, True)
        for h in range(4):
            for c in range(2):
                P.mm(ps_b[0:96, :], wuq[:, c, h * 96:(h + 1) * 96], cqb[i2][:, c, :], start=(c == 0), stop=(c == 1))
            P.copy(raw, ps_b[0:96, :], eng="act")
            qk_finish(raw, "qkq", QT[:, h, :].sub(h), tsl)
        for h in range(4):
            nkc = 4 * (ti + 1)

            def c0_of(kc):
                j = kc - 4 * ti
                return 0 if j <= 0 else j * 128

            def score(kc):
                c0 = c0_of(kc)
                P.mm(ps_s[kc % 2][:, c0:TT], KT[:, h, kc * 128:(kc + 1) * 128].sub(h), QT[:, h, c0:TT].sub(h))
            score(0)
            for kc in range(nkc):
                if kc + 1 < nkc:
                    score(kc + 1)
                j = kc - 4 * ti
                c0 = c0_of(kc)
                pss = ps_s[kc % 2]
                p_t = pt[kc % 3]
                P.act(p_t[:, c0:TT], pss[:, c0:TT], AF.Exp, scale=scale)
                if j >= 0:
                    P.tt(p_t[:, c0:c0 + 128], p_t[:, c0:c0 + 128], K["att_mask"], ALU.mult, eng="pool")
                P.mm(ps_o[0:64, c0:TT], VT[:, kc, h * 64:(h + 1) * 64], p_t[:, c0:TT], start=(kc == 0), stop=(kc == nkc - 1))
                P.mm(ps_l[0:64, c0:TT], ones_b, p_t[:, c0:TT], start=(kc == 0), stop=(kc == nkc - 1))
            P.act(lsb, ps_l[0:64, :], AF.Ln)
            P.act(lsb, lsb, AF.Exp, scale=-1.0)
            P.tt(osb, ps_o[0:64, :], lsb, ALU.mult)
            P.dma(oT[h * 64:(h + 1) * 64, tsl].sub(h), osb, q="pool")
    P.release(mk)


def neumann_inv(P, C, A0, B0, bufs, ps1, ps2, ps3):
    id4 = C.k["id4"]
    TTt = bufs["TT"]
    P.tt(TTt, B0, id4, ALU.add)
    A = [A0, bufs["A1"]]
    B = [B0, bufs["B1"]]
    for k in range(1, 6):
        a_prev, a_new = A[(k - 1) % 2], A[k % 2]
        b_prev, b_new = B[(k - 1) % 2], B[k % 2]
        for h in range(4):
            hs = slice(h * 64, (h + 1) * 64)
            P.mm(ps1[0:64, hs], b_prev[:, hs], a_prev[:, hs])
        if k < 5:
            for h in range(4):
                hs = slice(h * 64, (h + 1) * 64)
                P.mm(ps2[0:64, hs], a_prev[:, hs], b_prev[:, hs])
        P.copy(a_new, ps1[0:64, 0:256], eng="act")
        if k < 5:
            P.copy(b_new, ps2[0:64, 0:256], eng="dve")
        for h in range(4):
            hs = slice(h * 64, (h + 1) * 64)
            P.mm(ps3[0:64, hs], a_new[:, hs], TTt[:, hs])
        P.tt(TTt, TTt, ps3[0:64, 0:256], ALU.add)
    return TTt


def phase_gdn(P, C, S, zT, oT, ttl=512, psbase=None, release=True):
    mk = P.mark()
    TT = ttl
    cv = C.cv
    K = C.k
    nt = S // TT
    NCH = TT // 64
    ones = K["ones"]
    ident = K["ident"]
    cvw = lambda seg, h, j: cv[0:64, CV["conv"] + (seg * 4 + h) * 4 + j:CV["conv"] + (seg * 4 + h) * 4 + j + 1]
    St = P.tile([64, 4, 64], name="gS")
    P.memset(St, 0.0)
    Sb = P.tile([64, 4, 64], name="gSb", dtype=BF16)
    P.copy(Sb, St, eng="pool")
    nA = P.tile([4, 1], name="nA")
    P.act(nA, cv[0:4, CV["alog"]:CV["alog"] + 1], AF.Exp)
    P.ts(nA, nA, -1.0, ALU.mult)
    def mkbuf(i):
        b = {}
        for nm in ["q", "k", "kb", "qd"]:
            b[nm] = P.tile([64, 4, TT], name=f"g{nm}{i}", dtype=BF16)
        b["k32"] = P.tile([64, 4, TT], name=f"gk32{i}")
        b["q32"] = P.tile([64, 4, TT], name=f"gq32{i}")
        b["ktm"] = P.tile([64, NCH, 4, 64], name=f"gktm{i}", dtype=BF16)
        b["bv"] = P.tile([64, NCH, 4, 64], name=f"gbv{i}")
        b["bg"] = P.tile([64, NCH, 12], name=f"gbg{i}")
        b["c2"] = P.tile([64, NCH, 4], name=f"gc2{i}")
        b["ngc"] = P.tile([64, NCH, 4], name=f"gngc{i}")
        b["dl"] = P.tile([64, 4, NCH], name=f"gdl{i}")
        return b
    TB = [mkbuf(0), mkbuf(1)]
    xin = [P.tile([64, TT + 3], name=f"gxin{i}") for i in range(3)]
    acc = [P.tile([64, TT], name=f"gacc{i}") for i in range(2)]
    vfm = P.tile([64, 4, TT], name="gvfm")
    sq = P.tile([64, TT], name="gsq")
    rs = P.tile([64, TT], name="grs")
    bfm = P.tile([4, TT], name="gbfm")
    gfm = [P.tile([4, TT], name=f"ggfm{i}") for i in range(2)]
    efm = P.tile([4, TT], name="gefm")
    kdf = P.tile([4, TT], name="gkdf")
    gl4 = P.tile([4, NCH], name="ggl4")
    ob = [P.tile([64, 4, TT], name=f"gob{i}") for i in range(2)]
    gate = P.tile([64, TT], name="ggate")
    def mkch(i):
        d_ = {nm: P.tile([64, 256], name=f"gc_{nm}{i}") for nm in ["E", "F", "G1", "G2", "Gs"]}
        d_.update({nm: P.tile([64, 256], name=f"gc_{nm}{i}", dtype=BF16) for nm in ["A0", "B0", "A1", "B1", "TT", "Ain", "X", "vn"]})
        return d_
    CB = [mkch(0), mkch(1)]
    if psbase is None:
        psA, psB, psC, psD, psE, psF, psG, psH = [P.pbank(i) for i in range(8)]
    else:
        psA, psB, psC, psD, psE, psF, psG, psH = [P.pbank(psbase + j % 4) for j in range(8)]
    xk = 0
    for ti in range(nt):
        tb = TB[ti % 2]
        tsl = slice(ti * TT, (ti + 1) * TT)
        for seg, (g0, dst) in enumerate([(G_GQ, tb["q32"]), (G_GK, tb["k32"]), (G_GV, vfm)]):
            for h in range(4):
                x = xin[xk % 3]
                a = acc[xk % 2]
                xk += 1
                if ti == 0:
                    P.memset(x[:, 0:3], 0.0, writes=[x.sub("halo")])
                    P.dma(x[:, 3:TT + 3].sub("body"), zT[g0 + h, 0:64, 0:TT])
                else:
                    P.dma(x, zT[g0 + h, 0:64, ti * TT - 3:(ti + 1) * TT])
                P.ts(a, x[:, 0:TT], cvw(seg, h, 0), ALU.mult)
                for j in range(1, 4):
                    P.stt(a, x[:, j:TT + j], cvw(seg, h, j), a, ALU.mult, ALU.add)
                if seg == 2:
                    P.act(dst[:, h, :].sub(h), a, AF.Silu)
                else:
                    P.act(a, a, AF.Silu)
                    P.act(sq, a, AF.Square)
                    P.mm(psA[0:64, 0:TT], ones[0:64, 0:64], sq)
                    rstd_from_ps(P, rs, psA[0:64, 0:TT], 1.0, 1e-12)
                    P.stt(dst[:, h, :].sub(h), a, (0.125 if seg == 0 else 1.0), rs, ALU.mult, ALU.mult)
        P.dma(bfm, zT[G_GB, 0:4, tsl])
        P.act(bfm, bfm, AF.Sigmoid)
        g0t = gfm[0]
        P.dma(g0t, zT[G_GA, 0:4, tsl])
        P.act(g0t, g0t, AF.Exp, bias=cv[0:4, CV["dtb"]:CV["dtb"] + 1])
        P.act(g0t, g0t, AF.Ln, bias=1.0)
        P.ts(g0t, g0t, nA[:, 0:1], ALU.mult)
        cur = 0
        for sh in (1, 2, 4, 8, 16, 32):
            src = gfm[cur].re("h (n c) -> h n c", c=64)
            dstt = gfm[1 - cur].re("h (n c) -> h n c", c=64)
            P.copy(dstt[:, :, 0:sh], src[:, :, 0:sh], eng="pool", writes=[gfm[1 - cur].sub("a")])
            P.tt(dstt[:, :, sh:64], src[:, :, sh:64], src[:, :, 0:64 - sh], ALU.add, writes=[gfm[1 - cur].sub("b")])
            cur = 1 - cur
        gc = gfm[cur]
        P.act(efm, gc, AF.Exp)
        gc3 = gc.re("h (n c) -> h n c", c=64)
        P.copy(gl4, gc3[:, :, 63])
        P.tt(kdf.re("h (n c) -> h n c", c=64), V(gl4.ap.unsqueeze(2).to_broadcast([4, NCH, 64]), gl4.key), gc3, ALU.subtract)
        P.act(kdf, kdf, AF.Exp)
        for h in range(4):
            P.mm(psA[0:64, h * NCH:(h + 1) * NCH], K["sel4"][:, h * 64:(h + 1) * 64], gl4)
        P.act(tb["dl"].re("p h n -> p (h n)"), psA[0:64, 0:4 * NCH], AF.Exp)
        P.copy(tb["k"], tb["k32"], eng="pool")
        P.copy(tb["q"], tb["q32"], eng="pool")
        for h in range(4):
            P.mm(psB[0:64, 0:TT], K["sel4"][:, h * 64:(h + 1) * 64], bfm)
            P.tt(tb["kb"][:, h, :].sub(h), tb["k32"][:, h, :].sub(h), psB[0:64, 0:TT], ALU.mult)
            P.mm(psC[0:64, 0:TT], K["sel4"][:, h * 64:(h + 1) * 64], efm)
            P.tt(tb["qd"][:, h, :].sub(h), tb["q32"][:, h, :].sub(h), psC[0:64, 0:TT], ALU.mult)
        for n in range(NCH):
            cs = slice(n * 64, (n + 1) * 64)
            for h in range(4):
                P.transpose(psD[0:64, h * 64:(h + 1) * 64], tb["k32"][:, h, cs].sub(h), ident[0:64, 0:64])
            P.copy(tb["ktm"][:, n, :, :].re("p h d -> p (h d)"), psD[0:64, 0:256], eng="act")
            for h in range(4):
                P.transpose(psE[0:64, h * 64:(h + 1) * 64], vfm[:, h, cs].sub(h), ident[0:64, 0:64])
            P.copy(tb["bv"][:, n, :, :].re("p h d -> p (h d)"), psE[0:64, 0:256], eng="dve")
            P.transpose(psF[0:64, 0:4], bfm[:, cs], ident[0:4, 0:4])
            P.transpose(psF[0:64, 4:8], gc[:, cs], ident[0:4, 0:4])
            P.transpose(psF[0:64, 8:12], kdf[:, cs], ident[0:4, 0:4])
            P.copy(tb["bg"][:, n, :], psF[0:64, 0:12], eng="act")
        bg = tb["bg"]
        P.ts(tb["ngc"], bg[:, :, 4:8], -1.0, ALU.mult)
        P.act(tb["c2"], bg[:, :, 4:8], AF.Exp)
        P.stt(tb["c2"], tb["c2"], -1.0, bg[:, :, 0:4], ALU.mult, ALU.mult)
        P.tt(tb["ktm"], tb["ktm"], V(bg.ap[:, :, 8:12].unsqueeze(3).to_broadcast([64, NCH, 4, 64]), bg.key), ALU.mult)
        P.tt(tb["bv"], tb["bv"], V(bg.ap[:, :, 0:4].unsqueeze(3).to_broadcast([64, NCH, 4, 64]), bg.key), ALU.mult)
        o_t = ob[ti % 2]
        for n in range(NCH):
            cb = CB[n % 2]
            cs = slice(n * 64, (n + 1) * 64)
            gcn = V(bg.ap[:, n, 4:8].unsqueeze(2).to_broadcast([64, 4, 64]), bg.key)
            ngcn = V(tb["ngc"].ap[:, n, :].unsqueeze(2).to_broadcast([64, 4, 64]), tb["ngc"].key)
            E3 = cb["E"].re("p (h c) -> p h c", h=4)
            F3 = cb["F"].re("p (h c) -> p h c", h=4)
            P.tt(E3, K["id4"].re("p (h c) -> p h c", h=4), gcn, ALU.mult)
            P.tt(F3, K["neg4"].re("p (h c) -> p h c", h=4), ngcn, ALU.add)
            P.mm(psG[0:64, 0:256], ones[0:64, 0:64], cb["E"], start=True, stop=False)
            P.mm(psG[0:64, 0:256], ident[0:64, 0:64], cb["F"], start=False, stop=True)
            P.act(cb["G1"], psG[0:64, 0:256], AF.Exp)
            P.ts(cb["E"], cb["E"], -1.0, ALU.mult)
            P.tt(F3, K["neg4T"].re("p (h c) -> p h c", h=4), gcn, ALU.add)
            P.mm(psH[0:64, 0:256], ones[0:64, 0:64], cb["E"], start=True, stop=False)
            P.mm(psH[0:64, 0:256], ident[0:64, 0:64], cb["F"], start=False, stop=True)
            P.act(cb["G2"], psH[0:64, 0:256], AF.Exp)
            P.tt(cb["Gs"], cb["G1"], K["ms4"], ALU.mult, eng="pool")
            P.tt(cb["G2"], cb["G2"], K["ms4T"], ALU.mult, eng="pool")
            for h in range(4):
                hs = slice(h * 64, (h + 1) * 64)
                P.mm(psA[0:64, hs], tb["k"][:, h, cs].sub(h), tb["kb"][:, h, cs].sub(h))
                P.mm(psB[0:64, hs], tb["kb"][:, h, cs].sub(h), tb["k"][:, h, cs].sub(h))
                P.mm(psC[0:64, hs], tb["k"][:, h, cs].sub(h), tb["q"][:, h, cs].sub(h))
            P.stt(cb["B0"], psA[0:64, 0:256], -1.0, cb["Gs"], ALU.mult, ALU.mult)
            P.stt(cb["A0"], psB[0:64, 0:256], -1.0, cb["G2"], ALU.mult, ALU.mult)
            P.tt(cb["Ain"], psC[0:64, 0:256], cb["G1"], ALU.mult)
            TTm = neumann_inv(P, C, cb["A0"], cb["B0"], cb, psA, psB, psC)
            for h in range(4):
                hs = slice(h * 64, (h + 1) * 64)
                P.mm(psD[0:64, hs], tb["k"][:, h, cs].sub(h), Sb[:, h, :])
            X3 = cb["X"].re("p (h v) -> p h v", h=4)
            c2n = V(tb["c2"].ap[:, n, :].unsqueeze(2).to_broadcast([64, 4, 64]), tb["c2"].key)
            P.tt(X3, psD[0:64, 0:256].re("p (h v) -> p h v", h=4), c2n, ALU.mult)
            P.tt(X3, X3, tb["bv"][:, n, :, :], ALU.add)
            for h in range(4):
                hs = slice(h * 64, (h + 1) * 64)
                P.mm(psE[0:64, hs], TTm[:, hs], cb["X"][:, hs])
            P.copy(cb["vn"], psE[0:64, 0:256], eng="act")
            for h in range(4):
                hs = slice(h * 64, (h + 1) * 64)
                P.mm(psF[0:64, hs], Sb[:, h, :], tb["qd"][:, h, cs].sub(h), start=True, stop=False)
                P.mm(psF[0:64, hs], cb["vn"][:, hs], cb["Ain"][:, hs], start=False, stop=True)
            P.copy(o_t[:, :, cs], psF[0:64, 0:256].re("p (h c) -> p h c", h=4), eng="act")
            for h in range(4):
                hs = slice(h * 64, (h + 1) * 64)
                P.mm(psG[0:64, hs], tb["ktm"][:, n, h, :], cb["vn"][:, hs])
            dln = V(tb["dl"].ap[:, :, n].unsqueeze(2).to_broadcast([64, 4, 64]), tb["dl"].key)
            P.tt(St, St, dln, ALU.mult)
            P.tt(St, St, psG[0:64, 0:256].re("p (h v) -> p h v", h=4), ALU.add)
            P.copy(Sb, St, eng="pool")
        for h in range(4):
            P.dma(gate, zT[G_GG + h, 0:64, tsl])
            P.act(gate, gate, AF.Silu)
            P.act(sq, o_t[:, h, :], AF.Square)
            P.mm(psH[0:64, 0:TT], ones[0:64, 0:64], sq)
            rstd_from_ps(P, rs, psH[0:64, 0:TT], 64.0, 1e-6)
            P.stt(rs, rs, cv[0:64, CV["gng"]:CV["gng"] + 1], gate, ALU.mult, ALU.mult)
            P.tt(sq, o_t[:, h, :], rs, ALU.mult)
            P.dma(oT[h * 64:(h + 1) * 64, tsl].sub(h), sq, q="pool")
    if release:
        P.release(mk)


def phase_rwkv(P, C, S, L, zT, oT, w_up_d, a_up_d, g_up_d, vfT, uT, v_down_d, v_up_d, ttl=512, psbase=None, release=True):
    mk = P.mark()
    TT = ttl
    cv = C.cv
    K = C.k
    nt = S // TT
    NCH = TT // 64
    ones = K["ones"]
    ident = K["ident"]
    col = lambda nm, h: cv[0:64, CV[nm] + h:CV[nm] + h + 1]
    w_up = P.tile([64, 256], name="r_wup"); P.dma(w_up, w_up_d)
    a_up = P.tile([64, 256], name="r_aup"); P.dma(a_up, a_up_d)
    g_up = P.tile([128, 256], name="r_gup"); P.dma(g_up, g_up_d)
    if L > 0:
        v_dn = P.tile([128, 8, 32], name="r_vdn"); P.dma(v_dn, v_down_d.re("(c p) n -> p c n", p=128))
        v_upt = P.tile([32, 256], name="r_vup"); P.dma(v_upt, v_up_d)
    oma = P.tile([64, 4], name="r_oma")
    P.ts(oma, cv[0:64, CV["ka"]:CV["ka"] + 4], -1.0, ALU.mult, 1.0, ALU.add)
    ST = P.tile([64, 4, 64], name="rST")
    P.memset(ST, 0.0)
    STb = P.tile([64, 4, 64], name="rSTb", dtype=BF16)
    P.copy(STb, ST, eng="pool")
    T4 = lambda nm: P.tile([64, 4, TT], name=nm)
    T4b = lambda nm: P.tile([64, 4, TT], name=nm, dtype=BF16)
    at, bt, kt, rt = T4b("r_at"), T4b("r_bt"), T4b("r_kt"), T4b("r_rt")
    bon, gate4 = T4("r_bon"), T4("r_gate")
    t0, t1, t2, t3, t4_, t5 = [T4(f"r_t{i}") for i in range(6)]
    y4 = t0
    bh_tm = P.tile([64, NCH, 4, 64], name="r_bhtm", dtype=BF16)
    kh_tm = P.tile([64, NCH, 4, 64], name="r_khtm", dtype=BF16)
    v_tm = P.tile([64, NCH, 4, 64], name="r_vtm", dtype=BF16)
    WC = P.tile([64, 4, NCH], name="r_WC")
    xin = [P.tile([128, TT + 1], name=f"r_xin{i}") for i in range(2)]
    dd = P.tile([128, TT], name="r_dd")
    lo_w = P.tile([64, TT], name="r_low")
    lo_a = P.tile([64, TT], name="r_loa")
    lo_g = P.tile([128, TT], name="r_log")
    sq = P.tile([64, TT], name="r_sq")
    rs = P.tile([64, TT], name="r_rs")
    if L > 0:
        uxb = [P.tile([128, TT + 1], name=f"r_ux{i}") for i in range(2)]
        xvb = [P.tile([128, TT], name=f"r_xv{i}") for i in range(2)]
        vl = P.tile([32, TT], name="r_vl")
        vf = rs

    def mkch(i):
        d_ = {nm: P.tile([64, 256], name=f"rc_{nm}{i}", dtype=BF16) for nm in ["A0", "B0", "A1", "B1", "TT", "Bak", "Brb", "Brk"]}
        d_["X"] = d_["A1"]
        d_["U"] = d_["B1"]
        return d_
    CB = [mkch(0), mkch(1)]
    if psbase is None:
        psA, psB, psC, psD, psE, psF, psG, psH = [P.pbank(i) for i in range(8)]
    else:
        psA, psB, psC, psD, psE, psF, psG, psH = [P.pbank(psbase + j % 4) for j in range(8)]
    xk = 0

    def shifted(g, rows, ti, dst):
        nonlocal xk
        x = xin[xk % 2]
        xk += 1
        if ti == 0:
            P.memset(x[0:rows, 0:1], 0.0, writes=[x.sub("halo")])
            P.dma(x[0:rows, 1:TT + 1].sub("body"), zT[g, 0:rows, 0:TT])
        else:
            P.dma(x[0:rows, :], zT[g, 0:rows, ti * TT - 1:(ti + 1) * TT])
        P.tt(dd[0:rows, :], x[0:rows, 0:TT], x[0:rows, 1:TT + 1], ALU.subtract)
        P.stt(dst, dd[0:rows, :], cv[0:rows, CV["mu"] + g:CV["mu"] + g + 1], x[0:rows, 1:TT + 1], ALU.mult, ALU.add)

    for ti in range(nt):
        tsl = slice(ti * TT, (ti + 1) * TT)
        r4, k4, v4, kk4, ic4, lw4 = t0, t1, t2, t3, t4_, t5
        shifted(G_WLO, 64, ti, lo_w)
        P.act(lo_w, lo_w, AF.Tanh)
        shifted(G_ALO, 64, ti, lo_a)
        shifted(G_GLO, 128, ti, lo_g)
        P.act(lo_g, lo_g, AF.Sigmoid)
        if L > 0:
            uv = uT.re("(c p) t -> p c t", p=128)
            for c in range(8):
                ux = uxb[c % 2]
                xv = xvb[c % 2]
                if ti == 0:
                    P.memset(ux[:, 0:1], 0.0, writes=[ux.sub("halo")])
                    P.dma(ux[:, 1:TT + 1].sub("body"), uv[:, c, 0:TT])
                else:
                    P.dma(ux, uv[:, c, ti * TT - 1:(ti + 1) * TT])
                P.tt(xv, ux[:, 0:TT], ux[:, 1:TT + 1], ALU.subtract)
                P.stt(xv, xv, cv[:, CV["vmu"] + c:CV["vmu"] + c + 1], ux[:, 1:TT + 1], ALU.mult, ALU.add)
                P.mm(psH[0:32, 0:TT], v_dn[:, c, :], xv, start=(c == 0), stop=(c == 7))
            P.copy(vl, psH[0:32, 0:TT], eng="act")
        for h in range(4):
            hs = slice(h * 64, (h + 1) * 64)
            shifted(G_R + h, 64, ti, r4[:, h, :].sub(h))
            shifted(G_K + h, 64, ti, k4[:, h, :].sub(h))
            shifted(G_V + h, 64, ti, v4[:, h, :].sub(h))
            P.mm(psA[0:64, 0:TT], w_up[:, hs], lo_w)
            P.act(lw4[:, h, :].sub(h), psA[0:64, 0:TT], AF.Sigmoid, bias=col("w0", h))
            P.mm(psB[0:64, 0:TT], a_up[:, hs], lo_a)
            P.act(ic4[:, h, :].sub(h), psB[0:64, 0:TT], AF.Sigmoid, bias=col("a0", h))
            P.mm(psC[0:64, 0:TT], g_up[:, hs], lo_g)
            P.copy(gate4[:, h, :].sub(h), psC[0:64, 0:TT], eng="act")
            if L == 0:
                P.dma(vfT[h * 64:(h + 1) * 64, tsl].sub(h), v4[:, h, :].sub(h), q="pool")
            else:
                P.dma(vf, vfT[h * 64:(h + 1) * 64, tsl])
                P.mm(psD[0:64, 0:TT], v_upt[:, hs], vl)
                P.act(sq, psD[0:64, 0:TT], AF.Sigmoid, bias=col("vb", h))
                P.tt(vf, vf, v4[:, h, :].sub(h), ALU.subtract)
                P.tt(vf, vf, sq, ALU.mult)
                P.tt(v4[:, h, :].sub(h), v4[:, h, :].sub(h), vf, ALU.add)
            P.ts(kk4[:, h, :].sub(h), k4[:, h, :].sub(h), col("kk", h), ALU.mult)
            P.act(sq, kk4[:, h, :].sub(h), AF.Square)
            P.mm(psE[0:64, 0:TT], ones[0:64, 0:64], sq)
            rstd_from_ps(P, rs, psE[0:64, 0:TT], 1.0, 1e-12)
            P.tt(kk4[:, h, :].sub(h), kk4[:, h, :].sub(h), rs, ALU.mult)
            P.ts(sq, ic4[:, h, :].sub(h), col("ka", h), ALU.mult, oma[:, h:h + 1], ALU.add)
            P.tt(k4[:, h, :].sub(h), k4[:, h, :].sub(h), sq, ALU.mult)
            P.stt(sq, r4[:, h, :].sub(h), col("rk", h), k4[:, h, :].sub(h), ALU.mult, ALU.mult)
            P.mm(psF[0:64, 0:TT], ones[0:64, 0:64], sq)
            P.tt(bon[:, h, :].sub(h), psF[0:64, 0:TT], v4[:, h, :].sub(h), ALU.mult)
        P.ts(lw4, lw4, float(-np.exp(-0.5)), ALU.mult)
        for n in range(NCH):
            cs = slice(n * 64, (n + 1) * 64)
            for h in range(4):
                P.transpose(psG[0:64, h * 64:(h + 1) * 64], v4[:, h, cs], ident[0:64, 0:64])
            P.copy(v_tm[:, n, :, :].re("p h d -> p (h d)"), psG[0:64, 0:256], eng="act")
        P.tt(ic4, ic4, kk4, ALU.mult)
        cb_ = [lw4, v4]
        cur = 0
        for sh in (1, 2, 4, 8, 16, 32):
            src = cb_[cur].re("p h (n c) -> p (h n) c", c=64)
            dstt = cb_[1 - cur].re("p h (n c) -> p (h n) c", c=64)
            P.copy(dstt[:, :, 0:sh], src[:, :, 0:sh], eng="pool", writes=[cb_[1 - cur].sub("a")])
            P.tt(dstt[:, :, sh:64], src[:, :, sh:64], src[:, :, 0:64 - sh], ALU.add, writes=[cb_[1 - cur].sub("b")])
            cur = 1 - cur
        assert cur == 0
        cl = lw4
        cl3 = cl.re("p h (n c) -> p (h n) c", c=64)
        e = v4
        e3 = e.re("p h (n c) -> p (h n) c", c=64)
        P.act(e, cl, AF.Exp)
        P.tt(rt, r4, e, ALU.mult)
        P.memset(at.re("p h (n c) -> p (h n) c", c=64)[:, :, 0:1], 1.0, writes=[at.sub("a")])
        P.copy(at.re("p h (n c) -> p (h n) c", c=64)[:, :, 1:64], e3[:, :, 0:63], eng="pool", writes=[at.sub("b")])
        P.stt(at, at, -1.0, kk4, ALU.mult, ALU.mult)
        P.act(e, cl, AF.Exp, scale=-1.0)
        P.tt(bt, ic4, e, ALU.mult)
        P.tt(kt, k4, e, ALU.mult)
        cl4 = cl.re("p h (n c) -> p h n c", c=64)
        P.copy(WC, cl4[:, :, :, 63])
        P.tt(e.re("p h (n c) -> p h n c", c=64), V(WC.ap.unsqueeze(3).to_broadcast([64, 4, NCH, 64]), WC.key), cl4, ALU.subtract)
        P.act(e, e, AF.Exp)
        P.act(WC, WC, AF.Exp)
        P.tt(ic4, ic4, e, ALU.mult)
        P.tt(k4, k4, e, ALU.mult)
        for n in range(NCH):
            cs = slice(n * 64, (n + 1) * 64)
            for h in range(4):
                P.transpose(psG[0:64, h * 64:(h + 1) * 64], ic4[:, h, cs], ident[0:64, 0:64])
            P.copy(bh_tm[:, n, :, :].re("p h d -> p (h d)"), psG[0:64, 0:256], eng="act")
            for h in range(4):
                P.transpose(psH[0:64, h * 64:(h + 1) * 64], k4[:, h, cs], ident[0:64, 0:64])
            P.copy(kh_tm[:, n, :, :].re("p h d -> p (h d)"), psH[0:64, 0:256], eng="dve")
        for n in range(NCH):
            cb = CB[n % 2]
            cs = slice(n * 64, (n + 1) * 64)
            for h in range(4):
                hs = slice(h * 64, (h + 1) * 64)
                P.mm(psA[0:64, hs], bt[:, h, cs], at[:, h, cs])
                P.mm(psB[0:64, hs], at[:, h, cs], bt[:, h, cs])
                P.mm(psC[0:64, hs], kt[:, h, cs], at[:, h, cs])
                P.mm(psD[0:64, hs], bt[:, h, cs], rt[:, h, cs])
            P.tt(cb["B0"], psA[0:64, 0:256], K["ms4"], ALU.mult)
            P.tt(cb["A0"], psB[0:64, 0:256], K["ms4T"], ALU.mult)
            P.tt(cb["Bak"], psC[0:64, 0:256], K["ms4"], ALU.mult)
            P.tt(cb["Brb"], psD[0:64, 0:256], K["mi4"], ALU.mult)
            for h in range(4):
                hs = slice(h * 64, (h + 1) * 64)
                P.mm(psE[0:64, hs], kt[:, h, cs], rt[:, h, cs])
            P.tt(cb["Brk"], psE[0:64, 0:256], K["mi4"], ALU.mult)
            TTm = neumann_inv(P, C, cb["A0"], cb["B0"], cb, psA, psB, psC)
            for h in range(4):
                hs = slice(h * 64, (h + 1) * 64)
                P.mm(psD[0:64, hs], at[:, h, cs], STb[:, h, :], start=True, stop=False)
                P.mm(psD[0:64, hs], cb["Bak"][:, hs], v_tm[:, n, h, :], start=False, stop=True)
            P.copy(cb["X"], psD[0:64, 0:256], eng="act")
            for h in range(4):
                hs = slice(h * 64, (h + 1) * 64)
                P.mm(psE[0:64, hs], TTm[:, hs], cb["X"][:, hs])
            P.copy(cb["U"], psE[0:64, 0:256], eng="act")
            for h in range(4):
                hs = slice(h * 64, (h + 1) * 64)
                P.mm(psF[0:64, hs], STb[:, h, :], rt[:, h, cs], start=True, stop=False)
                P.mm(psF[0:64, hs], cb["U"][:, hs], cb["Brb"][:, hs], start=False, stop=False)
                P.mm(psF[0:64, hs], v_tm[:, n, h, :], cb["Brk"][:, hs], start=False, stop=True)
            P.copy(y4[:, :, cs], psF[0:64, 0:256].re("p (h c) -> p h c", h=4), eng="act")
            for h in range(4):
                hs = slice(h * 64, (h + 1) * 64)
                P.mm(psG[0:64, hs], bh_tm[:, n, h, :], cb["U"][:, hs], start=True, stop=False)
                P.mm(psG[0:64, hs], kh_tm[:, n, h, :], v_tm[:, n, h, :], start=False, stop=True)
            wcn = V(WC.ap[:, :, n].unsqueeze(2).to_broadcast([64, 4, 64]), WC.key)
            P.tt(ST, ST, wcn, ALU.mult)
            P.tt(ST, ST, psG[0:64, 0:256].re("p (h v) -> p h v", h=4), ALU.add)
            P.copy(STb, ST, eng="pool")
        for h in range(4):
            yh = y4[:, h, :]
            P.mm(psH[0:64, 0:TT], ones[0:64, 0:64], yh)
            P.stt(yh, psH[0:64, 0:TT], float(-1.0 / 64), yh, ALU.mult, ALU.add)
            P.act(sq, yh, AF.Square)
            P.mm(psH[0:64, 0:TT], ones[0:64, 0:64], sq)
            rstd_from_ps(P, rs, psH[0:64, 0:TT], 64.0, 64e-5)
            P.stt(yh, yh, col("lng", h), rs, ALU.mult, ALU.mult)
            P.stt(yh, yh, col("lnb", h), bon[:, h, :].sub(h), ALU.add, ALU.add)
            P.tt(sq, yh, gate4[:, h, :].sub(h), ALU.mult)
            P.dma(oT[h * 64:(h + 1) * 64, tsl].sub(h), sq, q="pool")
    if release:
        P.release(mk)


def phase_merge(P, C, NT, hT, oT, gT, wbr_d, wout_d, h1T, dyn=None):
    mk = P.mark()
    nt = NT // TT
    wstg = [P.tile([128, 4, 1024], name=f"f_wstg{i}") for i in range(2)]
    wbr = []
    for br in range(3):
        t = P.tile([128, 4, 1024], name=f"wbr{br}", dtype=BF16)
        P.dma(wstg[br % 2], wbr_d[br].re("(c p) n -> p c n", p=128))
        P.copy(t, wstg[br % 2], eng=("pool" if br % 2 == 0 else "act"))
        wbr.append(t)
    wout = P.tile([128, 8, 1024], name="wout", dtype=BF16)
    wov = wout_d.re("(c p) n -> p c n", p=128)
    for hh in range(2):
        P.dma(wstg[(hh + 1) % 2], wov[:, hh * 4:(hh + 1) * 4, :])
        P.copy(wout[:, hh * 4:(hh + 1) * 4, :].sub(hh), wstg[(hh + 1) % 2], eng=("act" if hh == 0 else "pool"))
    h = P.tile([128, 8, TT], name="f_h")
    o = P.tile([128, 12, TT], name="f_o")
    ob_ = P.tile([128, 12, TT], name="f_ob", dtype=BF16)
    mg = P.tile([128, 8, TT], name="f_mg32") if dyn is not None else None
    mgb = P.tile([128, 8, TT], name="f_mg", dtype=BF16)
    o2 = P.tile([128, 6, TT], name="f_o2") if dyn is not None else None
    g3 = [P.tile([128, 3, TT], name=f"f_g3{i}") for i in range(2)]
    tmp = [P.tile([128, TT], name=f"f_tmp{i}") for i in range(2)]
    tmp2 = [P.tile([128, TT], name=f"f_tmpb{i}") for i in range(2)]
    ps = [P.pbank(i) for i in range(8)]
    hv = hT.re("(c p) t -> p c t", p=128)
    ov = oT.re("(c p) t -> p c t", p=128)
    gv = gT.re("(b c p) t -> p b c t", b=3, p=128)
    h1v = h1T.re("(c p) t -> p c t", p=128)
    for ti in range(nt):
        tsl = slice(ti * TT, (ti + 1) * TT)
        if dyn is not None:
            m0 = C.cv[:, CV["m0"]:CV["m0"] + 1]
            m1 = C.cv[:, CV["m1"]:CV["m1"] + 1]
            tsl2 = slice(dyn + ti * TT, dyn + (ti + 1) * TT)
            P.dma(h, hv[:, :, tsl])
            P.dma(mg, hv[:, :, tsl2])
            P.ts(h, h, m0, ALU.mult)
            P.stt(h, mg, m1, h, ALU.mult, ALU.add)
            for part in range(2):
                cs_ = slice(part * 6, (part + 1) * 6)
                P.dma(o[:, cs_, :].sub(part), ov[:, cs_, tsl])
                P.dma(o2, ov[:, cs_, tsl2])
                P.ts(o[:, cs_, :].sub(part), o[:, cs_, :].sub(part), m0, ALU.mult)
                P.stt(o[:, cs_, :].sub(part), o2, m1, o[:, cs_, :].sub(part), ALU.mult, ALU.add)
        else:
            P.dma(h, hv[:, :, tsl])
            P.dma(o, ov[:, :, tsl])
        P.copy(ob_[:, 0:6, :].sub(0), o[:, 0:6, :], eng="dve")
        P.copy(ob_[:, 6:12, :].sub(1), o[:, 6:12, :], eng="act")
        for n in range(8):
            ns = slice(n * 128, (n + 1) * 128)
            g = g3[n % 2]
            P.dma(g, gv[:, :, n, tsl])
            for br in range(3):
                pp = ps[(n % 2) * 3 + br]
                for k in range(4):
                    P.mm(pp, wbr[br][:, k, ns], ob_[:, br * 4 + k, :], start=(k == 0), stop=(k == 3))
            tm_ = tmp[n % 2]
            P.tt(tm_, ps[(n % 2) * 3 + 0], g[:, 0, :], ALU.mult)
            P.tt(tmp2[n % 2], ps[(n % 2) * 3 + 1], g[:, 1, :], ALU.mult)
            P.tt(tm_, tm_, tmp2[n % 2], ALU.add, eng="pool")
            P.tt(tmp2[n % 2], ps[(n % 2) * 3 + 2], g[:, 2, :], ALU.mult)
            P.tt(mgb[:, n, :].sub(n), tm_, tmp2[n % 2], ALU.add, eng="pool")
        for n in range(8):
            ns = slice(n * 128, (n + 1) * 128)
            pp = ps[6 + n % 2]
            for k in range(8):
                P.mm(pp, wout[:, k, ns], mgb[:, k, :], start=(k == 0), stop=(k == 7))
            P.tt(h[:, n, :].sub(n), h[:, n, :].sub(n), pp, ALU.add)
        P.dma(h1v[:, :, tsl], h, q="pool")
    P.release(mk)


def phase_ffn(P, C, NT, h1T, h2T, gcol, experts, FF, router_d=None):
    mk = P.mark()
    cv = C.cv
    K = C.k
    ones = K["ones"]
    ident = K["ident"]
    nt = NT // TT
    NF = FF // 128
    CB = 512
    blocks = [(c0, min(CB, FF - c0)) for c0 in range(0, FF, CB)]
    h = P.tile([128, 8, TT], name="m_h")
    u = P.tile([128, 8, TT], name="m_u", dtype=BF16)
    rs = P.tile([128, TT], name="m_rs")
    hid_raw = P.tile([128, NF * TT // 2], name="m_hid")
    hid = V(hid_raw.ap.bitcast(BF16).rearrange("p (f t) -> p f t", f=NF), hid_raw.key)
    u32 = V(hid_raw.ap[:, 0:8 * TT].rearrange("p (c t) -> p c t", c=8), hid_raw.key)
    sg = [P.tile([128, TT], name=f"m_sg{i}") for i in range(2)]
    wgb = [P.tile([128, 8, CB], name=f"m_wg{i}") for i in range(2)]
    wub = [P.tile([128, 8, CB], name=f"m_wu{i}") for i in range(2)]
    wgc = [P.tile([128, 8, CB], name=f"m_wgc{i}", dtype=BF16) for i in range(2)]
    wuc = [P.tile([128, 8, CB], name=f"m_wuc{i}", dtype=BF16) for i in range(2)]
    wdb = [P.tile([128, 512], name=f"m_wd{i}") for i in range(3)]
    wdc = [P.tile([128, 512], name=f"m_wdc{i}", dtype=BF16) for i in range(3)]
    ps = [P.pbank(i) for i in range(8)]
    hv = h1T.re("(c p) t -> p c t", p=128)
    h2v = h2T.re("(c p) t -> p c t", p=128)
    ne = len(experts)
    if router_d is not None:
        sel8 = P.tile([8, 1024], name="c_sel8")
        P.dma(sel8, C.sel8_d)
        rt_w = P.tile([128, 8, 8], name="m_rw")
        P.dma(rt_w, router_d.re("(c p) e -> p c e", p=128))
        lg = P.tile([8, TT], name="m_lg")
        ltm = P.tile([128, 4, 8], name="m_ltm")
        l2 = P.tile([128, 4, 8], name="m_l2")
        eq1 = P.tile([128, 4, 8], name="m_eq1")
        eq2 = P.tile([128, 4, 8], name="m_eq2")
        m1 = P.tile([128, 4], name="m_m1")
        m2 = P.tile([128, 4], name="m_m2")
        w1 = P.tile([128, 4], name="m_w1")
        w2 = P.tile([128, 4], name="m_w2")
        gwf = P.tile([8, TT], name="m_gwf")
        gwe = [P.tile([128, TT], name=f"m_gwe{i}") for i in range(2)]
    wk = 0
    dk = 0
    for ti in range(nt):
        tsl = slice(ti * TT, (ti + 1) * TT)
        P.dma(h, hv[:, :, tsl])
        P.act(u32, h, AF.Square)
        for c in range(8):
            P.mm(ps[7], ones, u32[:, c, :], start=(c == 0), stop=(c == 7))
        rstd_from_ps(P, rs, ps[7], D, 1e-6)
        if router_d is not None:
            for c in range(8):
                P.stt(u32[:, c, :], h[:, c, :], cv[:, gcol + c:gcol + c + 1], rs, ALU.mult, ALU.mult)
            P.copy(u, u32, eng="act")
            for c in range(8):
                P.mm(ps[6][0:8, :], rt_w[:, c, :], u32[:, c, :], start=(c == 0), stop=(c == 7))
        else:
            for c in range(8):
                P.stt(u[:, c, :], h[:, c, :], cv[:, gcol + c:gcol + c + 1], rs, ALU.mult, ALU.mult)
        if router_d is not None:
            P.copy(lg, ps[6][0:8, :], eng="act")
            for j in range(4):
                P.transpose(ps[5][:, j * 8:(j + 1) * 8], lg[:, j * 128:(j + 1) * 128], ident[0:8, 0:8])
            P.copy(ltm.re("p j e -> p (j e)"), ps[5][:, 0:32])
            bc = lambda t: V(t.ap.unsqueeze(2).to_broadcast([128, 4, 8]), t.key)
            P.op("dve", lambda e: e.reduce_max(_ap(m1), _ap(ltm), AX.X), [ltm], [m1])
            P.tt(eq1, ltm, bc(m1), ALU.is_equal)
            P.stt(l2, eq1, -1e30, ltm, ALU.mult, ALU.add)
            P.op("dve", lambda e: e.reduce_max(_ap(m2), _ap(l2), AX.X), [l2], [m2])
            P.tt(eq2, l2, bc(m2), ALU.is_equal)
            P.tt(w2, m2, m1, ALU.subtract)
            P.act(w2, w2, AF.Exp)
            P.ts(w1, w2, 1.0, ALU.add)
            P.recip(w1, w1)
            P.tt(w2, w2, w1, ALU.mult)
            P.tt(eq1, eq1, bc(w1), ALU.mult)
            P.tt(eq2, eq2, bc(w2), ALU.mult)
            P.tt(eq1, eq1, eq2, ALU.add)
            for j in range(4):
                P.transpose(ps[5][0:8, j * 128:(j + 1) * 128], eq1[:, j, :], ident)
            P.copy(gwf, ps[5][0:8, :], eng="act")
        work = [(e_, bi) for e_ in range(ne) for bi in range(len(blocks))]

        def prefetch(e_, bi, slot):
            wg_d, wu_d, _ = experts[e_]
            c0_, wdt = blocks[bi]
            wgv = wg_d.re("(c p) f -> p c f", p=128)
            wuv = wu_d.re("(c p) f -> p c f", p=128)
            P.dma(wgb[slot][:, :, 0:wdt], wgv[:, :, c0_:c0_ + wdt])
            P.dma(wub[slot][:, :, 0:wdt], wuv[:, :, c0_:c0_ + wdt], q="act")
            P.copy(wgc[slot][:, :, 0:wdt], wgb[slot][:, :, 0:wdt], eng="dve")
            P.copy(wuc[slot][:, :, 0:wdt], wub[slot][:, :, 0:wdt], eng="act")
        prefetch(work[0][0], work[0][1], wk % 2)
        for wi, (e_, bi) in enumerate(work):
            slot = wk % 2
            wk += 1
            if wi + 1 < len(work):
                prefetch(work[wi + 1][0], work[wi + 1][1], wk % 2)
            c0_, wdt = blocks[bi]
            wg_t, wu_t = wgc[slot], wuc[slot]
            if router_d is not None and bi == 0:
                gw_e = gwe[e_ % 2]
                P.mm(ps[3], sel8[:, e_ * 128:(e_ + 1) * 128], gwf)
                P.copy(gw_e, ps[3], eng="act")
            for j in range(wdt // 128):
                f = c0_ // 128 + j
                pg = ps[4 + f % 2]
                pu = ps[6 + f % 2]
                for c in range(8):
                    P.mm(pg, wg_t[:, c, j * 128:(j + 1) * 128], u[:, c, :], start=(c == 0), stop=(c == 7))
                for c in range(8):
                    P.mm(pu, wu_t[:, c, j * 128:(j + 1) * 128], u[:, c, :], start=(c == 0), stop=(c == 7))
                s_ = sg[f % 2]
                P.act(s_, pg, AF.Silu)
                if router_d is not None:
                    P.tt(s_, s_, gw_e, ALU.mult, eng="pool")
                P.tt(hid[:, f, :].sub(f), s_, pu, ALU.mult)
            if bi == len(blocks) - 1:
                wdv = experts[e_][2].re("(f p) n -> p f n", p=128)
                for half in range(2):
                    for f in range(NF):
                        wd_s = wdb[dk % 3]
                        wd_t = wdc[dk % 3]
                        dk += 1
                        P.dma(wd_s, wdv[:, f, half * 512:(half + 1) * 512])
                        P.copy(wd_t, wd_s, eng=("dve" if dk % 2 == 0 else "act"))
                        for n4 in range(4):
                            P.mm(ps[n4], wd_t[:, n4 * 128:(n4 + 1) * 128], hid[:, f, :].sub(f), start=(f == 0), stop=(f == NF - 1))
                    for n4 in range(4):
                        n = half * 4 + n4
                        P.tt(h[:, n, :].sub(n), h[:, n, :].sub(n), ps[n4], ALU.add)
        P.dma(h2v[:, :, tsl], h, q="pool")
    P.release(mk)


def phase_ple(P, C, NT, h2T, pT, proj_d, pgate_d, gcol, h3T):
    mk = P.mark()
    cv = C.cv
    ones = C.k["ones"]
    nt = NT // TT
    pstg = [P.tile([128, 4, 1024], name=f"p_stg{i}") for i in range(2)]
    proj = P.tile([128, 2, 1024], name="p_proj", dtype=BF16)
    P.dma(pstg[0][:, 0:2, :], proj_d.re("(c p) n -> p c n", p=128))
    P.copy(proj, pstg[0][:, 0:2, :], eng="pool")
    pg = P.tile([128, 8, 1024], name="p_gate", dtype=BF16)
    pgv = pgate_d.re("(c p) n -> p c n", p=128)
    for hh in range(2):
        P.dma(pstg[(hh + 1) % 2], pgv[:, hh * 4:(hh + 1) * 4, :])
        P.copy(pg[:, hh * 4:(hh + 1) * 4, :].sub(hh), pstg[(hh + 1) % 2], eng=("act" if hh == 0 else "pool"))
    h = P.tile([128, 8, TT], name="p_h")
    hb_ = P.tile([128, 8, TT], name="p_hb", dtype=BF16)
    pt = P.tile([128, 2, TT], name="p_p")
    ptb = P.tile([128, 2, TT], name="p_pb", dtype=BF16)
    er = P.tile([128, 8, TT], name="p_er")
    ho = P.tile([128, 8, TT], name="p_ho")
    sq = P.tile([128, TT], name="p_sq")
    rs = P.tile([128, TT], name="p_rs")
    gp = [P.tile([128, TT], name=f"p_gp{i}") for i in range(2)]
    ps = [P.pbank(i) for i in range(8)]
    hv = h2T.re("(c p) t -> p c t", p=128)
    pv = pT.re("(c p) t -> p c t", p=128)
    h3v = h3T.re("(c p) t -> p c t", p=128)
    for ti in range(nt):
        tsl = slice(ti * TT, (ti + 1) * TT)
        P.dma(h, hv[:, :, tsl])
        P.dma(pt, pv[:, :, tsl])
        P.copy(ptb, pt, eng="dve")
        P.copy(hb_, h, eng="act")
        for n in range(8):
            ns = slice(n * 128, (n + 1) * 128)
            pp = ps[n % 2]
            P.mm(pp, proj[:, 0, ns], ptb[:, 0, :], start=True, stop=False)
            P.mm(pp, proj[:, 1, ns], ptb[:, 1, :], start=False, stop=True)
            P.copy(er[:, n, :].sub(n), pp, eng="act")
            P.act(sq, pp, AF.Square)
            P.mm(ps[2], ones, sq, start=(n == 0), stop=(n == 7))
        rstd_from_ps(P, rs, ps[2], D, 1e-6)
        for n in range(8):
            ns = slice(n * 128, (n + 1) * 128)
            pp = ps[3 + n % 2]
            for k in range(8):
                P.mm(pp, pg[:, k, ns], hb_[:, k, :], start=(k == 0), stop=(k == 7))
            g = gp[n % 2]
            P.act(g, pp, AF.Sigmoid)
            P.stt(er[:, n, :].sub(n), er[:, n, :].sub(n), cv[:, gcol + n:gcol + n + 1], rs, ALU.mult, ALU.mult)
            P.tt(g, g, er[:, n, :].sub(n), ALU.mult)
            P.tt(ho[:, n, :].sub(n), h[:, n, :], g, ALU.add)
        P.dma(h3v[:, :, tsl], ho, q="pool")
    P.release(mk)


def own(hg, width=64):
    return slice(hg * 4 * width, (hg + 1) * 4 * width)


def col4(v):
    return np.ascontiguousarray(v.reshape(4, 64).T)


def col2(v):
    return np.ascontiguousarray(v.reshape(2, 128).T)


def mixer_host_inputs(inp, L, b, hg):
    f = np.float32
    w_in = inp["w_in"][L]
    o = own(hg)
    cols = np.concatenate([
        np.arange(0, 512)[o], np.arange(512, 1024)[o], np.arange(1024, 1536)[o],
        np.arange(1536, 1600), np.arange(1600, 1664), np.arange(1664, 1792),
        np.arange(1792, 2048), np.arange(2048, 2176), np.arange(2176, 2208),
        np.arange(2208, 2720)[o], np.arange(2720, 3232)[o], np.arange(3232, 3744)[o],
        np.arange(3760, 4272)[o],
        np.arange(3744, 3752)[hg * 4:(hg + 1) * 4], np.arange(3752, 3760)[hg * 4:(hg + 1) * 4]])
    assert len(cols) == NZ
    d = {}
    d["w_in_m"] = np.ascontiguousarray(w_in[:, cols])
    cv = np.zeros((128, NCV), f)

    def put(nm, arr):
        arr = np.asarray(arr, f)
        cv[:arr.shape[0], CV[nm]:CV[nm] + arr.shape[1]] = arr
    put("nmg", inp["norm_mix_g"][L].reshape(8, 128).T)
    mu = inp["rwkv_mu"][L]
    mucols = np.zeros((128, 15), f)
    rcols = cols[:1024]
    for g in range(15):
        seg = mu[rcols[ZOFF[g]:ZOFF[g + 1]]]
        mucols[:len(seg), g] = seg
    put("mu", mucols)
    put("w0", col4(inp["rwkv_w0"][L][o]))
    put("a0", col4(inp["rwkv_a0"][L][o]))
    put("kk", col4(inp["rwkv_k_k"][L][o]))
    put("ka", col4(inp["rwkv_k_a"][L][o]))
    put("rk", col4(inp["rwkv_r_k"][L].reshape(512)[o]))
    put("lng", col4(inp["rwkv_ln_g"][L][o]))
    put("lnb", col4(inp["rwkv_ln_b"][L][o]))
    if L > 0:
        put("vmu", inp["vres_mu"][L - 1].reshape(8, 128).T)
        put("vb", col4(inp["vres_b"][L - 1][o]))
    put("qng", inp["mla_q_norm_g"][L].reshape(2, 128).T)
    put("kvng", inp["mla_kv_norm_g"][L].reshape(128, 1))
    put("qkq", inp["mla_qk_norm_q"][L].reshape(96, 1))
    put("qkk", inp["mla_qk_norm_k"][L].reshape(96, 1))
    invf = (1.0 / (10000.0 ** (np.arange(0, 32, 2, dtype=f) / f(32)))).astype(f)
    iv = np.zeros((96, 1), f)
    iv[64:80, 0] = invf
    iv[80:96, 0] = invf
    put("invf", iv)
    put("ropec", np.full((128, 1), -np.pi, f))
    cw = inp["gdn_conv_w"][L]
    convc = np.zeros((128, 48), f)
    for seg in range(3):
        cc = cw[:, seg * 512:(seg + 1) * 512][:, o]
        for hh in range(4):
            for j in range(4):
                convc[:64, (seg * 4 + hh) * 4 + j] = cc[j, hh * 64:(hh + 1) * 64]
    put("conv", convc)
    put("alog", inp["gdn_a_log"][L][hg * 4:(hg + 1) * 4].reshape(4, 1))
    put("dtb", inp["gdn_dt_bias"][L][hg * 4:(hg + 1) * 4].reshape(4, 1))
    put("gng", inp["gdn_norm_g"][L].reshape(64, 1))
    d["cv"] = cv
    d["w_up"] = np.ascontiguousarray(inp["rwkv_w_up"][L][:, o])
    d["a_up"] = np.ascontiguousarray(inp["rwkv_a_up"][L][:, o])
    d["g_up"] = np.ascontiguousarray(inp["rwkv_g_up"][L][:, o])
    if L > 0:
        d["v_down"] = np.ascontiguousarray(inp["vres_down"][L - 1])
        d["v_up"] = np.ascontiguousarray(inp["vres_up"][L - 1][:, o])
    d["w_uq"] = np.ascontiguousarray(inp["mla_w_uq"][L][:, hg * 384:(hg + 1) * 384])
    ukv = inp["mla_w_ukv"][L].reshape(128, 8, 128)[:, hg * 4:(hg + 1) * 4, :]
    d["w_uk"] = np.ascontiguousarray(ukv[:, :, :64].reshape(128, 256))
    d["w_uv"] = np.ascontiguousarray(ukv[:, :, 64:].reshape(128, 256))
    d["pos"] = np.ascontiguousarray(inp["positions"][b:b + 1].astype(np.int32))
    for k_, v_ in consts_np().items():
        d["c_" + k_] = v_
    return d
from concourse.bass_utils import run_bass_kernel_spmd

B_, S_, NCORE = 4, 4096, 8
M_CONSTS = ["ident", "ones", "att_mask", "ropeRT", "sel4", "id4", "neg4", "neg4T", "ms4", "ms4T", "mi4"]
F_CONSTS = ["ident", "ones"]
_PROG_CACHE = {}


def build_mixer(S, L):
    P = Prog()
    C = Ctx()
    names = []

    def din(name, shape, dt=F32):
        names.append(name)
        return P.dview(P.dram(name, shape, dt, kind="ExternalInput"))
    hT = din("hT", [1024, S])
    w_d = din("w_in_m", [1024, NZ])
    cv_d = din("cv", [128, NCV])
    pos_d = din("pos", [1, S], I32)
    w_uq, w_uk, w_uv = din("w_uq", [256, 384]), din("w_uk", [128, 256]), din("w_uv", [128, 256])
    w_up, a_up, g_up = din("w_up", [64, 256]), din("a_up", [64, 256]), din("g_up", [128, 256])
    zT = P.dview(P.dram("zT", [NG, 128, S], F32, kind="Internal"))
    oT = P.dview(P.dram("oT", [768, S], F32, kind="ExternalOutput"))
    uT = v_down = v_up = None
    if L == 0:
        vfT = P.dview(P.dram("vfT_out", [256, S], F32, kind="ExternalOutput"))
    else:
        vfT = din("vfT_in", [256, S])
        uT = P.dview(P.dram("uT", [1024, S], F32, kind="Internal"))
        v_down, v_up = din("v_down", [1024, 32]), din("v_up", [32, 256])
    C.k = load_consts(P, M_CONSTS)
    names.extend(["c_" + c for c in M_CONSTS])
    C.cv = P.tile([128, NCV], name="cv")
    P.dma(C.cv, cv_d)
    groups = [(int(ZOFF[g]), int(ZG[g])) for g in range(NG)]
    phase_proj(P, C, S, hT, w_d, NZ, groups, zT, CV["nmg"], uT=uT)
    phase_rwkv(P, C, S, L, zT, V(oT.ap[0:256, :], "oT_r"), w_up, a_up, g_up, vfT, uT, v_down, v_up)
    phase_mla(P, C, S, zT, pos_d, w_uq, w_uk, w_uv, V(oT.ap[256:512, :], "oT_m"))
    phase_gdn(P, C, S, zT, V(oT.ap[512:768, :], "oT_g"))
    P.finalize()
    return P.nc, names


def build_token(NT, L):
    P = Prog()
    C = Ctx()
    names = []

    def din(name, shape, dt=F32):
        names.append(name)
        return P.dview(P.dram(name, shape, dt, kind="ExternalInput"))
    hT = din("hT", [1024, NT])
    oT = din("oT_all", [1536, NT])
    pT = din("pT", [256, NT])
    cv_d = din("cv", [128, NCV])
    w_g = din("w_gate", [1024, 3072])
    wbr = [din(f"w_br{i}", [512, 1024]) for i in range(3)]
    wout = din("w_out", [1024, 1024])
    proj = din("ple_proj", [256, 1024])
    pgate = din("ple_gate", [1024, 1024])
    if L % 2 == 0:
        experts = [(din("ffn_wg", [1024, 2816]), din("ffn_wu", [1024, 2816]), din("ffn_wd", [2816, 1024]))]
        FF = 2816
        router = None
    else:
        wg_all = din("moe_wg", [8, 1024, 3584])
        wu_all = din("moe_wu", [8, 1024, 3584])
        wd_all = din("moe_wd", [8, 3584, 1024])
        experts = [(V(wg_all.ap[e], wg_all.key), V(wu_all.ap[e], wu_all.key), V(wd_all.ap[e], wd_all.key)) for e in range(8)]
        FF = 3584
        router = din("moe_router", [1024, 8])
    gT = P.dview(P.dram("gT", [3072, NT], F32, kind="Internal"))
    h1T = P.dview(P.dram("h1T", [1024, NT], F32, kind="Internal"))
    h2T = P.dview(P.dram("h2T", [1024, NT], F32, kind="Internal"))
    h3T = P.dview(P.dram("h3T", [1024, NT], F32, kind="ExternalOutput"))
    C.k = load_consts(P, F_CONSTS)
    names.extend(["c_" + c for c in F_CONSTS])
    C.sel8_d = din("c_sel8", [8, 1024])
    C.cv = P.tile([128, NCV], name="cv")
    P.dma(C.cv, cv_d)
    groups = [(g * 128, 128) for g in range(24)]
    phase_proj(P, C, NT, hT, w_g, 3072, groups, gT, CV["nmg"], func=AF.Sigmoid, nbuf=1, zflat=True)
    phase_merge(P, C, NT, hT, oT, gT, wbr, wout, h1T)
    phase_ffn(P, C, NT, h1T, h2T, CV["nfg"], experts, FF, router)
    phase_ple(P, C, NT, h2T, pT, proj, pgate, CV["png"], h3T)
    P.finalize()
    return P.nc, names


def token_host_inputs(inp, L, half=0):
    f = np.float32
    d = {}
    cv = np.zeros((128, NCV), f)
    cv[:, CV["m0"]] = 1.0 if half == 0 else 0.0
    cv[:, CV["m1"]] = 1.0 if half == 1 else 0.0
    cv[:, CV["nmg"]:CV["nmg"] + 8] = inp["norm_mix_g"][L].reshape(8, 128).T
    cv[:, CV["nfg"]:CV["nfg"] + 8] = inp["norm_ffn_g"][L].reshape(8, 128).T
    cv[:, CV["png"]:CV["png"] + 8] = inp["ple_norm_g"][L].reshape(8, 128).T
    d["cv"] = cv
    d["w_gate"] = np.ascontiguousarray(inp["w_in"][L][:, 4272:7344])
    d["w_br0"] = inp["w_br_rwkv"][L]
    d["w_br1"] = inp["w_br_mla"][L]
    d["w_br2"] = inp["w_br_gdn"][L]
    d["w_out"] = inp["w_out"][L]
    d["ple_proj"] = inp["ple_proj"][L]
    d["ple_gate"] = inp["ple_gate"][L]
    if L % 2 == 0:
        d["ffn_wg"], d["ffn_wu"], d["ffn_wd"] = inp["ffn_wg"][L // 2], inp["ffn_wu"][L // 2], inp["ffn_wd"][L // 2]
    else:
        d["moe_wg"], d["moe_wu"], d["moe_wd"] = inp["moe_wg"][L // 2], inp["moe_wu"][L // 2], inp["moe_wd"][L // 2]
        d["moe_router"] = inp["moe_router"][L // 2]
    cs = consts_np()
    for c in F_CONSTS + ["sel8"]:
        d["c_" + c] = cs[c]
    return d


def kernel(**inputs):
    inp = {k: np.asarray(v) for k, v in inputs.items()}
    x = inp["x"].astype(np.float32)
    Bn, S, Dm = x.shape
    NT = S // 2
    hT = [np.ascontiguousarray(x[b].T) for b in range(Bn)]
    vf = [None] * NCORE
    for L in range(2):
        key = ("M", S, L)
        if key not in _PROG_CACHE:
            _PROG_CACHE[key] = build_mixer(S, L)
        nc, names = _PROG_CACHE[key]
        in_maps = []
        for core in range(NCORE):
            b, hg = core // 2, core % 2
            d = mixer_host_inputs(inp, L, b, hg)
            d["hT"] = hT[b]
            if L > 0:
                d["vfT_in"] = vf[core]
            in_maps.append({n: np.ascontiguousarray(d[n]) for n in names})
        res = run_bass_kernel_spmd(nc, in_maps, core_ids=list(range(NCORE)))
        oTs = [r["oT"] for r in res.results]
        if L == 0:
            vf = [r["vfT_out"] for r in res.results]
        key = ("F", NT, L)
        if key not in _PROG_CACHE:
            _PROG_CACHE[key] = build_token(NT, L)
        nc, names = _PROG_CACHE[key]
        th = token_host_inputs(inp, L)
        in_maps = []
        for core in range(NCORE):
            b, half = core // 2, core % 2
            tsl = slice(half * NT, (half + 1) * NT)
            d = dict(th)
            d["hT"] = hT[b][:, tsl]
            o0, o1 = oTs[2 * b], oTs[2 * b + 1]
            d["oT_all"] = np.concatenate([o0[0:256, tsl], o1[0:256, tsl], o0[256:512, tsl], o1[256:512, tsl],
                                          o0[512:768, tsl], o1[512:768, tsl]], axis=0)
            d["pT"] = inp["p"][L, b, tsl, :].T
            in_maps.append({n: np.ascontiguousarray(d[n]) for n in names})
        res = run_bass_kernel_spmd(nc, in_maps, core_ids=list(range(NCORE)))
        for b in range(Bn):
            hT[b] = np.concatenate([res.results[2 * b]["h3T"], res.results[2 * b + 1]["h3T"]], axis=1)
    out = np.stack([hT[b].T for b in range(Bn)], axis=0)
    return np.ascontiguousarray(out.astype(np.float32))


ALL_M_KEYS = ["w_in_m", "cv", "w_uq", "w_uk", "w_uv", "w_up", "a_up", "g_up"]


def build_fused(S):
    NTH = S // 2
    P = Prog()
    C = Ctx()
    names = []

    def din(name, shape, dt=F32):
        names.append(name)
        return P.dview(P.dram(name, shape, dt, kind="ExternalInput"))
    hT0 = din("hT0", [1024, S])
    pos_d = din("pos", [1, S], I32)
    pT = [din("pT0", [256, S]), din("pT1", [256, NTH])]
    C.sel8_d = din("c_sel8", [8, 1024])
    mi = {}
    for L in range(2):
        for hg in range(2):
            pre = f"m{L}{hg}_"
            d = {"w_in_m": din(pre + "w_in_m", [1024, NZ]), "cv": din(pre + "cv", [128, NCV]),
                 "w_uq": din(pre + "w_uq", [256, 384]), "w_uk": din(pre + "w_uk", [128, 256]), "w_uv": din(pre + "w_uv", [128, 256]),
                 "w_up": din(pre + "w_up", [64, 256]), "a_up": din(pre + "a_up", [64, 256]), "g_up": din(pre + "g_up", [128, 256])}
            if L > 0:
                d["v_down"] = din(pre + "v_down", [1024, 32])
                d["v_up"] = din(pre + "v_up", [32, 256])
            mi[(L, hg)] = d
    ti_ = {}
    for L in range(2):
        pre = f"t{L}_"
        d = {"cv": din(pre + "cv", [128, NCV]), "w_gate": din(pre + "w_gate", [1024, 3072]),
             "wbr": [din(pre + f"w_br{i}", [512, 1024]) for i in range(3)], "w_out": din(pre + "w_out", [1024, 1024]),
             "ple_proj": din(pre + "ple_proj", [256, 1024]), "ple_gate": din(pre + "ple_gate", [1024, 1024])}
        if L % 2 == 0:
            d["experts"] = [(din(pre + "ffn_wg", [1024, 2816]), din(pre + "ffn_wu", [1024, 2816]), din(pre + "ffn_wd", [2816, 1024]))]
            d["FF"] = 2816
            d["router"] = None
        else:
            wg_all = din(pre + "moe_wg", [8, 1024, 3584])
            wu_all = din(pre + "moe_wu", [8, 1024, 3584])
            wd_all = din(pre + "moe_wd", [8, 3584, 1024])
            d["experts"] = [(V(wg_all.ap[e], wg_all.key), V(wu_all.ap[e], wu_all.key), V(wd_all.ap[e], wd_all.key)) for e in range(8)]
            d["FF"] = 3584
            d["router"] = din(pre + "moe_router", [1024, 8])
        ti_[L] = d
    zT = P.dview(P.dram("zT", [NG, 128, S], F32, kind="Internal"))
    uT = P.dview(P.dram("uT", [1024, S], F32, kind="Internal"))
    oTa = P.dview(P.dram("oT_all", [1536, S], F32, kind="Internal"))
    vfT = P.dview(P.dram("vfT", [512, S], F32, kind="Internal"))
    gT = P.dview(P.dram("gT", [3072, S], F32, kind="Internal"))
    h1T = P.dview(P.dram("h1T", [1024, S], F32, kind="Internal"))
    h2T = P.dview(P.dram("h2T", [1024, S], F32, kind="Internal"))
    hT1 = P.dview(P.dram("hT1", [1024, S], F32, kind="Internal"))
    h3T = P.dview(P.dram("h3T", [1024, NTH], F32, kind="ExternalOutput"))
    C.k = load_consts(P, M_CONSTS)
    names.extend(["c_" + c for c in M_CONSTS])
    C.cv = P.tile([128, NCV], name="cv")
    groups = [(int(ZOFF[g]), int(ZG[g])) for g in range(NG)]
    ggroups = [(g * 128, 128) for g in range(24)]
    hin = hT0
    for L in range(2):
        for hg in range(2):
            d = mi[(L, hg)]
            P.dma(C.cv, d["cv"])
            phase_proj(P, C, S, hin, d["w_in_m"], NZ, groups, zT, CV["nmg"], uT=(uT if L > 0 else None))
            sub = lambda br: V(oTa.ap[br * 512 + hg * 256:br * 512 + (hg + 1) * 256, :], f"oT_{br}_{hg}")
            vfv = V(vfT.ap[hg * 256:(hg + 1) * 256, :], f"vfT_{hg}")
            mk_ = P.mark()
            o_r, o_g = sub(0), sub(2)
            P.run_interleaved([
                lambda: phase_rwkv(P, C, S, L, zT, o_r, d["w_up"], d["a_up"], d["g_up"], vfv,
                                   (uT if L > 0 else None), d.get("v_down"), d.get("v_up"), ttl=256, psbase=0, release=False),
                lambda: phase_gdn(P, C, S, zT, o_g, ttl=256, psbase=4, release=False)])
            P.release(mk_)
            phase_mla(P, C, S, zT, pos_d, d["w_uq"], d["w_uk"], d["w_uv"], sub(1))
        t = ti_[L]
        P.dma(C.cv, t["cv"])
        if L == 0:
            NT, dyn, hout = S, None, hT1
        else:
            NT, dyn, hout = NTH, NTH, h3T
        gv = V(gT.ap[:, 0:NT], gT.key)
        h1v = V(h1T.ap[:, 0:NT], h1T.key)
        h2v = V(h2T.ap[:, 0:NT], h2T.key)
        phase_proj(P, C, NT, hin, t["w_gate"], 3072, ggroups, gv, CV["nmg"], func=AF.Sigmoid, nbuf=1, zflat=True, dyn=dyn)
        phase_merge(P, C, NT, hin, oTa, gv, t["wbr"], t["w_out"], h1v, dyn=dyn)
        phase_ffn(P, C, NT, h1v, h2v, CV["nfg"], t["experts"], t["FF"], t["router"])
        phase_ple(P, C, NT, h2v, pT[L], t["ple_proj"], t["ple_gate"], CV["png"], hout)
        hin = hT1
    P.finalize()
    return P.nc, names


def kernel_unfused(**inputs):
    return _kernel_unfused(**inputs)


_kernel_unfused = kernel


def kernel(**inputs):
    inp = {k: np.asarray(v) for k, v in inputs.items()}
    x = inp["x"].astype(np.float32)
    Bn, S, Dm = x.shape
    NTH = S // 2
    key = ("FUSED", S)
    if key not in _PROG_CACHE:
        _PROG_CACHE[key] = build_fused(S)
    nc, names = _PROG_CACHE[key]
    cs = consts_np()
    shared = {"c_" + k: v for k, v in cs.items()}
    tok = []
    for L in range(2):
        th = token_host_inputs(inp, L)
        tok.append({f"t{L}_" + k: v for k, v in th.items() if not k.startswith("c_")})
    in_maps = []
    for core in range(NCORE):
        b, half = core // 2, core % 2
        d = dict(shared)
        d["hT0"] = x[b].T
        d["pos"] = inp["positions"][b:b + 1].astype(np.int32)
        d["pT0"] = inp["p"][0, b].T
        d["pT1"] = inp["p"][1, b, half * NTH:(half + 1) * NTH, :].T
        for L in range(2):
            for hg in range(2):
                md = mixer_host_inputs(inp, L, b, hg)
                for k, v in md.items():
                    if not k.startswith("c_") and k != "pos":
                        d[f"m{L}{hg}_" + k] = v
            d.update(tok[L])
            cvt = tok[L][f"t{L}_cv"].copy()
            cvt[:, CV["m0"]] = 1.0 if half == 0 else 0.0
            cvt[:, CV["m1"]] = 1.0 if half == 1 else 0.0
            d[f"t{L}_cv"] = cvt
        in_maps.append({n: np.ascontiguousarray(d[n]) for n in names})
    res = run_bass_kernel_spmd(nc, in_maps, core_ids=list(range(NCORE)))
    out = np.empty((Bn, S, Dm), np.float32)
    for core in range(NCORE):
        b, half = core // 2, core % 2
        out[b, half * NTH:(half + 1) * NTH, :] = res.results[core]["h3T"].T
    return out
```

```python
import numpy as np
from contextlib import ExitStack
import concourse.bass as bass
import concourse.mybir as mybir

F32 = mybir.dt.float32
BF16 = mybir.dt.bfloat16
I32 = mybir.dt.int32
ALU = mybir.AluOpType
AF = mybir.ActivationFunctionType
AX = mybir.AxisListType

ENGS = ("pe", "act", "dve", "pool", "sp")
N_DMA_SEMS = 24


class Op:
    __slots__ = ("eng", "fn", "deps", "is_dma", "idx", "sig", "slot", "slot_target", "slot_prev", "epoch")

    def __init__(self, eng, fn, is_dma):
        self.eng = eng
        self.fn = fn
        self.is_dma = is_dma
        self.deps = set()
        self.sig = 0
        self.slot = None
        self.slot_target = 0
        self.slot_prev = None


class V:
    __slots__ = ("ap", "key")

    def __init__(self, ap, key):
        self.ap = ap
        self.key = key

    def __getitem__(self, idx):
        return V(self.ap[idx], self.key)

    def sub(self, k):
        base = self.key[0] if isinstance(self.key, tuple) else self.key
        return V(self.ap, (base, k))

    def re(self, pat, **kw):
        return V(self.ap.rearrange(pat, **kw), self.key)

    def bc(self, shape):
        return V(self.ap.to_broadcast(list(shape)), self.key)

    @property
    def shape(self):
        return self.ap.shape


def _ap(x):
    return x.ap if isinstance(x, V) else x


ARENA_F32 = 53000


class Prog:
    def __init__(self, name="k"):
        self.nc = bass.Bass("TRN2", target_bir_lowering=False)
        self.st = ExitStack()
        self.arena = None
        self.aoff = 0
        self.amax = 0
        self.psum = None
        self.bar_deps = None
        self.since_bar = []
        self.bar_seen = {}
        self.epoch = 0
        self.ep_cnt = {}
        self.ops = []
        self.track = {}
        self.n_dma = 0
        self.slot_last = [None] * N_DMA_SEMS
        self.slot_count = [0] * N_DMA_SEMS
        self.uid = 0

    def sb(self, shape, dtype=F32, name=None):
        self.uid += 1
        return self.st.enter_context(self.nc.sbuf_tensor(name or f"sb{self.uid}", list(shape), dtype))

    def ps(self, shape, dtype=F32, name=None):
        self.uid += 1
        return self.st.enter_context(self.nc.psum_tensor(name or f"ps{self.uid}", list(shape), dtype))

    def dram(self, name, shape, dtype=F32, kind="Internal"):
        return self.nc.dram_tensor(name, list(shape), dtype, kind=kind)

    def tile(self, shape, name=None, dtype=None):
        if self.arena is None:
            self.arena = self.st.enter_context(self.nc.sbuf_tensor("arena", [128, ARENA_F32], F32))
        self.uid += 1
        p = shape[0]
        n = int(np.prod(shape[1:]))
        if dtype == BF16:
            nw = (n + 1) // 2
            assert self.aoff + nw <= ARENA_F32, f"arena overflow {self.aoff}+{nw}"
            ap = self.arena[0:p, self.aoff:self.aoff + nw].bitcast(BF16)[:, 0:n]
            self.aoff += nw
        else:
            assert self.aoff + n <= ARENA_F32, f"arena overflow {self.aoff}+{n}"
            ap = self.arena[0:p, self.aoff:self.aoff + n]
            self.aoff += n
        self.amax = max(self.amax, self.aoff)
        if len(shape) == 3:
            ap = ap.rearrange("p (a b) -> p a b", a=shape[1])
        elif len(shape) == 4:
            ap = ap.rearrange("p (a b c) -> p a b c", a=shape[1], b=shape[2])
        return V(ap, name or f"t{self.uid}")

    def mark(self):
        return self.aoff

    def release(self, mark):
        self.barrier()
        self.aoff = mark

    def pbank(self, i):
        if self.psum is None:
            self.psum = [self.st.enter_context(self.nc.psum_tensor(f"psb{j}", [128, 512], F32)) for j in range(8)]
        return V(self.psum[i][:], f"psb{i}")

    def pslot(self, bank, half):
        self.pbank(0)
        return V(self.psum[bank][:, half * 256:(half + 1) * 256], f"psb{bank}_{half}")

    def run_interleaved(self, fns):
        import threading
        il = {"turn": 0, "alive": [True] * len(fns), "cv": threading.Condition(), "tl": threading.local()}
        errs = []

        def nxt(i):
            n = len(fns)
            for d in range(1, n + 1):
                j = (i + d) % n
                if il["alive"][j]:
                    return j
            return i

        def runner(i, fn):
            with il["cv"]:
                while il["turn"] != i:
                    il["cv"].wait()
            il["tl"].i = i
            try:
                fn()
            except BaseException as e:
                errs.append(e)
            finally:
                with il["cv"]:
                    il["alive"][i] = False
                    il["turn"] = nxt(i)
                    il["cv"].notify_all()

        def yield_turn():
            i = getattr(il["tl"], "i", None)
            if i is None:
                return
            with il["cv"]:
                j = nxt(i)
                if j == i:
                    return
                il["turn"] = j
                il["cv"].notify_all()
                while il["turn"] != i:
                    il["cv"].wait()

        self._yield = yield_turn
        ths = [threading.Thread(target=runner, args=(i, f)) for i, f in enumerate(fns)]
        for t in ths:
            t.start()
        for t in ths:
            t.join()
        self._yield = None
        if errs:
            raise errs[0]

    def dview(self, t, name=None):
        ap = t.ap() if hasattr(t, "ap") and callable(t.ap) else t
        return V(ap, name or ap.tensor.name)

    def barrier(self):
        self.bar_deps = list(self.since_bar) if self.bar_deps is None else self.bar_deps + self.since_bar
        last = {}
        dm = []
        for o in self.bar_deps:
            if o.is_dma:
                dm.append(o)
            else:
                last[o.eng] = o
        self.bar_deps = list(last.values()) + dm[-2 * N_DMA_SEMS:]
        self.since_bar = []
        self.bar_seen = {}
        if max(self.ep_cnt.values(), default=0) > 20000:
            self.epoch += 1
            self.ep_cnt = {}

    @staticmethod
    def _key(x):
        if isinstance(x, V):
            x = x.key
        if isinstance(x, tuple):
            t, sub = x
        else:
            t, sub = x, None
        nm = t if isinstance(t, str) else (t.name if hasattr(t, "name") else t.tensor.name)
        return nm, sub

    def _conf(self, nm, sub):
        ent = self.track.setdefault(nm, {})
        if sub is None:
            return list(ent.keys())
        ks = [k for k in ent.keys() if k is None or k == sub]
        return ks

    def op(self, eng, fn, reads=(), writes=(), is_dma=False):
        o = Op(eng, fn, is_dma)
        o.epoch = self.epoch
        if not is_dma:
            self.ep_cnt[eng] = self.ep_cnt.get(eng, 0) + 1
        for r in reads:
            nm, sub = self._key(r)
            ent = self.track.setdefault(nm, {})
            for k in self._conf(nm, sub):
                w = ent[k][0]
                if w is not None:
                    o.deps.add(w)
        for wv in writes:
            nm, sub = self._key(wv)
            ent = self.track.setdefault(nm, {})
            for k in self._conf(nm, sub):
                w, rs = ent[k]
                if w is not None:
                    o.deps.add(w)
                for r in rs:
                    o.deps.add(r)
        for r in reads:
            nm, sub = self._key(r)
            ent = self.track[nm]
            if sub not in ent:
                ent[sub] = [None, []]
            ent[sub][1].append(o)
        for wv in writes:
            nm, sub = self._key(wv)
            ent = self.track[nm]
            if sub is None:
                for k in list(ent.keys()):
                    del ent[k]
            ent[sub] = [o, []]
        o.deps.discard(o)
        if self.bar_deps is not None and not self.bar_seen.get(eng):
            self.bar_seen[eng] = True
            o.deps.update(self.bar_deps)
        self.since_bar.append(o)
        if is_dma:
            s = self.n_dma % N_DMA_SEMS
            self.n_dma += 1
            o.slot = s
            o.slot_prev = self.slot_last[s]
            self.slot_count[s] += 1
            o.slot_target = 16 * self.slot_count[s]
            self.slot_last[s] = o
        o.idx = len(self.ops)
        self.ops.append(o)
        if getattr(self, "_yield", None) is not None:
            self._yield()
        return o

    def dma(self, out, in_, reads=None, writes=None, q="sp", in_fn=None, **kw):
        if in_fn is not None:
            return self.op(q, lambda e: e.dma_start(out=_ap(out), in_=in_fn(e), **kw),
                           reads if reads is not None else [in_], writes if writes is not None else [out], is_dma=True)
        return self.op(q, lambda e: e.dma_start(out=_ap(out), in_=_ap(in_), **kw),
                       reads if reads is not None else [in_], writes if writes is not None else [out], is_dma=True)

    def mm(self, out, lhsT, rhs, start=True, stop=True, reads=None, writes=None, **kw):
        return self.op("pe", lambda e: e.matmul(_ap(out), _ap(lhsT), _ap(rhs), start=start, stop=stop, **kw),
                       reads if reads is not None else [lhsT, rhs], writes if writes is not None else [out])

    def transpose(self, out, in_, ident, reads=None, writes=None):
        return self.op("pe", lambda e: e.transpose(_ap(out), _ap(in_), _ap(ident)),
                       reads if reads is not None else [in_, ident], writes if writes is not None else [out])

    def act(self, out, in_, func, bias=None, scale=1.0, reads=None, writes=None, accum_out=None, eng="act"):
        kw = {}
        rd = [in_]
        if bias is not None:
            kw["bias"] = _ap(bias)
            if not isinstance(bias, (int, float)):
                rd.append(bias)
        if not isinstance(scale, (int, float)):
            rd.append(scale)
        wr = [out]
        if accum_out is not None:
            kw["accum_out"] = _ap(accum_out)
            wr.append(accum_out)
        return self.op(eng, lambda e: e.activation(_ap(out), _ap(in_), func, scale=_ap(scale), **kw),
                       reads if reads is not None else rd, writes if writes is not None else wr)

    def tt(self, out, in0, in1, op, eng="dve", reads=None, writes=None):
        return self.op(eng, lambda e: e.tensor_tensor(_ap(out), _ap(in0), _ap(in1), op),
                       reads if reads is not None else [in0, in1], writes if writes is not None else [out])

    def ts(self, out, in0, s1, op0, s2=None, op1=None, eng="dve", reads=None, writes=None):
        rd = [in0] + [s for s in (s1, s2) if s is not None and not isinstance(s, (int, float))]
        if op1 is None:
            f = lambda e: e.tensor_scalar(_ap(out), _ap(in0), _ap(s1), None, op0)
        else:
            f = lambda e: e.tensor_scalar(_ap(out), _ap(in0), _ap(s1), _ap(s2), op0, op1)
        return self.op(eng, f, reads if reads is not None else rd, writes if writes is not None else [out])

    def stt(self, out, in0, scalar, in1, op0, op1, eng="dve", reads=None, writes=None):
        rd = [in0, in1] + ([] if isinstance(scalar, (int, float)) else [scalar])
        return self.op(eng, lambda e: e.scalar_tensor_tensor(_ap(out), _ap(in0), _ap(scalar), _ap(in1), op0, op1),
                       reads if reads is not None else rd, writes if writes is not None else [out])

    def copy(self, out, in_, eng="dve", reads=None, writes=None):
        if eng == "act":
            f = lambda e: e.copy(_ap(out), _ap(in_))
        else:
            f = lambda e: e.tensor_copy(_ap(out), _ap(in_))
        return self.op(eng, f, reads if reads is not None else [in_], writes if writes is not None else [out])

    def memset(self, ap, val, eng="pool", writes=None):
        return self.op(eng, lambda e: e.memset(_ap(ap), val), [], writes if writes is not None else [ap])

    def recip(self, out, in_, reads=None, writes=None):
        return self.op("dve", lambda e: e.reciprocal(_ap(out), _ap(in_)),
                       reads if reads is not None else [in_], writes if writes is not None else [out])

    def finalize(self, final_waits=()):
        nc = self.nc
        ops = self.ops
        needed = set()
        for o in ops:
            for d in o.deps:
                if not d.is_dma:
                    needed.add(d.idx)
        cnt = {}
        for o in ops:
            if not o.is_dma and (o.idx in needed):
                k_ = (o.eng, o.epoch)
                cnt[k_] = cnt.get(k_, 0) + 1
                o.sig = cnt[k_]
            else:
                o.sig = 0
        sems = {k_: self.st.enter_context(nc.semaphore(f"s_{k_[0]}_{k_[1]}")) for k_ in cnt}
        dsems = [self.st.enter_context(nc.semaphore(f"s_d{i}")) for i in range(N_DMA_SEMS)]
        per_eng = {e: [o for o in ops if o.eng == e] for e in ENGS}
        final = list(final_waits)

        def body(eng_name):
            def _b(e):
                waited = {}
                def wait(key, sem, val):
                    if waited.get(key, 0) >= val:
                        return
                    e.wait_ge(sem, val)
                    waited[key] = val
                for o in per_eng[eng_name]:
                    for d in sorted(o.deps, key=lambda x: x.idx):
                        if d.is_dma:
                            wait(("d", d.slot), dsems[d.slot], d.slot_target)
                        else:
                            if d.eng == eng_name and eng_name == "pe":
                                continue
                            wait(("c", d.eng, d.epoch), sems[(d.eng, d.epoch)], d.sig)
                    if o.is_dma and o.slot_prev is not None:
                        wait(("d", o.slot), dsems[o.slot], o.slot_prev.slot_target)
                    ins = o.fn(e)
                    if o.is_dma:
                        ins.then_inc(dsems[o.slot], 16)
                    elif o.sig:
                        ins.then_inc(sems[(eng_name, o.epoch)], 1)
                if eng_name == "sp":
                    for s in range(N_DMA_SEMS):
                        if self.slot_last[s] is not None:
                            wait(("d", s), dsems[s], self.slot_last[s].slot_target)
            return _b

        with nc.Block() as block:
            block.tensor(body("pe"))
            block.scalar(body("act"))
            block.vector(body("dve"))
            block.gpsimd(body("pool"))
            block.sync(body("sp"))
        self.st.close()
        return nc

D = 1024
TT = 512
ZG = [64] * 12 + [64, 64, 128] + [128, 128, 128, 32] + [64] * 16 + [4, 4]
NG = len(ZG)
G_R, G_K, G_V, G_WLO, G_ALO, G_GLO = 0, 4, 8, 12, 13, 14
G_CQ, G_CKV, G_KPE = 15, 17, 18
G_GQ, G_GK, G_GV, G_GG, G_GB, G_GA = 19, 23, 27, 31, 35, 36
ZOFF = np.concatenate([[0], np.cumsum(ZG)]).astype(int)
NZ = int(ZOFF[-1])

CV = {}
_n = 0
for nm, k in [("nmg", 8), ("mu", 15), ("w0", 4), ("a0", 4), ("kk", 4), ("ka", 4), ("rk", 4), ("lng", 4), ("lnb", 4),
              ("vmu", 8), ("vb", 4), ("qng", 2), ("kvng", 1), ("qkq", 1), ("qkk", 1), ("invf", 1),
              ("conv", 48), ("alog", 1), ("dtb", 1), ("gng", 1), ("ropec", 1), ("nfg", 8), ("png", 8), ("m0", 1), ("m1", 1)]:
    CV[nm] = _n
    _n += k
NCV = _n


def consts_np():
    c = {}
    c["ident"] = np.eye(128, dtype=np.float32)
    c["ones"] = np.ones((128, 128), np.float32)
    bd = np.zeros((128, 128), np.float32)
    bd[:64, :64] = 1
    bd[64:, 64:] = 1
    c["bd64"] = bd
    s = np.arange(64)[:, None]
    t = np.arange(64)[None, :]
    incl = (t >= s).astype(np.float32)
    strict = (t > s).astype(np.float32)
    c["m_incl"] = np.concatenate([incl, incl], 0)
    c["m_strict"] = np.concatenate([strict, strict], 0)
    c["m_incl_T"] = np.concatenate([incl.T, incl.T], 0)
    c["m_strict_T"] = np.concatenate([strict.T, strict.T], 0)
    c["neg_incl"] = (1.0 - c["m_incl"]) * -1e4
    c["neg_incl_T"] = (1.0 - c["m_incl_T"]) * -1e4
    c["id64x2"] = np.concatenate([np.eye(64, dtype=np.float32)] * 2, 0)
    kk = np.arange(128)[:, None]
    qq = np.arange(128)[None, :]
    c["att_mask"] = (qq >= kk).astype(np.float32)
    R = np.zeros((96, 96), np.float32)
    for m in range(16):
        R[64 + m, 64 + m + 16] = -1.0
        R[64 + 16 + m, 64 + m] = 1.0
    c["ropeRT"] = np.ascontiguousarray(R.T)
    sel = np.zeros((4, 4, 64), np.float32)
    for h in range(4):
        sel[h, h, :] = 1.0
    c["sel4"] = sel.reshape(4, 256)
    sel8 = np.zeros((8, 8, 128), np.float32)
    for e in range(8):
        sel8[e, e, :] = 1.0
    c["sel8"] = sel8.reshape(8, 1024)
    c["id4"] = np.tile(np.eye(64, dtype=np.float32)[:, None, :], (1, 4, 1)).reshape(64, 256)
    c["neg4"] = np.tile(c["neg_incl"][:64][:, None, :], (1, 4, 1)).reshape(64, 256)
    c["neg4T"] = np.tile(c["neg_incl_T"][:64][:, None, :], (1, 4, 1)).reshape(64, 256)
    c["ms4"] = np.tile(c["m_strict"][:64][:, None, :], (1, 4, 1)).reshape(64, 256)
    c["ms4T"] = np.tile(c["m_strict_T"][:64][:, None, :], (1, 4, 1)).reshape(64, 256)
    c["mi4"] = np.tile(c["m_incl"][:64][:, None, :], (1, 4, 1)).reshape(64, 256)
    return c


CONST_SHAPES = {k: v.shape for k, v in consts_np().items()}


class Ctx:
    pass


def load_consts(P, names):
    out = {}
    for nm in names:
        shp = CONST_SHAPES[nm]
        d = P.dview(P.dram("c_" + nm, shp, F32, kind="ExternalInput"))
        t = P.tile(list(shp), name="c_" + nm)
        P.dma(t, d)
        out[nm] = t
    return out


def rstd_from_ps(P, out, ps, n, eps):
    P.act(out, ps, AF.Ln, scale=float(1.0 / n), bias=float(eps))
    P.act(out, out, AF.Exp, scale=-0.5)


def act_sigmoid(P, out, in_, scale=1.0, negbias=None):
    if negbias is None:
        P.act(out, in_, AF.Exp, scale=-float(scale))
    else:
        P.act(out, in_, AF.Exp, scale=-float(scale), bias=negbias)
    P.act(out, out, AF.Ln, bias=1.0)
    P.act(out, out, AF.Exp, scale=-1.0)


def phase_proj(P, C, S, hT, w_d, ncols, groups, zT, gcol, uT=None, func=None, nbuf=2, zflat=False, dyn=None):
    mk = P.mark()
    cv = C.cv
    w = P.tile([128, 8, ncols], name="w_in", dtype=BF16)
    wst = [P.tile([128, 8, 512], name=f"wst{i}") for i in range(2)]
    wv = w_d.re("(c p) n -> p c n", p=128)
    step = 512
    for i_, c0 in enumerate(range(0, ncols, step)):
        c1 = min(ncols, c0 + step)
        st_ = wst[i_ % 2]
        P.dma(st_[:, :, 0:c1 - c0], wv[:, :, c0:c1])
        P.copy(w[:, :, c0:c1].sub(c0), st_[:, :, 0:c1 - c0], eng=("pool" if i_ % 2 == 0 else "act"))
    hb = [P.tile([128, 8, TT], name=f"hb{i}") for i in range(nbuf)]
    sq = P.tile([128, 8, TT], name="sq")
    ub = [P.tile([128, 8, TT], name=f"ub{i}", dtype=BF16) for i in range(nbuf)]
    rs = P.tile([128, TT], name="rs")
    stg = [P.tile([128, TT], name=f"stg{i}") for i in range(4)]
    hv = hT.re("(c p) t -> p c t", p=128)
    ps_ss = P.pbank(0)
    pz = [P.pbank(1 + i) for i in range(4)]
    nt = S // TT
    k = 0
    for ti in range(nt):
        tsl = slice(ti * TT, (ti + 1) * TT)
        h = hb[ti % nbuf]
        u = ub[ti % nbuf]
        if dyn is not None:
            P.dma(h, hv[:, :, tsl])
            P.dma(sq, hv[:, :, dyn + ti * TT:dyn + (ti + 1) * TT])
            P.ts(h, h, cv[:, CV["m0"]:CV["m0"] + 1], ALU.mult)
            P.stt(h, sq, cv[:, CV["m1"]:CV["m1"] + 1], h, ALU.mult, ALU.add)
        else:
            P.dma(h, hv[:, :, tsl])
        P.act(sq, h, AF.Square)
        for c in range(8):
            P.mm(ps_ss, C.k["ones"], sq[:, c, :], start=(c == 0), stop=(c == 7))
        rstd_from_ps(P, rs, ps_ss, D, 1e-6)
        if uT is not None:
            for c in range(8):
                P.stt(sq[:, c, :], h[:, c, :], cv[:, gcol + c:gcol + c + 1], rs, ALU.mult, ALU.mult)
            P.dma(uT.re("(c p) t -> p c t", p=128)[:, :, tsl], sq, q="pool")
            P.copy(u, sq, eng="act")
        else:
            for c in range(8):
                P.stt(u[:, c, :], h[:, c, :], cv[:, gcol + c:gcol + c + 1], rs, ALU.mult, ALU.mult)
        for gi, (co, n) in enumerate(groups):
            pp = pz[k % 4]
            st = stg[k % 4]
            for c in range(8):
                P.mm(pp[0:n, :], w[:, c, co:co + n], u[:, c, :], start=(c == 0), stop=(c == 7))
            if func is not None:
                P.act(st[0:n, :], pp[0:n, :], func)
            else:
                P.copy(st[0:n, :], pp[0:n, :], eng=("act" if k % 2 == 0 else "dve"))
            if zflat:
                P.dma(zT[co:co + n, tsl].sub(gi), st[0:n, :], q="pool")
            else:
                P.dma(zT[gi, 0:n, tsl].sub(gi), st[0:n, :], q="pool")
            k += 1
    P.release(mk)


def phase_mla(P, C, S, zT, pos_d, w_uq_d, w_uk_d, w_uv_d, oT):
    mk = P.mark()
    cv = C.cv
    K = C.k
    nt = S // TT
    wuq_f = P.tile([128, 2, 384], name="wuq_f")
    P.dma(wuq_f, w_uq_d.re("(c p) n -> p c n", p=128))
    wuk_f = P.tile([128, 256], name="wuk_f")
    P.dma(wuk_f, w_uk_d)
    wuv_f = P.tile([128, 256], name="wuv_f")
    P.dma(wuv_f, w_uv_d)
    wuq = P.tile([128, 2, 384], name="wuq", dtype=BF16)
    wuk = P.tile([128, 256], name="wuk", dtype=BF16)
    wuv = P.tile([128, 256], name="wuv", dtype=BF16)
    P.copy(wuq, wuq_f, eng="pool")
    P.copy(wuk, wuk_f, eng="pool")
    P.copy(wuv, wuv_f, eng="pool")
    ones_b = P.tile([128, 64], name="ones_b", dtype=BF16)
    P.copy(ones_b, K["ones"][:, 0:64], eng="pool")
    KT = P.tile([96, 4, S], name="KT", dtype=BF16)
    VT = P.tile([128, S // 128, 256], name="Vtm", dtype=BF16)
    rc = P.tile([96, TT], name="rope_c")
    rsn = P.tile([96, TT], name="rope_s")
    posi = P.tile([96, TT], name="posi")
    posf = P.tile([96, TT], name="posf")
    ang = P.tile([96, TT], name="ang")
    tmp = P.tile([96, TT], name="ropetmp")
    tmp2 = P.tile([96, TT], name="ropetmp2")
    P.memset(rc[0:64, :], 1.0, writes=[rc.sub("lo")])
    P.memset(rsn[0:64, :], 0.0, writes=[rsn.sub("lo")])
    pi_ap = V(posi.ap.bitcast(I32), posi.key)
    invf = cv[64:96, CV["invf"]:CV["invf"] + 1]
    negpi = cv[64:96, CV["ropec"]:CV["ropec"] + 1]
    TWO_PI = float(2 * np.pi)

    def rope_tile(ti):
        tsl = slice(ti * TT, (ti + 1) * TT)
        P.dma(pi_ap[64:96, :], V(pos_d.ap[:, tsl].partition_broadcast(32), pos_d.key))
        P.copy(posf[64:96, :], pi_ap[64:96, :])
        P.ts(ang[64:96, :], posf[64:96, :], invf, ALU.mult)
        for dst, shift in ((rsn, 0.0), (rc, float(np.pi / 2))):
            a_ = ang[64:96, :]
            if shift:
                P.ts(tmp2[64:96, :], ang[64:96, :], shift, ALU.add)
                a_ = tmp2[64:96, :]
            P.ts(tmp[64:96, :], a_, float(1.0 / TWO_PI), ALU.mult)
            P.copy(pi_ap[64:96, :], tmp[64:96, :])
            P.copy(tmp[64:96, :], pi_ap[64:96, :])
            P.stt(tmp[64:96, :], tmp[64:96, :], -TWO_PI, a_, ALU.mult, ALU.add)
            P.ts(posf[64:96, :], tmp[64:96, :], float(np.pi), ALU.is_gt, TWO_PI, ALU.mult)
            P.tt(tmp[64:96, :], tmp[64:96, :], posf[64:96, :], ALU.subtract)
            P.act(dst[64:96, :], tmp[64:96, :], AF.Sin, writes=[dst.sub("hi")])

    ones = K["ones"]
    cq = [P.tile([128, 2, TT], name=f"cq{i}") for i in range(2)]
    ckv = [P.tile([128, TT], name=f"ckv{i}") for i in range(2)]
    cqb = [P.tile([128, 2, TT], name=f"cqb{i}", dtype=BF16) for i in range(2)]
    ckvb = [P.tile([128, TT], name=f"ckvb{i}", dtype=BF16) for i in range(2)]
    kpe = [P.tile([96, TT], name=f"kpe{i}") for i in range(2)]
    sq = P.tile([128, 2, TT], name="msq")
    rs = P.tile([128, TT], name="mrs")
    raw = P.tile([96, TT], name="raw")
    nrm = P.tile([96, TT], name="nrm")
    rot = P.tile([96, TT], name="rot")
    QT = P.tile([96, 4, TT], name="QT", dtype=BF16)
    ps_a = P.pbank(0)
    ps_b = P.pbank(1)
    ps_c = P.pbank(2)

    def qk_finish(src_raw, gcolname, dst, tsl):
        P.act(sq[0:96, 0, :], src_raw, AF.Square)
        P.mm(ps_b[0:96, :], ones[0:96, 0:96], sq[0:96, 0, :])
        rstd_from_ps(P, rs[0:96, :], ps_b[0:96, :], 96, 1e-6)
        g = cv[0:96, CV[gcolname]:CV[gcolname] + 1]
        P.stt(nrm, src_raw, g, rs[0:96, :], ALU.mult, ALU.mult)
        P.mm(ps_c[0:96, :], K["ropeRT"], nrm)
        P.tt(rot, ps_c[0:96, :], rsn, ALU.mult)
        P.tt(nrm, nrm, rc, ALU.mult, eng="pool")
        P.tt(dst, nrm, rot, ALU.add)

    def load_norm(ti, want_q):
        tsl = slice(ti * TT, (ti + 1) * TT)
        i2 = ti % 2
        if want_q:
            P.dma(cq[i2], zT[G_CQ:G_CQ + 2, :, tsl].re("g p t -> p g t"))
            P.act(sq, cq[i2], AF.Square)
            P.mm(ps_a, ones, sq[:, 0, :], start=True, stop=False)
            P.mm(ps_a, ones, sq[:, 1, :], start=False, stop=True)
            rstd_from_ps(P, rs, ps_a, 256, 1e-6)
            for c in range(2):
                P.stt(cqb[i2][:, c, :], cq[i2][:, c, :], cv[:, CV["qng"] + c:CV["qng"] + c + 1], rs, ALU.mult, ALU.mult)
        else:
            P.dma(ckv[i2], zT[G_CKV, :, tsl])
            P.dma(kpe[i2][64:96, :], zT[G_KPE, 0:32, tsl])
            P.act(sq[:, 0, :], ckv[i2], AF.Square)
            P.mm(ps_a, ones, sq[:, 0, :])
            rstd_from_ps(P, rs, ps_a, 128, 1e-6)
            P.stt(ckvb[i2], ckv[i2], cv[:, CV["kvng"]:CV["kvng"] + 1], rs, ALU.mult, ALU.mult)
        return tsl, i2

    for ti in range(nt):
        rope_tile(ti)
        tsl, i2 = load_norm(ti, False)
        for h in range(4):
            P.mm(ps_b[0:64, :], wuk[:, h * 64:(h + 1) * 64], ckvb[i2])
            P.copy(raw[0:64, :], ps_b[0:64, :], eng="act", writes=[raw.sub("lo")])
            P.copy(raw[64:96, :], kpe[i2][64:96, :], eng="pool", writes=[raw.sub("hi")])
            qk_finish(raw, "qkk", KT[:, h, tsl].sub(h), tsl)
        for j in range(TT // 128):
            P.mm(ps_c[:, 0:256], ckvb[i2][:, j * 128:(j + 1) * 128], wuv)
            P.copy(VT[:, ti * (TT // 128) + j, :], ps_c[:, 0:256], eng="act")

    pt = [P.tile([128, TT], name=f"pt{i}", dtype=BF16) for i in range(3)]
    osb = P.tile([64, TT], name="osb")
    lsb = P.tile([64, TT], name="lsb")
    ps_s = [P.pbank(3), P.pbank(4)]
    ps_o = P.pbank(5)
    ps_l = P.pbank(6)
    scale = float(96 ** -0.5)
    for ti in range(nt):
        rope_tile(ti)
        tsl, i2 = load_norm(ti, True)
        for h in range(4):
            for c in range(2):
                P.mm(ps_b[0:96, :], wuq[:, c, h * 96:(h + 1) * 96], cqb[i2][:, c, :], start=(c == 0), stop=(c == 1))
            P.copy(raw, ps_b[0:96, :], eng="act")
            qk_finish(raw, "qkq", QT[:, h, :].sub(h), tsl)
        for h in range(4):
            nkc = 4 * (ti + 1)

            def c0_of(kc):
                j = kc - 4 * ti
                return 0 if j <= 0 else j * 128

            def score(kc):
                c0 = c0_of(kc)
                P.mm(ps_s[kc % 2][:, c0:TT], KT[:, h, kc * 128:(kc + 1) * 128].sub(h), QT[:, h, c0:TT].sub(h))
            score(0)
            for kc in range(nkc):
                if kc + 1 < nkc:
                    score(kc + 1)
                j = kc - 4 * ti
                c0 = c0_of(kc)
                pss = ps_s[kc % 2]
                p_t = pt[kc % 3]
                P.act(p_t[:, c0:TT], pss[:, c0:TT], AF.Exp, scale=scale)
                if j >= 0:
                    P.tt(p_t[:, c0:c0 + 128], p_t[:, c0:c0 + 128], K["att_mask"], ALU.mult, eng="pool")
                P.mm(ps_o[0:64, c0:TT], VT[:, kc, h * 64:(h + 1) * 64], p_t[:, c0:TT], start=(kc == 0), stop=(kc == nkc - 1))
                P.mm(ps_l[0:64, c0:TT], ones_b, p_t[:, c0:TT], start=(kc == 0), stop=(kc == nkc - 1))
            P.act(lsb, ps_l[0:64, :], AF.Ln)
            P.act(lsb, lsb, AF.Exp, scale=-1.0)
            P.tt(osb, ps_o[0:64, :], lsb, ALU.mult)
            P.dma(oT[h * 64:(h + 1) * 64, tsl].sub(h), osb, q="pool")
    P.release(mk)


def neumann_inv(P, C, A0, B0, bufs, ps1, ps2, ps3):
    id4 = C.k["id4"]
    TTt = bufs["TT"]
    P.tt(TTt, B0, id4, ALU.add)
    A = [A0, bufs["A1"]]
    B = [B0, bufs["B1"]]
    for k in range(1, 6):
        a_prev, a_new = A[(k - 1) % 2], A[k % 2]
        b_prev, b_new = B[(k - 1) % 2], B[k % 2]
        for h in range(4):
            hs = slice(h * 64, (h + 1) * 64)
            P.mm(ps1[0:64, hs], b_prev[:, hs], a_prev[:, hs])
        if k < 5:
            for h in range(4):
                hs = slice(h * 64, (h + 1) * 64)
                P.mm(ps2[0:64, hs], a_prev[:, hs], b_prev[:, hs])
        P.copy(a_new, ps1[0:64, 0:256], eng="act")
        if k < 5:
            P.copy(b_new, ps2[0:64, 0:256], eng="dve")
        for h in range(4):
            hs = slice(h * 64, (h + 1) * 64)
            P.mm(ps3[0:64, hs], a_new[:, hs], TTt[:, hs])
        P.tt(TTt, TTt, ps3[0:64, 0:256], ALU.add)
    return TTt


def phase_gdn(P, C, S, zT, oT, ttl=512, psbase=None, release=True):
    mk = P.mark()
    TT = ttl
    cv = C.cv
    K = C.k
    nt = S // TT
    NCH = TT // 64
    ones = K["ones"]
    ident = K["ident"]
    cvw = lambda seg, h, j: cv[0:64, CV["conv"] + (seg * 4 + h) * 4 + j:CV["conv"] + (seg * 4 + h) * 4 + j + 1]
    St = P.tile([64, 4, 64], name="gS")
    P.memset(St, 0.0)
    Sb = P.tile([64, 4, 64], name="gSb", dtype=BF16)
    P.copy(Sb, St, eng="pool")
    nA = P.tile([4, 1], name="nA")
    P.act(nA, cv[0:4, CV["alog"]:CV["alog"] + 1], AF.Exp)
    P.ts(nA, nA, -1.0, ALU.mult)
    def mkbuf(i):
        b = {}
        for nm in ["q", "k", "kb", "qd"]:
            b[nm] = P.tile([64, 4, TT], name=f"g{nm}{i}", dtype=BF16)
        b["k32"] = P.tile([64, 4, TT], name=f"gk32{i}")
        b["q32"] = P.tile([64, 4, TT], name=f"gq32{i}")
        b["ktm"] = P.tile([64, NCH, 4, 64], name=f"gktm{i}", dtype=BF16)
        b["bv"] = P.tile([64, NCH, 4, 64], name=f"gbv{i}")
        b["bg"] = P.tile([64, NCH, 12], name=f"gbg{i}")
        b["c2"] = P.tile([64, NCH, 4], name=f"gc2{i}")
        b["ngc"] = P.tile([64, NCH, 4], name=f"gngc{i}")
        b["dl"] = P.tile([64, 4, NCH], name=f"gdl{i}")
        return b
    TB = [mkbuf(0), mkbuf(1)]
    xin = [P.tile([64, TT + 3], name=f"gxin{i}") for i in range(3)]
    acc = [P.tile([64, TT], name=f"gacc{i}") for i in range(2)]
    vfm = P.tile([64, 4, TT], name="gvfm")
    sq = P.tile([64, TT], name="gsq")
    rs = P.tile([64, TT], name="grs")
    bfm = P.tile([4, TT], name="gbfm")
    gfm = [P.tile([4, TT], name=f"ggfm{i}") for i in range(2)]
    efm = P.tile([4, TT], name="gefm")
    kdf = P.tile([4, TT], name="gkdf")
    gl4 = P.tile([4, NCH], name="ggl4")
    ob = [P.tile([64, 4, TT], name=f"gob{i}") for i in range(2)]
    gate = P.tile([64, TT], name="ggate")
    def mkch(i):
        d_ = {nm: P.tile([64, 256], name=f"gc_{nm}{i}") for nm in ["E", "F", "G1", "G2", "Gs"]}
        d_.update({nm: P.tile([64, 256], name=f"gc_{nm}{i}", dtype=BF16) for nm in ["A0", "B0", "A1", "B1", "TT", "Ain", "X", "vn"]})
        return d_
    CB = [mkch(0), mkch(1)]
    if psbase is None:
        psA, psB, psC, psD, psE, psF, psG, psH = [P.pbank(i) for i in range(8)]
    else:
        psA, psB, psC, psD, psE, psF, psG, psH = [P.pbank(psbase + j % 4) for j in range(8)]
    xk = 0
    for ti in range(nt):
        tb = TB[ti % 2]
        tsl = slice(ti * TT, (ti + 1) * TT)
        for seg, (g0, dst) in enumerate([(G_GQ, tb["q32"]), (G_GK, tb["k32"]), (G_GV, vfm)]):
            for h in range(4):
                x = xin[xk % 3]
                a = acc[xk % 2]
                xk += 1
                if ti == 0:
                    P.memset(x[:, 0:3], 0.0, writes=[x.sub("halo")])
                    P.dma(x[:, 3:TT + 3].sub("body"), zT[g0 + h, 0:64, 0:TT])
                else:
                    P.dma(x, zT[g0 + h, 0:64, ti * TT - 3:(ti + 1) * TT])
                P.ts(a, x[:, 0:TT], cvw(seg, h, 0), ALU.mult)
                for j in range(1, 4):
                    P.stt(a, x[:, j:TT + j], cvw(seg, h, j), a, ALU.mult, ALU.add)
                act_sigmoid(P, sq, a)
                if seg == 2:
                    P.tt(dst[:, h, :].sub(h), a, sq, ALU.mult)
                else:
                    P.tt(a, a, sq, ALU.mult)
                    P.act(sq, a, AF.Square)
                    P.mm(psA[0:64, 0:TT], ones[0:64, 0:64], sq)
                    rstd_from_ps(P, rs, psA[0:64, 0:TT], 1.0, 1e-12)
                    P.stt(dst[:, h, :].sub(h), a, (0.125 if seg == 0 else 1.0), rs, ALU.mult, ALU.mult)
        P.dma(bfm, zT[G_GB, 0:4, tsl])
        act_sigmoid(P, bfm, bfm)
        g0t = gfm[0]
        P.dma(g0t, zT[G_GA, 0:4, tsl])
        P.act(g0t, g0t, AF.Exp, bias=cv[0:4, CV["dtb"]:CV["dtb"] + 1])
        P.act(g0t, g0t, AF.Ln, bias=1.0)
        P.ts(g0t, g0t, nA[:, 0:1], ALU.mult)
        cur = 0
        for sh in (1, 2, 4, 8, 16, 32):
            src = gfm[cur].re("h (n c) -> h n c", c=64)
            dstt = gfm[1 - cur].re("h (n c) -> h n c", c=64)
            P.copy(dstt[:, :, 0:sh], src[:, :, 0:sh], eng="pool", writes=[gfm[1 - cur].sub("a")])
            P.tt(dstt[:, :, sh:64], src[:, :, sh:64], src[:, :, 0:64 - sh], ALU.add, writes=[gfm[1 - cur].sub("b")])
            cur = 1 - cur
        gc = gfm[cur]
        P.act(efm, gc, AF.Exp)
        gc3 = gc.re("h (n c) -> h n c", c=64)
        P.copy(gl4, gc3[:, :, 63])
        P.tt(kdf.re("h (n c) -> h n c", c=64), V(gl4.ap.unsqueeze(2).to_broadcast([4, NCH, 64]), gl4.key), gc3, ALU.subtract)
        P.act(kdf, kdf, AF.Exp)
        for h in range(4):
            P.mm(psA[0:64, h * NCH:(h + 1) * NCH], K["sel4"][:, h * 64:(h + 1) * 64], gl4)
        P.act(tb["dl"].re("p h n -> p (h n)"), psA[0:64, 0:4 * NCH], AF.Exp)
        P.copy(tb["k"], tb["k32"], eng="pool")
        P.copy(tb["q"], tb["q32"], eng="pool")
        for h in range(4):
            P.mm(psB[0:64, 0:TT], K["sel4"][:, h * 64:(h + 1) * 64], bfm)
            P.tt(tb["kb"][:, h, :].sub(h), tb["k32"][:, h, :].sub(h), psB[0:64, 0:TT], ALU.mult)
            P.mm(psC[0:64, 0:TT], K["sel4"][:, h * 64:(h + 1) * 64], efm)
            P.tt(tb["qd"][:, h, :].sub(h), tb["q32"][:, h, :].sub(h), psC[0:64, 0:TT], ALU.mult)
        for n in range(NCH):
            cs = slice(n * 64, (n + 1) * 64)
            for h in range(4):
                P.transpose(psD[0:64, h * 64:(h + 1) * 64], tb["k32"][:, h, cs].sub(h), ident[0:64, 0:64])
            P.copy(tb["ktm"][:, n, :, :].re("p h d -> p (h d)"), psD[0:64, 0:256], eng="act")
            for h in range(4):
                P.transpose(psE[0:64, h * 64:(h + 1) * 64], vfm[:, h, cs].sub(h), ident[0:64, 0:64])
            P.copy(tb["bv"][:, n, :, :].re("p h d -> p (h d)"), psE[0:64, 0:256], eng="dve")
            P.transpose(psF[0:64, 0:4], bfm[:, cs], ident[0:4, 0:4])
            P.transpose(psF[0:64, 4:8], gc[:, cs], ident[0:4, 0:4])
            P.transpose(psF[0:64, 8:12], kdf[:, cs], ident[0:4, 0:4])
            P.copy(tb["bg"][:, n, :], psF[0:64, 0:12], eng="act")
        bg = tb["bg"]
        P.ts(tb["ngc"], bg[:, :, 4:8], -1.0, ALU.mult)
        P.act(tb["c2"], bg[:, :, 4:8], AF.Exp)
        P.stt(tb["c2"], tb["c2"], -1.0, bg[:, :, 0:4], ALU.mult, ALU.mult)
        P.tt(tb["ktm"], tb["ktm"], V(bg.ap[:, :, 8:12].unsqueeze(3).to_broadcast([64, NCH, 4, 64]), bg.key), ALU.mult)
        P.tt(tb["bv"], tb["bv"], V(bg.ap[:, :, 0:4].unsqueeze(3).to_broadcast([64, NCH, 4, 64]), bg.key), ALU.mult)
        o_t = ob[ti % 2]

        def g_pre(n):
            cb = CB[n % 2]
            cs = slice(n * 64, (n + 1) * 64)
            gcn = V(bg.ap[:, n, 4:8].unsqueeze(2).to_broadcast([64, 4, 64]), bg.key)
            ngcn = V(tb["ngc"].ap[:, n, :].unsqueeze(2).to_broadcast([64, 4, 64]), tb["ngc"].key)
            E3 = cb["E"].re("p (h c) -> p h c", h=4)
            F3 = cb["F"].re("p (h c) -> p h c", h=4)
            P.tt(E3, K["id4"].re("p (h c) -> p h c", h=4), gcn, ALU.mult)
            P.tt(F3, K["neg4"].re("p (h c) -> p h c", h=4), ngcn, ALU.add)
            P.mm(psG[0:64, 0:256], ones[0:64, 0:64], cb["E"], start=True, stop=False)
            P.mm(psG[0:64, 0:256], ident[0:64, 0:64], cb["F"], start=False, stop=True)
            P.act(cb["G1"], psG[0:64, 0:256], AF.Exp)
            P.ts(cb["E"], cb["E"], -1.0, ALU.mult)
            P.tt(F3, K["neg4T"].re("p (h c) -> p h c", h=4), gcn, ALU.add)
            P.mm(psH[0:64, 0:256], ones[0:64, 0:64], cb["E"], start=True, stop=False)
            P.mm(psH[0:64, 0:256], ident[0:64, 0:64], cb["F"], start=False, stop=True)
            P.act(cb["G2"], psH[0:64, 0:256], AF.Exp)
            P.tt(cb["Gs"], cb["G1"], K["ms4"], ALU.mult, eng="pool")
            P.tt(cb["G2"], cb["G2"], K["ms4T"], ALU.mult, eng="pool")
            for h in range(4):
                hs = slice(h * 64, (h + 1) * 64)
                P.mm(psA[0:64, hs], tb["k"][:, h, cs].sub(h), tb["kb"][:, h, cs].sub(h))
                P.mm(psB[0:64, hs], tb["kb"][:, h, cs].sub(h), tb["k"][:, h, cs].sub(h))
                P.mm(psC[0:64, hs], tb["k"][:, h, cs].sub(h), tb["q"][:, h, cs].sub(h))
            P.stt(cb["B0"], psA[0:64, 0:256], -1.0, cb["Gs"], ALU.mult, ALU.mult)
            P.stt(cb["A0"], psB[0:64, 0:256], -1.0, cb["G2"], ALU.mult, ALU.mult)
            P.tt(cb["Ain"], psC[0:64, 0:256], cb["G1"], ALU.mult)
            return neumann_inv(P, C, cb["A0"], cb["B0"], cb, psA, psB, psC)

        def g_scan(n, TTm):
            cb = CB[n % 2]
            cs = slice(n * 64, (n + 1) * 64)
            for h in range(4):
                hs = slice(h * 64, (h + 1) * 64)
                P.mm(psD[0:64, hs], tb["k"][:, h, cs].sub(h), Sb[:, h, :])
            X3 = cb["X"].re("p (h v) -> p h v", h=4)
            c2n = V(tb["c2"].ap[:, n, :].unsqueeze(2).to_broadcast([64, 4, 64]), tb["c2"].key)
            P.tt(X3, psD[0:64, 0:256].re("p (h v) -> p h v", h=4), c2n, ALU.mult)
            P.tt(X3, X3, tb["bv"][:, n, :, :], ALU.add)
            for h in range(4):
                hs = slice(h * 64, (h + 1) * 64)
                P.mm(psE[0:64, hs], TTm[:, hs], cb["X"][:, hs])
            P.copy(cb["vn"], psE[0:64, 0:256], eng="act")
            for h in range(4):
                hs = slice(h * 64, (h + 1) * 64)
                P.mm(psF[0:64, hs], Sb[:, h, :], tb["qd"][:, h, cs].sub(h), start=True, stop=False)
                P.mm(psF[0:64, hs], cb["vn"][:, hs], cb["Ain"][:, hs], start=False, stop=True)
            P.copy(o_t[:, :, cs], psF[0:64, 0:256].re("p (h c) -> p h c", h=4), eng="act")
            for h in range(4):
                hs = slice(h * 64, (h + 1) * 64)
                P.mm(psG[0:64, hs], tb["ktm"][:, n, h, :], cb["vn"][:, hs])
            dln = V(tb["dl"].ap[:, :, n].unsqueeze(2).to_broadcast([64, 4, 64]), tb["dl"].key)
            P.tt(St, St, dln, ALU.mult)
            P.tt(St, St, psG[0:64, 0:256].re("p (h v) -> p h v", h=4), ALU.add)
            P.copy(Sb, St, eng="pool")

        tt_next = g_pre(0)
        for n in range(NCH):
            tt_cur = tt_next
            if n + 1 < NCH:
                tt_next = g_pre(n + 1)
            g_scan(n, tt_cur)
        for h in range(4):
            P.dma(gate, zT[G_GG + h, 0:64, tsl])
            act_sigmoid(P, rs, gate)
            P.tt(gate, gate, rs, ALU.mult)
            P.act(sq, o_t[:, h, :], AF.Square)
            P.mm(psH[0:64, 0:TT], ones[0:64, 0:64], sq)
            rstd_from_ps(P, rs, psH[0:64, 0:TT], 64.0, 1e-6)
            P.stt(rs, rs, cv[0:64, CV["gng"]:CV["gng"] + 1], gate, ALU.mult, ALU.mult)
            P.tt(sq, o_t[:, h, :], rs, ALU.mult)
            P.dma(oT[h * 64:(h + 1) * 64, tsl].sub(h), sq, q="pool")
    if release:
        P.release(mk)


def phase_rwkv(P, C, S, L, zT, oT, w_up_d, a_up_d, g_up_d, vfT, uT, v_down_d, v_up_d, ttl=512, psbase=None, release=True):
    mk = P.mark()
    TT = ttl
    cv = C.cv
    K = C.k
    nt = S // TT
    NCH = TT // 64
    ones = K["ones"]
    ident = K["ident"]
    col = lambda nm, h: cv[0:64, CV[nm] + h:CV[nm] + h + 1]
    w_up = P.tile([64, 256], name="r_wup"); P.dma(w_up, w_up_d)
    a_up = P.tile([64, 256], name="r_aup"); P.dma(a_up, a_up_d)
    g_up = P.tile([128, 256], name="r_gup"); P.dma(g_up, g_up_d)
    if L > 0:
        v_dn = P.tile([128, 8, 32], name="r_vdn"); P.dma(v_dn, v_down_d.re("(c p) n -> p c n", p=128))
        v_upt = P.tile([32, 256], name="r_vup"); P.dma(v_upt, v_up_d)
    ncv = P.tile([64, 12], name="r_ncv")
    P.ts(ncv[:, 0:4], cv[0:64, CV["w0"]:CV["w0"] + 4], -1.0, ALU.mult, writes=[ncv.sub(0)])
    P.ts(ncv[:, 4:8], cv[0:64, CV["a0"]:CV["a0"] + 4], -1.0, ALU.mult, writes=[ncv.sub(1)])
    P.ts(ncv[:, 8:12], cv[0:64, CV["vb"]:CV["vb"] + 4], -1.0, ALU.mult, writes=[ncv.sub(2)])
    oma = P.tile([64, 4], name="r_oma")
    P.ts(oma, cv[0:64, CV["ka"]:CV["ka"] + 4], -1.0, ALU.mult, 1.0, ALU.add)
    ST = P.tile([64, 4, 64], name="rST")
    P.memset(ST, 0.0)
    STb = P.tile([64, 4, 64], name="rSTb", dtype=BF16)
    P.copy(STb, ST, eng="pool")
    T4 = lambda nm: P.tile([64, 4, TT], name=nm)
    T4b = lambda nm: P.tile([64, 4, TT], name=nm, dtype=BF16)
    at, bt, kt, rt = T4b("r_at"), T4b("r_bt"), T4b("r_kt"), T4b("r_rt")
    bon, gate4 = T4("r_bon"), T4("r_gate")
    t0, t1, t2, t3, t4_, t5 = [T4(f"r_t{i}") for i in range(6)]
    y4 = t0
    bh_tm = P.tile([64, NCH, 4, 64], name="r_bhtm", dtype=BF16)
    kh_tm = P.tile([64, NCH, 4, 64], name="r_khtm", dtype=BF16)
    v_tm = P.tile([64, NCH, 4, 64], name="r_vtm", dtype=BF16)
    WC = P.tile([64, 4, NCH], name="r_WC")
    xin = [P.tile([128, TT + 1], name=f"r_xin{i}") for i in range(2)]
    dd = P.tile([128, TT], name="r_dd")
    lo_w = P.tile([64, TT], name="r_low")
    lo_a = P.tile([64, TT], name="r_loa")
    lo_g = P.tile([128, TT], name="r_log")
    sq = P.tile([64, TT], name="r_sq")
    rs = P.tile([64, TT], name="r_rs")
    if L > 0:
        uxb = [P.tile([128, TT + 1], name=f"r_ux{i}") for i in range(2)]
        xvb = [P.tile([128, TT], name=f"r_xv{i}") for i in range(2)]
        vl = P.tile([32, TT], name="r_vl")
        vf = rs

    def mkch(i):
        d_ = {nm: P.tile([64, 256], name=f"rc_{nm}{i}", dtype=BF16) for nm in ["A0", "B0", "A1", "B1", "TT", "Bak", "Brb", "Brk"]}
        d_["X"] = d_["A1"]
        d_["U"] = d_["B1"]
        return d_
    CB = [mkch(0), mkch(1)]
    if psbase is None:
        psA, psB, psC, psD, psE, psF, psG, psH = [P.pbank(i) for i in range(8)]
    else:
        psA, psB, psC, psD, psE, psF, psG, psH = [P.pbank(psbase + j % 4) for j in range(8)]
    xk = 0

    def shifted(g, rows, ti, dst):
        nonlocal xk
        x = xin[xk % 2]
        xk += 1
        if ti == 0:
            P.memset(x[0:rows, 0:1], 0.0, writes=[x.sub("halo")])
            P.dma(x[0:rows, 1:TT + 1].sub("body"), zT[g, 0:rows, 0:TT])
        else:
            P.dma(x[0:rows, :], zT[g, 0:rows, ti * TT - 1:(ti + 1) * TT])
        P.tt(dd[0:rows, :], x[0:rows, 0:TT], x[0:rows, 1:TT + 1], ALU.subtract)
        P.stt(dst, dd[0:rows, :], cv[0:rows, CV["mu"] + g:CV["mu"] + g + 1], x[0:rows, 1:TT + 1], ALU.mult, ALU.add)

    for ti in range(nt):
        tsl = slice(ti * TT, (ti + 1) * TT)
        r4, k4, v4, kk4, ic4, lw4 = t0, t1, t2, t3, t4_, t5
        shifted(G_WLO, 64, ti, lo_w)
        act_sigmoid(P, lo_w, lo_w, scale=2.0)
        P.ts(lo_w, lo_w, 2.0, ALU.mult, -1.0, ALU.add)
        shifted(G_ALO, 64, ti, lo_a)
        shifted(G_GLO, 128, ti, lo_g)
        act_sigmoid(P, lo_g, lo_g)
        if L > 0:
            uv = uT.re("(c p) t -> p c t", p=128)
            for c in range(8):
                ux = uxb[c % 2]
                xv = xvb[c % 2]
                if ti == 0:
                    P.memset(ux[:, 0:1], 0.0, writes=[ux.sub("halo")])
                    P.dma(ux[:, 1:TT + 1].sub("body"), uv[:, c, 0:TT])
                else:
                    P.dma(ux, uv[:, c, ti * TT - 1:(ti + 1) * TT])
                P.tt(xv, ux[:, 0:TT], ux[:, 1:TT + 1], ALU.subtract)
                P.stt(xv, xv, cv[:, CV["vmu"] + c:CV["vmu"] + c + 1], ux[:, 1:TT + 1], ALU.mult, ALU.add)
                P.mm(psH[0:32, 0:TT], v_dn[:, c, :], xv, start=(c == 0), stop=(c == 7))
            P.copy(vl, psH[0:32, 0:TT], eng="act")
        for h in range(4):
            hs = slice(h * 64, (h + 1) * 64)
            shifted(G_R + h, 64, ti, r4[:, h, :].sub(h))
            shifted(G_K + h, 64, ti, k4[:, h, :].sub(h))
            shifted(G_V + h, 64, ti, v4[:, h, :].sub(h))
            P.mm(psA[0:64, 0:TT], w_up[:, hs], lo_w)
            act_sigmoid(P, lw4[:, h, :].sub(h), psA[0:64, 0:TT], negbias=ncv[:, h:h + 1])
            P.mm(psB[0:64, 0:TT], a_up[:, hs], lo_a)
            act_sigmoid(P, ic4[:, h, :].sub(h), psB[0:64, 0:TT], negbias=ncv[:, 4 + h:5 + h])
            P.mm(psC[0:64, 0:TT], g_up[:, hs], lo_g)
            P.copy(gate4[:, h, :].sub(h), psC[0:64, 0:TT], eng="act")
            if L == 0:
                P.dma(vfT[h * 64:(h + 1) * 64, tsl].sub(h), v4[:, h, :].sub(h), q="pool")
            else:
                P.dma(vf, vfT[h * 64:(h + 1) * 64, tsl])
                P.mm(psD[0:64, 0:TT], v_upt[:, hs], vl)
                act_sigmoid(P, sq, psD[0:64, 0:TT], negbias=ncv[:, 8 + h:9 + h])
                P.tt(vf, vf, v4[:, h, :].sub(h), ALU.subtract)
                P.tt(vf, vf, sq, ALU.mult)
                P.tt(v4[:, h, :].sub(h), v4[:, h, :].sub(h), vf, ALU.add)
            P.ts(kk4[:, h, :].sub(h), k4[:, h, :].sub(h), col("kk", h), ALU.mult)
            P.act(sq, kk4[:, h, :].sub(h), AF.Square)
            P.mm(psE[0:64, 0:TT], ones[0:64, 0:64], sq)
            rstd_from_ps(P, rs, psE[0:64, 0:TT], 1.0, 1e-12)
            P.tt(kk4[:, h, :].sub(h), kk4[:, h, :].sub(h), rs, ALU.mult)
            P.ts(sq, ic4[:, h, :].sub(h), col("ka", h), ALU.mult, oma[:, h:h + 1], ALU.add)
            P.tt(k4[:, h, :].sub(h), k4[:, h, :].sub(h), sq, ALU.mult)
            P.stt(sq, r4[:, h, :].sub(h), col("rk", h), k4[:, h, :].sub(h), ALU.mult, ALU.mult)
            P.mm(psF[0:64, 0:TT], ones[0:64, 0:64], sq)
            P.tt(bon[:, h, :].sub(h), psF[0:64, 0:TT], v4[:, h, :].sub(h), ALU.mult)
        P.ts(lw4, lw4, float(-np.exp(-0.5)), ALU.mult)
        for n in range(NCH):
            cs = slice(n * 64, (n + 1) * 64)
            for h in range(4):
                P.transpose(psG[0:64, h * 64:(h + 1) * 64], v4[:, h, cs], ident[0:64, 0:64])
            P.copy(v_tm[:, n, :, :].re("p h d -> p (h d)"), psG[0:64, 0:256], eng="act")
        P.tt(ic4, ic4, kk4, ALU.mult)
        cb_ = [lw4, v4]
        cur = 0
        for sh in (1, 2, 4, 8, 16, 32):
            src = cb_[cur].re("p h (n c) -> p (h n) c", c=64)
            dstt = cb_[1 - cur].re("p h (n c) -> p (h n) c", c=64)
            P.copy(dstt[:, :, 0:sh], src[:, :, 0:sh], eng="pool", writes=[cb_[1 - cur].sub("a")])
            P.tt(dstt[:, :, sh:64], src[:, :, sh:64], src[:, :, 0:64 - sh], ALU.add, writes=[cb_[1 - cur].sub("b")])
            cur = 1 - cur
        assert cur == 0
        cl = lw4
        cl3 = cl.re("p h (n c) -> p (h n) c", c=64)
        e = v4
        e3 = e.re("p h (n c) -> p (h n) c", c=64)
        P.act(e, cl, AF.Exp)
        P.tt(rt, r4, e, ALU.mult)
        P.memset(at.re("p h (n c) -> p (h n) c", c=64)[:, :, 0:1], 1.0, writes=[at.sub("a")])
        P.copy(at.re("p h (n c) -> p (h n) c", c=64)[:, :, 1:64], e3[:, :, 0:63], eng="pool", writes=[at.sub("b")])
        P.stt(at, at, -1.0, kk4, ALU.mult, ALU.mult)
        P.act(e, cl, AF.Exp, scale=-1.0)
        P.tt(bt, ic4, e, ALU.mult)
        P.tt(kt, k4, e, ALU.mult)
        cl4 = cl.re("p h (n c) -> p h n c", c=64)
        P.copy(WC, cl4[:, :, :, 63])
        P.tt(e.re("p h (n c) -> p h n c", c=64), V(WC.ap.unsqueeze(3).to_broadcast([64, 4, NCH, 64]), WC.key), cl4, ALU.subtract)
        P.act(e, e, AF.Exp)
        P.act(WC, WC, AF.Exp)
        P.tt(ic4, ic4, e, ALU.mult)
        P.tt(k4, k4, e, ALU.mult)
        for n in range(NCH):
            cs = slice(n * 64, (n + 1) * 64)
            for h in range(4):
                P.transpose(psG[0:64, h * 64:(h + 1) * 64], ic4[:, h, cs], ident[0:64, 0:64])
            P.copy(bh_tm[:, n, :, :].re("p h d -> p (h d)"), psG[0:64, 0:256], eng="act")
            for h in range(4):
                P.transpose(psH[0:64, h * 64:(h + 1) * 64], k4[:, h, cs], ident[0:64, 0:64])
            P.copy(kh_tm[:, n, :, :].re("p h d -> p (h d)"), psH[0:64, 0:256], eng="dve")
        def r_pre(n):
            cb = CB[n % 2]
            cs = slice(n * 64, (n + 1) * 64)
            for h in range(4):
                hs = slice(h * 64, (h + 1) * 64)
                P.mm(psA[0:64, hs], bt[:, h, cs], at[:, h, cs])
                P.mm(psB[0:64, hs], at[:, h, cs], bt[:, h, cs])
                P.mm(psC[0:64, hs], kt[:, h, cs], at[:, h, cs])
                P.mm(psD[0:64, hs], bt[:, h, cs], rt[:, h, cs])
            P.tt(cb["B0"], psA[0:64, 0:256], K["ms4"], ALU.mult)
            P.tt(cb["A0"], psB[0:64, 0:256], K["ms4T"], ALU.mult)
            P.tt(cb["Bak"], psC[0:64, 0:256], K["ms4"], ALU.mult)
            P.tt(cb["Brb"], psD[0:64, 0:256], K["mi4"], ALU.mult)
            for h in range(4):
                hs = slice(h * 64, (h + 1) * 64)
                P.mm(psE[0:64, hs], kt[:, h, cs], rt[:, h, cs])
            P.tt(cb["Brk"], psE[0:64, 0:256], K["mi4"], ALU.mult)
            return neumann_inv(P, C, cb["A0"], cb["B0"], cb, psA, psB, psC)

        def r_scan(n, TTm):
            cb = CB[n % 2]
            cs = slice(n * 64, (n + 1) * 64)
            for h in range(4):
                hs = slice(h * 64, (h + 1) * 64)
                P.mm(psD[0:64, hs], at[:, h, cs], STb[:, h, :], start=True, stop=False)
                P.mm(psD[0:64, hs], cb["Bak"][:, hs], v_tm[:, n, h, :], start=False, stop=True)
            P.copy(cb["X"], psD[0:64, 0:256], eng="act")
            for h in range(4):
                hs = slice(h * 64, (h + 1) * 64)
                P.mm(psE[0:64, hs], TTm[:, hs], cb["X"][:, hs])
            P.copy(cb["U"], psE[0:64, 0:256], eng="act")
            for h in range(4):
                hs = slice(h * 64, (h + 1) * 64)
                P.mm(psF[0:64, hs], STb[:, h, :], rt[:, h, cs], start=True, stop=False)
                P.mm(psF[0:64, hs], cb["U"][:, hs], cb["Brb"][:, hs], start=False, stop=False)
                P.mm(psF[0:64, hs], v_tm[:, n, h, :], cb["Brk"][:, hs], start=False, stop=True)
            P.copy(y4[:, :, cs], psF[0:64, 0:256].re("p (h c) -> p h c", h=4), eng="act")
            for h in range(4):
                hs = slice(h * 64, (h + 1) * 64)
                P.mm(psG[0:64, hs], bh_tm[:, n, h, :], cb["U"][:, hs], start=True, stop=False)
                P.mm(psG[0:64, hs], kh_tm[:, n, h, :], v_tm[:, n, h, :], start=False, stop=True)
            wcn = V(WC.ap[:, :, n].unsqueeze(2).to_broadcast([64, 4, 64]), WC.key)
            P.tt(ST, ST, wcn, ALU.mult)
            P.tt(ST, ST, psG[0:64, 0:256].re("p (h v) -> p h v", h=4), ALU.add)
            P.copy(STb, ST, eng="pool")

        tt_next = r_pre(0)
        for n in range(NCH):
            tt_cur = tt_next
            if n + 1 < NCH:
                tt_next = r_pre(n + 1)
            r_scan(n, tt_cur)
        for h in range(4):
            yh = y4[:, h, :]
            P.mm(psH[0:64, 0:TT], ones[0:64, 0:64], yh)
            P.stt(yh, psH[0:64, 0:TT], float(-1.0 / 64), yh, ALU.mult, ALU.add)
            P.act(sq, yh, AF.Square)
            P.mm(psH[0:64, 0:TT], ones[0:64, 0:64], sq)
            rstd_from_ps(P, rs, psH[0:64, 0:TT], 64.0, 64e-5)
            P.stt(yh, yh, col("lng", h), rs, ALU.mult, ALU.mult)
            P.stt(yh, yh, col("lnb", h), bon[:, h, :].sub(h), ALU.add, ALU.add)
            P.tt(sq, yh, gate4[:, h, :].sub(h), ALU.mult)
            P.dma(oT[h * 64:(h + 1) * 64, tsl].sub(h), sq, q="pool")
    if release:
        P.release(mk)


def phase_merge(P, C, NT, hT, oT, gT, wbr_d, wout_d, h1T, dyn=None):
    mk = P.mark()
    nt = NT // TT
    wstg = [P.tile([128, 4, 1024], name=f"f_wstg{i}") for i in range(2)]
    wbr = []
    for br in range(3):
        t = P.tile([128, 4, 1024], name=f"wbr{br}", dtype=BF16)
        P.dma(wstg[br % 2], wbr_d[br].re("(c p) n -> p c n", p=128))
        P.copy(t, wstg[br % 2], eng=("pool" if br % 2 == 0 else "act"))
        wbr.append(t)
    wout = P.tile([128, 8, 1024], name="wout", dtype=BF16)
    wov = wout_d.re("(c p) n -> p c n", p=128)
    for hh in range(2):
        P.dma(wstg[(hh + 1) % 2], wov[:, hh * 4:(hh + 1) * 4, :])
        P.copy(wout[:, hh * 4:(hh + 1) * 4, :].sub(hh), wstg[(hh + 1) % 2], eng=("act" if hh == 0 else "pool"))
    h = P.tile([128, 8, TT], name="f_h")
    o = P.tile([128, 12, TT], name="f_o")
    ob_ = P.tile([128, 12, TT], name="f_ob", dtype=BF16)
    mg = P.tile([128, 8, TT], name="f_mg32") if dyn is not None else None
    mgb = P.tile([128, 8, TT], name="f_mg", dtype=BF16)
    o2 = P.tile([128, 6, TT], name="f_o2") if dyn is not None else None
    g3 = [P.tile([128, 3, TT], name=f"f_g3{i}") for i in range(2)]
    tmp = [P.tile([128, TT], name=f"f_tmp{i}") for i in range(2)]
    tmp2 = [P.tile([128, TT], name=f"f_tmpb{i}") for i in range(2)]
    ps = [P.pbank(i) for i in range(8)]
    hv = hT.re("(c p) t -> p c t", p=128)
    ov = oT.re("(c p) t -> p c t", p=128)
    gv = gT.re("(b c p) t -> p b c t", b=3, p=128)
    h1v = h1T.re("(c p) t -> p c t", p=128)
    for ti in range(nt):
        tsl = slice(ti * TT, (ti + 1) * TT)
        if dyn is not None:
            m0 = C.cv[:, CV["m0"]:CV["m0"] + 1]
            m1 = C.cv[:, CV["m1"]:CV["m1"] + 1]
            tsl2 = slice(dyn + ti * TT, dyn + (ti + 1) * TT)
            P.dma(h, hv[:, :, tsl])
            P.dma(mg, hv[:, :, tsl2])
            P.ts(h, h, m0, ALU.mult)
            P.stt(h, mg, m1, h, ALU.mult, ALU.add)
            for part in range(2):
                cs_ = slice(part * 6, (part + 1) * 6)
                P.dma(o[:, cs_, :].sub(part), ov[:, cs_, tsl])
                P.dma(o2, ov[:, cs_, tsl2])
                P.ts(o[:, cs_, :].sub(part), o[:, cs_, :].sub(part), m0, ALU.mult)
                P.stt(o[:, cs_, :].sub(part), o2, m1, o[:, cs_, :].sub(part), ALU.mult, ALU.add)
        else:
            P.dma(h, hv[:, :, tsl])
            P.dma(o, ov[:, :, tsl])
        P.copy(ob_[:, 0:6, :].sub(0), o[:, 0:6, :], eng="dve")
        P.copy(ob_[:, 6:12, :].sub(1), o[:, 6:12, :], eng="act")
        for n in range(8):
            ns = slice(n * 128, (n + 1) * 128)
            g = g3[n % 2]
            P.dma(g, gv[:, :, n, tsl])
            for br in range(3):
                pp = ps[(n % 2) * 3 + br]
                for k in range(4):
                    P.mm(pp, wbr[br][:, k, ns], ob_[:, br * 4 + k, :], start=(k == 0), stop=(k == 3))
            tm_ = tmp[n % 2]
            P.tt(tm_, ps[(n % 2) * 3 + 0], g[:, 0, :], ALU.mult)
            P.tt(tmp2[n % 2], ps[(n % 2) * 3 + 1], g[:, 1, :], ALU.mult)
            P.tt(tm_, tm_, tmp2[n % 2], ALU.add, eng="pool")
            P.tt(tmp2[n % 2], ps[(n % 2) * 3 + 2], g[:, 2, :], ALU.mult)
            P.tt(mgb[:, n, :].sub(n), tm_, tmp2[n % 2], ALU.add, eng="pool")
        for n in range(8):
            ns = slice(n * 128, (n + 1) * 128)
            pp = ps[6 + n % 2]
            for k in range(8):
                P.mm(pp, wout[:, k, ns], mgb[:, k, :], start=(k == 0), stop=(k == 7))
            P.tt(h[:, n, :].sub(n), h[:, n, :].sub(n), pp, ALU.add)
        P.dma(h1v[:, :, tsl], h, q="pool")
    P.release(mk)


def phase_ffn(P, C, NT, h1T, h2T, gcol, experts, FF, router_d=None):
    mk = P.mark()
    cv = C.cv
    K = C.k
    ones = K["ones"]
    ident = K["ident"]
    nt = NT // TT
    NF = FF // 128
    CB = 512
    blocks = [(c0, min(CB, FF - c0)) for c0 in range(0, FF, CB)]
    h = P.tile([128, 8, TT], name="m_h")
    u = P.tile([128, 8, TT], name="m_u", dtype=BF16)
    rs = P.tile([128, TT], name="m_rs")
    hid_raw = P.tile([128, NF * TT // 2], name="m_hid")
    hid = V(hid_raw.ap.bitcast(BF16).rearrange("p (f t) -> p f t", f=NF), hid_raw.key)
    u32 = V(hid_raw.ap[:, 0:8 * TT].rearrange("p (c t) -> p c t", c=8), hid_raw.key)
    sg = [P.tile([128, TT], name=f"m_sg{i}") for i in range(2)]
    wgb = [P.tile([128, 8, CB], name=f"m_wg{i}") for i in range(2)]
    wub = [P.tile([128, 8, CB], name=f"m_wu{i}") for i in range(2)]
    wgc = [P.tile([128, 8, CB], name=f"m_wgc{i}", dtype=BF16) for i in range(2)]
    wuc = [P.tile([128, 8, CB], name=f"m_wuc{i}", dtype=BF16) for i in range(2)]
    wdb = [P.tile([128, 512], name=f"m_wd{i}") for i in range(3)]
    wdc = [P.tile([128, 512], name=f"m_wdc{i}", dtype=BF16) for i in range(3)]
    ps = [P.pbank(i) for i in range(8)]
    hv = h1T.re("(c p) t -> p c t", p=128)
    h2v = h2T.re("(c p) t -> p c t", p=128)
    ne = len(experts)
    if router_d is not None:
        sel8 = P.tile([8, 1024], name="c_sel8")
        P.dma(sel8, C.sel8_d)
        rt_w = P.tile([128, 8, 8], name="m_rw")
        P.dma(rt_w, router_d.re("(c p) e -> p c e", p=128))
        lg = P.tile([8, TT], name="m_lg")
        ltm = P.tile([128, 4, 8], name="m_ltm")
        l2 = P.tile([128, 4, 8], name="m_l2")
        eq1 = P.tile([128, 4, 8], name="m_eq1")
        eq2 = P.tile([128, 4, 8], name="m_eq2")
        m1 = P.tile([128, 4], name="m_m1")
        m2 = P.tile([128, 4], name="m_m2")
        w1 = P.tile([128, 4], name="m_w1")
        w2 = P.tile([128, 4], name="m_w2")
        gwf = P.tile([8, TT], name="m_gwf")
        gwe = [P.tile([128, TT], name=f"m_gwe{i}") for i in range(2)]
    wk = 0
    dk = 0
    for ti in range(nt):
        tsl = slice(ti * TT, (ti + 1) * TT)
        P.dma(h, hv[:, :, tsl])
        P.act(u32, h, AF.Square)
        for c in range(8):
            P.mm(ps[7], ones, u32[:, c, :], start=(c == 0), stop=(c == 7))
        rstd_from_ps(P, rs, ps[7], D, 1e-6)
        if router_d is not None:
            for c in range(8):
                P.stt(u32[:, c, :], h[:, c, :], cv[:, gcol + c:gcol + c + 1], rs, ALU.mult, ALU.mult)
            P.copy(u, u32, eng="act")
            for c in range(8):
                P.mm(ps[6][0:8, :], rt_w[:, c, :], u32[:, c, :], start=(c == 0), stop=(c == 7))
        else:
            for c in range(8):
                P.stt(u[:, c, :], h[:, c, :], cv[:, gcol + c:gcol + c + 1], rs, ALU.mult, ALU.mult)
        if router_d is not None:
            P.copy(lg, ps[6][0:8, :], eng="act")
            for j in range(4):
                P.transpose(ps[5][:, j * 8:(j + 1) * 8], lg[:, j * 128:(j + 1) * 128], ident[0:8, 0:8])
            P.copy(ltm.re("p j e -> p (j e)"), ps[5][:, 0:32])
            bc = lambda t: V(t.ap.unsqueeze(2).to_broadcast([128, 4, 8]), t.key)
            P.op("dve", lambda e: e.reduce_max(_ap(m1), _ap(ltm), AX.X), [ltm], [m1])
            P.tt(eq1, ltm, bc(m1), ALU.is_equal)
            P.stt(l2, eq1, -1e30, ltm, ALU.mult, ALU.add)
            P.op("dve", lambda e: e.reduce_max(_ap(m2), _ap(l2), AX.X), [l2], [m2])
            P.tt(eq2, l2, bc(m2), ALU.is_equal)
            P.tt(w2, m2, m1, ALU.subtract)
            P.act(w2, w2, AF.Exp)
            P.ts(w1, w2, 1.0, ALU.add)
            P.recip(w1, w1)
            P.tt(w2, w2, w1, ALU.mult)
            P.tt(eq1, eq1, bc(w1), ALU.mult)
            P.tt(eq2, eq2, bc(w2), ALU.mult)
            P.tt(eq1, eq1, eq2, ALU.add)
            for j in range(4):
                P.transpose(ps[5][0:8, j * 128:(j + 1) * 128], eq1[:, j, :], ident)
            P.copy(gwf, ps[5][0:8, :], eng="act")
        work = [(e_, bi) for e_ in range(ne) for bi in range(len(blocks))]

        def prefetch(e_, bi, slot):
            wg_d, wu_d, _ = experts[e_]
            c0_, wdt = blocks[bi]
            wgv = wg_d.re("(c p) f -> p c f", p=128)
            wuv = wu_d.re("(c p) f -> p c f", p=128)
            P.dma(wgb[slot][:, :, 0:wdt], wgv[:, :, c0_:c0_ + wdt])
            P.dma(wub[slot][:, :, 0:wdt], wuv[:, :, c0_:c0_ + wdt], q="act")
            P.copy(wgc[slot][:, :, 0:wdt], wgb[slot][:, :, 0:wdt], eng="dve")
            P.copy(wuc[slot][:, :, 0:wdt], wub[slot][:, :, 0:wdt], eng="act")
        prefetch(work[0][0], work[0][1], wk % 2)
        for wi, (e_, bi) in enumerate(work):
            slot = wk % 2
            wk += 1
            if wi + 1 < len(work):
                prefetch(work[wi + 1][0], work[wi + 1][1], wk % 2)
            c0_, wdt = blocks[bi]
            wg_t, wu_t = wgc[slot], wuc[slot]
            if router_d is not None and bi == 0:
                gw_e = gwe[e_ % 2]
                P.mm(ps[3], sel8[:, e_ * 128:(e_ + 1) * 128], gwf)
                P.copy(gw_e, ps[3], eng="act")
            for j in range(wdt // 128):
                f = c0_ // 128 + j
                pg = ps[4 + f % 2]
                pu = ps[6 + f % 2]
                for c in range(8):
                    P.mm(pg, wg_t[:, c, j * 128:(j + 1) * 128], u[:, c, :], start=(c == 0), stop=(c == 7))
                for c in range(8):
                    P.mm(pu, wu_t[:, c, j * 128:(j + 1) * 128], u[:, c, :], start=(c == 0), stop=(c == 7))
                s_ = sg[f % 2]
                P.act(s_, pg, AF.Silu)
                if router_d is not None:
                    P.tt(s_, s_, gw_e, ALU.mult, eng="pool")
                P.tt(hid[:, f, :].sub(f), s_, pu, ALU.mult)
            if bi == len(blocks) - 1:
                wdv = experts[e_][2].re("(f p) n -> p f n", p=128)
                for half in range(2):
                    for f in range(NF):
                        wd_s = wdb[dk % 3]
                        wd_t = wdc[dk % 3]
                        dk += 1
                        P.dma(wd_s, wdv[:, f, half * 512:(half + 1) * 512])
                        P.copy(wd_t, wd_s, eng=("dve" if dk % 2 == 0 else "act"))
                        for n4 in range(4):
                            P.mm(ps[n4], wd_t[:, n4 * 128:(n4 + 1) * 128], hid[:, f, :].sub(f), start=(f == 0), stop=(f == NF - 1))
                    for n4 in range(4):
                        n = half * 4 + n4
                        P.tt(h[:, n, :].sub(n), h[:, n, :].sub(n), ps[n4], ALU.add)
        P.dma(h2v[:, :, tsl], h, q="pool")
    P.release(mk)


def phase_ple(P, C, NT, h2T, pT, proj_d, pgate_d, gcol, h3T):
    mk = P.mark()
    cv = C.cv
    ones = C.k["ones"]
    nt = NT // TT
    pstg = [P.tile([128, 4, 1024], name=f"p_stg{i}") for i in range(2)]
    proj = P.tile([128, 2, 1024], name="p_proj", dtype=BF16)
    P.dma(pstg[0][:, 0:2, :], proj_d.re("(c p) n -> p c n", p=128))
    P.copy(proj, pstg[0][:, 0:2, :], eng="pool")
    pg = P.tile([128, 8, 1024], name="p_gate", dtype=BF16)
    pgv = pgate_d.re("(c p) n -> p c n", p=128)
    for hh in range(2):
        P.dma(pstg[(hh + 1) % 2], pgv[:, hh * 4:(hh + 1) * 4, :])
        P.copy(pg[:, hh * 4:(hh + 1) * 4, :].sub(hh), pstg[(hh + 1) % 2], eng=("act" if hh == 0 else "pool"))
    h = P.tile([128, 8, TT], name="p_h")
    hb_ = P.tile([128, 8, TT], name="p_hb", dtype=BF16)
    pt = P.tile([128, 2, TT], name="p_p")
    ptb = P.tile([128, 2, TT], name="p_pb", dtype=BF16)
    er = P.tile([128, 8, TT], name="p_er")
    ho = P.tile([128, 8, TT], name="p_ho")
    sq = P.tile([128, TT], name="p_sq")
    rs = P.tile([128, TT], name="p_rs")
    gp = [P.tile([128, TT], name=f"p_gp{i}") for i in range(2)]
    ps = [P.pbank(i) for i in range(8)]
    hv = h2T.re("(c p) t -> p c t", p=128)
    pv = pT.re("(c p) t -> p c t", p=128)
    h3v = h3T.re("(c p) t -> p c t", p=128)
    for ti in range(nt):
        tsl = slice(ti * TT, (ti + 1) * TT)
        P.dma(h, hv[:, :, tsl])
        P.dma(pt, pv[:, :, tsl])
        P.copy(ptb, pt, eng="dve")
        P.copy(hb_, h, eng="act")
        for n in range(8):
            ns = slice(n * 128, (n + 1) * 128)
            pp = ps[n % 2]
            P.mm(pp, proj[:, 0, ns], ptb[:, 0, :], start=True, stop=False)
            P.mm(pp, proj[:, 1, ns], ptb[:, 1, :], start=False, stop=True)
            P.copy(er[:, n, :].sub(n), pp, eng="act")
            P.act(sq, pp, AF.Square)
            P.mm(ps[2], ones, sq, start=(n == 0), stop=(n == 7))
        rstd_from_ps(P, rs, ps[2], D, 1e-6)
        for n in range(8):
            ns = slice(n * 128, (n + 1) * 128)
            pp = ps[3 + n % 2]
            for k in range(8):
                P.mm(pp, pg[:, k, ns], hb_[:, k, :], start=(k == 0), stop=(k == 7))
            g = gp[n % 2]
            P.act(g, pp, AF.Sigmoid)
            P.stt(er[:, n, :].sub(n), er[:, n, :].sub(n), cv[:, gcol + n:gcol + n + 1], rs, ALU.mult, ALU.mult)
            P.tt(g, g, er[:, n, :].sub(n), ALU.mult)
            P.tt(ho[:, n, :].sub(n), h[:, n, :], g, ALU.add)
        P.dma(h3v[:, :, tsl], ho, q="pool")
    P.release(mk)


def own(hg, width=64):
    return slice(hg * 4 * width, (hg + 1) * 4 * width)


def col4(v):
    return np.ascontiguousarray(v.reshape(4, 64).T)


def col2(v):
    return np.ascontiguousarray(v.reshape(2, 128).T)


def mixer_host_inputs(inp, L, b, hg):
    f = np.float32
    w_in = inp["w_in"][L]
    o = own(hg)
    cols = np.concatenate([
        np.arange(0, 512)[o], np.arange(512, 1024)[o], np.arange(1024, 1536)[o],
        np.arange(1536, 1600), np.arange(1600, 1664), np.arange(1664, 1792),
        np.arange(1792, 2048), np.arange(2048, 2176), np.arange(2176, 2208),
        np.arange(2208, 2720)[o], np.arange(2720, 3232)[o], np.arange(3232, 3744)[o],
        np.arange(3760, 4272)[o],
        np.arange(3744, 3752)[hg * 4:(hg + 1) * 4], np.arange(3752, 3760)[hg * 4:(hg + 1) * 4]])
    assert len(cols) == NZ
    d = {}
    d["w_in_m"] = np.ascontiguousarray(w_in[:, cols])
    cv = np.zeros((128, NCV), f)

    def put(nm, arr):
        arr = np.asarray(arr, f)
        cv[:arr.shape[0], CV[nm]:CV[nm] + arr.shape[1]] = arr
    put("nmg", inp["norm_mix_g"][L].reshape(8, 128).T)
    mu = inp["rwkv_mu"][L]
    mucols = np.zeros((128, 15), f)
    rcols = cols[:1024]
    for g in range(15):
        seg = mu[rcols[ZOFF[g]:ZOFF[g + 1]]]
        mucols[:len(seg), g] = seg
    put("mu", mucols)
    put("w0", col4(inp["rwkv_w0"][L][o]))
    put("a0", col4(inp["rwkv_a0"][L][o]))
    put("kk", col4(inp["rwkv_k_k"][L][o]))
    put("ka", col4(inp["rwkv_k_a"][L][o]))
    put("rk", col4(inp["rwkv_r_k"][L].reshape(512)[o]))
    put("lng", col4(inp["rwkv_ln_g"][L][o]))
    put("lnb", col4(inp["rwkv_ln_b"][L][o]))
    if L > 0:
        put("vmu", inp["vres_mu"][L - 1].reshape(8, 128).T)
        put("vb", col4(inp["vres_b"][L - 1][o]))
    put("qng", inp["mla_q_norm_g"][L].reshape(2, 128).T)
    put("kvng", inp["mla_kv_norm_g"][L].reshape(128, 1))
    put("qkq", inp["mla_qk_norm_q"][L].reshape(96, 1))
    put("qkk", inp["mla_qk_norm_k"][L].reshape(96, 1))
    invf = (1.0 / (10000.0 ** (np.arange(0, 32, 2, dtype=f) / f(32)))).astype(f)
    iv = np.zeros((96, 1), f)
    iv[64:80, 0] = invf
    iv[80:96, 0] = invf
    put("invf", iv)
    put("ropec", np.full((128, 1), -np.pi, f))
    cw = inp["gdn_conv_w"][L]
    convc = np.zeros((128, 48), f)
    for seg in range(3):
        cc = cw[:, seg * 512:(seg + 1) * 512][:, o]
        for hh in range(4):
            for j in range(4):
                convc[:64, (seg * 4 + hh) * 4 + j] = cc[j, hh * 64:(hh + 1) * 64]
    put("conv", convc)
    put("alog", inp["gdn_a_log"][L][hg * 4:(hg + 1) * 4].reshape(4, 1))
    put("dtb", inp["gdn_dt_bias"][L][hg * 4:(hg + 1) * 4].reshape(4, 1))
    put("gng", inp["gdn_norm_g"][L].reshape(64, 1))
    d["cv"] = cv
    d["w_up"] = np.ascontiguousarray(inp["rwkv_w_up"][L][:, o])
    d["a_up"] = np.ascontiguousarray(inp["rwkv_a_up"][L][:, o])
    d["g_up"] = np.ascontiguousarray(inp["rwkv_g_up"][L][:, o])
    if L > 0:
        d["v_down"] = np.ascontiguousarray(inp["vres_down"][L - 1])
        d["v_up"] = np.ascontiguousarray(inp["vres_up"][L - 1][:, o])
    d["w_uq"] = np.ascontiguousarray(inp["mla_w_uq"][L][:, hg * 384:(hg + 1) * 384])
    ukv = inp["mla_w_ukv"][L].reshape(128, 8, 128)[:, hg * 4:(hg + 1) * 4, :]
    d["w_uk"] = np.ascontiguousarray(ukv[:, :, :64].reshape(128, 256))
    d["w_uv"] = np.ascontiguousarray(ukv[:, :, 64:].reshape(128, 256))
    d["pos"] = np.ascontiguousarray(inp["positions"][b:b + 1].astype(np.int32))
    for k_, v_ in consts_np().items():
        d["c_" + k_] = v_
    return d
from concourse.bass_utils import run_bass_kernel_spmd

B_, S_, NCORE = 4, 4096, 8
M_CONSTS = ["ident", "ones", "att_mask", "ropeRT", "sel4", "id4", "neg4", "neg4T", "ms4", "ms4T", "mi4"]
F_CONSTS = ["ident", "ones"]
_PROG_CACHE = {}


def build_mixer(S, L):
    P = Prog()
    C = Ctx()
    names = []

    def din(name, shape, dt=F32):
        names.append(name)
        return P.dview(P.dram(name, shape, dt, kind="ExternalInput"))
    hT = din("hT", [1024, S])
    w_d = din("w_in_m", [1024, NZ])
    cv_d = din("cv", [128, NCV])
    pos_d = din("pos", [1, S], I32)
    w_uq, w_uk, w_uv = din("w_uq", [256, 384]), din("w_uk", [128, 256]), din("w_uv", [128, 256])
    w_up, a_up, g_up = din("w_up", [64, 256]), din("a_up", [64, 256]), din("g_up", [128, 256])
    zT = P.dview(P.dram("zT", [NG, 128, S], F32, kind="Internal"))
    oT = P.dview(P.dram("oT", [768, S], F32, kind="ExternalOutput"))
    uT = v_down = v_up = None
    if L == 0:
        vfT = P.dview(P.dram("vfT_out", [256, S], F32, kind="ExternalOutput"))
    else:
        vfT = din("vfT_in", [256, S])
        uT = P.dview(P.dram("uT", [1024, S], F32, kind="Internal"))
        v_down, v_up = din("v_down", [1024, 32]), din("v_up", [32, 256])
    C.k = load_consts(P, M_CONSTS)
    names.extend(["c_" + c for c in M_CONSTS])
    C.cv = P.tile([128, NCV], name="cv")
    P.dma(C.cv, cv_d)
    groups = [(int(ZOFF[g]), int(ZG[g])) for g in range(NG)]
    phase_proj(P, C, S, hT, w_d, NZ, groups, zT, CV["nmg"], uT=uT)
    phase_rwkv(P, C, S, L, zT, V(oT.ap[0:256, :], "oT_r"), w_up, a_up, g_up, vfT, uT, v_down, v_up)
    phase_mla(P, C, S, zT, pos_d, w_uq, w_uk, w_uv, V(oT.ap[256:512, :], "oT_m"))
    phase_gdn(P, C, S, zT, V(oT.ap[512:768, :], "oT_g"))
    P.finalize()
    return P.nc, names


def build_token(NT, L):
    P = Prog()
    C = Ctx()
    names = []

    def din(name, shape, dt=F32):
        names.append(name)
        return P.dview(P.dram(name, shape, dt, kind="ExternalInput"))
    hT = din("hT", [1024, NT])
    oT = din("oT_all", [1536, NT])
    pT = din("pT", [256, NT])
    cv_d = din("cv", [128, NCV])
    w_g = din("w_gate", [1024, 3072])
    wbr = [din(f"w_br{i}", [512, 1024]) for i in range(3)]
    wout = din("w_out", [1024, 1024])
    proj = din("ple_proj", [256, 1024])
    pgate = din("ple_gate", [1024, 1024])
    if L % 2 == 0:
        experts = [(din("ffn_wg", [1024, 2816]), din("ffn_wu", [1024, 2816]), din("ffn_wd", [2816, 1024]))]
        FF = 2816
        router = None
    else:
        wg_all = din("moe_wg", [8, 1024, 3584])
        wu_all = din("moe_wu", [8, 1024, 3584])
        wd_all = din("moe_wd", [8, 3584, 1024])
        experts = [(V(wg_all.ap[e], wg_all.key), V(wu_all.ap[e], wu_all.key), V(wd_all.ap[e], wd_all.key)) for e in range(8)]
        FF = 3584
        router = din("moe_router", [1024, 8])
    gT = P.dview(P.dram("gT", [3072, NT], F32, kind="Internal"))
    h1T = P.dview(P.dram("h1T", [1024, NT], F32, kind="Internal"))
    h2T = P.dview(P.dram("h2T", [1024, NT], F32, kind="Internal"))
    h3T = P.dview(P.dram("h3T", [1024, NT], F32, kind="ExternalOutput"))
    C.k = load_consts(P, F_CONSTS)
    names.extend(["c_" + c for c in F_CONSTS])
    C.sel8_d = din("c_sel8", [8, 1024])
    C.cv = P.tile([128, NCV], name="cv")
    P.dma(C.cv, cv_d)
    groups = [(g * 128, 128) for g in range(24)]
    phase_proj(P, C, NT, hT, w_g, 3072, groups, gT, CV["nmg"], func=AF.Sigmoid, nbuf=1, zflat=True)
    phase_merge(P, C, NT, hT, oT, gT, wbr, wout, h1T)
    phase_ffn(P, C, NT, h1T, h2T, CV["nfg"], experts, FF, router)
    phase_ple(P, C, NT, h2T, pT, proj, pgate, CV["png"], h3T)
    P.finalize()
    return P.nc, names


def token_host_inputs(inp, L, half=0):
    f = np.float32
    d = {}
    cv = np.zeros((128, NCV), f)
    cv[:, CV["m0"]] = 1.0 if half == 0 else 0.0
    cv[:, CV["m1"]] = 1.0 if half == 1 else 0.0
    cv[:, CV["nmg"]:CV["nmg"] + 8] = inp["norm_mix_g"][L].reshape(8, 128).T
    cv[:, CV["nfg"]:CV["nfg"] + 8] = inp["norm_ffn_g"][L].reshape(8, 128).T
    cv[:, CV["png"]:CV["png"] + 8] = inp["ple_norm_g"][L].reshape(8, 128).T
    d["cv"] = cv
    d["w_gate"] = np.ascontiguousarray(inp["w_in"][L][:, 4272:7344])
    d["w_br0"] = inp["w_br_rwkv"][L]
    d["w_br1"] = inp["w_br_mla"][L]
    d["w_br2"] = inp["w_br_gdn"][L]
    d["w_out"] = inp["w_out"][L]
    d["ple_proj"] = inp["ple_proj"][L]
    d["ple_gate"] = inp["ple_gate"][L]
    if L % 2 == 0:
        d["ffn_wg"], d["ffn_wu"], d["ffn_wd"] = inp["ffn_wg"][L // 2], inp["ffn_wu"][L // 2], inp["ffn_wd"][L // 2]
    else:
        d["moe_wg"], d["moe_wu"], d["moe_wd"] = inp["moe_wg"][L // 2], inp["moe_wu"][L // 2], inp["moe_wd"][L // 2]
        d["moe_router"] = inp["moe_router"][L // 2]
    cs = consts_np()
    for c in F_CONSTS + ["sel8"]:
        d["c_" + c] = cs[c]
    return d


def kernel(**inputs):
    inp = {k: np.asarray(v) for k, v in inputs.items()}
    x = inp["x"].astype(np.float32)
    Bn, S, Dm = x.shape
    NT = S // 2
    hT = [np.ascontiguousarray(x[b].T) for b in range(Bn)]
    vf = [None] * NCORE
    for L in range(2):
        key = ("M", S, L)
        if key not in _PROG_CACHE:
            _PROG_CACHE[key] = build_mixer(S, L)
        nc, names = _PROG_CACHE[key]
        in_maps = []
        for core in range(NCORE):
            b, hg = core // 2, core % 2
            d = mixer_host_inputs(inp, L, b, hg)
            d["hT"] = hT[b]
            if L > 0:
                d["vfT_in"] = vf[core]
            in_maps.append({n: np.ascontiguousarray(d[n]) for n in names})
        res = run_bass_kernel_spmd(nc, in_maps, core_ids=list(range(NCORE)))
        oTs = [r["oT"] for r in res.results]
        if L == 0:
            vf = [r["vfT_out"] for r in res.results]
        key = ("F", NT, L)
        if key not in _PROG_CACHE:
            _PROG_CACHE[key] = build_token(NT, L)
        nc, names = _PROG_CACHE[key]
        th = token_host_inputs(inp, L)
        in_maps = []
        for core in range(NCORE):
            b, half = core // 2, core % 2
            tsl = slice(half * NT, (half + 1) * NT)
            d = dict(th)
            d["hT"] = hT[b][:, tsl]
            o0, o1 = oTs[2 * b], oTs[2 * b + 1]
            d["oT_all"] = np.concatenate([o0[0:256, tsl], o1[0:256, tsl], o0[256:512, tsl], o1[256:512, tsl],
                                          o0[512:768, tsl], o1[512:768, tsl]], axis=0)
            d["pT"] = inp["p"][L, b, tsl, :].T
            in_maps.append({n: np.ascontiguousarray(d[n]) for n in names})
        res = run_bass_kernel_spmd(nc, in_maps, core_ids=list(range(NCORE)))
        for b in range(Bn):
            hT[b] = np.concatenate([res.results[2 * b]["h3T"], res.results[2 * b + 1]["h3T"]], axis=1)
    out = np.stack([hT[b].T for b in range(Bn)], axis=0)
    return np.ascontiguousarray(out.astype(np.float32))


ALL_M_KEYS = ["w_in_m", "cv", "w_uq", "w_uk", "w_uv", "w_up", "a_up", "g_up"]


def build_fused(S):
    NTH = S // 2
    P = Prog()
    C = Ctx()
    names = []

    def din(name, shape, dt=F32):
        names.append(name)
        return P.dview(P.dram(name, shape, dt, kind="ExternalInput"))
    hT0 = din("hT0", [1024, S])
    pos_d = din("pos", [1, S], I32)
    pT = [din("pT0", [256, S]), din("pT1", [256, NTH])]
    C.sel8_d = din("c_sel8", [8, 1024])
    mi = {}
    for L in range(2):
        for hg in range(2):
            pre = f"m{L}{hg}_"
            d = {"w_in_m": din(pre + "w_in_m", [1024, NZ]), "cv": din(pre + "cv", [128, NCV]),
                 "w_uq": din(pre + "w_uq", [256, 384]), "w_uk": din(pre + "w_uk", [128, 256]), "w_uv": din(pre + "w_uv", [128, 256]),
                 "w_up": din(pre + "w_up", [64, 256]), "a_up": din(pre + "a_up", [64, 256]), "g_up": din(pre + "g_up", [128, 256])}
            if L > 0:
                d["v_down"] = din(pre + "v_down", [1024, 32])
                d["v_up"] = din(pre + "v_up", [32, 256])
            mi[(L, hg)] = d
    ti_ = {}
    for L in range(2):
        pre = f"t{L}_"
        d = {"cv": din(pre + "cv", [128, NCV]), "w_gate": din(pre + "w_gate", [1024, 3072]),
             "wbr": [din(pre + f"w_br{i}", [512, 1024]) for i in range(3)], "w_out": din(pre + "w_out", [1024, 1024]),
             "ple_proj": din(pre + "ple_proj", [256, 1024]), "ple_gate": din(pre + "ple_gate", [1024, 1024])}
        if L % 2 == 0:
            d["experts"] = [(din(pre + "ffn_wg", [1024, 2816]), din(pre + "ffn_wu", [1024, 2816]), din(pre + "ffn_wd", [2816, 1024]))]
            d["FF"] = 2816
            d["router"] = None
        else:
            wg_all = din(pre + "moe_wg", [8, 1024, 3584])
            wu_all = din(pre + "moe_wu", [8, 1024, 3584])
            wd_all = din(pre + "moe_wd", [8, 3584, 1024])
            d["experts"] = [(V(wg_all.ap[e], wg_all.key), V(wu_all.ap[e], wu_all.key), V(wd_all.ap[e], wd_all.key)) for e in range(8)]
            d["FF"] = 3584
            d["router"] = din(pre + "moe_router", [1024, 8])
        ti_[L] = d
    zT = P.dview(P.dram("zT", [NG, 128, S], F32, kind="Internal"))
    uT = P.dview(P.dram("uT", [1024, S], F32, kind="Internal"))
    oTa = P.dview(P.dram("oT_all", [1536, S], F32, kind="Internal"))
    vfT = P.dview(P.dram("vfT", [512, S], F32, kind="Internal"))
    gT = P.dview(P.dram("gT", [3072, S], F32, kind="Internal"))
    h1T = P.dview(P.dram("h1T", [1024, S], F32, kind="Internal"))
    h2T = P.dview(P.dram("h2T", [1024, S], F32, kind="Internal"))
    hT1 = P.dview(P.dram("hT1", [1024, S], F32, kind="Internal"))
    h3T = P.dview(P.dram("h3T", [1024, NTH], F32, kind="ExternalOutput"))
    C.k = load_consts(P, M_CONSTS)
    names.extend(["c_" + c for c in M_CONSTS])
    C.cv = P.tile([128, NCV], name="cv")
    groups = [(int(ZOFF[g]), int(ZG[g])) for g in range(NG)]
    ggroups = [(g * 128, 128) for g in range(24)]
    hin = hT0
    for L in range(2):
        for hg in range(2):
            d = mi[(L, hg)]
            P.dma(C.cv, d["cv"])
            phase_proj(P, C, S, hin, d["w_in_m"], NZ, groups, zT, CV["nmg"], uT=(uT if L > 0 else None))
            sub = lambda br: V(oTa.ap[br * 512 + hg * 256:br * 512 + (hg + 1) * 256, :], f"oT_{br}_{hg}")
            vfv = V(vfT.ap[hg * 256:(hg + 1) * 256, :], f"vfT_{hg}")
            mk_ = P.mark()
            o_r, o_g = sub(0), sub(2)
            P.run_interleaved([
                lambda: phase_rwkv(P, C, S, L, zT, o_r, d["w_up"], d["a_up"], d["g_up"], vfv,
                                   (uT if L > 0 else None), d.get("v_down"), d.get("v_up"), ttl=256, psbase=0, release=False),
                lambda: phase_gdn(P, C, S, zT, o_g, ttl=256, psbase=4, release=False)])
            P.release(mk_)
            phase_mla(P, C, S, zT, pos_d, d["w_uq"], d["w_uk"], d["w_uv"], sub(1))
        t = ti_[L]
        P.dma(C.cv, t["cv"])
        if L == 0:
            NT, dyn, hout = S, None, hT1
        else:
            NT, dyn, hout = NTH, NTH, h3T
        gv = V(gT.ap[:, 0:NT], gT.key)
        h1v = V(h1T.ap[:, 0:NT], h1T.key)
        h2v = V(h2T.ap[:, 0:NT], h2T.key)
        phase_proj(P, C, NT, hin, t["w_gate"], 3072, ggroups, gv, CV["nmg"], func=AF.Sigmoid, nbuf=1, zflat=True, dyn=dyn)
        phase_merge(P, C, NT, hin, oTa, gv, t["wbr"], t["w_out"], h1v, dyn=dyn)
        phase_ffn(P, C, NT, h1v, h2v, CV["nfg"], t["experts"], t["FF"], t["router"])
        phase_ple(P, C, NT, h2v, pT[L], t["ple_proj"], t["ple_gate"], CV["png"], hout)
        hin = hT1
    P.finalize()
    return P.nc, names


def kernel_unfused(**inputs):
    return _kernel_unfused(**inputs)


_kernel_unfused = kernel


def kernel(**inputs):
    inp = {k: np.asarray(v) for k, v in inputs.items()}
    x = inp["x"].astype(np.float32)
    Bn, S, Dm = x.shape
    NTH = S // 2
    key = ("FUSED", S)
    if key not in _PROG_CACHE:
        _PROG_CACHE[key] = build_fused(S)
    nc, names = _PROG_CACHE[key]
    cs = consts_np()
    shared = {"c_" + k: v for k, v in cs.items()}
    tok = []
    for L in range(2):
        th = token_host_inputs(inp, L)
        tok.append({f"t{L}_" + k: v for k, v in th.items() if not k.startswith("c_")})
    in_maps = []
    for core in range(NCORE):
        b, half = core // 2, core % 2
        d = dict(shared)
        d["hT0"] = x[b].T
        d["pos"] = inp["positions"][b:b + 1].astype(np.int32)
        d["pT0"] = inp["p"][0, b].T
        d["pT1"] = inp["p"][1, b, half * NTH:(half + 1) * NTH, :].T
        for L in range(2):
            for hg in range(2):
                md = mixer_host_inputs(inp, L, b, hg)
                for k, v in md.items():
                    if not k.startswith("c_") and k != "pos":
                        d[f"m{L}{hg}_" + k] = v
            d.update(tok[L])
            cvt = tok[L][f"t{L}_cv"].copy()
            cvt[:, CV["m0"]] = 1.0 if half == 0 else 0.0
            cvt[:, CV["m1"]] = 1.0 if half == 1 else 0.0
            d[f"t{L}_cv"] = cvt
        in_maps.append({n: np.ascontiguousarray(d[n]) for n in names})
    res = run_bass_kernel_spmd(nc, in_maps, core_ids=list(range(NCORE)))
    out = np.empty((Bn, S, Dm), np.float32)
    for core in range(NCORE):
        b, half = core // 2, core % 2
        out[b, half * NTH:(half + 1) * NTH, :] = res.results[core]["h3T"].T
    return out
```

```python
import numpy as np
from contextlib import ExitStack
import concourse.bass as bass
import concourse.mybir as mybir

F32 = mybir.dt.float32
BF16 = mybir.dt.bfloat16
I32 = mybir.dt.int32
ALU = mybir.AluOpType
AF = mybir.ActivationFunctionType
AX = mybir.AxisListType

ENGS = ("pe", "act", "dve", "pool", "sp")
N_DMA_SEMS = 24


class Op:
    __slots__ = ("eng", "fn", "deps", "is_dma", "idx", "sig", "slot", "slot_target", "slot_prev", "epoch")

    def __init__(self, eng, fn, is_dma):
        self.eng = eng
        self.fn = fn
        self.is_dma = is_dma
        self.deps = set()
        self.sig = 0
        self.slot = None
        self.slot_target = 0
        self.slot_prev = None


class V:
    __slots__ = ("ap", "key")

    def __init__(self, ap, key):
        self.ap = ap
        self.key = key

    def __getitem__(self, idx):
        return V(self.ap[idx], self.key)

    def sub(self, k):
        base = self.key[0] if isinstance(self.key, tuple) else self.key
        return V(self.ap, (base, k))

    def re(self, pat, **kw):
        return V(self.ap.rearrange(pat, **kw), self.key)

    def bc(self, shape):
        return V(self.ap.to_broadcast(list(shape)), self.key)

    @property
    def shape(self):
        return self.ap.shape


def _ap(x):
    return x.ap if isinstance(x, V) else x


ARENA_F32 = 53000


class Prog:
    def __init__(self, name="k"):
        self.nc = bass.Bass("TRN2", target_bir_lowering=False)
        self.st = ExitStack()
        self.arena = None
        self.aoff = 0
        self.amax = 0
        self.psum = None
        self.bar_deps = None
        self.since_bar = []
        self.bar_seen = {}
        self.epoch = 0
        self.ep_cnt = {}
        self.ops = []
        self.track = {}
        self.n_dma = 0
        self.slot_last = [None] * N_DMA_SEMS
        self.slot_count = [0] * N_DMA_SEMS
        self.uid = 0

    def sb(self, shape, dtype=F32, name=None):
        self.uid += 1
        return self.st.enter_context(self.nc.sbuf_tensor(name or f"sb{self.uid}", list(shape), dtype))

    def ps(self, shape, dtype=F32, name=None):
        self.uid += 1
        return self.st.enter_context(self.nc.psum_tensor(name or f"ps{self.uid}", list(shape), dtype))

    def dram(self, name, shape, dtype=F32, kind="Internal"):
        return self.nc.dram_tensor(name, list(shape), dtype, kind=kind)

    def tile(self, shape, name=None, dtype=None):
        if self.arena is None:
            self.arena = self.st.enter_context(self.nc.sbuf_tensor("arena", [128, ARENA_F32], F32))
        self.uid += 1
        p = shape[0]
        n = int(np.prod(shape[1:]))
        if dtype == BF16:
            nw = (n + 1) // 2
            assert self.aoff + nw <= ARENA_F32, f"arena overflow {self.aoff}+{nw}"
            ap = self.arena[0:p, self.aoff:self.aoff + nw].bitcast(BF16)[:, 0:n]
            self.aoff += nw
        else:
            assert self.aoff + n <= ARENA_F32, f"arena overflow {self.aoff}+{n}"
            ap = self.arena[0:p, self.aoff:self.aoff + n]
            self.aoff += n
        self.amax = max(self.amax, self.aoff)
        if len(shape) == 3:
            ap = ap.rearrange("p (a b) -> p a b", a=shape[1])
        elif len(shape) == 4:
            ap = ap.rearrange("p (a b c) -> p a b c", a=shape[1], b=shape[2])
        return V(ap, name or f"t{self.uid}")

    def mark(self):
        return self.aoff

    def release(self, mark):
        self.barrier()
        self.aoff = mark

    def pbank(self, i):
        if self.psum is None:
            self.psum = [self.st.enter_context(self.nc.psum_tensor(f"psb{j}", [128, 512], F32)) for j in range(8)]
        return V(self.psum[i][:], f"psb{i}")

    def pslot(self, bank, half):
        self.pbank(0)
        return V(self.psum[bank][:, half * 256:(half + 1) * 256], f"psb{bank}_{half}")

    def run_interleaved(self, fns):
        import threading
        il = {"turn": 0, "alive": [True] * len(fns), "cv": threading.Condition(), "tl": threading.local()}
        errs = []

        def nxt(i):
            n = len(fns)
            for d in range(1, n + 1):
                j = (i + d) % n
                if il["alive"][j]:
                    return j
            return i

        def runner(i, fn):
            with il["cv"]:
                while il["turn"] != i:
                    il["cv"].wait()
            il["tl"].i = i
            try:
                fn()
            except BaseException as e:
                errs.append(e)
            finally:
                with il["cv"]:
                    il["alive"][i] = False
                    il["turn"] = nxt(i)
                    il["cv"].notify_all()

        def yield_turn():
            i = getattr(il["tl"], "i", None)
            if i is None:
                return
            with il["cv"]:
                j = nxt(i)
                if j == i:
                    return
                il["turn"] = j
                il["cv"].notify_all()
                while il["turn"] != i:
                    il["cv"].wait()

        self._yield = yield_turn
        ths = [threading.Thread(target=runner, args=(i, f)) for i, f in enumerate(fns)]
        for t in ths:
            t.start()
        for t in ths:
            t.join()
        self._yield = None
        if errs:
            raise errs[0]

    def dview(self, t, name=None):
        ap = t.ap() if hasattr(t, "ap") and callable(t.ap) else t
        return V(ap, name or ap.tensor.name)

    def barrier(self):
        self.bar_deps = list(self.since_bar) if self.bar_deps is None else self.bar_deps + self.since_bar
        last = {}
        dm = []
        for o in self.bar_deps:
            if o.is_dma:
                dm.append(o)
            else:
                last[o.eng] = o
        self.bar_deps = list(last.values()) + dm[-2 * N_DMA_SEMS:]
        self.since_bar = []
        self.bar_seen = {}
        if max(self.ep_cnt.values(), default=0) > 20000:
            self.epoch += 1
            self.ep_cnt = {}

    @staticmethod
    def _key(x):
        if isinstance(x, V):
            x = x.key
        if isinstance(x, tuple):
            t, sub = x
        else:
            t, sub = x, None
        nm = t if isinstance(t, str) else (t.name if hasattr(t, "name") else t.tensor.name)
        return nm, sub

    def _conf(self, nm, sub):
        ent = self.track.setdefault(nm, {})
        if sub is None:
            return list(ent.keys())
        ks = [k for k in ent.keys() if k is None or k == sub]
        return ks

    def op(self, eng, fn, reads=(), writes=(), is_dma=False):
        o = Op(eng, fn, is_dma)
        o.epoch = self.epoch
        if not is_dma:
            self.ep_cnt[eng] = self.ep_cnt.get(eng, 0) + 1
        for r in reads:
            nm, sub = self._key(r)
            ent = self.track.setdefault(nm, {})
            for k in self._conf(nm, sub):
                w = ent[k][0]
                if w is not None:
                    o.deps.add(w)
        for wv in writes:
            nm, sub = self._key(wv)
            ent = self.track.setdefault(nm, {})
            for k in self._conf(nm, sub):
                w, rs = ent[k]
                if w is not None:
                    o.deps.add(w)
                for r in rs:
                    o.deps.add(r)
        for r in reads:
            nm, sub = self._key(r)
            ent = self.track[nm]
            if sub not in ent:
                ent[sub] = [None, []]
            ent[sub][1].append(o)
        for wv in writes:
            nm, sub = self._key(wv)
            ent = self.track[nm]
            if sub is None:
                for k in list(ent.keys()):
                    del ent[k]
            ent[sub] = [o, []]
        o.deps.discard(o)
        if self.bar_deps is not None and not self.bar_seen.get(eng):
            self.bar_seen[eng] = True
            o.deps.update(self.bar_deps)
        self.since_bar.append(o)
        if is_dma:
            s = self.n_dma % N_DMA_SEMS
            self.n_dma += 1
            o.slot = s
            o.slot_prev = self.slot_last[s]
            self.slot_count[s] += 1
            o.slot_target = 16 * self.slot_count[s]
            self.slot_last[s] = o
        o.idx = len(self.ops)
        self.ops.append(o)
        if getattr(self, "_yield", None) is not None:
            self._yield()
        return o

    def dma(self, out, in_, reads=None, writes=None, q="sp", in_fn=None, **kw):
        if in_fn is not None:
            return self.op(q, lambda e: e.dma_start(out=_ap(out), in_=in_fn(e), **kw),
                           reads if reads is not None else [in_], writes if writes is not None else [out], is_dma=True)
        return self.op(q, lambda e: e.dma_start(out=_ap(out), in_=_ap(in_), **kw),
                       reads if reads is not None else [in_], writes if writes is not None else [out], is_dma=True)

    def mm(self, out, lhsT, rhs, start=True, stop=True, reads=None, writes=None, **kw):
        return self.op("pe", lambda e: e.matmul(_ap(out), _ap(lhsT), _ap(rhs), start=start, stop=stop, **kw),
                       reads if reads is not None else [lhsT, rhs], writes if writes is not None else [out])

    def transpose(self, out, in_, ident, reads=None, writes=None):
        return self.op("pe", lambda e: e.transpose(_ap(out), _ap(in_), _ap(ident)),
                       reads if reads is not None else [in_, ident], writes if writes is not None else [out])

    def act(self, out, in_, func, bias=None, scale=1.0, reads=None, writes=None, accum_out=None, eng="act"):
        kw = {}
        rd = [in_]
        if bias is not None:
            kw["bias"] = _ap(bias)
            if not isinstance(bias, (int, float)):
                rd.append(bias)
        if not isinstance(scale, (int, float)):
            rd.append(scale)
        wr = [out]
        if accum_out is not None:
            kw["accum_out"] = _ap(accum_out)
            wr.append(accum_out)
        return self.op(eng, lambda e: e.activation(_ap(out), _ap(in_), func, scale=_ap(scale), **kw),
                       reads if reads is not None else rd, writes if writes is not None else wr)

    def tt(self, out, in0, in1, op, eng="dve", reads=None, writes=None):
        return self.op(eng, lambda e: e.tensor_tensor(_ap(out), _ap(in0), _ap(in1), op),
                       reads if reads is not None else [in0, in1], writes if writes is not None else [out])

    def ts(self, out, in0, s1, op0, s2=None, op1=None, eng="dve", reads=None, writes=None):
        rd = [in0] + [s for s in (s1, s2) if s is not None and not isinstance(s, (int, float))]
        if op1 is None:
            f = lambda e: e.tensor_scalar(_ap(out), _ap(in0), _ap(s1), None, op0)
        else:
            f = lambda e: e.tensor_scalar(_ap(out), _ap(in0), _ap(s1), _ap(s2), op0, op1)
        return self.op(eng, f, reads if reads is not None else rd, writes if writes is not None else [out])

    def stt(self, out, in0, scalar, in1, op0, op1, eng="dve", reads=None, writes=None):
        rd = [in0, in1] + ([] if isinstance(scalar, (int, float)) else [scalar])
        return self.op(eng, lambda e: e.scalar_tensor_tensor(_ap(out), _ap(in0), _ap(scalar), _ap(in1), op0, op1),
                       reads if reads is not None else rd, writes if writes is not None else [out])

    def copy(self, out, in_, eng="dve", reads=None, writes=None):
        if eng == "act":
            f = lambda e: e.copy(_ap(out), _ap(in_))
        else:
            f = lambda e: e.tensor_copy(_ap(out), _ap(in_))
        return self.op(eng, f, reads if reads is not None else [in_], writes if writes is not None else [out])

    def memset(self, ap, val, eng="pool", writes=None):
        return self.op(eng, lambda e: e.memset(_ap(ap), val), [], writes if writes is not None else [ap])

    def recip(self, out, in_, reads=None, writes=None):
        return self.op("dve", lambda e: e.reciprocal(_ap(out), _ap(in_)),
                       reads if reads is not None else [in_], writes if writes is not None else [out])

    def finalize(self, final_waits=()):
        nc = self.nc
        ops = self.ops
        needed = set()
        for o in ops:
            for d in o.deps:
                if not d.is_dma:
                    needed.add(d.idx)
        cnt = {}
        for o in ops:
            if not o.is_dma and (o.idx in needed):
                k_ = (o.eng, o.epoch)
                cnt[k_] = cnt.get(k_, 0) + 1
                o.sig = cnt[k_]
            else:
                o.sig = 0
        sems = {k_: self.st.enter_context(nc.semaphore(f"s_{k_[0]}_{k_[1]}")) for k_ in cnt}
        dsems = [self.st.enter_context(nc.semaphore(f"s_d{i}")) for i in range(N_DMA_SEMS)]
        per_eng = {e: [o for o in ops if o.eng == e] for e in ENGS}
        final = list(final_waits)

        def body(eng_name):
            def _b(e):
                waited = {}
                def wait(key, sem, val):
                    if waited.get(key, 0) >= val:
                        return
                    e.wait_ge(sem, val)
                    waited[key] = val
                for o in per_eng[eng_name]:
                    for d in sorted(o.deps, key=lambda x: x.idx):
                        if d.is_dma:
                            wait(("d", d.slot), dsems[d.slot], d.slot_target)
                        else:
                            if d.eng == eng_name and eng_name == "pe":
                                continue
                            wait(("c", d.eng, d.epoch), sems[(d.eng, d.epoch)], d.sig)
                    if o.is_dma and o.slot_prev is not None:
                        wait(("d", o.slot), dsems[o.slot], o.slot_prev.slot_target)
                    ins = o.fn(e)
                    if o.is_dma:
                        ins.then_inc(dsems[o.slot], 16)
                    elif o.sig:
                        ins.then_inc(sems[(eng_name, o.epoch)], 1)
                if eng_name == "sp":
                    for s in range(N_DMA_SEMS):
                        if self.slot_last[s] is not None:
                            wait(("d", s), dsems[s], self.slot_last[s].slot_target)
            return _b

        with nc.Block() as block:
            block.tensor(body("pe"))
            block.scalar(body("act"))
            block.vector(body("dve"))
            block.gpsimd(body("pool"))
            block.sync(body("sp"))
        self.st.close()
        return nc

D = 1024
TT = 512
ZG = [64] * 12 + [64, 64, 128] + [128, 128, 128, 32] + [64] * 16 + [4, 4]
NG = len(ZG)
G_R, G_K, G_V, G_WLO, G_ALO, G_GLO = 0, 4, 8, 12, 13, 14
G_CQ, G_CKV, G_KPE = 15, 17, 18
G_GQ, G_GK, G_GV, G_GG, G_GB, G_GA = 19, 23, 27, 31, 35, 36
ZOFF = np.concatenate([[0], np.cumsum(ZG)]).astype(int)
NZ = int(ZOFF[-1])

CV = {}
_n = 0
for nm, k in [("nmg", 8), ("mu", 15), ("w0", 4), ("a0", 4), ("kk", 4), ("ka", 4), ("rk", 4), ("lng", 4), ("lnb", 4),
              ("vmu", 8), ("vb", 4), ("qng", 2), ("kvng", 1), ("qkq", 1), ("qkk", 1), ("invf", 1),
              ("conv", 48), ("alog", 1), ("dtb", 1), ("gng", 1), ("ropec", 1), ("nfg", 8), ("png", 8), ("m0", 1), ("m1", 1)]:
    CV[nm] = _n
    _n += k
NCV = _n


def consts_np():
    c = {}
    c["ident"] = np.eye(128, dtype=np.float32)
    c["ones"] = np.ones((128, 128), np.float32)
    bd = np.zeros((128, 128), np.float32)
    bd[:64, :64] = 1
    bd[64:, 64:] = 1
    c["bd64"] = bd
    s = np.arange(64)[:, None]
    t = np.arange(64)[None, :]
    incl = (t >= s).astype(np.float32)
    strict = (t > s).astype(np.float32)
    c["m_incl"] = np.concatenate([incl, incl], 0)
    c["m_strict"] = np.concatenate([strict, strict], 0)
    c["m_incl_T"] = np.concatenate([incl.T, incl.T], 0)
    c["m_strict_T"] = np.concatenate([strict.T, strict.T], 0)
    c["neg_incl"] = (1.0 - c["m_incl"]) * -1e4
    c["neg_incl_T"] = (1.0 - c["m_incl_T"]) * -1e4
    c["id64x2"] = np.concatenate([np.eye(64, dtype=np.float32)] * 2, 0)
    kk = np.arange(128)[:, None]
    qq = np.arange(128)[None, :]
    c["att_mask"] = (qq >= kk).astype(np.float32)
    R = np.zeros((96, 96), np.float32)
    for m in range(16):
        R[64 + m, 64 + m + 16] = -1.0
        R[64 + 16 + m, 64 + m] = 1.0
    c["ropeRT"] = np.ascontiguousarray(R.T)
    sel = np.zeros((4, 4, 64), np.float32)
    for h in range(4):
        sel[h, h, :] = 1.0
    c["sel4"] = sel.reshape(4, 256)
    sel8 = np.zeros((8, 8, 128), np.float32)
    for e in range(8):
        sel8[e, e, :] = 1.0
    c["sel8"] = sel8.reshape(8, 1024)
    c["id4"] = np.tile(np.eye(64, dtype=np.float32)[:, None, :], (1, 4, 1)).reshape(64, 256)
    c["neg4"] = np.tile(c["neg_incl"][:64][:, None, :], (1, 4, 1)).reshape(64, 256)
    c["neg4T"] = np.tile(c["neg_incl_T"][:64][:, None, :], (1, 4, 1)).reshape(64, 256)
    c["ms4"] = np.tile(c["m_strict"][:64][:, None, :], (1, 4, 1)).reshape(64, 256)
    c["ms4T"] = np.tile(c["m_strict_T"][:64][:, None, :], (1, 4, 1)).reshape(64, 256)
    c["mi4"] = np.tile(c["m_incl"][:64][:, None, :], (1, 4, 1)).reshape(64, 256)
    return c


CONST_SHAPES = {k: v.shape for k, v in consts_np().items()}


class Ctx:
    pass


def load_consts(P, names):
    out = {}
    for nm in names:
        shp = CONST_SHAPES[nm]
        d = P.dview(P.dram("c_" + nm, shp, F32, kind="ExternalInput"))
        t = P.tile(list(shp), name="c_" + nm)
        P.dma(t, d)
        out[nm] = t
    return out


def rstd_from_ps(P, out, ps, n, eps):
    P.act(out, ps, AF.Ln, scale=float(1.0 / n), bias=float(eps))
    P.act(out, out, AF.Exp, scale=-0.5)


def act_sigmoid(P, out, in_, scale=1.0, negbias=None):
    if negbias is None:
        P.act(out, in_, AF.Exp, scale=-float(scale))
    else:
        P.act(out, in_, AF.Exp, scale=-float(scale), bias=negbias)
    P.act(out, out, AF.Ln, bias=1.0)
    P.act(out, out, AF.Exp, scale=-1.0)


def phase_proj(P, C, S, hT, w_d, ncols, groups, zT, gcol, uT=None, func=None, nbuf=2, zflat=False, dyn=None):
    mk = P.mark()
    cv = C.cv
    w = P.tile([128, 8, ncols], name="w_in", dtype=BF16)
    wst = [P.tile([128, 8, 512], name=f"wst{i}") for i in range(2)]
    wv = w_d.re("(c p) n -> p c n", p=128)
    step = 512
    for i_, c0 in enumerate(range(0, ncols, step)):
        c1 = min(ncols, c0 + step)
        st_ = wst[i_ % 2]
        P.dma(st_[:, :, 0:c1 - c0], wv[:, :, c0:c1])
        P.copy(w[:, :, c0:c1].sub(c0), st_[:, :, 0:c1 - c0], eng=("pool" if i_ % 2 == 0 else "act"))
    hb = [P.tile([128, 8, TT], name=f"hb{i}") for i in range(nbuf)]
    sq = P.tile([128, 8, TT], name="sq")
    ub = [P.tile([128, 8, TT], name=f"ub{i}", dtype=BF16) for i in range(nbuf)]
    rs = P.tile([128, TT], name="rs")
    stg = [P.tile([128, TT], name=f"stg{i}") for i in range(4)]
    hv = hT.re("(c p) t -> p c t", p=128)
    ps_ss = P.pbank(0)
    pz = [P.pbank(1 + i) for i in range(4)]
    nt = S // TT
    k = 0
    for ti in range(nt):
        tsl = slice(ti * TT, (ti + 1) * TT)
        h = hb[ti % nbuf]
        u = ub[ti % nbuf]
        if dyn is not None:
            P.dma(h, hv[:, :, tsl])
            P.dma(sq, hv[:, :, dyn + ti * TT:dyn + (ti + 1) * TT])
            P.ts(h, h, cv[:, CV["m0"]:CV["m0"] + 1], ALU.mult)
            P.stt(h, sq, cv[:, CV["m1"]:CV["m1"] + 1], h, ALU.mult, ALU.add)
        else:
            P.dma(h, hv[:, :, tsl])
        P.act(sq, h, AF.Square)
        for c in range(8):
            P.mm(ps_ss, C.k["ones"], sq[:, c, :], start=(c == 0), stop=(c == 7))
        rstd_from_ps(P, rs, ps_ss, D, 1e-6)
        if uT is not None:
            for c in range(8):
                P.stt(sq[:, c, :], h[:, c, :], cv[:, gcol + c:gcol + c + 1], rs, ALU.mult, ALU.mult)
            P.dma(uT.re("(c p) t -> p c t", p=128)[:, :, tsl], sq, q="pool")
            P.copy(u, sq, eng="act")
        else:
            for c in range(8):
                P.stt(u[:, c, :], h[:, c, :], cv[:, gcol + c:gcol + c + 1], rs, ALU.mult, ALU.mult)
        for gi, (co, n) in enumerate(groups):
            pp = pz[k % 4]
            st = stg[k % 4]
            for c in range(8):
                wsl = w[:, c, co:co + n]
                if co // step == (co + n - 1) // step:
                    wsl = wsl.sub((co // step) * step)
                P.mm(pp[0:n, :], wsl, u[:, c, :], start=(c == 0), stop=(c == 7))
            if func is not None:
                P.act(st[0:n, :], pp[0:n, :], func)
            else:
                P.copy(st[0:n, :], pp[0:n, :], eng=("act" if k % 2 == 0 else "dve"))
            if zflat:
                P.dma(zT[co:co + n, tsl].sub(gi), st[0:n, :], q="pool")
            else:
                P.dma(zT[gi, 0:n, tsl].sub(gi), st[0:n, :], q="pool")
            k += 1
    P.release(mk)


def phase_mla(P, C, S, zT, pos_d, w_uq_d, w_uk_d, w_uv_d, oT):
    mk = P.mark()
    cv = C.cv
    K = C.k
    nt = S // TT
    wuq_f = P.tile([128, 2, 384], name="wuq_f")
    P.dma(wuq_f, w_uq_d.re("(c p) n -> p c n", p=128))
    wuk_f = P.tile([128, 256], name="wuk_f")
    P.dma(wuk_f, w_uk_d)
    wuv_f = P.tile([128, 256], name="wuv_f")
    P.dma(wuv_f, w_uv_d)
    wuq = P.tile([128, 2, 384], name="wuq", dtype=BF16)
    wuk = P.tile([128, 256], name="wuk", dtype=BF16)
    wuv = P.tile([128, 256], name="wuv", dtype=BF16)
    P.copy(wuq, wuq_f, eng="pool")
    P.copy(wuk, wuk_f, eng="pool")
    P.copy(wuv, wuv_f, eng="pool")
    ones_b = P.tile([128, 64], name="ones_b", dtype=BF16)
    P.copy(ones_b, K["ones"][:, 0:64], eng="pool")
    KT = P.tile([96, 4, S], name="KT", dtype=BF16)
    VT = P.tile([128, S // 128, 256], name="Vtm", dtype=BF16)
    rc = P.tile([96, TT], name="rope_c")
    rsn = P.tile([96, TT], name="rope_s")
    posi = P.tile([96, TT], name="posi")
    posf = P.tile([96, TT], name="posf")
    ang = P.tile([96, TT], name="ang")
    tmp = P.tile([96, TT], name="ropetmp")
    tmp2 = P.tile([96, TT], name="ropetmp2")
    P.memset(rc[0:64, :], 1.0, writes=[rc.sub("lo")])
    P.memset(rsn[0:64, :], 0.0, writes=[rsn.sub("lo")])
    pi_ap = V(posi.ap.bitcast(I32), posi.key)
    invf = cv[64:96, CV["invf"]:CV["invf"] + 1]
    negpi = cv[64:96, CV["ropec"]:CV["ropec"] + 1]
    TWO_PI = float(2 * np.pi)

    def rope_tile(ti):
        tsl = slice(ti * TT, (ti + 1) * TT)
        P.dma(pi_ap[64:96, :], V(pos_d.ap[:, tsl].partition_broadcast(32), pos_d.key))
        P.copy(posf[64:96, :], pi_ap[64:96, :])
        P.ts(ang[64:96, :], posf[64:96, :], invf, ALU.mult)
        for dst, shift in ((rsn, 0.0), (rc, float(np.pi / 2))):
            a_ = ang[64:96, :]
            if shift:
                P.ts(tmp2[64:96, :], ang[64:96, :], shift, ALU.add)
                a_ = tmp2[64:96, :]
            P.ts(tmp[64:96, :], a_, float(1.0 / TWO_PI), ALU.mult)
            P.copy(pi_ap[64:96, :], tmp[64:96, :])
            P.copy(tmp[64:96, :], pi_ap[64:96, :])
            P.stt(tmp[64:96, :], tmp[64:96, :], -TWO_PI, a_, ALU.mult, ALU.add)
            P.ts(posf[64:96, :], tmp[64:96, :], float(np.pi), ALU.is_gt, TWO_PI, ALU.mult)
            P.tt(tmp[64:96, :], tmp[64:96, :], posf[64:96, :], ALU.subtract)
            P.act(dst[64:96, :], tmp[64:96, :], AF.Sin, writes=[dst.sub("hi")])

    ones = K["ones"]
    cq = [P.tile([128, 2, TT], name=f"cq{i}") for i in range(2)]
    ckv = [P.tile([128, TT], name=f"ckv{i}") for i in range(2)]
    cqb = [P.tile([128, 2, TT], name=f"cqb{i}", dtype=BF16) for i in range(2)]
    ckvb = [P.tile([128, TT], name=f"ckvb{i}", dtype=BF16) for i in range(2)]
    kpe = [P.tile([96, TT], name=f"kpe{i}") for i in range(2)]
    sq = P.tile([128, 2, TT], name="msq")
    rs = P.tile([128, TT], name="mrs")
    raw = P.tile([96, TT], name="raw")
    nrm = P.tile([96, TT], name="nrm")
    rot = P.tile([96, TT], name="rot")
    QT = P.tile([96, 4, TT], name="QT", dtype=BF16)
    ps_a = P.pbank(0)
    ps_b = P.pbank(1)
    ps_c = P.pbank(2)

    def qk_finish(src_raw, gcolname, dst, tsl):
        P.act(sq[0:96, 0, :], src_raw, AF.Square)
        P.mm(ps_b[0:96, :], ones[0:96, 0:96], sq[0:96, 0, :])
        rstd_from_ps(P, rs[0:96, :], ps_b[0:96, :], 96, 1e-6)
        g = cv[0:96, CV[gcolname]:CV[gcolname] + 1]
        P.stt(nrm, src_raw, g, rs[0:96, :], ALU.mult, ALU.mult)
        P.mm(ps_c[0:96, :], K["ropeRT"], nrm)
        P.tt(rot, ps_c[0:96, :], rsn, ALU.mult)
        P.tt(nrm, nrm, rc, ALU.mult, eng="pool")
        P.tt(dst, nrm, rot, ALU.add)

    def load_norm(ti, want_q):
        tsl = slice(ti * TT, (ti + 1) * TT)
        i2 = ti % 2
        if want_q:
            P.dma(cq[i2], zT[G_CQ:G_CQ + 2, :, tsl].re("g p t -> p g t"))
            P.act(sq, cq[i2], AF.Square)
            P.mm(ps_a, ones, sq[:, 0, :], start=True, stop=False)
            P.mm(ps_a, ones, sq[:, 1, :], start=False, stop=True)
            rstd_from_ps(P, rs, ps_a, 256, 1e-6)
            for c in range(2):
                P.stt(cqb[i2][:, c, :], cq[i2][:, c, :], cv[:, CV["qng"] + c:CV["qng"] + c + 1], rs, ALU.mult, ALU.mult)
        else:
            P.dma(ckv[i2], zT[G_CKV, :, tsl])
            P.dma(kpe[i2][64:96, :], zT[G_KPE, 0:32, tsl])
            P.act(sq[:, 0, :], ckv[i2], AF.Square)
            P.mm(ps_a, ones, sq[:, 0, :])
            rstd_from_ps(P, rs, ps_a, 128, 1e-6)
            P.stt(ckvb[i2], ckv[i2], cv[:, CV["kvng"]:CV["kvng"] + 1], rs, ALU.mult, ALU.mult)
        return tsl, i2

    for ti in range(nt):
        rope_tile(ti)
        tsl, i2 = load_norm(ti, False)
        for h in range(4):
            P.mm(ps_b[0:64, :], wuk[:, h * 64:(h + 1) * 64], ckvb[i2])
            P.copy(raw[0:64, :], ps_b[0:64, :], eng="act", writes=[raw.sub("lo")])
            P.copy(raw[64:96, :], kpe[i2][64:96, :], eng="pool", writes=[raw.sub("hi")])
            qk_finish(raw, "qkk", KT[:, h, tsl].sub(h), tsl)
        for j in range(TT // 128):
            P.mm(ps_c[:, 0:256], ckvb[i2][:, j * 128:(j + 1) * 128], wuv)
            P.copy(VT[:, ti * (TT // 128) + j, :], ps_c[:, 0:256], eng="act")

    pt = [P.tile([128, TT], name=f"pt{i}", dtype=BF16) for i in range(3)]
    osb = P.tile([64, TT], name="osb")
    lsb = P.tile([64, TT], name="lsb")
    ps_s = [P.pbank(3), P.pbank(4)]
    ps_o = P.pbank(5)
    ps_l = P.pbank(6)
    scale = float(96 ** -0.5)
    for ti in range(nt):
        rope_tile(ti)
        tsl, i2 = load_norm(ti, True)
        for h in range(4):
            for c in range(2):
                P.mm(ps_b[0:96, :], wuq[:, c, h * 96:(h + 1) * 96], cqb[i2][:, c, :], start=(c == 0), stop=(c == 1))
            P.copy(raw, ps_b[0:96, :], eng="act")
            qk_finish(raw, "qkq", QT[:, h, :].sub(h), tsl)
        for h in range(4):
            nkc = 4 * (ti + 1)

            def c0_of(kc):
                j = kc - 4 * ti
                return 0 if j <= 0 else j * 128

            def score(kc):
                c0 = c0_of(kc)
                P.mm(ps_s[kc % 2][:, c0:TT], KT[:, h, kc * 128:(kc + 1) * 128].sub(h), QT[:, h, c0:TT].sub(h))
            score(0)
            for kc in range(nkc):
                if kc + 1 < nkc:
                    score(kc + 1)
                j = kc - 4 * ti
                c0 = c0_of(kc)
                pss = ps_s[kc % 2]
                p_t = pt[kc % 3]
                P.act(p_t[:, c0:TT], pss[:, c0:TT], AF.Exp, scale=scale)
                if j >= 0:
                    P.tt(p_t[:, c0:c0 + 128], p_t[:, c0:c0 + 128], K["att_mask"], ALU.mult, eng="pool")
                P.mm(ps_o[0:64, c0:TT], VT[:, kc, h * 64:(h + 1) * 64], p_t[:, c0:TT], start=(kc == 0), stop=(kc == nkc - 1))
                P.mm(ps_l[0:64, c0:TT], ones_b, p_t[:, c0:TT], start=(kc == 0), stop=(kc == nkc - 1))
            P.act(lsb, ps_l[0:64, :], AF.Ln)
            P.act(lsb, lsb, AF.Exp, scale=-1.0)
            P.tt(osb, ps_o[0:64, :], lsb, ALU.mult)
            P.dma(oT[h * 64:(h + 1) * 64, tsl].sub(h), osb, q="pool")
    P.release(mk)


def neumann_inv(P, C, A0, B0, bufs, ps1, ps2, ps3):
    id4 = C.k["id4"]
    TTt = bufs["TT"]
    P.tt(TTt, B0, id4, ALU.add)
    A = [A0, bufs["A1"]]
    B = [B0, bufs["B1"]]
    for k in range(1, 6):
        a_prev, a_new = A[(k - 1) % 2], A[k % 2]
        b_prev, b_new = B[(k - 1) % 2], B[k % 2]
        for h in range(4):
            hs = slice(h * 64, (h + 1) * 64)
            P.mm(ps1[0:64, hs], b_prev[:, hs], a_prev[:, hs])
        if k < 5:
            for h in range(4):
                hs = slice(h * 64, (h + 1) * 64)
                P.mm(ps2[0:64, hs], a_prev[:, hs], b_prev[:, hs])
        P.copy(a_new, ps1[0:64, 0:256], eng="act")
        if k < 5:
            P.copy(b_new, ps2[0:64, 0:256], eng="dve")
        for h in range(4):
            hs = slice(h * 64, (h + 1) * 64)
            P.mm(ps3[0:64, hs], a_new[:, hs], TTt[:, hs])
        P.tt(TTt, TTt, ps3[0:64, 0:256], ALU.add)
    return TTt


def phase_gdn(P, C, S, zT, oT, ttl=512, psbase=None, release=True):
    mk = P.mark()
    TT = ttl
    cv = C.cv
    K = C.k
    nt = S // TT
    NCH = TT // 64
    ones = K["ones"]
    ident = K["ident"]
    cvw = lambda seg, h, j: cv[0:64, CV["conv"] + (seg * 4 + h) * 4 + j:CV["conv"] + (seg * 4 + h) * 4 + j + 1]
    St = P.tile([64, 4, 64], name="gS")
    P.memset(St, 0.0)
    Sb = P.tile([64, 4, 64], name="gSb", dtype=BF16)
    P.copy(Sb, St, eng="pool")
    nA = P.tile([4, 1], name="nA")
    P.act(nA, cv[0:4, CV["alog"]:CV["alog"] + 1], AF.Exp)
    P.ts(nA, nA, -1.0, ALU.mult)
    def mkbuf(i):
        b = {}
        for nm in ["q", "k", "kb", "qd"]:
            b[nm] = P.tile([64, 4, TT], name=f"g{nm}{i}", dtype=BF16)
        b["k32"] = P.tile([64, 4, TT], name=f"gk32{i}")
        b["q32"] = P.tile([64, 4, TT], name=f"gq32{i}")
        b["ktm"] = P.tile([64, NCH, 4, 64], name=f"gktm{i}", dtype=BF16)
        b["bv"] = P.tile([64, NCH, 4, 64], name=f"gbv{i}")
        b["bg"] = P.tile([64, NCH, 12], name=f"gbg{i}")
        b["c2"] = P.tile([64, NCH, 4], name=f"gc2{i}")
        b["ngc"] = P.tile([64, NCH, 4], name=f"gngc{i}")
        b["dl"] = P.tile([64, 4, NCH], name=f"gdl{i}")
        return b
    TB = [mkbuf(0), mkbuf(1)]
    xin = [P.tile([64, TT + 3], name=f"gxin{i}") for i in range(3)]
    acc = [P.tile([64, TT], name=f"gacc{i}") for i in range(2)]
    vfm = P.tile([64, 4, TT], name="gvfm")
    sq = P.tile([64, TT], name="gsq")
    rs = P.tile([64, TT], name="grs")
    bfm = P.tile([4, TT], name="gbfm")
    gfm = [P.tile([4, TT], name=f"ggfm{i}") for i in range(2)]
    efm = P.tile([4, TT], name="gefm")
    kdf = P.tile([4, TT], name="gkdf")
    gl4 = P.tile([4, NCH], name="ggl4")
    ob = [P.tile([64, 4, TT], name=f"gob{i}") for i in range(2)]
    gate = P.tile([64, TT], name="ggate")
    def mkch(i):
        d_ = {nm: P.tile([64, 256], name=f"gc_{nm}{i}") for nm in ["E", "F", "G1", "G2", "Gs"]}
        d_.update({nm: P.tile([64, 256], name=f"gc_{nm}{i}", dtype=BF16) for nm in ["A0", "B0", "A1", "B1", "TT", "Ain", "X", "vn"]})
        return d_
    CB = [mkch(0), mkch(1)]
    if psbase is None:
        psA, psB, psC, psD, psE, psF, psG, psH = [P.pbank(i) for i in range(8)]
    else:
        psA, psB, psC, psD, psE, psF, psG, psH = [P.pbank(psbase + j % 4) for j in range(8)]
    xk = 0
    for ti in range(nt):
        tb = TB[ti % 2]
        tsl = slice(ti * TT, (ti + 1) * TT)
        for seg, (g0, dst) in enumerate([(G_GQ, tb["q32"]), (G_GK, tb["k32"]), (G_GV, vfm)]):
            for h in range(4):
                x = xin[xk % 3]
                a = acc[xk % 2]
                xk += 1
                if ti == 0:
                    P.memset(x[:, 0:3], 0.0, writes=[x.sub("halo")])
                    P.dma(x[:, 3:TT + 3].sub("body"), zT[g0 + h, 0:64, 0:TT])
                else:
                    P.dma(x, zT[g0 + h, 0:64, ti * TT - 3:(ti + 1) * TT])
                P.ts(a, x[:, 0:TT], cvw(seg, h, 0), ALU.mult)
                for j in range(1, 4):
                    P.stt(a, x[:, j:TT + j], cvw(seg, h, j), a, ALU.mult, ALU.add)
                act_sigmoid(P, sq, a)
                if seg == 2:
                    P.tt(dst[:, h, :].sub(h), a, sq, ALU.mult)
                else:
                    P.tt(a, a, sq, ALU.mult)
                    P.act(sq, a, AF.Square)
                    P.mm(psA[0:64, 0:TT], ones[0:64, 0:64], sq)
                    rstd_from_ps(P, rs, psA[0:64, 0:TT], 1.0, 1e-12)
                    P.stt(dst[:, h, :].sub(h), a, (0.125 if seg == 0 else 1.0), rs, ALU.mult, ALU.mult)
        P.dma(bfm, zT[G_GB, 0:4, tsl])
        act_sigmoid(P, bfm, bfm)
        g0t = gfm[0]
        P.dma(g0t, zT[G_GA, 0:4, tsl])
        P.act(g0t, g0t, AF.Exp, bias=cv[0:4, CV["dtb"]:CV["dtb"] + 1])
        P.act(g0t, g0t, AF.Ln, bias=1.0)
        P.ts(g0t, g0t, nA[:, 0:1], ALU.mult)
        cur = 0
        for sh in (1, 2, 4, 8, 16, 32):
            src = gfm[cur].re("h (n c) -> h n c", c=64)
            dstt = gfm[1 - cur].re("h (n c) -> h n c", c=64)
            P.copy(dstt[:, :, 0:sh], src[:, :, 0:sh], eng="pool", writes=[gfm[1 - cur].sub("a")])
            P.tt(dstt[:, :, sh:64], src[:, :, sh:64], src[:, :, 0:64 - sh], ALU.add, writes=[gfm[1 - cur].sub("b")])
            cur = 1 - cur
        gc = gfm[cur]
        P.act(efm, gc, AF.Exp)
        gc3 = gc.re("h (n c) -> h n c", c=64)
        P.copy(gl4, gc3[:, :, 63])
        P.tt(kdf.re("h (n c) -> h n c", c=64), V(gl4.ap.unsqueeze(2).to_broadcast([4, NCH, 64]), gl4.key), gc3, ALU.subtract)
        P.act(kdf, kdf, AF.Exp)
        for h in range(4):
            P.mm(psA[0:64, h * NCH:(h + 1) * NCH], K["sel4"][:, h * 64:(h + 1) * 64], gl4)
        P.act(tb["dl"].re("p h n -> p (h n)"), psA[0:64, 0:4 * NCH], AF.Exp)
        P.copy(tb["k"], tb["k32"], eng="pool")
        P.copy(tb["q"], tb["q32"], eng="pool")
        for h in range(4):
            P.mm(psB[0:64, 0:TT], K["sel4"][:, h * 64:(h + 1) * 64], bfm)
            P.tt(tb["kb"][:, h, :].sub(h), tb["k32"][:, h, :].sub(h), psB[0:64, 0:TT], ALU.mult)
            P.mm(psC[0:64, 0:TT], K["sel4"][:, h * 64:(h + 1) * 64], efm)
            P.tt(tb["qd"][:, h, :].sub(h), tb["q32"][:, h, :].sub(h), psC[0:64, 0:TT], ALU.mult)
        for n in range(NCH):
            cs = slice(n * 64, (n + 1) * 64)
            for h in range(4):
                P.transpose(psD[0:64, h * 64:(h + 1) * 64], tb["k32"][:, h, cs].sub(h), ident[0:64, 0:64])
            P.copy(tb["ktm"][:, n, :, :].re("p h d -> p (h d)"), psD[0:64, 0:256], eng="act")
            for h in range(4):
                P.transpose(psE[0:64, h * 64:(h + 1) * 64], vfm[:, h, cs].sub(h), ident[0:64, 0:64])
            P.copy(tb["bv"][:, n, :, :].re("p h d -> p (h d)"), psE[0:64, 0:256], eng="dve")
            P.transpose(psF[0:64, 0:4], bfm[:, cs], ident[0:4, 0:4])
            P.transpose(psF[0:64, 4:8], gc[:, cs], ident[0:4, 0:4])
            P.transpose(psF[0:64, 8:12], kdf[:, cs], ident[0:4, 0:4])
            P.copy(tb["bg"][:, n, :], psF[0:64, 0:12], eng="act")
        bg = tb["bg"]
        P.ts(tb["ngc"], bg[:, :, 4:8], -1.0, ALU.mult)
        P.act(tb["c2"], bg[:, :, 4:8], AF.Exp)
        P.stt(tb["c2"], tb["c2"], -1.0, bg[:, :, 0:4], ALU.mult, ALU.mult)
        P.tt(tb["ktm"], tb["ktm"], V(bg.ap[:, :, 8:12].unsqueeze(3).to_broadcast([64, NCH, 4, 64]), bg.key), ALU.mult)
        P.tt(tb["bv"], tb["bv"], V(bg.ap[:, :, 0:4].unsqueeze(3).to_broadcast([64, NCH, 4, 64]), bg.key), ALU.mult)
        o_t = ob[ti % 2]

        def g_pre(n):
            cb = CB[n % 2]
            cs = slice(n * 64, (n + 1) * 64)
            gcn = V(bg.ap[:, n, 4:8].unsqueeze(2).to_broadcast([64, 4, 64]), bg.key)
            ngcn = V(tb["ngc"].ap[:, n, :].unsqueeze(2).to_broadcast([64, 4, 64]), tb["ngc"].key)
            E3 = cb["E"].re("p (h c) -> p h c", h=4)
            F3 = cb["F"].re("p (h c) -> p h c", h=4)
            P.tt(E3, K["id4"].re("p (h c) -> p h c", h=4), gcn, ALU.mult)
            P.tt(F3, K["neg4"].re("p (h c) -> p h c", h=4), ngcn, ALU.add)
            P.mm(psG[0:64, 0:256], ones[0:64, 0:64], cb["E"], start=True, stop=False)
            P.mm(psG[0:64, 0:256], ident[0:64, 0:64], cb["F"], start=False, stop=True)
            P.act(cb["G1"], psG[0:64, 0:256], AF.Exp)
            P.ts(cb["E"], cb["E"], -1.0, ALU.mult)
            P.tt(F3, K["neg4T"].re("p (h c) -> p h c", h=4), gcn, ALU.add)
            P.mm(psH[0:64, 0:256], ones[0:64, 0:64], cb["E"], start=True, stop=False)
            P.mm(psH[0:64, 0:256], ident[0:64, 0:64], cb["F"], start=False, stop=True)
            P.act(cb["G2"], psH[0:64, 0:256], AF.Exp)
            P.tt(cb["Gs"], cb["G1"], K["ms4"], ALU.mult, eng="pool")
            P.tt(cb["G2"], cb["G2"], K["ms4T"], ALU.mult, eng="pool")
            for h in range(4):
                hs = slice(h * 64, (h + 1) * 64)
                P.mm(psA[0:64, hs], tb["k"][:, h, cs].sub(h), tb["kb"][:, h, cs].sub(h))
                P.mm(psB[0:64, hs], tb["kb"][:, h, cs].sub(h), tb["k"][:, h, cs].sub(h))
                P.mm(psC[0:64, hs], tb["k"][:, h, cs].sub(h), tb["q"][:, h, cs].sub(h))
            P.stt(cb["B0"], psA[0:64, 0:256], -1.0, cb["Gs"], ALU.mult, ALU.mult)
            P.stt(cb["A0"], psB[0:64, 0:256], -1.0, cb["G2"], ALU.mult, ALU.mult)
            P.tt(cb["Ain"], psC[0:64, 0:256], cb["G1"], ALU.mult)
            return neumann_inv(P, C, cb["A0"], cb["B0"], cb, psA, psB, psC)

        def g_scan(n, TTm):
            cb = CB[n % 2]
            cs = slice(n * 64, (n + 1) * 64)
            for h in range(4):
                hs = slice(h * 64, (h + 1) * 64)
                P.mm(psD[0:64, hs], tb["k"][:, h, cs].sub(h), Sb[:, h, :])
            X3 = cb["X"].re("p (h v) -> p h v", h=4)
            c2n = V(tb["c2"].ap[:, n, :].unsqueeze(2).to_broadcast([64, 4, 64]), tb["c2"].key)
            P.tt(X3, psD[0:64, 0:256].re("p (h v) -> p h v", h=4), c2n, ALU.mult)
            P.tt(X3, X3, tb["bv"][:, n, :, :], ALU.add)
            for h in range(4):
                hs = slice(h * 64, (h + 1) * 64)
                P.mm(psE[0:64, hs], TTm[:, hs], cb["X"][:, hs])
            P.copy(cb["vn"], psE[0:64, 0:256], eng="act")
            for h in range(4):
                hs = slice(h * 64, (h + 1) * 64)
                P.mm(psF[0:64, hs], Sb[:, h, :], tb["qd"][:, h, cs].sub(h), start=True, stop=False)
                P.mm(psF[0:64, hs], cb["vn"][:, hs], cb["Ain"][:, hs], start=False, stop=True)
            P.copy(o_t[:, :, cs], psF[0:64, 0:256].re("p (h c) -> p h c", h=4), eng="act")
            for h in range(4):
                hs = slice(h * 64, (h + 1) * 64)
                P.mm(psG[0:64, hs], tb["ktm"][:, n, h, :], cb["vn"][:, hs])
            dln = V(tb["dl"].ap[:, :, n].unsqueeze(2).to_broadcast([64, 4, 64]), tb["dl"].key)
            P.tt(St, St, dln, ALU.mult)
            P.tt(St, St, psG[0:64, 0:256].re("p (h v) -> p h v", h=4), ALU.add)
            P.copy(Sb, St, eng="pool")

        tt_next = g_pre(0)
        for n in range(NCH):
            tt_cur = tt_next
            if n + 1 < NCH:
                tt_next = g_pre(n + 1)
            g_scan(n, tt_cur)
        for h in range(4):
            P.dma(gate, zT[G_GG + h, 0:64, tsl])
            act_sigmoid(P, rs, gate)
            P.tt(gate, gate, rs, ALU.mult)
            P.act(sq, o_t[:, h, :], AF.Square)
            P.mm(psH[0:64, 0:TT], ones[0:64, 0:64], sq)
            rstd_from_ps(P, rs, psH[0:64, 0:TT], 64.0, 1e-6)
            P.stt(rs, rs, cv[0:64, CV["gng"]:CV["gng"] + 1], gate, ALU.mult, ALU.mult)
            P.tt(sq, o_t[:, h, :], rs, ALU.mult)
            P.dma(oT[h * 64:(h + 1) * 64, tsl].sub(h), sq, q="pool")
    if release:
        P.release(mk)


def phase_rwkv(P, C, S, L, zT, oT, w_up_d, a_up_d, g_up_d, vfT, uT, v_down_d, v_up_d, ttl=512, psbase=None, release=True):
    mk = P.mark()
    TT = ttl
    cv = C.cv
    K = C.k
    nt = S // TT
    NCH = TT // 64
    ones = K["ones"]
    ident = K["ident"]
    col = lambda nm, h: cv[0:64, CV[nm] + h:CV[nm] + h + 1]
    w_up = P.tile([64, 256], name="r_wup"); P.dma(w_up, w_up_d)
    a_up = P.tile([64, 256], name="r_aup"); P.dma(a_up, a_up_d)
    g_up = P.tile([128, 256], name="r_gup"); P.dma(g_up, g_up_d)
    if L > 0:
        v_dn = P.tile([128, 8, 32], name="r_vdn"); P.dma(v_dn, v_down_d.re("(c p) n -> p c n", p=128))
        v_upt = P.tile([32, 256], name="r_vup"); P.dma(v_upt, v_up_d)
    ncv = P.tile([64, 12], name="r_ncv")
    P.ts(ncv[:, 0:4], cv[0:64, CV["w0"]:CV["w0"] + 4], -1.0, ALU.mult, writes=[ncv.sub(0)])
    P.ts(ncv[:, 4:8], cv[0:64, CV["a0"]:CV["a0"] + 4], -1.0, ALU.mult, writes=[ncv.sub(1)])
    P.ts(ncv[:, 8:12], cv[0:64, CV["vb"]:CV["vb"] + 4], -1.0, ALU.mult, writes=[ncv.sub(2)])
    oma = P.tile([64, 4], name="r_oma")
    P.ts(oma, cv[0:64, CV["ka"]:CV["ka"] + 4], -1.0, ALU.mult, 1.0, ALU.add)
    ST = P.tile([64, 4, 64], name="rST")
    P.memset(ST, 0.0)
    STb = P.tile([64, 4, 64], name="rSTb", dtype=BF16)
    P.copy(STb, ST, eng="pool")
    T4 = lambda nm: P.tile([64, 4, TT], name=nm)
    T4b = lambda nm: P.tile([64, 4, TT], name=nm, dtype=BF16)
    at, bt, kt, rt = T4b("r_at"), T4b("r_bt"), T4b("r_kt"), T4b("r_rt")
    bon, gate4 = T4("r_bon"), T4("r_gate")
    t0, t1, t2, t3, t4_, t5 = [T4(f"r_t{i}") for i in range(6)]
    y4 = t0
    bh_tm = P.tile([64, NCH, 4, 64], name="r_bhtm", dtype=BF16)
    kh_tm = P.tile([64, NCH, 4, 64], name="r_khtm", dtype=BF16)
    v_tm = P.tile([64, NCH, 4, 64], name="r_vtm", dtype=BF16)
    WC = P.tile([64, 4, NCH], name="r_WC")
    xin = [P.tile([128, TT + 1], name=f"r_xin{i}") for i in range(2)]
    dd = P.tile([128, TT], name="r_dd")
    lo_w = P.tile([64, TT], name="r_low")
    lo_a = P.tile([64, TT], name="r_loa")
    lo_g = P.tile([128, TT], name="r_log")
    sq = P.tile([64, TT], name="r_sq")
    rs = P.tile([64, TT], name="r_rs")
    if L > 0:
        uxb = [P.tile([128, TT + 1], name=f"r_ux{i}") for i in range(2)]
        xvb = [P.tile([128, TT], name=f"r_xv{i}") for i in range(2)]
        vl = P.tile([32, TT], name="r_vl")
        vf = rs

    def mkch(i):
        d_ = {nm: P.tile([64, 256], name=f"rc_{nm}{i}", dtype=BF16) for nm in ["A0", "B0", "A1", "B1", "TT", "Bak", "Brb", "Brk"]}
        d_["X"] = d_["A1"]
        d_["U"] = d_["B1"]
        return d_
    CB = [mkch(0), mkch(1)]
    if psbase is None:
        psA, psB, psC, psD, psE, psF, psG, psH = [P.pbank(i) for i in range(8)]
    else:
        psA, psB, psC, psD, psE, psF, psG, psH = [P.pbank(psbase + j % 4) for j in range(8)]
    xk = 0

    def shifted(g, rows, ti, dst):
        nonlocal xk
        x = xin[xk % 2]
        xk += 1
        if ti == 0:
            P.memset(x[0:rows, 0:1], 0.0, writes=[x.sub("halo")])
            P.dma(x[0:rows, 1:TT + 1].sub("body"), zT[g, 0:rows, 0:TT])
        else:
            P.dma(x[0:rows, :], zT[g, 0:rows, ti * TT - 1:(ti + 1) * TT])
        P.tt(dd[0:rows, :], x[0:rows, 0:TT], x[0:rows, 1:TT + 1], ALU.subtract)
        P.stt(dst, dd[0:rows, :], cv[0:rows, CV["mu"] + g:CV["mu"] + g + 1], x[0:rows, 1:TT + 1], ALU.mult, ALU.add)

    for ti in range(nt):
        tsl = slice(ti * TT, (ti + 1) * TT)
        r4, k4, v4, kk4, ic4, lw4 = t0, t1, t2, t3, t4_, t5
        shifted(G_WLO, 64, ti, lo_w)
        act_sigmoid(P, lo_w, lo_w, scale=2.0)
        P.ts(lo_w, lo_w, 2.0, ALU.mult, -1.0, ALU.add)
        shifted(G_ALO, 64, ti, lo_a)
        shifted(G_GLO, 128, ti, lo_g)
        act_sigmoid(P, lo_g, lo_g)
        if L > 0:
            uv = uT.re("(c p) t -> p c t", p=128)
            for c in range(8):
                ux = uxb[c % 2]
                xv = xvb[c % 2]
                if ti == 0:
                    P.memset(ux[:, 0:1], 0.0, writes=[ux.sub("halo")])
                    P.dma(ux[:, 1:TT + 1].sub("body"), uv[:, c, 0:TT])
                else:
                    P.dma(ux, uv[:, c, ti * TT - 1:(ti + 1) * TT])
                P.tt(xv, ux[:, 0:TT], ux[:, 1:TT + 1], ALU.subtract)
                P.stt(xv, xv, cv[:, CV["vmu"] + c:CV["vmu"] + c + 1], ux[:, 1:TT + 1], ALU.mult, ALU.add)
                P.mm(psH[0:32, 0:TT], v_dn[:, c, :], xv, start=(c == 0), stop=(c == 7))
            P.copy(vl, psH[0:32, 0:TT], eng="act")
        for h in range(4):
            hs = slice(h * 64, (h + 1) * 64)
            shifted(G_R + h, 64, ti, r4[:, h, :].sub(h))
            shifted(G_K + h, 64, ti, k4[:, h, :].sub(h))
            shifted(G_V + h, 64, ti, v4[:, h, :].sub(h))
            P.mm(psA[0:64, 0:TT], w_up[:, hs], lo_w)
            act_sigmoid(P, lw4[:, h, :].sub(h), psA[0:64, 0:TT], negbias=ncv[:, h:h + 1])
            P.mm(psB[0:64, 0:TT], a_up[:, hs], lo_a)
            act_sigmoid(P, ic4[:, h, :].sub(h), psB[0:64, 0:TT], negbias=ncv[:, 4 + h:5 + h])
            P.mm(psC[0:64, 0:TT], g_up[:, hs], lo_g)
            P.copy(gate4[:, h, :].sub(h), psC[0:64, 0:TT], eng="act")
            if L == 0:
                P.dma(vfT[h * 64:(h + 1) * 64, tsl].sub(h), v4[:, h, :].sub(h), q="pool")
            else:
                P.dma(vf, vfT[h * 64:(h + 1) * 64, tsl])
                P.mm(psD[0:64, 0:TT], v_upt[:, hs], vl)
                act_sigmoid(P, sq, psD[0:64, 0:TT], negbias=ncv[:, 8 + h:9 + h])
                P.tt(vf, vf, v4[:, h, :].sub(h), ALU.subtract)
                P.tt(vf, vf, sq, ALU.mult)
                P.tt(v4[:, h, :].sub(h), v4[:, h, :].sub(h), vf, ALU.add)
            P.ts(kk4[:, h, :].sub(h), k4[:, h, :].sub(h), col("kk", h), ALU.mult)
            P.act(sq, kk4[:, h, :].sub(h), AF.Square)
            P.mm(psE[0:64, 0:TT], ones[0:64, 0:64], sq)
            rstd_from_ps(P, rs, psE[0:64, 0:TT], 1.0, 1e-12)
            P.tt(kk4[:, h, :].sub(h), kk4[:, h, :].sub(h), rs, ALU.mult)
            P.ts(sq, ic4[:, h, :].sub(h), col("ka", h), ALU.mult, oma[:, h:h + 1], ALU.add)
            P.tt(k4[:, h, :].sub(h), k4[:, h, :].sub(h), sq, ALU.mult)
            P.stt(sq, r4[:, h, :].sub(h), col("rk", h), k4[:, h, :].sub(h), ALU.mult, ALU.mult)
            P.mm(psF[0:64, 0:TT], ones[0:64, 0:64], sq)
            P.tt(bon[:, h, :].sub(h), psF[0:64, 0:TT], v4[:, h, :].sub(h), ALU.mult)
        P.ts(lw4, lw4, float(-np.exp(-0.5)), ALU.mult)
        for n in range(NCH):
            cs = slice(n * 64, (n + 1) * 64)
            for h in range(4):
                P.transpose(psG[0:64, h * 64:(h + 1) * 64], v4[:, h, cs], ident[0:64, 0:64])
            P.copy(v_tm[:, n, :, :].re("p h d -> p (h d)"), psG[0:64, 0:256], eng="act")
        P.tt(ic4, ic4, kk4, ALU.mult)
        cb_ = [lw4, v4]
        cur = 0
        for sh in (1, 2, 4, 8, 16, 32):
            src = cb_[cur].re("p h (n c) -> p (h n) c", c=64)
            dstt = cb_[1 - cur].re("p h (n c) -> p (h n) c", c=64)
            P.copy(dstt[:, :, 0:sh], src[:, :, 0:sh], eng="pool", writes=[cb_[1 - cur].sub("a")])
            P.tt(dstt[:, :, sh:64], src[:, :, sh:64], src[:, :, 0:64 - sh], ALU.add, writes=[cb_[1 - cur].sub("b")])
            cur = 1 - cur
        assert cur == 0
        cl = lw4
        cl3 = cl.re("p h (n c) -> p (h n) c", c=64)
        e = v4
        e3 = e.re("p h (n c) -> p (h n) c", c=64)
        P.act(e, cl, AF.Exp)
        P.tt(rt, r4, e, ALU.mult)
        P.memset(at.re("p h (n c) -> p (h n) c", c=64)[:, :, 0:1], 1.0, writes=[at.sub("a")])
        P.copy(at.re("p h (n c) -> p (h n) c", c=64)[:, :, 1:64], e3[:, :, 0:63], eng="pool", writes=[at.sub("b")])
        P.stt(at, at, -1.0, kk4, ALU.mult, ALU.mult)
        P.act(e, cl, AF.Exp, scale=-1.0)
        P.tt(bt, ic4, e, ALU.mult)
        P.tt(kt, k4, e, ALU.mult)
        cl4 = cl.re("p h (n c) -> p h n c", c=64)
        P.copy(WC, cl4[:, :, :, 63])
        P.tt(e.re("p h (n c) -> p h n c", c=64), V(WC.ap.unsqueeze(3).to_broadcast([64, 4, NCH, 64]), WC.key), cl4, ALU.subtract)
        P.act(e, e, AF.Exp)
        P.act(WC, WC, AF.Exp)
        P.tt(ic4, ic4, e, ALU.mult)
        P.tt(k4, k4, e, ALU.mult)
        for n in range(NCH):
            cs = slice(n * 64, (n + 1) * 64)
            for h in range(4):
                P.transpose(psG[0:64, h * 64:(h + 1) * 64], ic4[:, h, cs], ident[0:64, 0:64])
            P.copy(bh_tm[:, n, :, :].re("p h d -> p (h d)"), psG[0:64, 0:256], eng="act")
            for h in range(4):
                P.transpose(psH[0:64, h * 64:(h + 1) * 64], k4[:, h, cs], ident[0:64, 0:64])
            P.copy(kh_tm[:, n, :, :].re("p h d -> p (h d)"), psH[0:64, 0:256], eng="dve")
        def r_pre(n):
            cb = CB[n % 2]
            cs = slice(n * 64, (n + 1) * 64)
            for h in range(4):
                hs = slice(h * 64, (h + 1) * 64)
                P.mm(psA[0:64, hs], bt[:, h, cs], at[:, h, cs])
                P.mm(psB[0:64, hs], at[:, h, cs], bt[:, h, cs])
                P.mm(psC[0:64, hs], kt[:, h, cs], at[:, h, cs])
                P.mm(psD[0:64, hs], bt[:, h, cs], rt[:, h, cs])
            P.tt(cb["B0"], psA[0:64, 0:256], K["ms4"], ALU.mult)
            P.tt(cb["A0"], psB[0:64, 0:256], K["ms4T"], ALU.mult)
            P.tt(cb["Bak"], psC[0:64, 0:256], K["ms4"], ALU.mult)
            P.tt(cb["Brb"], psD[0:64, 0:256], K["mi4"], ALU.mult)
            for h in range(4):
                hs = slice(h * 64, (h + 1) * 64)
                P.mm(psE[0:64, hs], kt[:, h, cs], rt[:, h, cs])
            P.tt(cb["Brk"], psE[0:64, 0:256], K["mi4"], ALU.mult)
            return neumann_inv(P, C, cb["A0"], cb["B0"], cb, psA, psB, psC)

        def r_scan(n, TTm):
            cb = CB[n % 2]
            cs = slice(n * 64, (n + 1) * 64)
            for h in range(4):
                hs = slice(h * 64, (h + 1) * 64)
                P.mm(psD[0:64, hs], at[:, h, cs], STb[:, h, :], start=True, stop=False)
                P.mm(psD[0:64, hs], cb["Bak"][:, hs], v_tm[:, n, h, :], start=False, stop=True)
            P.copy(cb["X"], psD[0:64, 0:256], eng="act")
            for h in range(4):
                hs = slice(h * 64, (h + 1) * 64)
                P.mm(psE[0:64, hs], TTm[:, hs], cb["X"][:, hs])
            P.copy(cb["U"], psE[0:64, 0:256], eng="act")
            for h in range(4):
                hs = slice(h * 64, (h + 1) * 64)
                P.mm(psF[0:64, hs], STb[:, h, :], rt[:, h, cs], start=True, stop=False)
                P.mm(psF[0:64, hs], cb["U"][:, hs], cb["Brb"][:, hs], start=False, stop=False)
                P.mm(psF[0:64, hs], v_tm[:, n, h, :], cb["Brk"][:, hs], start=False, stop=True)
            P.copy(y4[:, :, cs], psF[0:64, 0:256].re("p (h c) -> p h c", h=4), eng="act")
            for h in range(4):
                hs = slice(h * 64, (h + 1) * 64)
                P.mm(psG[0:64, hs], bh_tm[:, n, h, :], cb["U"][:, hs], start=True, stop=False)
                P.mm(psG[0:64, hs], kh_tm[:, n, h, :], v_tm[:, n, h, :], start=False, stop=True)
            wcn = V(WC.ap[:, :, n].unsqueeze(2).to_broadcast([64, 4, 64]), WC.key)
            P.tt(ST, ST, wcn, ALU.mult)
            P.tt(ST, ST, psG[0:64, 0:256].re("p (h v) -> p h v", h=4), ALU.add)
            P.copy(STb, ST, eng="pool")

        tt_next = r_pre(0)
        for n in range(NCH):
            tt_cur = tt_next
            if n + 1 < NCH:
                tt_next = r_pre(n + 1)
            r_scan(n, tt_cur)
        for h in range(4):
            yh = y4[:, h, :]
            P.mm(psH[0:64, 0:TT], ones[0:64, 0:64], yh)
            P.stt(yh, psH[0:64, 0:TT], float(-1.0 / 64), yh, ALU.mult, ALU.add)
            P.act(sq, yh, AF.Square)
            P.mm(psH[0:64, 0:TT], ones[0:64, 0:64], sq)
            rstd_from_ps(P, rs, psH[0:64, 0:TT], 64.0, 64e-5)
            P.stt(yh, yh, col("lng", h), rs, ALU.mult, ALU.mult)
            P.stt(yh, yh, col("lnb", h), bon[:, h, :].sub(h), ALU.add, ALU.add)
            P.tt(sq, yh, gate4[:, h, :].sub(h), ALU.mult)
            P.dma(oT[h * 64:(h + 1) * 64, tsl].sub(h), sq, q="pool")
    if release:
        P.release(mk)


def phase_merge(P, C, NT, hT, oT, gT, wbr_d, wout_d, h1T, dyn=None):
    mk = P.mark()
    nt = NT // TT
    wstg = [P.tile([128, 4, 1024], name=f"f_wstg{i}") for i in range(2)]
    wbr = []
    for br in range(3):
        t = P.tile([128, 4, 1024], name=f"wbr{br}", dtype=BF16)
        P.dma(wstg[br % 2], wbr_d[br].re("(c p) n -> p c n", p=128))
        P.copy(t, wstg[br % 2], eng=("pool" if br % 2 == 0 else "act"))
        wbr.append(t)
    wout = P.tile([128, 8, 1024], name="wout", dtype=BF16)
    wov = wout_d.re("(c p) n -> p c n", p=128)
    for hh in range(2):
        P.dma(wstg[(hh + 1) % 2], wov[:, hh * 4:(hh + 1) * 4, :])
        P.copy(wout[:, hh * 4:(hh + 1) * 4, :].sub(hh), wstg[(hh + 1) % 2], eng=("act" if hh == 0 else "pool"))
    h = P.tile([128, 8, TT], name="f_h")
    o = P.tile([128, 12, TT], name="f_o")
    ob_ = P.tile([128, 12, TT], name="f_ob", dtype=BF16)
    mg = P.tile([128, 8, TT], name="f_mg32") if dyn is not None else None
    mgb = P.tile([128, 8, TT], name="f_mg", dtype=BF16)
    o2 = P.tile([128, 6, TT], name="f_o2") if dyn is not None else None
    g3 = [P.tile([128, 3, TT], name=f"f_g3{i}") for i in range(2)]
    tmp = [P.tile([128, TT], name=f"f_tmp{i}") for i in range(2)]
    tmp2 = [P.tile([128, TT], name=f"f_tmpb{i}") for i in range(2)]
    ps = [P.pbank(i) for i in range(8)]
    hv = hT.re("(c p) t -> p c t", p=128)
    ov = oT.re("(c p) t -> p c t", p=128)
    gv = gT.re("(b c p) t -> p b c t", b=3, p=128)
    h1v = h1T.re("(c p) t -> p c t", p=128)
    for ti in range(nt):
        tsl = slice(ti * TT, (ti + 1) * TT)
        if dyn is not None:
            m0 = C.cv[:, CV["m0"]:CV["m0"] + 1]
            m1 = C.cv[:, CV["m1"]:CV["m1"] + 1]
            tsl2 = slice(dyn + ti * TT, dyn + (ti + 1) * TT)
            P.dma(h, hv[:, :, tsl])
            P.dma(mg, hv[:, :, tsl2])
            P.ts(h, h, m0, ALU.mult)
            P.stt(h, mg, m1, h, ALU.mult, ALU.add)
            for part in range(2):
                cs_ = slice(part * 6, (part + 1) * 6)
                P.dma(o[:, cs_, :].sub(part), ov[:, cs_, tsl])
                P.dma(o2, ov[:, cs_, tsl2])
                P.ts(o[:, cs_, :].sub(part), o[:, cs_, :].sub(part), m0, ALU.mult)
                P.stt(o[:, cs_, :].sub(part), o2, m1, o[:, cs_, :].sub(part), ALU.mult, ALU.add)
        else:
            P.dma(h, hv[:, :, tsl])
            P.dma(o, ov[:, :, tsl])
        P.copy(ob_[:, 0:6, :].sub(0), o[:, 0:6, :], eng="dve")
        P.copy(ob_[:, 6:12, :].sub(1), o[:, 6:12, :], eng="act")
        for n in range(8):
            ns = slice(n * 128, (n + 1) * 128)
            g = g3[n % 2]
            P.dma(g, gv[:, :, n, tsl])
            for br in range(3):
                pp = ps[(n % 2) * 3 + br]
                for k in range(4):
                    P.mm(pp, wbr[br][:, k, ns], ob_[:, br * 4 + k, :], start=(k == 0), stop=(k == 3))
            tm_ = tmp[n % 2]
            P.tt(tm_, ps[(n % 2) * 3 + 0], g[:, 0, :], ALU.mult)
            P.tt(tmp2[n % 2], ps[(n % 2) * 3 + 1], g[:, 1, :], ALU.mult)
            P.tt(tm_, tm_, tmp2[n % 2], ALU.add, eng="pool")
            P.tt(tmp2[n % 2], ps[(n % 2) * 3 + 2], g[:, 2, :], ALU.mult)
            P.tt(mgb[:, n, :].sub(n), tm_, tmp2[n % 2], ALU.add, eng="pool")
        for n in range(8):
            ns = slice(n * 128, (n + 1) * 128)
            pp = ps[6 + n % 2]
            for k in range(8):
                P.mm(pp, wout[:, k, ns], mgb[:, k, :], start=(k == 0), stop=(k == 7))
            P.tt(h[:, n, :].sub(n), h[:, n, :].sub(n), pp, ALU.add)
        P.dma(h1v[:, :, tsl], h, q="pool")
    P.release(mk)


def phase_ffn(P, C, NT, h1T, h2T, gcol, experts, FF, router_d=None):
    mk = P.mark()
    cv = C.cv
    K = C.k
    ones = K["ones"]
    ident = K["ident"]
    nt = NT // TT
    NF = FF // 128
    CB = 512
    blocks = [(c0, min(CB, FF - c0)) for c0 in range(0, FF, CB)]
    h = P.tile([128, 8, TT], name="m_h")
    u = P.tile([128, 8, TT], name="m_u", dtype=BF16)
    rs = P.tile([128, TT], name="m_rs")
    hid_raw = P.tile([128, NF * TT // 2], name="m_hid")
    hid = V(hid_raw.ap.bitcast(BF16).rearrange("p (f t) -> p f t", f=NF), hid_raw.key)
    u32 = V(hid_raw.ap[:, 0:8 * TT].rearrange("p (c t) -> p c t", c=8), hid_raw.key)
    sg = [P.tile([128, TT], name=f"m_sg{i}") for i in range(2)]
    wgb = [P.tile([128, 8, CB], name=f"m_wg{i}") for i in range(2)]
    wub = [P.tile([128, 8, CB], name=f"m_wu{i}") for i in range(2)]
    wgc = [P.tile([128, 8, CB], name=f"m_wgc{i}", dtype=BF16) for i in range(2)]
    wuc = [P.tile([128, 8, CB], name=f"m_wuc{i}", dtype=BF16) for i in range(2)]
    wdb = [P.tile([128, 512], name=f"m_wd{i}") for i in range(3)]
    wdc = [P.tile([128, 512], name=f"m_wdc{i}", dtype=BF16) for i in range(3)]
    ps = [P.pbank(i) for i in range(8)]
    hv = h1T.re("(c p) t -> p c t", p=128)
    h2v = h2T.re("(c p) t -> p c t", p=128)
    ne = len(experts)
    if router_d is not None:
        sel8 = P.tile([8, 1024], name="c_sel8")
        P.dma(sel8, C.sel8_d)
        rt_w = P.tile([128, 8, 8], name="m_rw")
        P.dma(rt_w, router_d.re("(c p) e -> p c e", p=128))
        lg = P.tile([8, TT], name="m_lg")
        ltm = P.tile([128, 4, 8], name="m_ltm")
        l2 = P.tile([128, 4, 8], name="m_l2")
        eq1 = P.tile([128, 4, 8], name="m_eq1")
        eq2 = P.tile([128, 4, 8], name="m_eq2")
        m1 = P.tile([128, 4], name="m_m1")
        m2 = P.tile([128, 4], name="m_m2")
        w1 = P.tile([128, 4], name="m_w1")
        w2 = P.tile([128, 4], name="m_w2")
        gwf = P.tile([8, TT], name="m_gwf")
        gwe = [P.tile([128, TT], name=f"m_gwe{i}") for i in range(2)]
    wk = 0
    dk = 0
    for ti in range(nt):
        tsl = slice(ti * TT, (ti + 1) * TT)
        P.dma(h, hv[:, :, tsl])
        P.act(u32, h, AF.Square)
        for c in range(8):
            P.mm(ps[7], ones, u32[:, c, :], start=(c == 0), stop=(c == 7))
        rstd_from_ps(P, rs, ps[7], D, 1e-6)
        if router_d is not None:
            for c in range(8):
                P.stt(u32[:, c, :], h[:, c, :], cv[:, gcol + c:gcol + c + 1], rs, ALU.mult, ALU.mult)
            P.copy(u, u32, eng="act")
            for c in range(8):
                P.mm(ps[6][0:8, :], rt_w[:, c, :], u32[:, c, :], start=(c == 0), stop=(c == 7))
        else:
            for c in range(8):
                P.stt(u[:, c, :], h[:, c, :], cv[:, gcol + c:gcol + c + 1], rs, ALU.mult, ALU.mult)
        if router_d is not None:
            P.copy(lg, ps[6][0:8, :], eng="act")
            for j in range(4):
                P.transpose(ps[5][:, j * 8:(j + 1) * 8], lg[:, j * 128:(j + 1) * 128], ident[0:8, 0:8])
            P.copy(ltm.re("p j e -> p (j e)"), ps[5][:, 0:32])
            bc = lambda t: V(t.ap.unsqueeze(2).to_broadcast([128, 4, 8]), t.key)
            P.op("dve", lambda e: e.reduce_max(_ap(m1), _ap(ltm), AX.X), [ltm], [m1])
            P.tt(eq1, ltm, bc(m1), ALU.is_equal)
            P.stt(l2, eq1, -1e30, ltm, ALU.mult, ALU.add)
            P.op("dve", lambda e: e.reduce_max(_ap(m2), _ap(l2), AX.X), [l2], [m2])
            P.tt(eq2, l2, bc(m2), ALU.is_equal)
            P.tt(w2, m2, m1, ALU.subtract)
            P.act(w2, w2, AF.Exp)
            P.ts(w1, w2, 1.0, ALU.add)
            P.recip(w1, w1)
            P.tt(w2, w2, w1, ALU.mult)
            P.tt(eq1, eq1, bc(w1), ALU.mult)
            P.tt(eq2, eq2, bc(w2), ALU.mult)
            P.tt(eq1, eq1, eq2, ALU.add)
            for j in range(4):
                P.transpose(ps[5][0:8, j * 128:(j + 1) * 128], eq1[:, j, :], ident)
            P.copy(gwf, ps[5][0:8, :], eng="act")
        work = [(e_, bi) for e_ in range(ne) for bi in range(len(blocks))]

        def prefetch(e_, bi, slot):
            wg_d, wu_d, _ = experts[e_]
            c0_, wdt = blocks[bi]
            wgv = wg_d.re("(c p) f -> p c f", p=128)
            wuv = wu_d.re("(c p) f -> p c f", p=128)
            P.dma(wgb[slot][:, :, 0:wdt], wgv[:, :, c0_:c0_ + wdt])
            P.dma(wub[slot][:, :, 0:wdt], wuv[:, :, c0_:c0_ + wdt], q="act")
            P.copy(wgc[slot][:, :, 0:wdt], wgb[slot][:, :, 0:wdt], eng="dve")
            P.copy(wuc[slot][:, :, 0:wdt], wub[slot][:, :, 0:wdt], eng="act")
        prefetch(work[0][0], work[0][1], wk % 2)
        for wi, (e_, bi) in enumerate(work):
            slot = wk % 2
            wk += 1
            if wi + 1 < len(work):
                prefetch(work[wi + 1][0], work[wi + 1][1], wk % 2)
            c0_, wdt = blocks[bi]
            wg_t, wu_t = wgc[slot], wuc[slot]
            if router_d is not None and bi == 0:
                gw_e = gwe[e_ % 2]
                P.mm(ps[3], sel8[:, e_ * 128:(e_ + 1) * 128], gwf)
                P.copy(gw_e, ps[3], eng="act")
            for j in range(wdt // 128):
                f = c0_ // 128 + j
                pg = ps[4 + f % 2]
                pu = ps[6 + f % 2]
                for c in range(8):
                    P.mm(pg, wg_t[:, c, j * 128:(j + 1) * 128], u[:, c, :], start=(c == 0), stop=(c == 7))
                for c in range(8):
                    P.mm(pu, wu_t[:, c, j * 128:(j + 1) * 128], u[:, c, :], start=(c == 0), stop=(c == 7))
                s_ = sg[f % 2]
                P.act(s_, pg, AF.Silu)
                if router_d is not None:
                    P.tt(s_, s_, gw_e, ALU.mult, eng="pool")
                P.tt(hid[:, f, :].sub(f), s_, pu, ALU.mult)
            if bi == len(blocks) - 1:
                wdv = experts[e_][2].re("(f p) n -> p f n", p=128)
                for half in range(2):
                    for f in range(NF):
                        wd_s = wdb[dk % 3]
                        wd_t = wdc[dk % 3]
                        dk += 1
                        P.dma(wd_s, wdv[:, f, half * 512:(half + 1) * 512])
                        P.copy(wd_t, wd_s, eng=("dve" if dk % 2 == 0 else "act"))
                        for n4 in range(4):
                            P.mm(ps[n4], wd_t[:, n4 * 128:(n4 + 1) * 128], hid[:, f, :].sub(f), start=(f == 0), stop=(f == NF - 1))
                    for n4 in range(4):
                        n = half * 4 + n4
                        P.tt(h[:, n, :].sub(n), h[:, n, :].sub(n), ps[n4], ALU.add)
        P.dma(h2v[:, :, tsl], h, q="pool")
    P.release(mk)


def phase_ple(P, C, NT, h2T, pT, proj_d, pgate_d, gcol, h3T):
    mk = P.mark()
    cv = C.cv
    ones = C.k["ones"]
    nt = NT // TT
    pstg = [P.tile([128, 4, 1024], name=f"p_stg{i}") for i in range(2)]
    proj = P.tile([128, 2, 1024], name="p_proj", dtype=BF16)
    P.dma(pstg[0][:, 0:2, :], proj_d.re("(c p) n -> p c n", p=128))
    P.copy(proj, pstg[0][:, 0:2, :], eng="pool")
    pg = P.tile([128, 8, 1024], name="p_gate", dtype=BF16)
    pgv = pgate_d.re("(c p) n -> p c n", p=128)
    for hh in range(2):
        P.dma(pstg[(hh + 1) % 2], pgv[:, hh * 4:(hh + 1) * 4, :])
        P.copy(pg[:, hh * 4:(hh + 1) * 4, :].sub(hh), pstg[(hh + 1) % 2], eng=("act" if hh == 0 else "pool"))
    h = P.tile([128, 8, TT], name="p_h")
    hb_ = P.tile([128, 8, TT], name="p_hb", dtype=BF16)
    pt = P.tile([128, 2, TT], name="p_p")
    ptb = P.tile([128, 2, TT], name="p_pb", dtype=BF16)
    er = P.tile([128, 8, TT], name="p_er")
    ho = P.tile([128, 8, TT], name="p_ho")
    sq = P.tile([128, TT], name="p_sq")
    rs = P.tile([128, TT], name="p_rs")
    gp = [P.tile([128, TT], name=f"p_gp{i}") for i in range(2)]
    ps = [P.pbank(i) for i in range(8)]
    hv = h2T.re("(c p) t -> p c t", p=128)
    pv = pT.re("(c p) t -> p c t", p=128)
    h3v = h3T.re("(c p) t -> p c t", p=128)
    for ti in range(nt):
        tsl = slice(ti * TT, (ti + 1) * TT)
        P.dma(h, hv[:, :, tsl])
        P.dma(pt, pv[:, :, tsl])
        P.copy(ptb, pt, eng="dve")
        P.copy(hb_, h, eng="act")
        for n in range(8):
            ns = slice(n * 128, (n + 1) * 128)
            pp = ps[n % 2]
            P.mm(pp, proj[:, 0, ns], ptb[:, 0, :], start=True, stop=False)
            P.mm(pp, proj[:, 1, ns], ptb[:, 1, :], start=False, stop=True)
            P.copy(er[:, n, :].sub(n), pp, eng="act")
            P.act(sq, pp, AF.Square)
            P.mm(ps[2], ones, sq, start=(n == 0), stop=(n == 7))
        rstd_from_ps(P, rs, ps[2], D, 1e-6)
        for n in range(8):
            ns = slice(n * 128, (n + 1) * 128)
            pp = ps[3 + n % 2]
            for k in range(8):
                P.mm(pp, pg[:, k, ns], hb_[:, k, :], start=(k == 0), stop=(k == 7))
            g = gp[n % 2]
            P.act(g, pp, AF.Sigmoid)
            P.stt(er[:, n, :].sub(n), er[:, n, :].sub(n), cv[:, gcol + n:gcol + n + 1], rs, ALU.mult, ALU.mult)
            P.tt(g, g, er[:, n, :].sub(n), ALU.mult)
            P.tt(ho[:, n, :].sub(n), h[:, n, :], g, ALU.add)
        P.dma(h3v[:, :, tsl], ho, q="pool")
    P.release(mk)


def own(hg, width=64):
    return slice(hg * 4 * width, (hg + 1) * 4 * width)


def col4(v):
    return np.ascontiguousarray(v.reshape(4, 64).T)


def col2(v):
    return np.ascontiguousarray(v.reshape(2, 128).T)


def mixer_host_inputs(inp, L, b, hg):
    f = np.float32
    w_in = inp["w_in"][L]
    o = own(hg)
    cols = np.concatenate([
        np.arange(0, 512)[o], np.arange(512, 1024)[o], np.arange(1024, 1536)[o],
        np.arange(1536, 1600), np.arange(1600, 1664), np.arange(1664, 1792),
        np.arange(1792, 2048), np.arange(2048, 2176), np.arange(2176, 2208),
        np.arange(2208, 2720)[o], np.arange(2720, 3232)[o], np.arange(3232, 3744)[o],
        np.arange(3760, 4272)[o],
        np.arange(3744, 3752)[hg * 4:(hg + 1) * 4], np.arange(3752, 3760)[hg * 4:(hg + 1) * 4]])
    assert len(cols) == NZ
    d = {}
    d["w_in_m"] = np.ascontiguousarray(w_in[:, cols])
    cv = np.zeros((128, NCV), f)

    def put(nm, arr):
        arr = np.asarray(arr, f)
        cv[:arr.shape[0], CV[nm]:CV[nm] + arr.shape[1]] = arr
    put("nmg", inp["norm_mix_g"][L].reshape(8, 128).T)
    mu = inp["rwkv_mu"][L]
    mucols = np.zeros((128, 15), f)
    rcols = cols[:1024]
    for g in range(15):
        seg = mu[rcols[ZOFF[g]:ZOFF[g + 1]]]
        mucols[:len(seg), g] = seg
    put("mu", mucols)
    put("w0", col4(inp["rwkv_w0"][L][o]))
    put("a0", col4(inp["rwkv_a0"][L][o]))
    put("kk", col4(inp["rwkv_k_k"][L][o]))
    put("ka", col4(inp["rwkv_k_a"][L][o]))
    put("rk", col4(inp["rwkv_r_k"][L].reshape(512)[o]))
    put("lng", col4(inp["rwkv_ln_g"][L][o]))
    put("lnb", col4(inp["rwkv_ln_b"][L][o]))
    if L > 0:
        put("vmu", inp["vres_mu"][L - 1].reshape(8, 128).T)
        put("vb", col4(inp["vres_b"][L - 1][o]))
    put("qng", inp["mla_q_norm_g"][L].reshape(2, 128).T)
    put("kvng", inp["mla_kv_norm_g"][L].reshape(128, 1))
    put("qkq", inp["mla_qk_norm_q"][L].reshape(96, 1))
    put("qkk", inp["mla_qk_norm_k"][L].reshape(96, 1))
    invf = (1.0 / (10000.0 ** (np.arange(0, 32, 2, dtype=f) / f(32)))).astype(f)
    iv = np.zeros((96, 1), f)
    iv[64:80, 0] = invf
    iv[80:96, 0] = invf
    put("invf", iv)
    put("ropec", np.full((128, 1), -np.pi, f))
    cw = inp["gdn_conv_w"][L]
    convc = np.zeros((128, 48), f)
    for seg in range(3):
        cc = cw[:, seg * 512:(seg + 1) * 512][:, o]
        for hh in range(4):
            for j in range(4):
                convc[:64, (seg * 4 + hh) * 4 + j] = cc[j, hh * 64:(hh + 1) * 64]
    put("conv", convc)
    put("alog", inp["gdn_a_log"][L][hg * 4:(hg + 1) * 4].reshape(4, 1))
    put("dtb", inp["gdn_dt_bias"][L][hg * 4:(hg + 1) * 4].reshape(4, 1))
    put("gng", inp["gdn_norm_g"][L].reshape(64, 1))
    d["cv"] = cv
    d["w_up"] = np.ascontiguousarray(inp["rwkv_w_up"][L][:, o])
    d["a_up"] = np.ascontiguousarray(inp["rwkv_a_up"][L][:, o])
    d["g_up"] = np.ascontiguousarray(inp["rwkv_g_up"][L][:, o])
    if L > 0:
        d["v_down"] = np.ascontiguousarray(inp["vres_down"][L - 1])
        d["v_up"] = np.ascontiguousarray(inp["vres_up"][L - 1][:, o])
    d["w_uq"] = np.ascontiguousarray(inp["mla_w_uq"][L][:, hg * 384:(hg + 1) * 384])
    ukv = inp["mla_w_ukv"][L].reshape(128, 8, 128)[:, hg * 4:(hg + 1) * 4, :]
    d["w_uk"] = np.ascontiguousarray(ukv[:, :, :64].reshape(128, 256))
    d["w_uv"] = np.ascontiguousarray(ukv[:, :, 64:].reshape(128, 256))
    d["pos"] = np.ascontiguousarray(inp["positions"][b:b + 1].astype(np.int32))
    for k_, v_ in consts_np().items():
        d["c_" + k_] = v_
    return d
from concourse.bass_utils import run_bass_kernel_spmd

B_, S_, NCORE = 4, 4096, 8
M_CONSTS = ["ident", "ones", "att_mask", "ropeRT", "sel4", "id4", "neg4", "neg4T", "ms4", "ms4T", "mi4"]
F_CONSTS = ["ident", "ones"]
_PROG_CACHE = {}


def build_mixer(S, L):
    P = Prog()
    C = Ctx()
    names = []

    def din(name, shape, dt=F32):
        names.append(name)
        return P.dview(P.dram(name, shape, dt, kind="ExternalInput"))
    hT = din("hT", [1024, S])
    w_d = din("w_in_m", [1024, NZ])
    cv_d = din("cv", [128, NCV])
    pos_d = din("pos", [1, S], I32)
    w_uq, w_uk, w_uv = din("w_uq", [256, 384]), din("w_uk", [128, 256]), din("w_uv", [128, 256])
    w_up, a_up, g_up = din("w_up", [64, 256]), din("a_up", [64, 256]), din("g_up", [128, 256])
    zT = P.dview(P.dram("zT", [NG, 128, S], F32, kind="Internal"))
    oT = P.dview(P.dram("oT", [768, S], F32, kind="ExternalOutput"))
    uT = v_down = v_up = None
    if L == 0:
        vfT = P.dview(P.dram("vfT_out", [256, S], F32, kind="ExternalOutput"))
    else:
        vfT = din("vfT_in", [256, S])
        uT = P.dview(P.dram("uT", [1024, S], F32, kind="Internal"))
        v_down, v_up = din("v_down", [1024, 32]), din("v_up", [32, 256])
    C.k = load_consts(P, M_CONSTS)
    names.extend(["c_" + c for c in M_CONSTS])
    C.cv = P.tile([128, NCV], name="cv")
    P.dma(C.cv, cv_d)
    groups = [(int(ZOFF[g]), int(ZG[g])) for g in range(NG)]
    phase_proj(P, C, S, hT, w_d, NZ, groups, zT, CV["nmg"], uT=uT)
    phase_rwkv(P, C, S, L, zT, V(oT.ap[0:256, :], "oT_r"), w_up, a_up, g_up, vfT, uT, v_down, v_up)
    phase_mla(P, C, S, zT, pos_d, w_uq, w_uk, w_uv, V(oT.ap[256:512, :], "oT_m"))
    phase_gdn(P, C, S, zT, V(oT.ap[512:768, :], "oT_g"))
    P.finalize()
    return P.nc, names


def build_token(NT, L):
    P = Prog()
    C = Ctx()
    names = []

    def din(name, shape, dt=F32):
        names.append(name)
        return P.dview(P.dram(name, shape, dt, kind="ExternalInput"))
    hT = din("hT", [1024, NT])
    oT = din("oT_all", [1536, NT])
    pT = din("pT", [256, NT])
    cv_d = din("cv", [128, NCV])
    w_g = din("w_gate", [1024, 3072])
    wbr = [din(f"w_br{i}", [512, 1024]) for i in range(3)]
    wout = din("w_out", [1024, 1024])
    proj = din("ple_proj", [256, 1024])
    pgate = din("ple_gate", [1024, 1024])
    if L % 2 == 0:
        experts = [(din("ffn_wg", [1024, 2816]), din("ffn_wu", [1024, 2816]), din("ffn_wd", [2816, 1024]))]
        FF = 2816
        router = None
    else:
        wg_all = din("moe_wg", [8, 1024, 3584])
        wu_all = din("moe_wu", [8, 1024, 3584])
        wd_all = din("moe_wd", [8, 3584, 1024])
        experts = [(V(wg_all.ap[e], wg_all.key), V(wu_all.ap[e], wu_all.key), V(wd_all.ap[e], wd_all.key)) for e in range(8)]
        FF = 3584
        router = din("moe_router", [1024, 8])
    gT = P.dview(P.dram("gT", [3072, NT], F32, kind="Internal"))
    h1T = P.dview(P.dram("h1T", [1024, NT], F32, kind="Internal"))
    h2T = P.dview(P.dram("h2T", [1024, NT], F32, kind="Internal"))
    h3T = P.dview(P.dram("h3T", [1024, NT], F32, kind="ExternalOutput"))
    C.k = load_consts(P, F_CONSTS)
    names.extend(["c_" + c for c in F_CONSTS])
    C.sel8_d = din("c_sel8", [8, 1024])
    C.cv = P.tile([128, NCV], name="cv")
    P.dma(C.cv, cv_d)
    groups = [(g * 128, 128) for g in range(24)]
    phase_proj(P, C, NT, hT, w_g, 3072, groups, gT, CV["nmg"], func=AF.Sigmoid, nbuf=1, zflat=True)
    phase_merge(P, C, NT, hT, oT, gT, wbr, wout, h1T)
    phase_ffn(P, C, NT, h1T, h2T, CV["nfg"], experts, FF, router)
    phase_ple(P, C, NT, h2T, pT, proj, pgate, CV["png"], h3T)
    P.finalize()
    return P.nc, names


def token_host_inputs(inp, L, half=0):
    f = np.float32
    d = {}
    cv = np.zeros((128, NCV), f)
    cv[:, CV["m0"]] = 1.0 if half == 0 else 0.0
    cv[:, CV["m1"]] = 1.0 if half == 1 else 0.0
    cv[:, CV["nmg"]:CV["nmg"] + 8] = inp["norm_mix_g"][L].reshape(8, 128).T
    cv[:, CV["nfg"]:CV["nfg"] + 8] = inp["norm_ffn_g"][L].reshape(8, 128).T
    cv[:, CV["png"]:CV["png"] + 8] = inp["ple_norm_g"][L].reshape(8, 128).T
    d["cv"] = cv
    d["w_gate"] = np.ascontiguousarray(inp["w_in"][L][:, 4272:7344])
    d["w_br0"] = inp["w_br_rwkv"][L]
    d["w_br1"] = inp["w_br_mla"][L]
    d["w_br2"] = inp["w_br_gdn"][L]
    d["w_out"] = inp["w_out"][L]
    d["ple_proj"] = inp["ple_proj"][L]
    d["ple_gate"] = inp["ple_gate"][L]
    if L % 2 == 0:
        d["ffn_wg"], d["ffn_wu"], d["ffn_wd"] = inp["ffn_wg"][L // 2], inp["ffn_wu"][L // 2], inp["ffn_wd"][L // 2]
    else:
        d["moe_wg"], d["moe_wu"], d["moe_wd"] = inp["moe_wg"][L // 2], inp["moe_wu"][L // 2], inp["moe_wd"][L // 2]
        d["moe_router"] = inp["moe_router"][L // 2]
    cs = consts_np()
    for c in F_CONSTS + ["sel8"]:
        d["c_" + c] = cs[c]
    return d


def kernel(**inputs):
    inp = {k: np.asarray(v) for k, v in inputs.items()}
    x = inp["x"].astype(np.float32)
    Bn, S, Dm = x.shape
    NT = S // 2
    hT = [np.ascontiguousarray(x[b].T) for b in range(Bn)]
    vf = [None] * NCORE
    for L in range(2):
        key = ("M", S, L)
        if key not in _PROG_CACHE:
            _PROG_CACHE[key] = build_mixer(S, L)
        nc, names = _PROG_CACHE[key]
        in_maps = []
        for core in range(NCORE):
            b, hg = core // 2, core % 2
            d = mixer_host_inputs(inp, L, b, hg)
            d["hT"] = hT[b]
            if L > 0:
                d["vfT_in"] = vf[core]
            in_maps.append({n: np.ascontiguousarray(d[n]) for n in names})
        res = run_bass_kernel_spmd(nc, in_maps, core_ids=list(range(NCORE)))
        oTs = [r["oT"] for r in res.results]
        if L == 0:
            vf = [r["vfT_out"] for r in res.results]
        key = ("F", NT, L)
        if key not in _PROG_CACHE:
            _PROG_CACHE[key] = build_token(NT, L)
        nc, names = _PROG_CACHE[key]
        th = token_host_inputs(inp, L)
        in_maps = []
        for core in range(NCORE):
            b, half = core // 2, core % 2
            tsl = slice(half * NT, (half + 1) * NT)
            d = dict(th)
            d["hT"] = hT[b][:, tsl]
            o0, o1 = oTs[2 * b], oTs[2 * b + 1]
            d["oT_all"] = np.concatenate([o0[0:256, tsl], o1[0:256, tsl], o0[256:512, tsl], o1[256:512, tsl],
                                          o0[512:768, tsl], o1[512:768, tsl]], axis=0)
            d["pT"] = inp["p"][L, b, tsl, :].T
            in_maps.append({n: np.ascontiguousarray(d[n]) for n in names})
        res = run_bass_kernel_spmd(nc, in_maps, core_ids=list(range(NCORE)))
        for b in range(Bn):
            hT[b] = np.concatenate([res.results[2 * b]["h3T"], res.results[2 * b + 1]["h3T"]], axis=1)
    out = np.stack([hT[b].T for b in range(Bn)], axis=0)
    return np.ascontiguousarray(out.astype(np.float32))


ALL_M_KEYS = ["w_in_m", "cv", "w_uq", "w_uk", "w_uv", "w_up", "a_up", "g_up"]


def build_fused(S):
    NTH = S // 2
    P = Prog()
    C = Ctx()
    names = []

    def din(name, shape, dt=F32):
        names.append(name)
        return P.dview(P.dram(name, shape, dt, kind="ExternalInput"))
    hT0 = din("hT0", [1024, S])
    pos_d = din("pos", [1, S], I32)
    pT = [din("pT0", [256, S]), din("pT1", [256, NTH])]
    C.sel8_d = din("c_sel8", [8, 1024])
    mi = {}
    for L in range(2):
        for hg in range(2):
            pre = f"m{L}{hg}_"
            d = {"w_in_m": din(pre + "w_in_m", [1024, NZ]), "cv": din(pre + "cv", [128, NCV]),
                 "w_uq": din(pre + "w_uq", [256, 384]), "w_uk": din(pre + "w_uk", [128, 256]), "w_uv": din(pre + "w_uv", [128, 256]),
                 "w_up": din(pre + "w_up", [64, 256]), "a_up": din(pre + "a_up", [64, 256]), "g_up": din(pre + "g_up", [128, 256])}
            if L > 0:
                d["v_down"] = din(pre + "v_down", [1024, 32])
                d["v_up"] = din(pre + "v_up", [32, 256])
            mi[(L, hg)] = d
    ti_ = {}
    for L in range(2):
        pre = f"t{L}_"
        d = {"cv": din(pre + "cv", [128, NCV]), "w_gate": din(pre + "w_gate", [1024, 3072]),
             "wbr": [din(pre + f"w_br{i}", [512, 1024]) for i in range(3)], "w_out": din(pre + "w_out", [1024, 1024]),
             "ple_proj": din(pre + "ple_proj", [256, 1024]), "ple_gate": din(pre + "ple_gate", [1024, 1024])}
        if L % 2 == 0:
            d["experts"] = [(din(pre + "ffn_wg", [1024, 2816]), din(pre + "ffn_wu", [1024, 2816]), din(pre + "ffn_wd", [2816, 1024]))]
            d["FF"] = 2816
            d["router"] = None
        else:
            wg_all = din(pre + "moe_wg", [8, 1024, 3584])
            wu_all = din(pre + "moe_wu", [8, 1024, 3584])
            wd_all = din(pre + "moe_wd", [8, 3584, 1024])
            d["experts"] = [(V(wg_all.ap[e], wg_all.key), V(wu_all.ap[e], wu_all.key), V(wd_all.ap[e], wd_all.key)) for e in range(8)]
            d["FF"] = 3584
            d["router"] = din(pre + "moe_router", [1024, 8])
        ti_[L] = d
    zT = P.dview(P.dram("zT", [NG, 128, S], F32, kind="Internal"))
    uT = P.dview(P.dram("uT", [1024, S], F32, kind="Internal"))
    oTa = P.dview(P.dram("oT_all", [1536, S], F32, kind="Internal"))
    vfT = P.dview(P.dram("vfT", [512, S], F32, kind="Internal"))
    gT = P.dview(P.dram("gT", [3072, S], F32, kind="Internal"))
    h1T = P.dview(P.dram("h1T", [1024, S], F32, kind="Internal"))
    h2T = P.dview(P.dram("h2T", [1024, S], F32, kind="Internal"))
    hT1 = P.dview(P.dram("hT1", [1024, S], F32, kind="Internal"))
    h3T = P.dview(P.dram("h3T", [1024, NTH], F32, kind="ExternalOutput"))
    C.k = load_consts(P, M_CONSTS)
    names.extend(["c_" + c for c in M_CONSTS])
    C.cv = P.tile([128, NCV], name="cv")
    groups = [(int(ZOFF[g]), int(ZG[g])) for g in range(NG)]
    ggroups = [(g * 128, 128) for g in range(24)]
    hin = hT0
    for L in range(2):
        for hg in range(2):
            d = mi[(L, hg)]
            P.dma(C.cv, d["cv"])
            phase_proj(P, C, S, hin, d["w_in_m"], NZ, groups, zT, CV["nmg"], uT=(uT if L > 0 else None))
            sub = lambda br: V(oTa.ap[br * 512 + hg * 256:br * 512 + (hg + 1) * 256, :], f"oT_{br}_{hg}")
            vfv = V(vfT.ap[hg * 256:(hg + 1) * 256, :], f"vfT_{hg}")
            mk_ = P.mark()
            o_r, o_g = sub(0), sub(2)
            P.run_interleaved([
                lambda: phase_rwkv(P, C, S, L, zT, o_r, d["w_up"], d["a_up"], d["g_up"], vfv,
                                   (uT if L > 0 else None), d.get("v_down"), d.get("v_up"), ttl=256, psbase=0, release=False),
                lambda: phase_gdn(P, C, S, zT, o_g, ttl=256, psbase=4, release=False)])
            P.release(mk_)
            phase_mla(P, C, S, zT, pos_d, d["w_uq"], d["w_uk"], d["w_uv"], sub(1))
        t = ti_[L]
        P.dma(C.cv, t["cv"])
        if L == 0:
            NT, dyn, hout = S, None, hT1
        else:
            NT, dyn, hout = NTH, NTH, h3T
        gv = V(gT.ap[:, 0:NT], gT.key)
        h1v = V(h1T.ap[:, 0:NT], h1T.key)
        h2v = V(h2T.ap[:, 0:NT], h2T.key)
        phase_proj(P, C, NT, hin, t["w_gate"], 3072, ggroups, gv, CV["nmg"], func=AF.Sigmoid, nbuf=1, zflat=True, dyn=dyn)
        phase_merge(P, C, NT, hin, oTa, gv, t["wbr"], t["w_out"], h1v, dyn=dyn)
        phase_ffn(P, C, NT, h1v, h2v, CV["nfg"], t["experts"], t["FF"], t["router"])
        phase_ple(P, C, NT, h2v, pT[L], t["ple_proj"], t["ple_gate"], CV["png"], hout)
        hin = hT1
    P.finalize()
    return P.nc, names


def kernel_unfused(**inputs):
    return _kernel_unfused(**inputs)


_kernel_unfused = kernel


def kernel(**inputs):
    inp = {k: np.asarray(v) for k, v in inputs.items()}
    x = inp["x"].astype(np.float32)
    Bn, S, Dm = x.shape
    NTH = S // 2
    key = ("FUSED", S)
    if key not in _PROG_CACHE:
        _PROG_CACHE[key] = build_fused(S)
    nc, names = _PROG_CACHE[key]
    cs = consts_np()
    shared = {"c_" + k: v for k, v in cs.items()}
    tok = []
    for L in range(2):
        th = token_host_inputs(inp, L)
        tok.append({f"t{L}_" + k: v for k, v in th.items() if not k.startswith("c_")})
    in_maps = []
    for core in range(NCORE):
        b, half = core // 2, core % 2
        d = dict(shared)
        d["hT0"] = x[b].T
        d["pos"] = inp["positions"][b:b + 1].astype(np.int32)
        d["pT0"] = inp["p"][0, b].T
        d["pT1"] = inp["p"][1, b, half * NTH:(half + 1) * NTH, :].T
        for L in range(2):
            for hg in range(2):
                md = mixer_host_inputs(inp, L, b, hg)
                for k, v in md.items():
                    if not k.startswith("c_") and k != "pos":
                        d[f"m{L}{hg}_" + k] = v
            d.update(tok[L])
            cvt = tok[L][f"t{L}_cv"].copy()
            cvt[:, CV["m0"]] = 1.0 if half == 0 else 0.0
            cvt[:, CV["m1"]] = 1.0 if half == 1 else 0.0
            d[f"t{L}_cv"] = cvt
        in_maps.append({n: np.ascontiguousarray(d[n]) for n in names})
    res = run_bass_kernel_spmd(nc, in_maps, core_ids=list(range(NCORE)))
    out = np.empty((Bn, S, Dm), np.float32)
    for core in range(NCORE):
        b, half = core // 2, core % 2
        out[b, half * NTH:(half + 1) * NTH, :] = res.results[core]["h3T"].T
    return out
```

```python
import numpy as np
from contextlib import ExitStack
import concourse.bass as bass
import concourse.mybir as mybir

F32 = mybir.dt.float32
BF16 = mybir.dt.bfloat16
I32 = mybir.dt.int32
ALU = mybir.AluOpType
AF = mybir.ActivationFunctionType
AX = mybir.AxisListType

ENGS = ("pe", "act", "dve", "pool", "sp")
N_DMA_SEMS = 24


class Op:
    __slots__ = ("eng", "fn", "deps", "is_dma", "idx", "sig", "slot", "slot_target", "slot_prev", "epoch")

    def __init__(self, eng, fn, is_dma):
        self.eng = eng
        self.fn = fn
        self.is_dma = is_dma
        self.deps = set()
        self.sig = 0
        self.slot = None
        self.slot_target = 0
        self.slot_prev = None


class V:
    __slots__ = ("ap", "key")

    def __init__(self, ap, key):
        self.ap = ap
        self.key = key

    def __getitem__(self, idx):
        return V(self.ap[idx], self.key)

    def sub(self, k):
        base = self.key[0] if isinstance(self.key, tuple) else self.key
        return V(self.ap, (base, k))

    def re(self, pat, **kw):
        return V(self.ap.rearrange(pat, **kw), self.key)

    def bc(self, shape):
        return V(self.ap.to_broadcast(list(shape)), self.key)

    @property
    def shape(self):
        return self.ap.shape


def _ap(x):
    return x.ap if isinstance(x, V) else x


ARENA_F32 = 53000


class Prog:
    def __init__(self, name="k"):
        self.nc = bass.Bass("TRN2", target_bir_lowering=False)
        self.st = ExitStack()
        self.arena = None
        self.aoff = 0
        self.amax = 0
        self.psum = None
        self.bar_deps = None
        self.since_bar = []
        self.bar_seen = {}
        self.epoch = 0
        self.ep_cnt = {}
        self.ops = []
        self.track = {}
        self.n_dma = 0
        self.slot_last = [None] * N_DMA_SEMS
        self.slot_count = [0] * N_DMA_SEMS
        self.uid = 0

    def sb(self, shape, dtype=F32, name=None):
        self.uid += 1
        return self.st.enter_context(self.nc.sbuf_tensor(name or f"sb{self.uid}", list(shape), dtype))

    def ps(self, shape, dtype=F32, name=None):
        self.uid += 1
        return self.st.enter_context(self.nc.psum_tensor(name or f"ps{self.uid}", list(shape), dtype))

    def dram(self, name, shape, dtype=F32, kind="Internal"):
        return self.nc.dram_tensor(name, list(shape), dtype, kind=kind)

    def tile(self, shape, name=None, dtype=None):
        if self.arena is None:
            self.arena = self.st.enter_context(self.nc.sbuf_tensor("arena", [128, ARENA_F32], F32))
        self.uid += 1
        p = shape[0]
        n = int(np.prod(shape[1:]))
        if dtype == BF16:
            nw = (n + 1) // 2
            assert self.aoff + nw <= ARENA_F32, f"arena overflow {self.aoff}+{nw}"
            ap = self.arena[0:p, self.aoff:self.aoff + nw].bitcast(BF16)[:, 0:n]
            self.aoff += nw
        else:
            assert self.aoff + n <= ARENA_F32, f"arena overflow {self.aoff}+{n}"
            ap = self.arena[0:p, self.aoff:self.aoff + n]
            self.aoff += n
        self.amax = max(self.amax, self.aoff)
        if len(shape) == 3:
            ap = ap.rearrange("p (a b) -> p a b", a=shape[1])
        elif len(shape) == 4:
            ap = ap.rearrange("p (a b c) -> p a b c", a=shape[1], b=shape[2])
        return V(ap, name or f"t{self.uid}")

    def mark(self):
        return self.aoff

    def release(self, mark):
        self.barrier()
        self.aoff = mark

    def pbank(self, i):
        if self.psum is None:
            self.psum = [self.st.enter_context(self.nc.psum_tensor(f"psb{j}", [128, 512], F32)) for j in range(8)]
        return V(self.psum[i][:], f"psb{i}")

    def pslot(self, bank, half):
        self.pbank(0)
        return V(self.psum[bank][:, half * 256:(half + 1) * 256], f"psb{bank}_{half}")

    def run_interleaved(self, fns):
        import threading
        il = {"turn": 0, "alive": [True] * len(fns), "cv": threading.Condition(), "tl": threading.local()}
        errs = []

        def nxt(i):
            n = len(fns)
            for d in range(1, n + 1):
                j = (i + d) % n
                if il["alive"][j]:
                    return j
            return i

        def runner(i, fn):
            with il["cv"]:
                while il["turn"] != i:
                    il["cv"].wait()
            il["tl"].i = i
            try:
                fn()
            except BaseException as e:
                errs.append(e)
            finally:
                with il["cv"]:
                    il["alive"][i] = False
                    il["turn"] = nxt(i)
                    il["cv"].notify_all()

        def yield_turn():
            i = getattr(il["tl"], "i", None)
            if i is None:
                return
            with il["cv"]:
                j = nxt(i)
                if j == i:
                    return
                il["turn"] = j
                il["cv"].notify_all()
                while il["turn"] != i:
                    il["cv"].wait()

        self._yield = yield_turn
        ths = [threading.Thread(target=runner, args=(i, f)) for i, f in enumerate(fns)]
        for t in ths:
            t.start()
        for t in ths:
            t.join()
        self._yield = None
        if errs:
            raise errs[0]

    def dview(self, t, name=None):
        ap = t.ap() if hasattr(t, "ap") and callable(t.ap) else t
        return V(ap, name or ap.tensor.name)

    def barrier(self):
        self.bar_deps = list(self.since_bar) if self.bar_deps is None else self.bar_deps + self.since_bar
        last = {}
        dm = []
        for o in self.bar_deps:
            if o.is_dma:
                dm.append(o)
            else:
                last[o.eng] = o
        self.bar_deps = list(last.values()) + dm[-2 * N_DMA_SEMS:]
        self.since_bar = []
        self.bar_seen = {}
        if max(self.ep_cnt.values(), default=0) > 20000:
            self.epoch += 1
            self.ep_cnt = {}

    @staticmethod
    def _key(x):
        if isinstance(x, V):
            x = x.key
        if isinstance(x, tuple):
            t, sub = x
        else:
            t, sub = x, None
        nm = t if isinstance(t, str) else (t.name if hasattr(t, "name") else t.tensor.name)
        return nm, sub

    def _conf(self, nm, sub):
        ent = self.track.setdefault(nm, {})
        if sub is None:
            return list(ent.keys())
        ks = [k for k in ent.keys() if k is None or k == sub]
        return ks

    def op(self, eng, fn, reads=(), writes=(), is_dma=False):
        o = Op(eng, fn, is_dma)
        o.epoch = self.epoch
        if not is_dma:
            self.ep_cnt[eng] = self.ep_cnt.get(eng, 0) + 1
        for r in reads:
            nm, sub = self._key(r)
            ent = self.track.setdefault(nm, {})
            for k in self._conf(nm, sub):
                w = ent[k][0]
                if w is not None:
                    o.deps.add(w)
        for wv in writes:
            nm, sub = self._key(wv)
            ent = self.track.setdefault(nm, {})
            for k in self._conf(nm, sub):
                w, rs = ent[k]
                if w is not None:
                    o.deps.add(w)
                for r in rs:
                    o.deps.add(r)
        for r in reads:
            nm, sub = self._key(r)
            ent = self.track[nm]
            if sub not in ent:
                ent[sub] = [None, []]
            ent[sub][1].append(o)
        for wv in writes:
            nm, sub = self._key(wv)
            ent = self.track[nm]
            if sub is None:
                for k in list(ent.keys()):
                    del ent[k]
            ent[sub] = [o, []]
        o.deps.discard(o)
        if self.bar_deps is not None and not self.bar_seen.get(eng):
            self.bar_seen[eng] = True
            o.deps.update(self.bar_deps)
        self.since_bar.append(o)
        if is_dma:
            s = self.n_dma % N_DMA_SEMS
            self.n_dma += 1
            o.slot = s
            o.slot_prev = self.slot_last[s]
            self.slot_count[s] += 1
            o.slot_target = 16 * self.slot_count[s]
            self.slot_last[s] = o
        o.idx = len(self.ops)
        self.ops.append(o)
        if getattr(self, "_yield", None) is not None:
            self._yield()
        return o

    def dma(self, out, in_, reads=None, writes=None, q="sp", in_fn=None, **kw):
        if in_fn is not None:
            return self.op(q, lambda e: e.dma_start(out=_ap(out), in_=in_fn(e), **kw),
                           reads if reads is not None else [in_], writes if writes is not None else [out], is_dma=True)
        return self.op(q, lambda e: e.dma_start(out=_ap(out), in_=_ap(in_), **kw),
                       reads if reads is not None else [in_], writes if writes is not None else [out], is_dma=True)

    def mm(self, out, lhsT, rhs, start=True, stop=True, reads=None, writes=None, **kw):
        return self.op("pe", lambda e: e.matmul(_ap(out), _ap(lhsT), _ap(rhs), start=start, stop=stop, **kw),
                       reads if reads is not None else [lhsT, rhs], writes if writes is not None else [out])

    def transpose(self, out, in_, ident, reads=None, writes=None):
        return self.op("pe", lambda e: e.transpose(_ap(out), _ap(in_), _ap(ident)),
                       reads if reads is not None else [in_, ident], writes if writes is not None else [out])

    def act(self, out, in_, func, bias=None, scale=1.0, reads=None, writes=None, accum_out=None, eng="act"):
        kw = {}
        rd = [in_]
        if bias is not None:
            kw["bias"] = _ap(bias)
            if not isinstance(bias, (int, float)):
                rd.append(bias)
        if not isinstance(scale, (int, float)):
            rd.append(scale)
        wr = [out]
        if accum_out is not None:
            kw["accum_out"] = _ap(accum_out)
            wr.append(accum_out)
        return self.op(eng, lambda e: e.activation(_ap(out), _ap(in_), func, scale=_ap(scale), **kw),
                       reads if reads is not None else rd, writes if writes is not None else wr)

    def tt(self, out, in0, in1, op, eng="dve", reads=None, writes=None):
        return self.op(eng, lambda e: e.tensor_tensor(_ap(out), _ap(in0), _ap(in1), op),
                       reads if reads is not None else [in0, in1], writes if writes is not None else [out])

    def ts(self, out, in0, s1, op0, s2=None, op1=None, eng="dve", reads=None, writes=None):
        rd = [in0] + [s for s in (s1, s2) if s is not None and not isinstance(s, (int, float))]
        if op1 is None:
            f = lambda e: e.tensor_scalar(_ap(out), _ap(in0), _ap(s1), None, op0)
        else:
            f = lambda e: e.tensor_scalar(_ap(out), _ap(in0), _ap(s1), _ap(s2), op0, op1)
        return self.op(eng, f, reads if reads is not None else rd, writes if writes is not None else [out])

    def stt(self, out, in0, scalar, in1, op0, op1, eng="dve", reads=None, writes=None):
        rd = [in0, in1] + ([] if isinstance(scalar, (int, float)) else [scalar])
        return self.op(eng, lambda e: e.scalar_tensor_tensor(_ap(out), _ap(in0), _ap(scalar), _ap(in1), op0, op1),
                       reads if reads is not None else rd, writes if writes is not None else [out])

    def copy(self, out, in_, eng="dve", reads=None, writes=None):
        if eng == "act":
            f = lambda e: e.copy(_ap(out), _ap(in_))
        else:
            f = lambda e: e.tensor_copy(_ap(out), _ap(in_))
        return self.op(eng, f, reads if reads is not None else [in_], writes if writes is not None else [out])

    def memset(self, ap, val, eng="pool", writes=None):
        return self.op(eng, lambda e: e.memset(_ap(ap), val), [], writes if writes is not None else [ap])

    def recip(self, out, in_, reads=None, writes=None):
        return self.op("dve", lambda e: e.reciprocal(_ap(out), _ap(in_)),
                       reads if reads is not None else [in_], writes if writes is not None else [out])

    def finalize(self, final_waits=()):
        nc = self.nc
        ops = self.ops
        needed = set()
        for o in ops:
            for d in o.deps:
                if not d.is_dma:
                    needed.add(d.idx)
        cnt = {}
        for o in ops:
            if not o.is_dma and (o.idx in needed):
                k_ = (o.eng, o.epoch)
                cnt[k_] = cnt.get(k_, 0) + 1
                o.sig = cnt[k_]
            else:
                o.sig = 0
        sems = {k_: self.st.enter_context(nc.semaphore(f"s_{k_[0]}_{k_[1]}")) for k_ in cnt}
        dsems = [self.st.enter_context(nc.semaphore(f"s_d{i}")) for i in range(N_DMA_SEMS)]
        per_eng = {e: [o for o in ops if o.eng == e] for e in ENGS}
        final = list(final_waits)

        def body(eng_name):
            def _b(e):
                waited = {}
                def wait(key, sem, val):
                    if waited.get(key, 0) >= val:
                        return
                    e.wait_ge(sem, val)
                    waited[key] = val
                for o in per_eng[eng_name]:
                    for d in sorted(o.deps, key=lambda x: x.idx):
                        if d.is_dma:
                            wait(("d", d.slot), dsems[d.slot], d.slot_target)
                        else:
                            if d.eng == eng_name and eng_name == "pe":
                                continue
                            wait(("c", d.eng, d.epoch), sems[(d.eng, d.epoch)], d.sig)
                    if o.is_dma and o.slot_prev is not None:
                        wait(("d", o.slot), dsems[o.slot], o.slot_prev.slot_target)
                    ins = o.fn(e)
                    if o.is_dma:
                        ins.then_inc(dsems[o.slot], 16)
                    elif o.sig:
                        ins.then_inc(sems[(eng_name, o.epoch)], 1)
                if eng_name == "sp":
                    for s in range(N_DMA_SEMS):
                        if self.slot_last[s] is not None:
                            wait(("d", s), dsems[s], self.slot_last[s].slot_target)
            return _b

        with nc.Block() as block:
            block.tensor(body("pe"))
            block.scalar(body("act"))
            block.vector(body("dve"))
            block.gpsimd(body("pool"))
            block.sync(body("sp"))
        self.st.close()
        return nc

D = 1024
TT = 512
ZG = [64] * 12 + [64, 64, 128] + [128, 128, 128, 32] + [64] * 16 + [4, 4]
NG = len(ZG)
G_R, G_K, G_V, G_WLO, G_ALO, G_GLO = 0, 4, 8, 12, 13, 14
G_CQ, G_CKV, G_KPE = 15, 17, 18
G_GQ, G_GK, G_GV, G_GG, G_GB, G_GA = 19, 23, 27, 31, 35, 36
ZOFF = np.concatenate([[0], np.cumsum(ZG)]).astype(int)
NZ = int(ZOFF[-1])

CV = {}
_n = 0
for nm, k in [("nmg", 8), ("mu", 15), ("w0", 4), ("a0", 4), ("kk", 4), ("ka", 4), ("rk", 4), ("lng", 4), ("lnb", 4),
              ("vmu", 8), ("vb", 4), ("qng", 2), ("kvng", 1), ("qkq", 1), ("qkk", 1), ("invf", 1),
              ("conv", 48), ("alog", 1), ("dtb", 1), ("gng", 1), ("ropec", 1), ("nfg", 8), ("png", 8), ("m0", 1), ("m1", 1)]:
    CV[nm] = _n
    _n += k
NCV = _n


def consts_np():
    c = {}
    c["ident"] = np.eye(128, dtype=np.float32)
    c["ones"] = np.ones((128, 128), np.float32)
    bd = np.zeros((128, 128), np.float32)
    bd[:64, :64] = 1
    bd[64:, 64:] = 1
    c["bd64"] = bd
    s = np.arange(64)[:, None]
    t = np.arange(64)[None, :]
    incl = (t >= s).astype(np.float32)
    strict = (t > s).astype(np.float32)
    c["m_incl"] = np.concatenate([incl, incl], 0)
    c["m_strict"] = np.concatenate([strict, strict], 0)
    c["m_incl_T"] = np.concatenate([incl.T, incl.T], 0)
    c["m_strict_T"] = np.concatenate([strict.T, strict.T], 0)
    c["neg_incl"] = (1.0 - c["m_incl"]) * -1e4
    c["neg_incl_T"] = (1.0 - c["m_incl_T"]) * -1e4
    c["id64x2"] = np.concatenate([np.eye(64, dtype=np.float32)] * 2, 0)
    kk = np.arange(128)[:, None]
    qq = np.arange(128)[None, :]
    c["att_mask"] = (qq >= kk).astype(np.float32)
    R = np.zeros((96, 96), np.float32)
    for m in range(16):
        R[64 + m, 64 + m + 16] = -1.0
        R[64 + 16 + m, 64 + m] = 1.0
    c["ropeRT"] = np.ascontiguousarray(R.T)
    sel = np.zeros((4, 4, 64), np.float32)
    for h in range(4):
        sel[h, h, :] = 1.0
    c["sel4"] = sel.reshape(4, 256)
    sel8 = np.zeros((8, 8, 128), np.float32)
    for e in range(8):
        sel8[e, e, :] = 1.0
    c["sel8"] = sel8.reshape(8, 1024)
    c["id4"] = np.tile(np.eye(64, dtype=np.float32)[:, None, :], (1, 4, 1)).reshape(64, 256)
    c["neg4"] = np.tile(c["neg_incl"][:64][:, None, :], (1, 4, 1)).reshape(64, 256)
    c["neg4T"] = np.tile(c["neg_incl_T"][:64][:, None, :], (1, 4, 1)).reshape(64, 256)
    c["ms4"] = np.tile(c["m_strict"][:64][:, None, :], (1, 4, 1)).reshape(64, 256)
    c["ms4T"] = np.tile(c["m_strict_T"][:64][:, None, :], (1, 4, 1)).reshape(64, 256)
    c["mi4"] = np.tile(c["m_incl"][:64][:, None, :], (1, 4, 1)).reshape(64, 256)
    return c


CONST_SHAPES = {k: v.shape for k, v in consts_np().items()}


class Ctx:
    pass


def load_consts(P, names):
    out = {}
    for nm in names:
        shp = CONST_SHAPES[nm]
        d = P.dview(P.dram("c_" + nm, shp, F32, kind="ExternalInput"))
        t = P.tile(list(shp), name="c_" + nm)
        P.dma(t, d)
        out[nm] = t
    return out


def rstd_from_ps(P, out, ps, n, eps):
    P.act(out, ps, AF.Ln, scale=float(1.0 / n), bias=float(eps))
    P.act(out, out, AF.Exp, scale=-0.5)


def act_sigmoid(P, out, in_, scale=1.0, negbias=None):
    if negbias is None:
        P.act(out, in_, AF.Exp, scale=-float(scale))
    else:
        P.act(out, in_, AF.Exp, scale=-float(scale), bias=negbias)
    P.act(out, out, AF.Ln, bias=1.0)
    P.act(out, out, AF.Exp, scale=-1.0)


def phase_proj(P, C, S, hT, w_d, ncols, groups, zT, gcol, uT=None, func=None, nbuf=2, zflat=False, dyn=None):
    mk = P.mark()
    cv = C.cv
    w = P.tile([128, 8, ncols], name="w_in", dtype=BF16)
    wst = [P.tile([128, 8, 512], name=f"wst{i}") for i in range(2)]
    wv = w_d.re("(c p) n -> p c n", p=128)
    step = 512
    for i_, c0 in enumerate(range(0, ncols, step)):
        c1 = min(ncols, c0 + step)
        st_ = wst[i_ % 2]
        P.dma(st_[:, :, 0:c1 - c0], wv[:, :, c0:c1])
        P.copy(w[:, :, c0:c1].sub(c0), st_[:, :, 0:c1 - c0], eng=("pool" if i_ % 2 == 0 else "act"))
    hb = [P.tile([128, 8, TT], name=f"hb{i}") for i in range(nbuf)]
    sq = P.tile([128, 8, TT], name="sq")
    ub = [P.tile([128, 8, TT], name=f"ub{i}", dtype=BF16) for i in range(nbuf)]
    rs = P.tile([128, TT], name="rs")
    stg = [P.tile([128, TT], name=f"stg{i}") for i in range(4)]
    hv = hT.re("(c p) t -> p c t", p=128)
    ps_ss = P.pbank(0)
    pz = [P.pbank(1 + i) for i in range(4)]
    nt = S // TT
    k = 0
    for ti in range(nt):
        tsl = slice(ti * TT, (ti + 1) * TT)
        h = hb[ti % nbuf]
        u = ub[ti % nbuf]
        if dyn is not None:
            P.dma(h, hv[:, :, tsl])
            P.dma(sq, hv[:, :, dyn + ti * TT:dyn + (ti + 1) * TT])
            P.ts(h, h, cv[:, CV["m0"]:CV["m0"] + 1], ALU.mult)
            P.stt(h, sq, cv[:, CV["m1"]:CV["m1"] + 1], h, ALU.mult, ALU.add)
        else:
            P.dma(h, hv[:, :, tsl])
        P.act(sq, h, AF.Square)
        for c in range(8):
            P.mm(ps_ss, C.k["ones"], sq[:, c, :], start=(c == 0), stop=(c == 7))
        rstd_from_ps(P, rs, ps_ss, D, 1e-6)
        if uT is not None:
            for c in range(8):
                P.stt(sq[:, c, :], h[:, c, :], cv[:, gcol + c:gcol + c + 1], rs, ALU.mult, ALU.mult)
            P.dma(uT.re("(c p) t -> p c t", p=128)[:, :, tsl], sq, q="pool")
            P.copy(u, sq, eng="act")
        else:
            for c in range(8):
                P.stt(u[:, c, :], h[:, c, :], cv[:, gcol + c:gcol + c + 1], rs, ALU.mult, ALU.mult)
        for gi, (co, n) in enumerate(groups):
            pp = pz[k % 4]
            st = stg[k % 4]
            for c in range(8):
                wsl = w[:, c, co:co + n]
                if co // step == (co + n - 1) // step:
                    wsl = wsl.sub((co // step) * step)
                P.mm(pp[0:n, :], wsl, u[:, c, :], start=(c == 0), stop=(c == 7))
            if func is not None:
                P.act(st[0:n, :], pp[0:n, :], func)
            else:
                P.copy(st[0:n, :], pp[0:n, :], eng=("act" if k % 2 == 0 else "dve"))
            if zflat:
                P.dma(zT[co:co + n, tsl].sub(gi), st[0:n, :], q="pool")
            else:
                P.dma(zT[gi, 0:n, tsl].sub(gi), st[0:n, :], q="pool")
            k += 1
    P.release(mk)


def phase_mla(P, C, S, zT, pos_d, w_uq_d, w_uk_d, w_uv_d, oT):
    mk = P.mark()
    cv = C.cv
    K = C.k
    nt = S // TT
    wuq_f = P.tile([128, 2, 384], name="wuq_f")
    P.dma(wuq_f, w_uq_d.re("(c p) n -> p c n", p=128))
    wuk_f = P.tile([128, 256], name="wuk_f")
    P.dma(wuk_f, w_uk_d)
    wuv_f = P.tile([128, 256], name="wuv_f")
    P.dma(wuv_f, w_uv_d)
    wuq = P.tile([128, 2, 384], name="wuq", dtype=BF16)
    wuk = P.tile([128, 256], name="wuk", dtype=BF16)
    wuv = P.tile([128, 256], name="wuv", dtype=BF16)
    P.copy(wuq, wuq_f, eng="pool")
    P.copy(wuk, wuk_f, eng="pool")
    P.copy(wuv, wuv_f, eng="pool")
    ones_b = P.tile([128, 64], name="ones_b", dtype=BF16)
    P.copy(ones_b, K["ones"][:, 0:64], eng="pool")
    KT = P.tile([96, 4, S], name="KT", dtype=BF16)
    VT = P.tile([128, S // 128, 256], name="Vtm", dtype=BF16)
    rc = P.tile([96, TT], name="rope_c")
    rsn = P.tile([96, TT], name="rope_s")
    posi = P.tile([96, TT], name="posi")
    posf = P.tile([96, TT], name="posf")
    ang = P.tile([96, TT], name="ang")
    tmp = P.tile([96, TT], name="ropetmp")
    tmp2 = P.tile([96, TT], name="ropetmp2")
    P.memset(rc[0:64, :], 1.0, writes=[rc.sub("lo")])
    P.memset(rsn[0:64, :], 0.0, writes=[rsn.sub("lo")])
    pi_ap = V(posi.ap.bitcast(I32), posi.key)
    invf = cv[64:96, CV["invf"]:CV["invf"] + 1]
    negpi = cv[64:96, CV["ropec"]:CV["ropec"] + 1]
    TWO_PI = float(2 * np.pi)

    def rope_tile(ti):
        tsl = slice(ti * TT, (ti + 1) * TT)
        P.dma(pi_ap[64:96, :], V(pos_d.ap[:, tsl].partition_broadcast(32), pos_d.key))
        P.copy(posf[64:96, :], pi_ap[64:96, :])
        P.ts(ang[64:96, :], posf[64:96, :], invf, ALU.mult)
        for dst, shift in ((rsn, 0.0), (rc, float(np.pi / 2))):
            a_ = ang[64:96, :]
            if shift:
                P.ts(tmp2[64:96, :], ang[64:96, :], shift, ALU.add)
                a_ = tmp2[64:96, :]
            P.ts(tmp[64:96, :], a_, float(1.0 / TWO_PI), ALU.mult)
            P.copy(pi_ap[64:96, :], tmp[64:96, :])
            P.copy(tmp[64:96, :], pi_ap[64:96, :])
            P.stt(tmp[64:96, :], tmp[64:96, :], -TWO_PI, a_, ALU.mult, ALU.add)
            P.ts(posf[64:96, :], tmp[64:96, :], float(np.pi), ALU.is_gt, TWO_PI, ALU.mult)
            P.tt(tmp[64:96, :], tmp[64:96, :], posf[64:96, :], ALU.subtract)
            P.act(dst[64:96, :], tmp[64:96, :], AF.Sin, writes=[dst.sub("hi")])

    ones = K["ones"]
    cq = [P.tile([128, 2, TT], name=f"cq{i}") for i in range(2)]
    ckv = [P.tile([128, TT], name=f"ckv{i}") for i in range(2)]
    cqb = [P.tile([128, 2, TT], name=f"cqb{i}", dtype=BF16) for i in range(2)]
    ckvb = [P.tile([128, TT], name=f"ckvb{i}", dtype=BF16) for i in range(2)]
    kpe = [P.tile([96, TT], name=f"kpe{i}") for i in range(2)]
    sq = P.tile([128, 2, TT], name="msq")
    rs = P.tile([128, TT], name="mrs")
    raw = P.tile([96, TT], name="raw")
    nrm = P.tile([96, TT], name="nrm")
    rot = P.tile([96, TT], name="rot")
    QT = P.tile([96, 4, TT], name="QT", dtype=BF16)
    ps_a = P.pbank(0)
    ps_b = P.pbank(1)
    ps_c = P.pbank(2)

    def qk_finish(src_raw, gcolname, dst, tsl):
        P.act(sq[0:96, 0, :], src_raw, AF.Square)
        P.mm(ps_b[0:96, :], ones[0:96, 0:96], sq[0:96, 0, :])
        rstd_from_ps(P, rs[0:96, :], ps_b[0:96, :], 96, 1e-6)
        g = cv[0:96, CV[gcolname]:CV[gcolname] + 1]
        P.stt(nrm, src_raw, g, rs[0:96, :], ALU.mult, ALU.mult)
        P.mm(ps_c[0:96, :], K["ropeRT"], nrm)
        P.tt(rot, ps_c[0:96, :], rsn, ALU.mult)
        P.tt(nrm, nrm, rc, ALU.mult, eng="pool")
        P.tt(dst, nrm, rot, ALU.add)

    def load_norm(ti, want_q):
        tsl = slice(ti * TT, (ti + 1) * TT)
        i2 = ti % 2
        if want_q:
            P.dma(cq[i2], zT[G_CQ:G_CQ + 2, :, tsl].re("g p t -> p g t"))
            P.act(sq, cq[i2], AF.Square)
            P.mm(ps_a, ones, sq[:, 0, :], start=True, stop=False)
            P.mm(ps_a, ones, sq[:, 1, :], start=False, stop=True)
            rstd_from_ps(P, rs, ps_a, 256, 1e-6)
            for c in range(2):
                P.stt(cqb[i2][:, c, :], cq[i2][:, c, :], cv[:, CV["qng"] + c:CV["qng"] + c + 1], rs, ALU.mult, ALU.mult)
        else:
            P.dma(ckv[i2], zT[G_CKV, :, tsl])
            P.dma(kpe[i2][64:96, :], zT[G_KPE, 0:32, tsl])
            P.act(sq[:, 0, :], ckv[i2], AF.Square)
            P.mm(ps_a, ones, sq[:, 0, :])
            rstd_from_ps(P, rs, ps_a, 128, 1e-6)
            P.stt(ckvb[i2], ckv[i2], cv[:, CV["kvng"]:CV["kvng"] + 1], rs, ALU.mult, ALU.mult)
        return tsl, i2

    for ti in range(nt):
        rope_tile(ti)
        tsl, i2 = load_norm(ti, False)
        for h in range(4):
            P.mm(ps_b[0:64, :], wuk[:, h * 64:(h + 1) * 64], ckvb[i2])
            P.copy(raw[0:64, :], ps_b[0:64, :], eng="act", writes=[raw.sub("lo")])
            P.copy(raw[64:96, :], kpe[i2][64:96, :], eng="pool", writes=[raw.sub("hi")])
            qk_finish(raw, "qkk", KT[:, h, tsl].sub(h), tsl)
        for j in range(TT // 128):
            P.mm(ps_c[:, 0:256], ckvb[i2][:, j * 128:(j + 1) * 128], wuv)
            P.copy(VT[:, ti * (TT // 128) + j, :], ps_c[:, 0:256], eng="act")

    pt = [P.tile([128, TT], name=f"pt{i}", dtype=BF16) for i in range(3)]
    osb = P.tile([64, TT], name="osb")
    lsb = P.tile([64, TT], name="lsb")
    ps_s = [P.pbank(3), P.pbank(4)]
    ps_o = P.pbank(5)
    ps_l = P.pbank(6)
    scale = float(96 ** -0.5)
    for ti in range(nt):
        rope_tile(ti)
        tsl, i2 = load_norm(ti, True)
        for h in range(4):
            for c in range(2):
                P.mm(ps_b[0:96, :], wuq[:, c, h * 96:(h + 1) * 96], cqb[i2][:, c, :], start=(c == 0), stop=(c == 1))
            P.copy(raw, ps_b[0:96, :], eng="act")
            qk_finish(raw, "qkq", QT[:, h, :].sub(h), tsl)
        for h in range(4):
            nkc = 4 * (ti + 1)

            def c0_of(kc):
                j = kc - 4 * ti
                return 0 if j <= 0 else j * 128

            def score(kc):
                c0 = c0_of(kc)
                P.mm(ps_s[kc % 2][:, c0:TT], KT[:, h, kc * 128:(kc + 1) * 128].sub(h), QT[:, h, c0:TT].sub(h))
            score(0)
            for kc in range(nkc):
                if kc + 1 < nkc:
                    score(kc + 1)
                j = kc - 4 * ti
                c0 = c0_of(kc)
                pss = ps_s[kc % 2]
                p_t = pt[kc % 3]
                P.act(p_t[:, c0:TT], pss[:, c0:TT], AF.Exp, scale=scale)
                if j >= 0:
                    P.tt(p_t[:, c0:c0 + 128], p_t[:, c0:c0 + 128], K["att_mask"], ALU.mult, eng="pool")
                P.mm(ps_o[0:64, c0:TT], VT[:, kc, h * 64:(h + 1) * 64], p_t[:, c0:TT], start=(kc == 0), stop=(kc == nkc - 1))
                P.mm(ps_l[0:64, c0:TT], ones_b, p_t[:, c0:TT], start=(kc == 0), stop=(kc == nkc - 1))
            P.act(lsb, ps_l[0:64, :], AF.Ln)
            P.act(lsb, lsb, AF.Exp, scale=-1.0)
            P.tt(osb, ps_o[0:64, :], lsb, ALU.mult)
            P.dma(oT[h * 64:(h + 1) * 64, tsl].sub(h), osb, q="pool")
    P.release(mk)


def neumann_inv(P, C, A0, B0, bufs, ps1, ps2, ps3):
    id4 = C.k["id4"]
    TTt = bufs["TT"]
    P.tt(TTt, B0, id4, ALU.add)
    A = [A0, bufs["A1"]]
    B = [B0, bufs["B1"]]
    for k in range(1, 6):
        a_prev, a_new = A[(k - 1) % 2], A[k % 2]
        b_prev, b_new = B[(k - 1) % 2], B[k % 2]
        for h in range(4):
            hs = slice(h * 64, (h + 1) * 64)
            P.mm(ps1[0:64, hs], b_prev[:, hs], a_prev[:, hs])
        if k < 5:
            for h in range(4):
                hs = slice(h * 64, (h + 1) * 64)
                P.mm(ps2[0:64, hs], a_prev[:, hs], b_prev[:, hs])
        P.copy(a_new, ps1[0:64, 0:256], eng="act")
        if k < 5:
            P.copy(b_new, ps2[0:64, 0:256], eng="dve")
        for h in range(4):
            hs = slice(h * 64, (h + 1) * 64)
            P.mm(ps3[0:64, hs], a_new[:, hs], TTt[:, hs])
        P.tt(TTt, TTt, ps3[0:64, 0:256], ALU.add)
    return TTt


def phase_gdn(P, C, S, zT, oT, ttl=512, psbase=None, release=True):
    mk = P.mark()
    TT = ttl
    cv = C.cv
    K = C.k
    nt = S // TT
    NCH = TT // 64
    ones = K["ones"]
    ident = K["ident"]
    cvw = lambda seg, h, j: cv[0:64, CV["conv"] + (seg * 4 + h) * 4 + j:CV["conv"] + (seg * 4 + h) * 4 + j + 1]
    St = P.tile([64, 4, 64], name="gS")
    P.memset(St, 0.0)
    Sb = P.tile([64, 4, 64], name="gSb", dtype=BF16)
    P.copy(Sb, St, eng="pool")
    nA = P.tile([4, 1], name="nA")
    P.act(nA, cv[0:4, CV["alog"]:CV["alog"] + 1], AF.Exp)
    P.ts(nA, nA, -1.0, ALU.mult)
    def mkbuf(i):
        b = {}
        for nm in ["q", "k", "kb", "qd"]:
            b[nm] = P.tile([64, 4, TT], name=f"g{nm}{i}", dtype=BF16)
        b["k32"] = P.tile([64, 4, TT], name=f"gk32{i}")
        b["q32"] = P.tile([64, 4, TT], name=f"gq32{i}")
        b["ktm"] = P.tile([64, NCH, 4, 64], name=f"gktm{i}", dtype=BF16)
        b["bv"] = P.tile([64, NCH, 4, 64], name=f"gbv{i}")
        b["bg"] = P.tile([64, NCH, 12], name=f"gbg{i}")
        b["c2"] = P.tile([64, NCH, 4], name=f"gc2{i}")
        b["ngc"] = P.tile([64, NCH, 4], name=f"gngc{i}")
        b["dl"] = P.tile([64, 4, NCH], name=f"gdl{i}")
        return b
    TB = [mkbuf(0), mkbuf(1)]
    xin4 = [P.tile([64, TT + 3], name=f"gxin{i}") for i in range(4)]
    acc4 = [P.tile([64, TT], name=f"gacc{i}") for i in range(4)]
    sq4 = [P.tile([64, TT], name=f"gsq4{i}") for i in range(4)]
    rs4 = [P.tile([64, TT], name=f"grs4{i}") for i in range(4)]
    vfm = P.tile([64, 4, TT], name="gvfm")
    sq = P.tile([64, TT], name="gsq")
    rs = P.tile([64, TT], name="grs")
    bfm = P.tile([4, TT], name="gbfm")
    gfm = [P.tile([4, TT], name=f"ggfm{i}") for i in range(2)]
    efm = P.tile([4, TT], name="gefm")
    kdf = P.tile([4, TT], name="gkdf")
    gl4 = P.tile([4, NCH], name="ggl4")
    ob = [P.tile([64, 4, TT], name=f"gob{i}") for i in range(2)]
    gate = P.tile([64, TT], name="ggate")
    def mkch(i):
        d_ = {nm: P.tile([64, 256], name=f"gc_{nm}{i}") for nm in ["E", "F", "G1", "G2", "Gs"]}
        d_.update({nm: P.tile([64, 256], name=f"gc_{nm}{i}", dtype=BF16) for nm in ["A0", "B0", "A1", "B1", "TT", "Ain", "X", "vn"]})
        return d_
    CB = [mkch(0), mkch(1)]
    if psbase is None:
        psA, psB, psC, psD, psE, psF, psG, psH = [P.pbank(i) for i in range(8)]
    else:
        psA, psB, psC, psD, psE, psF, psG, psH = [P.pbank(psbase + j % 4) for j in range(8)]
    xk = 0
    for ti in range(nt):
        tb = TB[ti % 2]
        tsl = slice(ti * TT, (ti + 1) * TT)
        for seg, (g0, dst) in enumerate([(G_GQ, tb["q32"]), (G_GK, tb["k32"]), (G_GV, vfm)]):
            for h in range(4):
                x = xin4[h]
                a = acc4[h]
                if ti == 0:
                    P.memset(x[:, 0:3], 0.0, writes=[x.sub("halo")])
                    P.dma(x[:, 3:TT + 3].sub("body"), zT[g0 + h, 0:64, 0:TT])
                else:
                    P.dma(x, zT[g0 + h, 0:64, ti * TT - 3:(ti + 1) * TT])
                P.ts(a, x[:, 0:TT], cvw(seg, h, 0), ALU.mult)
                for j in range(1, 4):
                    P.stt(a, x[:, j:TT + j], cvw(seg, h, j), a, ALU.mult, ALU.add)
            for h in range(4):
                act_sigmoid(P, sq4[h], acc4[h])
            for h in range(4):
                if seg == 2:
                    P.tt(dst[:, h, :].sub(h), acc4[h], sq4[h], ALU.mult)
                else:
                    P.tt(acc4[h], acc4[h], sq4[h], ALU.mult)
            if seg < 2:
                pss_ = [psA, psB, psC, psD]
                for h in range(4):
                    P.act(sq4[h], acc4[h], AF.Square)
                for h in range(4):
                    P.mm(pss_[h][0:64, 0:TT], ones[0:64, 0:64], sq4[h])
                for h in range(4):
                    P.act(rs4[h], pss_[h][0:64, 0:TT], AF.Ln, scale=1.0, bias=1e-12)
                for h in range(4):
                    P.act(rs4[h], rs4[h], AF.Exp, scale=-0.5)
                for h in range(4):
                    P.stt(dst[:, h, :].sub(h), acc4[h], (0.125 if seg == 0 else 1.0), rs4[h], ALU.mult, ALU.mult)
        P.dma(bfm, zT[G_GB, 0:4, tsl])
        act_sigmoid(P, bfm, bfm)
        g0t = gfm[0]
        P.dma(g0t, zT[G_GA, 0:4, tsl])
        P.act(g0t, g0t, AF.Exp, bias=cv[0:4, CV["dtb"]:CV["dtb"] + 1])
        P.act(g0t, g0t, AF.Ln, bias=1.0)
        P.ts(g0t, g0t, nA[:, 0:1], ALU.mult)
        cur = 0
        for sh in (1, 2, 4, 8, 16, 32):
            src = gfm[cur].re("h (n c) -> h n c", c=64)
            dstt = gfm[1 - cur].re("h (n c) -> h n c", c=64)
            P.copy(dstt[:, :, 0:sh], src[:, :, 0:sh], eng="pool", writes=[gfm[1 - cur].sub("a")])
            P.tt(dstt[:, :, sh:64], src[:, :, sh:64], src[:, :, 0:64 - sh], ALU.add, writes=[gfm[1 - cur].sub("b")])
            cur = 1 - cur
        gc = gfm[cur]
        P.act(efm, gc, AF.Exp)
        gc3 = gc.re("h (n c) -> h n c", c=64)
        P.copy(gl4, gc3[:, :, 63])
        P.tt(kdf.re("h (n c) -> h n c", c=64), V(gl4.ap.unsqueeze(2).to_broadcast([4, NCH, 64]), gl4.key), gc3, ALU.subtract)
        P.act(kdf, kdf, AF.Exp)
        for h in range(4):
            P.mm(psA[0:64, h * NCH:(h + 1) * NCH], K["sel4"][:, h * 64:(h + 1) * 64], gl4)
        P.act(tb["dl"].re("p h n -> p (h n)"), psA[0:64, 0:4 * NCH], AF.Exp)
        P.copy(tb["k"], tb["k32"], eng="pool")
        P.copy(tb["q"], tb["q32"], eng="pool")
        for h in range(4):
            P.mm(psB[0:64, 0:TT], K["sel4"][:, h * 64:(h + 1) * 64], bfm)
            P.tt(tb["kb"][:, h, :].sub(h), tb["k32"][:, h, :].sub(h), psB[0:64, 0:TT], ALU.mult)
            P.mm(psC[0:64, 0:TT], K["sel4"][:, h * 64:(h + 1) * 64], efm)
            P.tt(tb["qd"][:, h, :].sub(h), tb["q32"][:, h, :].sub(h), psC[0:64, 0:TT], ALU.mult)
        for n in range(NCH):
            cs = slice(n * 64, (n + 1) * 64)
            for h in range(4):
                P.transpose(psD[0:64, h * 64:(h + 1) * 64], tb["k32"][:, h, cs].sub(h), ident[0:64, 0:64])
            P.copy(tb["ktm"][:, n, :, :].re("p h d -> p (h d)"), psD[0:64, 0:256], eng="act")
            for h in range(4):
                P.transpose(psE[0:64, h * 64:(h + 1) * 64], vfm[:, h, cs].sub(h), ident[0:64, 0:64])
            P.copy(tb["bv"][:, n, :, :].re("p h d -> p (h d)"), psE[0:64, 0:256], eng="dve")
            P.transpose(psF[0:64, 0:4], bfm[:, cs], ident[0:4, 0:4])
            P.transpose(psF[0:64, 4:8], gc[:, cs], ident[0:4, 0:4])
            P.transpose(psF[0:64, 8:12], kdf[:, cs], ident[0:4, 0:4])
            P.copy(tb["bg"][:, n, :], psF[0:64, 0:12], eng="act")
        bg = tb["bg"]
        P.ts(tb["ngc"], bg[:, :, 4:8], -1.0, ALU.mult)
        P.act(tb["c2"], bg[:, :, 4:8], AF.Exp)
        P.stt(tb["c2"], tb["c2"], -1.0, bg[:, :, 0:4], ALU.mult, ALU.mult)
        P.tt(tb["ktm"], tb["ktm"], V(bg.ap[:, :, 8:12].unsqueeze(3).to_broadcast([64, NCH, 4, 64]), bg.key), ALU.mult)
        P.tt(tb["bv"], tb["bv"], V(bg.ap[:, :, 0:4].unsqueeze(3).to_broadcast([64, NCH, 4, 64]), bg.key), ALU.mult)
        o_t = ob[ti % 2]

        def g_pre(n):
            cb = CB[n % 2]
            cs = slice(n * 64, (n + 1) * 64)
            gcn = V(bg.ap[:, n, 4:8].unsqueeze(2).to_broadcast([64, 4, 64]), bg.key)
            ngcn = V(tb["ngc"].ap[:, n, :].unsqueeze(2).to_broadcast([64, 4, 64]), tb["ngc"].key)
            E3 = cb["E"].re("p (h c) -> p h c", h=4)
            F3 = cb["F"].re("p (h c) -> p h c", h=4)
            P.tt(E3, K["id4"].re("p (h c) -> p h c", h=4), gcn, ALU.mult)
            P.tt(F3, K["neg4"].re("p (h c) -> p h c", h=4), ngcn, ALU.add)
            P.mm(psG[0:64, 0:256], ones[0:64, 0:64], cb["E"], start=True, stop=False)
            P.mm(psG[0:64, 0:256], ident[0:64, 0:64], cb["F"], start=False, stop=True)
            P.act(cb["G1"], psG[0:64, 0:256], AF.Exp)
            P.ts(cb["E"], cb["E"], -1.0, ALU.mult)
            P.tt(F3, K["neg4T"].re("p (h c) -> p h c", h=4), gcn, ALU.add)
            P.mm(psH[0:64, 0:256], ones[0:64, 0:64], cb["E"], start=True, stop=False)
            P.mm(psH[0:64, 0:256], ident[0:64, 0:64], cb["F"], start=False, stop=True)
            P.act(cb["G2"], psH[0:64, 0:256], AF.Exp)
            P.tt(cb["Gs"], cb["G1"], K["ms4"], ALU.mult, eng="pool")
            P.tt(cb["G2"], cb["G2"], K["ms4T"], ALU.mult, eng="pool")
            for h in range(4):
                hs = slice(h * 64, (h + 1) * 64)
                P.mm(psA[0:64, hs], tb["k"][:, h, cs].sub(h), tb["kb"][:, h, cs].sub(h))
                P.mm(psB[0:64, hs], tb["kb"][:, h, cs].sub(h), tb["k"][:, h, cs].sub(h))
                P.mm(psC[0:64, hs], tb["k"][:, h, cs].sub(h), tb["q"][:, h, cs].sub(h))
            P.stt(cb["B0"], psA[0:64, 0:256], -1.0, cb["Gs"], ALU.mult, ALU.mult)
            P.stt(cb["A0"], psB[0:64, 0:256], -1.0, cb["G2"], ALU.mult, ALU.mult)
            P.tt(cb["Ain"], psC[0:64, 0:256], cb["G1"], ALU.mult)
            return neumann_inv(P, C, cb["A0"], cb["B0"], cb, psA, psB, psC)

        def g_scan(n, TTm):
            cb = CB[n % 2]
            cs = slice(n * 64, (n + 1) * 64)
            for h in range(4):
                hs = slice(h * 64, (h + 1) * 64)
                P.mm(psD[0:64, hs], tb["k"][:, h, cs].sub(h), Sb[:, h, :])
            X3 = cb["X"].re("p (h v) -> p h v", h=4)
            c2n = V(tb["c2"].ap[:, n, :].unsqueeze(2).to_broadcast([64, 4, 64]), tb["c2"].key)
            P.tt(X3, psD[0:64, 0:256].re("p (h v) -> p h v", h=4), c2n, ALU.mult)
            P.tt(X3, X3, tb["bv"][:, n, :, :], ALU.add)
            for h in range(4):
                hs = slice(h * 64, (h + 1) * 64)
                P.mm(psE[0:64, hs], TTm[:, hs], cb["X"][:, hs])
            P.copy(cb["vn"], psE[0:64, 0:256], eng="act")
            for h in range(4):
                hs = slice(h * 64, (h + 1) * 64)
                P.mm(psF[0:64, hs], Sb[:, h, :], tb["qd"][:, h, cs].sub(h), start=True, stop=False)
                P.mm(psF[0:64, hs], cb["vn"][:, hs], cb["Ain"][:, hs], start=False, stop=True)
            P.copy(o_t[:, :, cs], psF[0:64, 0:256].re("p (h c) -> p h c", h=4), eng="act")
            for h in range(4):
                hs = slice(h * 64, (h + 1) * 64)
                P.mm(psG[0:64, hs], tb["ktm"][:, n, h, :], cb["vn"][:, hs])
            dln = V(tb["dl"].ap[:, :, n].unsqueeze(2).to_broadcast([64, 4, 64]), tb["dl"].key)
            P.tt(St, St, dln, ALU.mult)
            P.tt(St, St, psG[0:64, 0:256].re("p (h v) -> p h v", h=4), ALU.add)
            P.copy(Sb, St, eng="pool")

        tt_next = g_pre(0)
        for n in range(NCH):
            tt_cur = tt_next
            if n + 1 < NCH:
                tt_next = g_pre(n + 1)
            g_scan(n, tt_cur)
        for h in range(4):
            P.dma(gate, zT[G_GG + h, 0:64, tsl])
            act_sigmoid(P, rs, gate)
            P.tt(gate, gate, rs, ALU.mult)
            P.act(sq, o_t[:, h, :], AF.Square)
            P.mm(psH[0:64, 0:TT], ones[0:64, 0:64], sq)
            rstd_from_ps(P, rs, psH[0:64, 0:TT], 64.0, 1e-6)
            P.stt(rs, rs, cv[0:64, CV["gng"]:CV["gng"] + 1], gate, ALU.mult, ALU.mult)
            P.tt(sq, o_t[:, h, :], rs, ALU.mult)
            P.dma(oT[h * 64:(h + 1) * 64, tsl].sub(h), sq, q="pool")
    if release:
        P.release(mk)


def phase_rwkv(P, C, S, L, zT, oT, w_up_d, a_up_d, g_up_d, vfT, uT, v_down_d, v_up_d, ttl=512, psbase=None, release=True):
    mk = P.mark()
    TT = ttl
    cv = C.cv
    K = C.k
    nt = S // TT
    NCH = TT // 64
    ones = K["ones"]
    ident = K["ident"]
    col = lambda nm, h: cv[0:64, CV[nm] + h:CV[nm] + h + 1]
    w_up = P.tile([64, 256], name="r_wup"); P.dma(w_up, w_up_d)
    a_up = P.tile([64, 256], name="r_aup"); P.dma(a_up, a_up_d)
    g_up = P.tile([128, 256], name="r_gup"); P.dma(g_up, g_up_d)
    if L > 0:
        v_dn = P.tile([128, 8, 32], name="r_vdn"); P.dma(v_dn, v_down_d.re("(c p) n -> p c n", p=128))
        v_upt = P.tile([32, 256], name="r_vup"); P.dma(v_upt, v_up_d)
    ncv = P.tile([64, 12], name="r_ncv")
    P.ts(ncv[:, 0:4], cv[0:64, CV["w0"]:CV["w0"] + 4], -1.0, ALU.mult, writes=[ncv.sub(0)])
    P.ts(ncv[:, 4:8], cv[0:64, CV["a0"]:CV["a0"] + 4], -1.0, ALU.mult, writes=[ncv.sub(1)])
    P.ts(ncv[:, 8:12], cv[0:64, CV["vb"]:CV["vb"] + 4], -1.0, ALU.mult, writes=[ncv.sub(2)])
    oma = P.tile([64, 4], name="r_oma")
    P.ts(oma, cv[0:64, CV["ka"]:CV["ka"] + 4], -1.0, ALU.mult, 1.0, ALU.add)
    ST = P.tile([64, 4, 64], name="rST")
    P.memset(ST, 0.0)
    STb = P.tile([64, 4, 64], name="rSTb", dtype=BF16)
    P.copy(STb, ST, eng="pool")
    T4 = lambda nm: P.tile([64, 4, TT], name=nm)
    T4b = lambda nm: P.tile([64, 4, TT], name=nm, dtype=BF16)
    at, bt, kt, rt = T4b("r_at"), T4b("r_bt"), T4b("r_kt"), T4b("r_rt")
    bon, gate4 = T4("r_bon"), T4("r_gate")
    t0, t1, t2, t3, t4_, t5 = [T4(f"r_t{i}") for i in range(6)]
    y4 = t0
    bh_tm = P.tile([64, NCH, 4, 64], name="r_bhtm", dtype=BF16)
    kh_tm = P.tile([64, NCH, 4, 64], name="r_khtm", dtype=BF16)
    v_tm = P.tile([64, NCH, 4, 64], name="r_vtm", dtype=BF16)
    WC = P.tile([64, 4, NCH], name="r_WC")
    xin = [P.tile([128, TT + 1], name=f"r_xin{i}") for i in range(2)]
    dd = P.tile([128, TT], name="r_dd")
    lo_w = P.tile([64, TT], name="r_low")
    lo_a = P.tile([64, TT], name="r_loa")
    lo_g = P.tile([128, TT], name="r_log")
    sq = P.tile([64, TT], name="r_sq")
    rs = P.tile([64, TT], name="r_rs")
    if L > 0:
        uxb = [P.tile([128, TT + 1], name=f"r_ux{i}") for i in range(2)]
        xvb = [P.tile([128, TT], name=f"r_xv{i}") for i in range(2)]
        vl = P.tile([32, TT], name="r_vl")
        vf = rs

    def mkch(i):
        d_ = {nm: P.tile([64, 256], name=f"rc_{nm}{i}", dtype=BF16) for nm in ["A0", "B0", "A1", "B1", "TT", "Bak", "Brb", "Brk"]}
        d_["X"] = d_["A1"]
        d_["U"] = d_["B1"]
        return d_
    CB = [mkch(0), mkch(1)]
    if psbase is None:
        psA, psB, psC, psD, psE, psF, psG, psH = [P.pbank(i) for i in range(8)]
    else:
        psA, psB, psC, psD, psE, psF, psG, psH = [P.pbank(psbase + j % 4) for j in range(8)]
    xk = 0

    def shifted(g, rows, ti, dst):
        nonlocal xk
        x = xin[xk % 2]
        xk += 1
        if ti == 0:
            P.memset(x[0:rows, 0:1], 0.0, writes=[x.sub("halo")])
            P.dma(x[0:rows, 1:TT + 1].sub("body"), zT[g, 0:rows, 0:TT])
        else:
            P.dma(x[0:rows, :], zT[g, 0:rows, ti * TT - 1:(ti + 1) * TT])
        P.tt(dd[0:rows, :], x[0:rows, 0:TT], x[0:rows, 1:TT + 1], ALU.subtract)
        P.stt(dst, dd[0:rows, :], cv[0:rows, CV["mu"] + g:CV["mu"] + g + 1], x[0:rows, 1:TT + 1], ALU.mult, ALU.add)

    for ti in range(nt):
        tsl = slice(ti * TT, (ti + 1) * TT)
        r4, k4, v4, kk4, ic4, lw4 = t0, t1, t2, t3, t4_, t5
        shifted(G_WLO, 64, ti, lo_w)
        act_sigmoid(P, lo_w, lo_w, scale=2.0)
        P.ts(lo_w, lo_w, 2.0, ALU.mult, -1.0, ALU.add)
        shifted(G_ALO, 64, ti, lo_a)
        shifted(G_GLO, 128, ti, lo_g)
        act_sigmoid(P, lo_g, lo_g)
        if L > 0:
            uv = uT.re("(c p) t -> p c t", p=128)
            for c in range(8):
                ux = uxb[c % 2]
                xv = xvb[c % 2]
                if ti == 0:
                    P.memset(ux[:, 0:1], 0.0, writes=[ux.sub("halo")])
                    P.dma(ux[:, 1:TT + 1].sub("body"), uv[:, c, 0:TT])
                else:
                    P.dma(ux, uv[:, c, ti * TT - 1:(ti + 1) * TT])
                P.tt(xv, ux[:, 0:TT], ux[:, 1:TT + 1], ALU.subtract)
                P.stt(xv, xv, cv[:, CV["vmu"] + c:CV["vmu"] + c + 1], ux[:, 1:TT + 1], ALU.mult, ALU.add)
                P.mm(psH[0:32, 0:TT], v_dn[:, c, :], xv, start=(c == 0), stop=(c == 7))
            P.copy(vl, psH[0:32, 0:TT], eng="act")
        for h in range(4):
            hs = slice(h * 64, (h + 1) * 64)
            shifted(G_R + h, 64, ti, r4[:, h, :].sub(h))
            shifted(G_K + h, 64, ti, k4[:, h, :].sub(h))
            shifted(G_V + h, 64, ti, v4[:, h, :].sub(h))
            P.mm(psA[0:64, 0:TT], w_up[:, hs], lo_w)
            act_sigmoid(P, lw4[:, h, :].sub(h), psA[0:64, 0:TT], negbias=ncv[:, h:h + 1])
            P.mm(psB[0:64, 0:TT], a_up[:, hs], lo_a)
            act_sigmoid(P, ic4[:, h, :].sub(h), psB[0:64, 0:TT], negbias=ncv[:, 4 + h:5 + h])
            P.mm(psC[0:64, 0:TT], g_up[:, hs], lo_g)
            P.copy(gate4[:, h, :].sub(h), psC[0:64, 0:TT], eng="act")
            if L == 0:
                P.dma(vfT[h * 64:(h + 1) * 64, tsl].sub(h), v4[:, h, :].sub(h), q="pool")
            else:
                P.dma(vf, vfT[h * 64:(h + 1) * 64, tsl])
                P.mm(psD[0:64, 0:TT], v_upt[:, hs], vl)
                act_sigmoid(P, sq, psD[0:64, 0:TT], negbias=ncv[:, 8 + h:9 + h])
                P.tt(vf, vf, v4[:, h, :].sub(h), ALU.subtract)
                P.tt(vf, vf, sq, ALU.mult)
                P.tt(v4[:, h, :].sub(h), v4[:, h, :].sub(h), vf, ALU.add)
            P.ts(kk4[:, h, :].sub(h), k4[:, h, :].sub(h), col("kk", h), ALU.mult)
            P.act(sq, kk4[:, h, :].sub(h), AF.Square)
            P.mm(psE[0:64, 0:TT], ones[0:64, 0:64], sq)
            rstd_from_ps(P, rs, psE[0:64, 0:TT], 1.0, 1e-12)
            P.tt(kk4[:, h, :].sub(h), kk4[:, h, :].sub(h), rs, ALU.mult)
            P.ts(sq, ic4[:, h, :].sub(h), col("ka", h), ALU.mult, oma[:, h:h + 1], ALU.add)
            P.tt(k4[:, h, :].sub(h), k4[:, h, :].sub(h), sq, ALU.mult)
            P.stt(sq, r4[:, h, :].sub(h), col("rk", h), k4[:, h, :].sub(h), ALU.mult, ALU.mult)
            P.mm(psF[0:64, 0:TT], ones[0:64, 0:64], sq)
            P.tt(bon[:, h, :].sub(h), psF[0:64, 0:TT], v4[:, h, :].sub(h), ALU.mult)
        P.ts(lw4, lw4, float(-np.exp(-0.5)), ALU.mult)
        for n in range(NCH):
            cs = slice(n * 64, (n + 1) * 64)
            for h in range(4):
                P.transpose(psG[0:64, h * 64:(h + 1) * 64], v4[:, h, cs], ident[0:64, 0:64])
            P.copy(v_tm[:, n, :, :].re("p h d -> p (h d)"), psG[0:64, 0:256], eng="act")
        P.tt(ic4, ic4, kk4, ALU.mult)
        cb_ = [lw4, v4]
        cur = 0
        for sh in (1, 2, 4, 8, 16, 32):
            src = cb_[cur].re("p h (n c) -> p (h n) c", c=64)
            dstt = cb_[1 - cur].re("p h (n c) -> p (h n) c", c=64)
            P.copy(dstt[:, :, 0:sh], src[:, :, 0:sh], eng="pool", writes=[cb_[1 - cur].sub("a")])
            P.tt(dstt[:, :, sh:64], src[:, :, sh:64], src[:, :, 0:64 - sh], ALU.add, writes=[cb_[1 - cur].sub("b")])
            cur = 1 - cur
        assert cur == 0
        cl = lw4
        cl3 = cl.re("p h (n c) -> p (h n) c", c=64)
        e = v4
        e3 = e.re("p h (n c) -> p (h n) c", c=64)
        P.act(e, cl, AF.Exp)
        P.tt(rt, r4, e, ALU.mult)
        P.memset(at.re("p h (n c) -> p (h n) c", c=64)[:, :, 0:1], 1.0, writes=[at.sub("a")])
        P.copy(at.re("p h (n c) -> p (h n) c", c=64)[:, :, 1:64], e3[:, :, 0:63], eng="pool", writes=[at.sub("b")])
        P.stt(at, at, -1.0, kk4, ALU.mult, ALU.mult)
        P.act(e, cl, AF.Exp, scale=-1.0)
        P.tt(bt, ic4, e, ALU.mult)
        P.tt(kt, k4, e, ALU.mult)
        cl4 = cl.re("p h (n c) -> p h n c", c=64)
        P.copy(WC, cl4[:, :, :, 63])
        P.tt(e.re("p h (n c) -> p h n c", c=64), V(WC.ap.unsqueeze(3).to_broadcast([64, 4, NCH, 64]), WC.key), cl4, ALU.subtract)
        P.act(e, e, AF.Exp)
        P.act(WC, WC, AF.Exp)
        P.tt(ic4, ic4, e, ALU.mult)
        P.tt(k4, k4, e, ALU.mult)
        for n in range(NCH):
            cs = slice(n * 64, (n + 1) * 64)
            for h in range(4):
                P.transpose(psG[0:64, h * 64:(h + 1) * 64], ic4[:, h, cs], ident[0:64, 0:64])
            P.copy(bh_tm[:, n, :, :].re("p h d -> p (h d)"), psG[0:64, 0:256], eng="act")
            for h in range(4):
                P.transpose(psH[0:64, h * 64:(h + 1) * 64], k4[:, h, cs], ident[0:64, 0:64])
            P.copy(kh_tm[:, n, :, :].re("p h d -> p (h d)"), psH[0:64, 0:256], eng="dve")
        def r_pre(n):
            cb = CB[n % 2]
            cs = slice(n * 64, (n + 1) * 64)
            for h in range(4):
                hs = slice(h * 64, (h + 1) * 64)
                P.mm(psA[0:64, hs], bt[:, h, cs], at[:, h, cs])
                P.mm(psB[0:64, hs], at[:, h, cs], bt[:, h, cs])
                P.mm(psC[0:64, hs], kt[:, h, cs], at[:, h, cs])
                P.mm(psD[0:64, hs], bt[:, h, cs], rt[:, h, cs])
            P.tt(cb["B0"], psA[0:64, 0:256], K["ms4"], ALU.mult)
            P.tt(cb["A0"], psB[0:64, 0:256], K["ms4T"], ALU.mult)
            P.tt(cb["Bak"], psC[0:64, 0:256], K["ms4"], ALU.mult)
            P.tt(cb["Brb"], psD[0:64, 0:256], K["mi4"], ALU.mult)
            for h in range(4):
                hs = slice(h * 64, (h + 1) * 64)
                P.mm(psE[0:64, hs], kt[:, h, cs], rt[:, h, cs])
            P.tt(cb["Brk"], psE[0:64, 0:256], K["mi4"], ALU.mult)
            return neumann_inv(P, C, cb["A0"], cb["B0"], cb, psA, psB, psC)

        def r_scan(n, TTm):
            cb = CB[n % 2]
            cs = slice(n * 64, (n + 1) * 64)
            for h in range(4):
                hs = slice(h * 64, (h + 1) * 64)
                P.mm(psD[0:64, hs], at[:, h, cs], STb[:, h, :], start=True, stop=False)
                P.mm(psD[0:64, hs], cb["Bak"][:, hs], v_tm[:, n, h, :], start=False, stop=True)
            P.copy(cb["X"], psD[0:64, 0:256], eng="act")
            for h in range(4):
                hs = slice(h * 64, (h + 1) * 64)
                P.mm(psE[0:64, hs], TTm[:, hs], cb["X"][:, hs])
            P.copy(cb["U"], psE[0:64, 0:256], eng="act")
            for h in range(4):
                hs = slice(h * 64, (h + 1) * 64)
                P.mm(psF[0:64, hs], STb[:, h, :], rt[:, h, cs], start=True, stop=False)
                P.mm(psF[0:64, hs], cb["U"][:, hs], cb["Brb"][:, hs], start=False, stop=False)
                P.mm(psF[0:64, hs], v_tm[:, n, h, :], cb["Brk"][:, hs], start=False, stop=True)
            P.copy(y4[:, :, cs], psF[0:64, 0:256].re("p (h c) -> p h c", h=4), eng="act")
            for h in range(4):
                hs = slice(h * 64, (h + 1) * 64)
                P.mm(psG[0:64, hs], bh_tm[:, n, h, :], cb["U"][:, hs], start=True, stop=False)
                P.mm(psG[0:64, hs], kh_tm[:, n, h, :], v_tm[:, n, h, :], start=False, stop=True)
            wcn = V(WC.ap[:, :, n].unsqueeze(2).to_broadcast([64, 4, 64]), WC.key)
            P.tt(ST, ST, wcn, ALU.mult)
            P.tt(ST, ST, psG[0:64, 0:256].re("p (h v) -> p h v", h=4), ALU.add)
            P.copy(STb, ST, eng="pool")

        tt_next = r_pre(0)
        for n in range(NCH):
            tt_cur = tt_next
            if n + 1 < NCH:
                tt_next = r_pre(n + 1)
            r_scan(n, tt_cur)
        for h in range(4):
            yh = y4[:, h, :]
            P.mm(psH[0:64, 0:TT], ones[0:64, 0:64], yh)
            P.stt(yh, psH[0:64, 0:TT], float(-1.0 / 64), yh, ALU.mult, ALU.add)
            P.act(sq, yh, AF.Square)
            P.mm(psH[0:64, 0:TT], ones[0:64, 0:64], sq)
            rstd_from_ps(P, rs, psH[0:64, 0:TT], 64.0, 64e-5)
            P.stt(yh, yh, col("lng", h), rs, ALU.mult, ALU.mult)
            P.stt(yh, yh, col("lnb", h), bon[:, h, :].sub(h), ALU.add, ALU.add)
            P.tt(sq, yh, gate4[:, h, :].sub(h), ALU.mult)
            P.dma(oT[h * 64:(h + 1) * 64, tsl].sub(h), sq, q="pool")
    if release:
        P.release(mk)


def phase_merge(P, C, NT, hT, oT, gT, wbr_d, wout_d, h1T, dyn=None):
    mk = P.mark()
    nt = NT // TT
    wstg = [P.tile([128, 4, 1024], name=f"f_wstg{i}") for i in range(2)]
    wbr = []
    for br in range(3):
        t = P.tile([128, 4, 1024], name=f"wbr{br}", dtype=BF16)
        P.dma(wstg[br % 2], wbr_d[br].re("(c p) n -> p c n", p=128))
        P.copy(t, wstg[br % 2], eng=("pool" if br % 2 == 0 else "act"))
        wbr.append(t)
    wout = P.tile([128, 8, 1024], name="wout", dtype=BF16)
    wov = wout_d.re("(c p) n -> p c n", p=128)
    for hh in range(2):
        P.dma(wstg[(hh + 1) % 2], wov[:, hh * 4:(hh + 1) * 4, :])
        P.copy(wout[:, hh * 4:(hh + 1) * 4, :].sub(hh), wstg[(hh + 1) % 2], eng=("act" if hh == 0 else "pool"))
    h = P.tile([128, 8, TT], name="f_h")
    o = P.tile([128, 12, TT], name="f_o")
    ob_ = P.tile([128, 12, TT], name="f_ob", dtype=BF16)
    mg = P.tile([128, 8, TT], name="f_mg32") if dyn is not None else None
    mgb = P.tile([128, 8, TT], name="f_mg", dtype=BF16)
    o2 = P.tile([128, 6, TT], name="f_o2") if dyn is not None else None
    g3 = [P.tile([128, 3, TT], name=f"f_g3{i}") for i in range(2)]
    tmp = [P.tile([128, TT], name=f"f_tmp{i}") for i in range(2)]
    tmp2 = [P.tile([128, TT], name=f"f_tmpb{i}") for i in range(2)]
    ps = [P.pbank(i) for i in range(8)]
    hv = hT.re("(c p) t -> p c t", p=128)
    ov = oT.re("(c p) t -> p c t", p=128)
    gv = gT.re("(b c p) t -> p b c t", b=3, p=128)
    h1v = h1T.re("(c p) t -> p c t", p=128)
    for ti in range(nt):
        tsl = slice(ti * TT, (ti + 1) * TT)
        if dyn is not None:
            m0 = C.cv[:, CV["m0"]:CV["m0"] + 1]
            m1 = C.cv[:, CV["m1"]:CV["m1"] + 1]
            tsl2 = slice(dyn + ti * TT, dyn + (ti + 1) * TT)
            P.dma(h, hv[:, :, tsl])
            P.dma(mg, hv[:, :, tsl2])
            P.ts(h, h, m0, ALU.mult)
            P.stt(h, mg, m1, h, ALU.mult, ALU.add)
            for part in range(2):
                cs_ = slice(part * 6, (part + 1) * 6)
                P.dma(o[:, cs_, :].sub(part), ov[:, cs_, tsl])
                P.dma(o2, ov[:, cs_, tsl2])
                P.ts(o[:, cs_, :].sub(part), o[:, cs_, :].sub(part), m0, ALU.mult)
                P.stt(o[:, cs_, :].sub(part), o2, m1, o[:, cs_, :].sub(part), ALU.mult, ALU.add)
        else:
            P.dma(h, hv[:, :, tsl])
            P.dma(o, ov[:, :, tsl])
        P.copy(ob_[:, 0:6, :].sub(0), o[:, 0:6, :], eng="dve")
        P.copy(ob_[:, 6:12, :].sub(1), o[:, 6:12, :], eng="act")
        for n in range(8):
            ns = slice(n * 128, (n + 1) * 128)
            g = g3[n % 2]
            P.dma(g, gv[:, :, n, tsl])
            for br in range(3):
                pp = ps[(n % 2) * 3 + br]
                for k in range(4):
                    P.mm(pp, wbr[br][:, k, ns], ob_[:, br * 4 + k, :], start=(k == 0), stop=(k == 3))
            tm_ = tmp[n % 2]
            P.tt(tm_, ps[(n % 2) * 3 + 0], g[:, 0, :], ALU.mult)
            P.tt(tmp2[n % 2], ps[(n % 2) * 3 + 1], g[:, 1, :], ALU.mult)
            P.tt(tm_, tm_, tmp2[n % 2], ALU.add, eng="pool")
            P.tt(tmp2[n % 2], ps[(n % 2) * 3 + 2], g[:, 2, :], ALU.mult)
            P.tt(mgb[:, n, :].sub(n), tm_, tmp2[n % 2], ALU.add, eng="pool")
        for n in range(8):
            ns = slice(n * 128, (n + 1) * 128)
            pp = ps[6 + n % 2]
            for k in range(8):
                P.mm(pp, wout[:, k, ns], mgb[:, k, :], start=(k == 0), stop=(k == 7))
            P.tt(h[:, n, :].sub(n), h[:, n, :].sub(n), pp, ALU.add)
        P.dma(h1v[:, :, tsl], h, q="pool")
    P.release(mk)


def phase_ffn(P, C, NT, h1T, h2T, gcol, experts, FF, router_d=None):
    mk = P.mark()
    cv = C.cv
    K = C.k
    ones = K["ones"]
    ident = K["ident"]
    nt = NT // TT
    NF = FF // 128
    CB = 512
    blocks = [(c0, min(CB, FF - c0)) for c0 in range(0, FF, CB)]
    h = P.tile([128, 8, TT], name="m_h")
    u = P.tile([128, 8, TT], name="m_u", dtype=BF16)
    rs = P.tile([128, TT], name="m_rs")
    hid_raw = P.tile([128, NF * TT // 2], name="m_hid")
    hid = V(hid_raw.ap.bitcast(BF16).rearrange("p (f t) -> p f t", f=NF), hid_raw.key)
    u32 = V(hid_raw.ap[:, 0:8 * TT].rearrange("p (c t) -> p c t", c=8), hid_raw.key)
    sg = [P.tile([128, TT], name=f"m_sg{i}") for i in range(2)]
    wgb = [P.tile([128, 8, CB], name=f"m_wg{i}") for i in range(2)]
    wub = [P.tile([128, 8, CB], name=f"m_wu{i}") for i in range(2)]
    wgc = [P.tile([128, 8, CB], name=f"m_wgc{i}", dtype=BF16) for i in range(2)]
    wuc = [P.tile([128, 8, CB], name=f"m_wuc{i}", dtype=BF16) for i in range(2)]
    wdb = [P.tile([128, 512], name=f"m_wd{i}") for i in range(3)]
    wdc = [P.tile([128, 512], name=f"m_wdc{i}", dtype=BF16) for i in range(3)]
    ps = [P.pbank(i) for i in range(8)]
    hv = h1T.re("(c p) t -> p c t", p=128)
    h2v = h2T.re("(c p) t -> p c t", p=128)
    ne = len(experts)
    if router_d is not None:
        sel8 = P.tile([8, 1024], name="c_sel8")
        P.dma(sel8, C.sel8_d)
        rt_w = P.tile([128, 8, 8], name="m_rw")
        P.dma(rt_w, router_d.re("(c p) e -> p c e", p=128))
        lg = P.tile([8, TT], name="m_lg")
        ltm = P.tile([128, 4, 8], name="m_ltm")
        l2 = P.tile([128, 4, 8], name="m_l2")
        eq1 = P.tile([128, 4, 8], name="m_eq1")
        eq2 = P.tile([128, 4, 8], name="m_eq2")
        m1 = P.tile([128, 4], name="m_m1")
        m2 = P.tile([128, 4], name="m_m2")
        w1 = P.tile([128, 4], name="m_w1")
        w2 = P.tile([128, 4], name="m_w2")
        gwf = P.tile([8, TT], name="m_gwf")
        gwe = [P.tile([128, TT], name=f"m_gwe{i}") for i in range(2)]
    wk = 0
    dk = 0
    for ti in range(nt):
        tsl = slice(ti * TT, (ti + 1) * TT)
        P.dma(h, hv[:, :, tsl])
        P.act(u32, h, AF.Square)
        for c in range(8):
            P.mm(ps[7], ones, u32[:, c, :], start=(c == 0), stop=(c == 7))
        rstd_from_ps(P, rs, ps[7], D, 1e-6)
        if router_d is not None:
            for c in range(8):
                P.stt(u32[:, c, :], h[:, c, :], cv[:, gcol + c:gcol + c + 1], rs, ALU.mult, ALU.mult)
            P.copy(u, u32, eng="act")
            for c in range(8):
                P.mm(ps[6][0:8, :], rt_w[:, c, :], u32[:, c, :], start=(c == 0), stop=(c == 7))
        else:
            for c in range(8):
                P.stt(u[:, c, :], h[:, c, :], cv[:, gcol + c:gcol + c + 1], rs, ALU.mult, ALU.mult)
        if router_d is not None:
            P.copy(lg, ps[6][0:8, :], eng="act")
            for j in range(4):
                P.transpose(ps[5][:, j * 8:(j + 1) * 8], lg[:, j * 128:(j + 1) * 128], ident[0:8, 0:8])
            P.copy(ltm.re("p j e -> p (j e)"), ps[5][:, 0:32])
            bc = lambda t: V(t.ap.unsqueeze(2).to_broadcast([128, 4, 8]), t.key)
            P.op("dve", lambda e: e.reduce_max(_ap(m1), _ap(ltm), AX.X), [ltm], [m1])
            P.tt(eq1, ltm, bc(m1), ALU.is_equal)
            P.stt(l2, eq1, -1e30, ltm, ALU.mult, ALU.add)
            P.op("dve", lambda e: e.reduce_max(_ap(m2), _ap(l2), AX.X), [l2], [m2])
            P.tt(eq2, l2, bc(m2), ALU.is_equal)
            P.tt(w2, m2, m1, ALU.subtract)
            P.act(w2, w2, AF.Exp)
            P.ts(w1, w2, 1.0, ALU.add)
            P.recip(w1, w1)
            P.tt(w2, w2, w1, ALU.mult)
            P.tt(eq1, eq1, bc(w1), ALU.mult)
            P.tt(eq2, eq2, bc(w2), ALU.mult)
            P.tt(eq1, eq1, eq2, ALU.add)
            for j in range(4):
                P.transpose(ps[5][0:8, j * 128:(j + 1) * 128], eq1[:, j, :], ident)
            P.copy(gwf, ps[5][0:8, :], eng="act")
        work = [(e_, bi) for e_ in range(ne) for bi in range(len(blocks))]

        def prefetch(e_, bi, slot):
            wg_d, wu_d, _ = experts[e_]
            c0_, wdt = blocks[bi]
            wgv = wg_d.re("(c p) f -> p c f", p=128)
            wuv = wu_d.re("(c p) f -> p c f", p=128)
            P.dma(wgb[slot][:, :, 0:wdt], wgv[:, :, c0_:c0_ + wdt])
            P.dma(wub[slot][:, :, 0:wdt], wuv[:, :, c0_:c0_ + wdt], q="act")
            P.copy(wgc[slot][:, :, 0:wdt], wgb[slot][:, :, 0:wdt], eng="dve")
            P.copy(wuc[slot][:, :, 0:wdt], wub[slot][:, :, 0:wdt], eng="act")
        prefetch(work[0][0], work[0][1], wk % 2)
        for wi, (e_, bi) in enumerate(work):
            slot = wk % 2
            wk += 1
            if wi + 1 < len(work):
                prefetch(work[wi + 1][0], work[wi + 1][1], wk % 2)
            c0_, wdt = blocks[bi]
            wg_t, wu_t = wgc[slot], wuc[slot]
            if router_d is not None and bi == 0:
                gw_e = gwe[e_ % 2]
                P.mm(ps[3], sel8[:, e_ * 128:(e_ + 1) * 128], gwf)
                P.copy(gw_e, ps[3], eng="act")
            for j in range(wdt // 128):
                f = c0_ // 128 + j
                pg = ps[4 + f % 2]
                pu = ps[6 + f % 2]
                for c in range(8):
                    P.mm(pg, wg_t[:, c, j * 128:(j + 1) * 128], u[:, c, :], start=(c == 0), stop=(c == 7))
                for c in range(8):
                    P.mm(pu, wu_t[:, c, j * 128:(j + 1) * 128], u[:, c, :], start=(c == 0), stop=(c == 7))
                s_ = sg[f % 2]
                P.act(s_, pg, AF.Silu)
                if router_d is not None:
                    P.tt(s_, s_, gw_e, ALU.mult, eng="pool")
                P.tt(hid[:, f, :].sub(f), s_, pu, ALU.mult)
            if bi == len(blocks) - 1:
                wdv = experts[e_][2].re("(f p) n -> p f n", p=128)
                for half in range(2):
                    for f in range(NF):
                        wd_s = wdb[dk % 3]
                        wd_t = wdc[dk % 3]
                        dk += 1
                        P.dma(wd_s, wdv[:, f, half * 512:(half + 1) * 512])
                        P.copy(wd_t, wd_s, eng=("dve" if dk % 2 == 0 else "act"))
                        for n4 in range(4):
                            P.mm(ps[n4], wd_t[:, n4 * 128:(n4 + 1) * 128], hid[:, f, :].sub(f), start=(f == 0), stop=(f == NF - 1))
                    for n4 in range(4):
                        n = half * 4 + n4
                        P.tt(h[:, n, :].sub(n), h[:, n, :].sub(n), ps[n4], ALU.add)
        P.dma(h2v[:, :, tsl], h, q="pool")
    P.release(mk)


def phase_ple(P, C, NT, h2T, pT, proj_d, pgate_d, gcol, h3T):
    mk = P.mark()
    cv = C.cv
    ones = C.k["ones"]
    nt = NT // TT
    pstg = [P.tile([128, 4, 1024], name=f"p_stg{i}") for i in range(2)]
    proj = P.tile([128, 2, 1024], name="p_proj", dtype=BF16)
    P.dma(pstg[0][:, 0:2, :], proj_d.re("(c p) n -> p c n", p=128))
    P.copy(proj, pstg[0][:, 0:2, :], eng="pool")
    pg = P.tile([128, 8, 1024], name="p_gate", dtype=BF16)
    pgv = pgate_d.re("(c p) n -> p c n", p=128)
    for hh in range(2):
        P.dma(pstg[(hh + 1) % 2], pgv[:, hh * 4:(hh + 1) * 4, :])
        P.copy(pg[:, hh * 4:(hh + 1) * 4, :].sub(hh), pstg[(hh + 1) % 2], eng=("act" if hh == 0 else "pool"))
    h = P.tile([128, 8, TT], name="p_h")
    hb_ = P.tile([128, 8, TT], name="p_hb", dtype=BF16)
    pt = P.tile([128, 2, TT], name="p_p")
    ptb = P.tile([128, 2, TT], name="p_pb", dtype=BF16)
    er = P.tile([128, 8, TT], name="p_er")
    ho = P.tile([128, 8, TT], name="p_ho")
    sq = P.tile([128, TT], name="p_sq")
    rs = P.tile([128, TT], name="p_rs")
    gp = [P.tile([128, TT], name=f"p_gp{i}") for i in range(2)]
    ps = [P.pbank(i) for i in range(8)]
    hv = h2T.re("(c p) t -> p c t", p=128)
    pv = pT.re("(c p) t -> p c t", p=128)
    h3v = h3T.re("(c p) t -> p c t", p=128)
    for ti in range(nt):
        tsl = slice(ti * TT, (ti + 1) * TT)
        P.dma(h, hv[:, :, tsl])
        P.dma(pt, pv[:, :, tsl])
        P.copy(ptb, pt, eng="dve")
        P.copy(hb_, h, eng="act")
        for n in range(8):
            ns = slice(n * 128, (n + 1) * 128)
            pp = ps[n % 2]
            P.mm(pp, proj[:, 0, ns], ptb[:, 0, :], start=True, stop=False)
            P.mm(pp, proj[:, 1, ns], ptb[:, 1, :], start=False, stop=True)
            P.copy(er[:, n, :].sub(n), pp, eng="act")
            P.act(sq, pp, AF.Square)
            P.mm(ps[2], ones, sq, start=(n == 0), stop=(n == 7))
        rstd_from_ps(P, rs, ps[2], D, 1e-6)
        for n in range(8):
            ns = slice(n * 128, (n + 1) * 128)
            pp = ps[3 + n % 2]
            for k in range(8):
                P.mm(pp, pg[:, k, ns], hb_[:, k, :], start=(k == 0), stop=(k == 7))
            g = gp[n % 2]
            P.act(g, pp, AF.Sigmoid)
            P.stt(er[:, n, :].sub(n), er[:, n, :].sub(n), cv[:, gcol + n:gcol + n + 1], rs, ALU.mult, ALU.mult)
            P.tt(g, g, er[:, n, :].sub(n), ALU.mult)
            P.tt(ho[:, n, :].sub(n), h[:, n, :], g, ALU.add)
        P.dma(h3v[:, :, tsl], ho, q="pool")
    P.release(mk)


def own(hg, width=64):
    return slice(hg * 4 * width, (hg + 1) * 4 * width)


def col4(v):
    return np.ascontiguousarray(v.reshape(4, 64).T)


def col2(v):
    return np.ascontiguousarray(v.reshape(2, 128).T)


def mixer_host_inputs(inp, L, b, hg):
    f = np.float32
    w_in = inp["w_in"][L]
    o = own(hg)
    cols = np.concatenate([
        np.arange(0, 512)[o], np.arange(512, 1024)[o], np.arange(1024, 1536)[o],
        np.arange(1536, 1600), np.arange(1600, 1664), np.arange(1664, 1792),
        np.arange(1792, 2048), np.arange(2048, 2176), np.arange(2176, 2208),
        np.arange(2208, 2720)[o], np.arange(2720, 3232)[o], np.arange(3232, 3744)[o],
        np.arange(3760, 4272)[o],
        np.arange(3744, 3752)[hg * 4:(hg + 1) * 4], np.arange(3752, 3760)[hg * 4:(hg + 1) * 4]])
    assert len(cols) == NZ
    d = {}
    d["w_in_m"] = np.ascontiguousarray(w_in[:, cols])
    cv = np.zeros((128, NCV), f)

    def put(nm, arr):
        arr = np.asarray(arr, f)
        cv[:arr.shape[0], CV[nm]:CV[nm] + arr.shape[1]] = arr
    put("nmg", inp["norm_mix_g"][L].reshape(8, 128).T)
    mu = inp["rwkv_mu"][L]
    mucols = np.zeros((128, 15), f)
    rcols = cols[:1024]
    for g in range(15):
        seg = mu[rcols[ZOFF[g]:ZOFF[g + 1]]]
        mucols[:len(seg), g] = seg
    put("mu", mucols)
    put("w0", col4(inp["rwkv_w0"][L][o]))
    put("a0", col4(inp["rwkv_a0"][L][o]))
    put("kk", col4(inp["rwkv_k_k"][L][o]))
    put("ka", col4(inp["rwkv_k_a"][L][o]))
    put("rk", col4(inp["rwkv_r_k"][L].reshape(512)[o]))
    put("lng", col4(inp["rwkv_ln_g"][L][o]))
    put("lnb", col4(inp["rwkv_ln_b"][L][o]))
    if L > 0:
        put("vmu", inp["vres_mu"][L - 1].reshape(8, 128).T)
        put("vb", col4(inp["vres_b"][L - 1][o]))
    put("qng", inp["mla_q_norm_g"][L].reshape(2, 128).T)
    put("kvng", inp["mla_kv_norm_g"][L].reshape(128, 1))
    put("qkq", inp["mla_qk_norm_q"][L].reshape(96, 1))
    put("qkk", inp["mla_qk_norm_k"][L].reshape(96, 1))
    invf = (1.0 / (10000.0 ** (np.arange(0, 32, 2, dtype=f) / f(32)))).astype(f)
    iv = np.zeros((96, 1), f)
    iv[64:80, 0] = invf
    iv[80:96, 0] = invf
    put("invf", iv)
    put("ropec", np.full((128, 1), -np.pi, f))
    cw = inp["gdn_conv_w"][L]
    convc = np.zeros((128, 48), f)
    for seg in range(3):
        cc = cw[:, seg * 512:(seg + 1) * 512][:, o]
        for hh in range(4):
            for j in range(4):
                convc[:64, (seg * 4 + hh) * 4 + j] = cc[j, hh * 64:(hh + 1) * 64]
    put("conv", convc)
    put("alog", inp["gdn_a_log"][L][hg * 4:(hg + 1) * 4].reshape(4, 1))
    put("dtb", inp["gdn_dt_bias"][L][hg * 4:(hg + 1) * 4].reshape(4, 1))
    put("gng", inp["gdn_norm_g"][L].reshape(64, 1))
    d["cv"] = cv
    d["w_up"] = np.ascontiguousarray(inp["rwkv_w_up"][L][:, o])
    d["a_up"] = np.ascontiguousarray(inp["rwkv_a_up"][L][:, o])
    d["g_up"] = np.ascontiguousarray(inp["rwkv_g_up"][L][:, o])
    if L > 0:
        d["v_down"] = np.ascontiguousarray(inp["vres_down"][L - 1])
        d["v_up"] = np.ascontiguousarray(inp["vres_up"][L - 1][:, o])
    d["w_uq"] = np.ascontiguousarray(inp["mla_w_uq"][L][:, hg * 384:(hg + 1) * 384])
    ukv = inp["mla_w_ukv"][L].reshape(128, 8, 128)[:, hg * 4:(hg + 1) * 4, :]
    d["w_uk"] = np.ascontiguousarray(ukv[:, :, :64].reshape(128, 256))
    d["w_uv"] = np.ascontiguousarray(ukv[:, :, 64:].reshape(128, 256))
    d["pos"] = np.ascontiguousarray(inp["positions"][b:b + 1].astype(np.int32))
    for k_, v_ in consts_np().items():
        d["c_" + k_] = v_
    return d
from concourse.bass_utils import run_bass_kernel_spmd

B_, S_, NCORE = 4, 4096, 8
M_CONSTS = ["ident", "ones", "att_mask", "ropeRT", "sel4", "id4", "neg4", "neg4T", "ms4", "ms4T", "mi4"]
F_CONSTS = ["ident", "ones"]
_PROG_CACHE = {}


def build_mixer(S, L):
    P = Prog()
    C = Ctx()
    names = []

    def din(name, shape, dt=F32):
        names.append(name)
        return P.dview(P.dram(name, shape, dt, kind="ExternalInput"))
    hT = din("hT", [1024, S])
    w_d = din("w_in_m", [1024, NZ])
    cv_d = din("cv", [128, NCV])
    pos_d = din("pos", [1, S], I32)
    w_uq, w_uk, w_uv = din("w_uq", [256, 384]), din("w_uk", [128, 256]), din("w_uv", [128, 256])
    w_up, a_up, g_up = din("w_up", [64, 256]), din("a_up", [64, 256]), din("g_up", [128, 256])
    zT = P.dview(P.dram("zT", [NG, 128, S], F32, kind="Internal"))
    oT = P.dview(P.dram("oT", [768, S], F32, kind="ExternalOutput"))
    uT = v_down = v_up = None
    if L == 0:
        vfT = P.dview(P.dram("vfT_out", [256, S], F32, kind="ExternalOutput"))
    else:
        vfT = din("vfT_in", [256, S])
        uT = P.dview(P.dram("uT", [1024, S], F32, kind="Internal"))
        v_down, v_up = din("v_down", [1024, 32]), din("v_up", [32, 256])
    C.k = load_consts(P, M_CONSTS)
    names.extend(["c_" + c for c in M_CONSTS])
    C.cv = P.tile([128, NCV], name="cv")
    P.dma(C.cv, cv_d)
    groups = [(int(ZOFF[g]), int(ZG[g])) for g in range(NG)]
    phase_proj(P, C, S, hT, w_d, NZ, groups, zT, CV["nmg"], uT=uT)
    phase_rwkv(P, C, S, L, zT, V(oT.ap[0:256, :], "oT_r"), w_up, a_up, g_up, vfT, uT, v_down, v_up)
    phase_mla(P, C, S, zT, pos_d, w_uq, w_uk, w_uv, V(oT.ap[256:512, :], "oT_m"))
    phase_gdn(P, C, S, zT, V(oT.ap[512:768, :], "oT_g"))
    P.finalize()
    return P.nc, names


def build_token(NT, L):
    P = Prog()
    C = Ctx()
    names = []

    def din(name, shape, dt=F32):
        names.append(name)
        return P.dview(P.dram(name, shape, dt, kind="ExternalInput"))
    hT = din("hT", [1024, NT])
    oT = din("oT_all", [1536, NT])
    pT = din("pT", [256, NT])
    cv_d = din("cv", [128, NCV])
    w_g = din("w_gate", [1024, 3072])
    wbr = [din(f"w_br{i}", [512, 1024]) for i in range(3)]
    wout = din("w_out", [1024, 1024])
    proj = din("ple_proj", [256, 1024])
    pgate = din("ple_gate", [1024, 1024])
    if L % 2 == 0:
        experts = [(din("ffn_wg", [1024, 2816]), din("ffn_wu", [1024, 2816]), din("ffn_wd", [2816, 1024]))]
        FF = 2816
        router = None
    else:
        wg_all = din("moe_wg", [8, 1024, 3584])
        wu_all = din("moe_wu", [8, 1024, 3584])
        wd_all = din("moe_wd", [8, 3584, 1024])
        experts = [(V(wg_all.ap[e], wg_all.key), V(wu_all.ap[e], wu_all.key), V(wd_all.ap[e], wd_all.key)) for e in range(8)]
        FF = 3584
        router = din("moe_router", [1024, 8])
    gT = P.dview(P.dram("gT", [3072, NT], F32, kind="Internal"))
    h1T = P.dview(P.dram("h1T", [1024, NT], F32, kind="Internal"))
    h2T = P.dview(P.dram("h2T", [1024, NT], F32, kind="Internal"))
    h3T = P.dview(P.dram("h3T", [1024, NT], F32, kind="ExternalOutput"))
    C.k = load_consts(P, F_CONSTS)
    names.extend(["c_" + c for c in F_CONSTS])
    C.sel8_d = din("c_sel8", [8, 1024])
    C.cv = P.tile([128, NCV], name="cv")
    P.dma(C.cv, cv_d)
    groups = [(g * 128, 128) for g in range(24)]
    phase_proj(P, C, NT, hT, w_g, 3072, groups, gT, CV["nmg"], func=AF.Sigmoid, nbuf=1, zflat=True)
    phase_merge(P, C, NT, hT, oT, gT, wbr, wout, h1T)
    phase_ffn(P, C, NT, h1T, h2T, CV["nfg"], experts, FF, router)
    phase_ple(P, C, NT, h2T, pT, proj, pgate, CV["png"], h3T)
    P.finalize()
    return P.nc, names


def token_host_inputs(inp, L, half=0):
    f = np.float32
    d = {}
    cv = np.zeros((128, NCV), f)
    cv[:, CV["m0"]] = 1.0 if half == 0 else 0.0
    cv[:, CV["m1"]] = 1.0 if half == 1 else 0.0
    cv[:, CV["nmg"]:CV["nmg"] + 8] = inp["norm_mix_g"][L].reshape(8, 128).T
    cv[:, CV["nfg"]:CV["nfg"] + 8] = inp["norm_ffn_g"][L].reshape(8, 128).T
    cv[:, CV["png"]:CV["png"] + 8] = inp["ple_norm_g"][L].reshape(8, 128).T
    d["cv"] = cv
    d["w_gate"] = np.ascontiguousarray(inp["w_in"][L][:, 4272:7344])
    d["w_br0"] = inp["w_br_rwkv"][L]
    d["w_br1"] = inp["w_br_mla"][L]
    d["w_br2"] = inp["w_br_gdn"][L]
    d["w_out"] = inp["w_out"][L]
    d["ple_proj"] = inp["ple_proj"][L]
    d["ple_gate"] = inp["ple_gate"][L]
    if L % 2 == 0:
        d["ffn_wg"], d["ffn_wu"], d["ffn_wd"] = inp["ffn_wg"][L // 2], inp["ffn_wu"][L // 2], inp["ffn_wd"][L // 2]
    else:
        d["moe_wg"], d["moe_wu"], d["moe_wd"] = inp["moe_wg"][L // 2], inp["moe_wu"][L // 2], inp["moe_wd"][L // 2]
        d["moe_router"] = inp["moe_router"][L // 2]
    cs = consts_np()
    for c in F_CONSTS + ["sel8"]:
        d["c_" + c] = cs[c]
    return d


def kernel(**inputs):
    inp = {k: np.asarray(v) for k, v in inputs.items()}
    x = inp["x"].astype(np.float32)
    Bn, S, Dm = x.shape
    NT = S // 2
    hT = [np.ascontiguousarray(x[b].T) for b in range(Bn)]
    vf = [None] * NCORE
    for L in range(2):
        key = ("M", S, L)
        if key not in _PROG_CACHE:
            _PROG_CACHE[key] = build_mixer(S, L)
        nc, names = _PROG_CACHE[key]
        in_maps = []
        for core in range(NCORE):
            b, hg = core // 2, core % 2
            d = mixer_host_inputs(inp, L, b, hg)
            d["hT"] = hT[b]
            if L > 0:
                d["vfT_in"] = vf[core]
            in_maps.append({n: np.ascontiguousarray(d[n]) for n in names})
        res = run_bass_kernel_spmd(nc, in_maps, core_ids=list(range(NCORE)))
        oTs = [r["oT"] for r in res.results]
        if L == 0:
            vf = [r["vfT_out"] for r in res.results]
        key = ("F", NT, L)
        if key not in _PROG_CACHE:
            _PROG_CACHE[key] = build_token(NT, L)
        nc, names = _PROG_CACHE[key]
        th = token_host_inputs(inp, L)
        in_maps = []
        for core in range(NCORE):
            b, half = core // 2, core % 2
            tsl = slice(half * NT, (half + 1) * NT)
            d = dict(th)
            d["hT"] = hT[b][:, tsl]
            o0, o1 = oTs[2 * b], oTs[2 * b + 1]
            d["oT_all"] = np.concatenate([o0[0:256, tsl], o1[0:256, tsl], o0[256:512, tsl], o1[256:512, tsl],
                                          o0[512:768, tsl], o1[512:768, tsl]], axis=0)
            d["pT"] = inp["p"][L, b, tsl, :].T
            in_maps.append({n: np.ascontiguousarray(d[n]) for n in names})
        res = run_bass_kernel_spmd(nc, in_maps, core_ids=list(range(NCORE)))
        for b in range(Bn):
            hT[b] = np.concatenate([res.results[2 * b]["h3T"], res.results[2 * b + 1]["h3T"]], axis=1)
    out = np.stack([hT[b].T for b in range(Bn)], axis=0)
    return np.ascontiguousarray(out.astype(np.float32))


ALL_M_KEYS = ["w_in_m", "cv", "w_uq", "w_uk", "w_uv", "w_up", "a_up", "g_up"]


def build_fused(S):
    NTH = S // 2
    P = Prog()
    C = Ctx()
    names = []

    def din(name, shape, dt=F32):
        names.append(name)
        return P.dview(P.dram(name, shape, dt, kind="ExternalInput"))
    hT0 = din("hT0", [1024, S])
    pos_d = din("pos", [1, S], I32)
    pT = [din("pT0", [256, S]), din("pT1", [256, NTH])]
    C.sel8_d = din("c_sel8", [8, 1024])
    mi = {}
    for L in range(2):
        for hg in range(2):
            pre = f"m{L}{hg}_"
            d = {"w_in_m": din(pre + "w_in_m", [1024, NZ]), "cv": din(pre + "cv", [128, NCV]),
                 "w_uq": din(pre + "w_uq", [256, 384]), "w_uk": din(pre + "w_uk", [128, 256]), "w_uv": din(pre + "w_uv", [128, 256]),
                 "w_up": din(pre + "w_up", [64, 256]), "a_up": din(pre + "a_up", [64, 256]), "g_up": din(pre + "g_up", [128, 256])}
            if L > 0:
                d["v_down"] = din(pre + "v_down", [1024, 32])
                d["v_up"] = din(pre + "v_up", [32, 256])
            mi[(L, hg)] = d
    ti_ = {}
    for L in range(2):
        pre = f"t{L}_"
        d = {"cv": din(pre + "cv", [128, NCV]), "w_gate": din(pre + "w_gate", [1024, 3072]),
             "wbr": [din(pre + f"w_br{i}", [512, 1024]) for i in range(3)], "w_out": din(pre + "w_out", [1024, 1024]),
             "ple_proj": din(pre + "ple_proj", [256, 1024]), "ple_gate": din(pre + "ple_gate", [1024, 1024])}
        if L % 2 == 0:
            d["experts"] = [(din(pre + "ffn_wg", [1024, 2816]), din(pre + "ffn_wu", [1024, 2816]), din(pre + "ffn_wd", [2816, 1024]))]
            d["FF"] = 2816
            d["router"] = None
        else:
            wg_all = din(pre + "moe_wg", [8, 1024, 3584])
            wu_all = din(pre + "moe_wu", [8, 1024, 3584])
            wd_all = din(pre + "moe_wd", [8, 3584, 1024])
            d["experts"] = [(V(wg_all.ap[e], wg_all.key), V(wu_all.ap[e], wu_all.key), V(wd_all.ap[e], wd_all.key)) for e in range(8)]
            d["FF"] = 3584
            d["router"] = din(pre + "moe_router", [1024, 8])
        ti_[L] = d
    zT = P.dview(P.dram("zT", [NG, 128, S], F32, kind="Internal"))
    uT = P.dview(P.dram("uT", [1024, S], F32, kind="Internal"))
    oTa = P.dview(P.dram("oT_all", [1536, S], F32, kind="Internal"))
    vfT = P.dview(P.dram("vfT", [512, S], F32, kind="Internal"))
    gT = P.dview(P.dram("gT", [3072, S], F32, kind="Internal"))
    h1T = P.dview(P.dram("h1T", [1024, S], F32, kind="Internal"))
    h2T = P.dview(P.dram("h2T", [1024, S], F32, kind="Internal"))
    hT1 = P.dview(P.dram("hT1", [1024, S], F32, kind="Internal"))
    h3T = P.dview(P.dram("h3T", [1024, NTH], F32, kind="ExternalOutput"))
    C.k = load_consts(P, M_CONSTS)
    names.extend(["c_" + c for c in M_CONSTS])
    C.cv = P.tile([128, NCV], name="cv")
    groups = [(int(ZOFF[g]), int(ZG[g])) for g in range(NG)]
    ggroups = [(g * 128, 128) for g in range(24)]
    hin = hT0
    for L in range(2):
        for hg in range(2):
            d = mi[(L, hg)]
            P.dma(C.cv, d["cv"])
            phase_proj(P, C, S, hin, d["w_in_m"], NZ, groups, zT, CV["nmg"], uT=(uT if L > 0 else None))
            sub = lambda br: V(oTa.ap[br * 512 + hg * 256:br * 512 + (hg + 1) * 256, :], f"oT_{br}_{hg}")
            vfv = V(vfT.ap[hg * 256:(hg + 1) * 256, :], f"vfT_{hg}")
            mk_ = P.mark()
            o_r, o_g = sub(0), sub(2)
            P.run_interleaved([
                lambda: phase_rwkv(P, C, S, L, zT, o_r, d["w_up"], d["a_up"], d["g_up"], vfv,
                                   (uT if L > 0 else None), d.get("v_down"), d.get("v_up"), ttl=256, psbase=0, release=False),
                lambda: phase_gdn(P, C, S, zT, o_g, ttl=256, psbase=4, release=False)])
            P.release(mk_)
            phase_mla(P, C, S, zT, pos_d, d["w_uq"], d["w_uk"], d["w_uv"], sub(1))
        t = ti_[L]
        P.dma(C.cv, t["cv"])
        if L == 0:
            NT, dyn, hout = S, None, hT1
        else:
            NT, dyn, hout = NTH, NTH, h3T
        gv = V(gT.ap[:, 0:NT], gT.key)
        h1v = V(h1T.ap[:, 0:NT], h1T.key)
        h2v = V(h2T.ap[:, 0:NT], h2T.key)
        phase_proj(P, C, NT, hin, t["w_gate"], 3072, ggroups, gv, CV["nmg"], func=AF.Sigmoid, nbuf=1, zflat=True, dyn=dyn)
        phase_merge(P, C, NT, hin, oTa, gv, t["wbr"], t["w_out"], h1v, dyn=dyn)
        phase_ffn(P, C, NT, h1v, h2v, CV["nfg"], t["experts"], t["FF"], t["router"])
        phase_ple(P, C, NT, h2v, pT[L], t["ple_proj"], t["ple_gate"], CV["png"], hout)
        hin = hT1
    P.finalize()
    return P.nc, names


def kernel_unfused(**inputs):
    return _kernel_unfused(**inputs)


_kernel_unfused = kernel


def kernel(**inputs):
    inp = {k: np.asarray(v) for k, v in inputs.items()}
    x = inp["x"].astype(np.float32)
    Bn, S, Dm = x.shape
    NTH = S // 2
    key = ("FUSED", S)
    if key not in _PROG_CACHE:
        _PROG_CACHE[key] = build_fused(S)
    nc, names = _PROG_CACHE[key]
    cs = consts_np()
    shared = {"c_" + k: v for k, v in cs.items()}
    tok = []
    for L in range(2):
        th = token_host_inputs(inp, L)
        tok.append({f"t{L}_" + k: v for k, v in th.items() if not k.startswith("c_")})
    in_maps = []
    for core in range(NCORE):
        b, half = core // 2, core % 2
        d = dict(shared)
        d["hT0"] = x[b].T
        d["pos"] = inp["positions"][b:b + 1].astype(np.int32)
        d["pT0"] = inp["p"][0, b].T
        d["pT1"] = inp["p"][1, b, half * NTH:(half + 1) * NTH, :].T
        for L in range(2):
            for hg in range(2):
                md = mixer_host_inputs(inp, L, b, hg)
                for k, v in md.items():
                    if not k.startswith("c_") and k != "pos":
                        d[f"m{L}{hg}_" + k] = v
            d.update(tok[L])
            cvt = tok[L][f"t{L}_cv"].copy()
            cvt[:, CV["m0"]] = 1.0 if half == 0 else 0.0
            cvt[:, CV["m1"]] = 1.0 if half == 1 else 0.0
            d[f"t{L}_cv"] = cvt
        in_maps.append({n: np.ascontiguousarray(d[n]) for n in names})
    res = run_bass_kernel_spmd(nc, in_maps, core_ids=list(range(NCORE)))
    out = np.empty((Bn, S, Dm), np.float32)
    for core in range(NCORE):
        b, half = core // 2, core % 2
        out[b, half * NTH:(half + 1) * NTH, :] = res.results[core]["h3T"].T
    return out
```

```python
import numpy as np
from contextlib import ExitStack
import concourse.bass as bass
import concourse.mybir as mybir

F32 = mybir.dt.float32
BF16 = mybir.dt.bfloat16
I32 = mybir.dt.int32
ALU = mybir.AluOpType
AF = mybir.ActivationFunctionType
AX = mybir.AxisListType

ENGS = ("pe", "act", "dve", "pool", "sp")
N_DMA_SEMS = 24


class Op:
    __slots__ = ("eng", "fn", "deps", "is_dma", "idx", "sig", "slot", "slot_target", "slot_prev", "epoch")

    def __init__(self, eng, fn, is_dma):
        self.eng = eng
        self.fn = fn
        self.is_dma = is_dma
        self.deps = set()
        self.sig = 0
        self.slot = None
        self.slot_target = 0
        self.slot_prev = None


class V:
    __slots__ = ("ap", "key")

    def __init__(self, ap, key):
        self.ap = ap
        self.key = key

    def __getitem__(self, idx):
        return V(self.ap[idx], self.key)

    def sub(self, k):
        base = self.key[0] if isinstance(self.key, tuple) else self.key
        return V(self.ap, (base, k))

    def re(self, pat, **kw):
        return V(self.ap.rearrange(pat, **kw), self.key)

    def bc(self, shape):
        return V(self.ap.to_broadcast(list(shape)), self.key)

    @property
    def shape(self):
        return self.ap.shape


def _ap(x):
    return x.ap if isinstance(x, V) else x


ARENA_F32 = 53000


class Prog:
    def __init__(self, name="k"):
        self.nc = bass.Bass("TRN2", target_bir_lowering=False)
        self.st = ExitStack()
        self.arena = None
        self.aoff = 0
        self.amax = 0
        self.psum = None
        self.bar_deps = None
        self.since_bar = []
        self.bar_seen = {}
        self.epoch = 0
        self.ep_cnt = {}
        self.ops = []
        self.track = {}
        self.n_dma = 0
        self.slot_last = [None] * N_DMA_SEMS
        self.slot_count = [0] * N_DMA_SEMS
        self.uid = 0

    def sb(self, shape, dtype=F32, name=None):
        self.uid += 1
        return self.st.enter_context(self.nc.sbuf_tensor(name or f"sb{self.uid}", list(shape), dtype))

    def ps(self, shape, dtype=F32, name=None):
        self.uid += 1
        return self.st.enter_context(self.nc.psum_tensor(name or f"ps{self.uid}", list(shape), dtype))

    def dram(self, name, shape, dtype=F32, kind="Internal"):
        return self.nc.dram_tensor(name, list(shape), dtype, kind=kind)

    def tile(self, shape, name=None, dtype=None):
        if self.arena is None:
            self.arena = self.st.enter_context(self.nc.sbuf_tensor("arena", [128, ARENA_F32], F32))
        self.uid += 1
        p = shape[0]
        n = int(np.prod(shape[1:]))
        if dtype == BF16:
            nw = (n + 1) // 2
            assert self.aoff + nw <= ARENA_F32, f"arena overflow {self.aoff}+{nw}"
            ap = self.arena[0:p, self.aoff:self.aoff + nw].bitcast(BF16)[:, 0:n]
            self.aoff += nw
        else:
            assert self.aoff + n <= ARENA_F32, f"arena overflow {self.aoff}+{n}"
            ap = self.arena[0:p, self.aoff:self.aoff + n]
            self.aoff += n
        self.amax = max(self.amax, self.aoff)
        if len(shape) == 3:
            ap = ap.rearrange("p (a b) -> p a b", a=shape[1])
        elif len(shape) == 4:
            ap = ap.rearrange("p (a b c) -> p a b c", a=shape[1], b=shape[2])
        return V(ap, name or f"t{self.uid}")

    def mark(self):
        return self.aoff

    def release(self, mark):
        self.barrier()
        self.aoff = mark

    def pbank(self, i):
        if self.psum is None:
            self.psum = [self.st.enter_context(self.nc.psum_tensor(f"psb{j}", [128, 512], F32)) for j in range(8)]
        return V(self.psum[i][:], f"psb{i}")

    def pslot(self, bank, half):
        self.pbank(0)
        return V(self.psum[bank][:, half * 256:(half + 1) * 256], f"psb{bank}_{half}")

    def run_interleaved(self, fns):
        import threading
        il = {"turn": 0, "alive": [True] * len(fns), "cv": threading.Condition(), "tl": threading.local()}
        errs = []

        def nxt(i):
            n = len(fns)
            for d in range(1, n + 1):
                j = (i + d) % n
                if il["alive"][j]:
                    return j
            return i

        def runner(i, fn):
            with il["cv"]:
                while il["turn"] != i:
                    il["cv"].wait()
            il["tl"].i = i
            try:
                fn()
            except BaseException as e:
                errs.append(e)
            finally:
                with il["cv"]:
                    il["alive"][i] = False
                    il["turn"] = nxt(i)
                    il["cv"].notify_all()

        def yield_turn():
            i = getattr(il["tl"], "i", None)
            if i is None:
                return
            with il["cv"]:
                j = nxt(i)
                if j == i:
                    return
                il["turn"] = j
                il["cv"].notify_all()
                while il["turn"] != i:
                    il["cv"].wait()

        self._yield = yield_turn
        ths = [threading.Thread(target=runner, args=(i, f)) for i, f in enumerate(fns)]
        for t in ths:
            t.start()
        for t in ths:
            t.join()
        self._yield = None
        if errs:
            raise errs[0]

    def dview(self, t, name=None):
        ap = t.ap() if hasattr(t, "ap") and callable(t.ap) else t
        return V(ap, name or ap.tensor.name)

    def barrier(self):
        self.bar_deps = list(self.since_bar) if self.bar_deps is None else self.bar_deps + self.since_bar
        last = {}
        dm = []
        for o in self.bar_deps:
            if o.is_dma:
                dm.append(o)
            else:
                last[o.eng] = o
        self.bar_deps = list(last.values()) + dm[-2 * N_DMA_SEMS:]
        self.since_bar = []
        self.bar_seen = {}
        if max(self.ep_cnt.values(), default=0) > 20000:
            self.epoch += 1
            self.ep_cnt = {}

    @staticmethod
    def _key(x):
        if isinstance(x, V):
            x = x.key
        if isinstance(x, tuple):
            t, sub = x
        else:
            t, sub = x, None
        nm = t if isinstance(t, str) else (t.name if hasattr(t, "name") else t.tensor.name)
        return nm, sub

    def _conf(self, nm, sub):
        ent = self.track.setdefault(nm, {})
        if sub is None:
            return list(ent.keys())
        ks = [k for k in ent.keys() if k is None or k == sub]
        return ks

    def op(self, eng, fn, reads=(), writes=(), is_dma=False):
        o = Op(eng, fn, is_dma)
        o.epoch = self.epoch
        if not is_dma:
            self.ep_cnt[eng] = self.ep_cnt.get(eng, 0) + 1
        for r in reads:
            nm, sub = self._key(r)
            ent = self.track.setdefault(nm, {})
            for k in self._conf(nm, sub):
                w = ent[k][0]
                if w is not None:
                    o.deps.add(w)
        for wv in writes:
            nm, sub = self._key(wv)
            ent = self.track.setdefault(nm, {})
            for k in self._conf(nm, sub):
                w, rs = ent[k]
                if w is not None:
                    o.deps.add(w)
                for r in rs:
                    o.deps.add(r)
        for r in reads:
            nm, sub = self._key(r)
            ent = self.track[nm]
            if sub not in ent:
                ent[sub] = [None, []]
            ent[sub][1].append(o)
        for wv in writes:
            nm, sub = self._key(wv)
            ent = self.track[nm]
            if sub is None:
                for k in list(ent.keys()):
                    del ent[k]
            ent[sub] = [o, []]
        o.deps.discard(o)
        if self.bar_deps is not None and not self.bar_seen.get(eng):
            self.bar_seen[eng] = True
            o.deps.update(self.bar_deps)
        self.since_bar.append(o)
        if is_dma:
            s = self.n_dma % N_DMA_SEMS
            self.n_dma += 1
            o.slot = s
            o.slot_prev = self.slot_last[s]
            self.slot_count[s] += 1
            o.slot_target = 16 * self.slot_count[s]
            self.slot_last[s] = o
        o.idx = len(self.ops)
        self.ops.append(o)
        if getattr(self, "_yield", None) is not None:
            self._yield()
        return o

    def dma(self, out, in_, reads=None, writes=None, q="sp", in_fn=None, **kw):
        if in_fn is not None:
            return self.op(q, lambda e: e.dma_start(out=_ap(out), in_=in_fn(e), **kw),
                           reads if reads is not None else [in_], writes if writes is not None else [out], is_dma=True)
        return self.op(q, lambda e: e.dma_start(out=_ap(out), in_=_ap(in_), **kw),
                       reads if reads is not None else [in_], writes if writes is not None else [out], is_dma=True)

    def mm(self, out, lhsT, rhs, start=True, stop=True, reads=None, writes=None, **kw):
        return self.op("pe", lambda e: e.matmul(_ap(out), _ap(lhsT), _ap(rhs), start=start, stop=stop, **kw),
                       reads if reads is not None else [lhsT, rhs], writes if writes is not None else [out])

    def transpose(self, out, in_, ident, reads=None, writes=None):
        return self.op("pe", lambda e: e.transpose(_ap(out), _ap(in_), _ap(ident)),
                       reads if reads is not None else [in_, ident], writes if writes is not None else [out])

    def act(self, out, in_, func, bias=None, scale=1.0, reads=None, writes=None, accum_out=None, eng="act"):
        kw = {}
        rd = [in_]
        if bias is not None:
            kw["bias"] = _ap(bias)
            if not isinstance(bias, (int, float)):
                rd.append(bias)
        if not isinstance(scale, (int, float)):
            rd.append(scale)
        wr = [out]
        if accum_out is not None:
            kw["accum_out"] = _ap(accum_out)
            wr.append(accum_out)
        return self.op(eng, lambda e: e.activation(_ap(out), _ap(in_), func, scale=_ap(scale), **kw),
                       reads if reads is not None else rd, writes if writes is not None else wr)

    def tt(self, out, in0, in1, op, eng="dve", reads=None, writes=None):
        return self.op(eng, lambda e: e.tensor_tensor(_ap(out), _ap(in0), _ap(in1), op),
                       reads if reads is not None else [in0, in1], writes if writes is not None else [out])

    def ts(self, out, in0, s1, op0, s2=None, op1=None, eng="dve", reads=None, writes=None):
        rd = [in0] + [s for s in (s1, s2) if s is not None and not isinstance(s, (int, float))]
        if op1 is None:
            f = lambda e: e.tensor_scalar(_ap(out), _ap(in0), _ap(s1), None, op0)
        else:
            f = lambda e: e.tensor_scalar(_ap(out), _ap(in0), _ap(s1), _ap(s2), op0, op1)
        return self.op(eng, f, reads if reads is not None else rd, writes if writes is not None else [out])

    def stt(self, out, in0, scalar, in1, op0, op1, eng="dve", reads=None, writes=None):
        rd = [in0, in1] + ([] if isinstance(scalar, (int, float)) else [scalar])
        return self.op(eng, lambda e: e.scalar_tensor_tensor(_ap(out), _ap(in0), _ap(scalar), _ap(in1), op0, op1),
                       reads if reads is not None else rd, writes if writes is not None else [out])

    def copy(self, out, in_, eng="dve", reads=None, writes=None):
        if eng == "act":
            f = lambda e: e.copy(_ap(out), _ap(in_))
        else:
            f = lambda e: e.tensor_copy(_ap(out), _ap(in_))
        return self.op(eng, f, reads if reads is not None else [in_], writes if writes is not None else [out])

    def memset(self, ap, val, eng="pool", writes=None):
        return self.op(eng, lambda e: e.memset(_ap(ap), val), [], writes if writes is not None else [ap])

    def recip(self, out, in_, reads=None, writes=None):
        return self.op("dve", lambda e: e.reciprocal(_ap(out), _ap(in_)),
                       reads if reads is not None else [in_], writes if writes is not None else [out])

    def finalize(self, final_waits=()):
        nc = self.nc
        ops = self.ops
        needed = set()
        for o in ops:
            for d in o.deps:
                if not d.is_dma:
                    needed.add(d.idx)
        cnt = {}
        for o in ops:
            if not o.is_dma and (o.idx in needed):
                k_ = (o.eng, o.epoch)
                cnt[k_] = cnt.get(k_, 0) + 1
                o.sig = cnt[k_]
            else:
                o.sig = 0
        sems = {k_: self.st.enter_context(nc.semaphore(f"s_{k_[0]}_{k_[1]}")) for k_ in cnt}
        dsems = [self.st.enter_context(nc.semaphore(f"s_d{i}")) for i in range(N_DMA_SEMS)]
        per_eng = {e: [o for o in ops if o.eng == e] for e in ENGS}
        final = list(final_waits)

        def body(eng_name):
            def _b(e):
                waited = {}
                def wait(key, sem, val):
                    if waited.get(key, 0) >= val:
                        return
                    e.wait_ge(sem, val)
                    waited[key] = val
                for o in per_eng[eng_name]:
                    for d in sorted(o.deps, key=lambda x: x.idx):
                        if d.is_dma:
                            wait(("d", d.slot), dsems[d.slot], d.slot_target)
                        else:
                            if d.eng == eng_name and eng_name == "pe":
                                continue
                            wait(("c", d.eng, d.epoch), sems[(d.eng, d.epoch)], d.sig)
                    if o.is_dma and o.slot_prev is not None:
                        wait(("d", o.slot), dsems[o.slot], o.slot_prev.slot_target)
                    ins = o.fn(e)
                    if o.is_dma:
                        ins.then_inc(dsems[o.slot], 16)
                    elif o.sig:
                        ins.then_inc(sems[(eng_name, o.epoch)], 1)
                if eng_name == "sp":
                    for s in range(N_DMA_SEMS):
                        if self.slot_last[s] is not None:
                            wait(("d", s), dsems[s], self.slot_last[s].slot_target)
            return _b

        with nc.Block() as block:
            block.tensor(body("pe"))
            block.scalar(body("act"))
            block.vector(body("dve"))
            block.gpsimd(body("pool"))
            block.sync(body("sp"))
        self.st.close()
        return nc

D = 1024
TT = 512
ZG = [64] * 12 + [64, 64, 128] + [128, 128, 128, 32] + [64] * 16 + [4, 4]
NG = len(ZG)
G_R, G_K, G_V, G_WLO, G_ALO, G_GLO = 0, 4, 8, 12, 13, 14
G_CQ, G_CKV, G_KPE = 15, 17, 18
G_GQ, G_GK, G_GV, G_GG, G_GB, G_GA = 19, 23, 27, 31, 35, 36
ZOFF = np.concatenate([[0], np.cumsum(ZG)]).astype(int)
NZ = int(ZOFF[-1])

CV = {}
_n = 0
for nm, k in [("nmg", 8), ("mu", 15), ("w0", 4), ("a0", 4), ("kk", 4), ("ka", 4), ("rk", 4), ("lng", 4), ("lnb", 4),
              ("vmu", 8), ("vb", 4), ("qng", 2), ("kvng", 1), ("qkq", 1), ("qkk", 1), ("invf", 1),
              ("conv", 48), ("alog", 1), ("dtb", 1), ("gng", 1), ("ropec", 1), ("nfg", 8), ("png", 8), ("m0", 1), ("m1", 1)]:
    CV[nm] = _n
    _n += k
NCV = _n


def consts_np():
    c = {}
    c["ident"] = np.eye(128, dtype=np.float32)
    c["ones"] = np.ones((128, 128), np.float32)
    bd = np.zeros((128, 128), np.float32)
    bd[:64, :64] = 1
    bd[64:, 64:] = 1
    c["bd64"] = bd
    s = np.arange(64)[:, None]
    t = np.arange(64)[None, :]
    incl = (t >= s).astype(np.float32)
    strict = (t > s).astype(np.float32)
    c["m_incl"] = np.concatenate([incl, incl], 0)
    c["m_strict"] = np.concatenate([strict, strict], 0)
    c["m_incl_T"] = np.concatenate([incl.T, incl.T], 0)
    c["m_strict_T"] = np.concatenate([strict.T, strict.T], 0)
    c["neg_incl"] = (1.0 - c["m_incl"]) * -1e4
    c["neg_incl_T"] = (1.0 - c["m_incl_T"]) * -1e4
    c["id64x2"] = np.concatenate([np.eye(64, dtype=np.float32)] * 2, 0)
    kk = np.arange(128)[:, None]
    qq = np.arange(128)[None, :]
    c["att_mask"] = (qq >= kk).astype(np.float32)
    R = np.zeros((96, 96), np.float32)
    for m in range(16):
        R[64 + m, 64 + m + 16] = -1.0
        R[64 + 16 + m, 64 + m] = 1.0
    c["ropeRT"] = np.ascontiguousarray(R.T)
    sel = np.zeros((4, 4, 64), np.float32)
    for h in range(4):
        sel[h, h, :] = 1.0
    c["sel4"] = sel.reshape(4, 256)
    sel8 = np.zeros((8, 8, 128), np.float32)
    for e in range(8):
        sel8[e, e, :] = 1.0
    c["sel8"] = sel8.reshape(8, 1024)
    c["id4"] = np.tile(np.eye(64, dtype=np.float32)[:, None, :], (1, 4, 1)).reshape(64, 256)
    c["neg4"] = np.tile(c["neg_incl"][:64][:, None, :], (1, 4, 1)).reshape(64, 256)
    c["neg4T"] = np.tile(c["neg_incl_T"][:64][:, None, :], (1, 4, 1)).reshape(64, 256)
    c["ms4"] = np.tile(c["m_strict"][:64][:, None, :], (1, 4, 1)).reshape(64, 256)
    c["ms4T"] = np.tile(c["m_strict_T"][:64][:, None, :], (1, 4, 1)).reshape(64, 256)
    c["mi4"] = np.tile(c["m_incl"][:64][:, None, :], (1, 4, 1)).reshape(64, 256)
    return c


CONST_SHAPES = {k: v.shape for k, v in consts_np().items()}


class Ctx:
    pass


def load_consts(P, names):
    out = {}
    for nm in names:
        shp = CONST_SHAPES[nm]
        d = P.dview(P.dram("c_" + nm, shp, F32, kind="ExternalInput"))
        t = P.tile(list(shp), name="c_" + nm)
        P.dma(t, d)
        out[nm] = t
    return out


def rstd_from_ps(P, out, ps, n, eps):
    P.act(out, ps, AF.Ln, scale=float(1.0 / n), bias=float(eps))
    P.act(out, out, AF.Exp, scale=-0.5)


def act_sigmoid(P, out, in_, scale=1.0, negbias=None):
    if negbias is None:
        P.act(out, in_, AF.Exp, scale=-float(scale))
    else:
        P.act(out, in_, AF.Exp, scale=-float(scale), bias=negbias)
    P.act(out, out, AF.Ln, bias=1.0)
    P.act(out, out, AF.Exp, scale=-1.0)


def phase_proj(P, C, S, hT, w_d, ncols, groups, zT, gcol, uT=None, func=None, nbuf=2, zflat=False, dyn=None):
    mk = P.mark()
    cv = C.cv
    w = P.tile([128, 8, ncols], name="w_in", dtype=BF16)
    wst = [P.tile([128, 8, 512], name=f"wst{i}") for i in range(2)]
    wv = w_d.re("(c p) n -> p c n", p=128)
    step = 512
    for i_, c0 in enumerate(range(0, ncols, step)):
        c1 = min(ncols, c0 + step)
        st_ = wst[i_ % 2]
        P.dma(st_[:, :, 0:c1 - c0], wv[:, :, c0:c1])
        P.copy(w[:, :, c0:c1].sub(c0), st_[:, :, 0:c1 - c0], eng=("pool" if i_ % 2 == 0 else "act"))
    hb = [P.tile([128, 8, TT], name=f"hb{i}") for i in range(nbuf)]
    sq = P.tile([128, 8, TT], name="sq")
    ub = [P.tile([128, 8, TT], name=f"ub{i}", dtype=BF16) for i in range(nbuf)]
    rs = P.tile([128, TT], name="rs")
    stg = [P.tile([128, TT], name=f"stg{i}") for i in range(4)]
    hv = hT.re("(c p) t -> p c t", p=128)
    ps_ss = P.pbank(0)
    pz = [P.pbank(1 + i) for i in range(4)]
    nt = S // TT
    k = 0
    for ti in range(nt):
        tsl = slice(ti * TT, (ti + 1) * TT)
        h = hb[ti % nbuf]
        u = ub[ti % nbuf]
        if dyn is not None:
            P.dma(h, hv[:, :, tsl])
            P.dma(sq, hv[:, :, dyn + ti * TT:dyn + (ti + 1) * TT])
            P.ts(h, h, cv[:, CV["m0"]:CV["m0"] + 1], ALU.mult)
            P.stt(h, sq, cv[:, CV["m1"]:CV["m1"] + 1], h, ALU.mult, ALU.add)
        else:
            P.dma(h, hv[:, :, tsl])
        P.act(sq, h, AF.Square)
        for c in range(8):
            P.mm(ps_ss, C.k["ones"], sq[:, c, :], start=(c == 0), stop=(c == 7))
        rstd_from_ps(P, rs, ps_ss, D, 1e-6)
        if uT is not None:
            for c in range(8):
                P.stt(sq[:, c, :], h[:, c, :], cv[:, gcol + c:gcol + c + 1], rs, ALU.mult, ALU.mult)
            P.dma(uT.re("(c p) t -> p c t", p=128)[:, :, tsl], sq, q="pool")
            P.copy(u, sq, eng="act")
        else:
            for c in range(8):
                P.stt(u[:, c, :], h[:, c, :], cv[:, gcol + c:gcol + c + 1], rs, ALU.mult, ALU.mult)
        for gi, (co, n) in enumerate(groups):
            pp = pz[k % 4]
            st = stg[k % 4]
            for c in range(8):
                wsl = w[:, c, co:co + n]
                if co // step == (co + n - 1) // step:
                    wsl = wsl.sub((co // step) * step)
                P.mm(pp[0:n, :], wsl, u[:, c, :], start=(c == 0), stop=(c == 7))
            if func is not None:
                P.act(st[0:n, :], pp[0:n, :], func)
            else:
                P.copy(st[0:n, :], pp[0:n, :], eng=("act" if k % 2 == 0 else "dve"))
            if zflat:
                P.dma(zT[co:co + n, tsl].sub(gi), st[0:n, :], q="pool")
            else:
                P.dma(zT[gi, 0:n, tsl].sub(gi), st[0:n, :], q="pool")
            k += 1
    P.release(mk)


def phase_mla(P, C, S, zT, pos_d, w_uq_d, w_uk_d, w_uv_d, oT):
    mk = P.mark()
    cv = C.cv
    K = C.k
    nt = S // TT
    wuq_f = P.tile([128, 2, 384], name="wuq_f")
    P.dma(wuq_f, w_uq_d.re("(c p) n -> p c n", p=128))
    wuk_f = P.tile([128, 256], name="wuk_f")
    P.dma(wuk_f, w_uk_d)
    wuv_f = P.tile([128, 256], name="wuv_f")
    P.dma(wuv_f, w_uv_d)
    wuq = P.tile([128, 2, 384], name="wuq", dtype=BF16)
    wuk = P.tile([128, 256], name="wuk", dtype=BF16)
    wuv = P.tile([128, 256], name="wuv", dtype=BF16)
    P.copy(wuq, wuq_f, eng="pool")
    P.copy(wuk, wuk_f, eng="pool")
    P.copy(wuv, wuv_f, eng="pool")
    ones_b = P.tile([128, 64], name="ones_b", dtype=BF16)
    P.copy(ones_b, K["ones"][:, 0:64], eng="pool")
    KT = P.tile([96, 4, S], name="KT", dtype=BF16)
    VT = P.tile([128, S // 128, 256], name="Vtm", dtype=BF16)
    rc = P.tile([96, TT], name="rope_c")
    rsn = P.tile([96, TT], name="rope_s")
    posi = P.tile([96, TT], name="posi")
    posf = P.tile([96, TT], name="posf")
    ang = P.tile([96, TT], name="ang")
    tmp = P.tile([96, TT], name="ropetmp")
    tmp2 = P.tile([96, TT], name="ropetmp2")
    P.memset(rc[0:64, :], 1.0, writes=[rc.sub("lo")])
    P.memset(rsn[0:64, :], 0.0, writes=[rsn.sub("lo")])
    pi_ap = V(posi.ap.bitcast(I32), posi.key)
    invf = cv[64:96, CV["invf"]:CV["invf"] + 1]
    negpi = cv[64:96, CV["ropec"]:CV["ropec"] + 1]
    TWO_PI = float(2 * np.pi)

    def rope_tile(ti):
        tsl = slice(ti * TT, (ti + 1) * TT)
        P.dma(pi_ap[64:96, :], V(pos_d.ap[:, tsl].partition_broadcast(32), pos_d.key))
        P.copy(posf[64:96, :], pi_ap[64:96, :])
        P.ts(ang[64:96, :], posf[64:96, :], invf, ALU.mult)
        for dst, shift in ((rsn, 0.0), (rc, float(np.pi / 2))):
            a_ = ang[64:96, :]
            if shift:
                P.ts(tmp2[64:96, :], ang[64:96, :], shift, ALU.add)
                a_ = tmp2[64:96, :]
            P.ts(tmp[64:96, :], a_, float(1.0 / TWO_PI), ALU.mult)
            P.copy(pi_ap[64:96, :], tmp[64:96, :])
            P.copy(tmp[64:96, :], pi_ap[64:96, :])
            P.stt(tmp[64:96, :], tmp[64:96, :], -TWO_PI, a_, ALU.mult, ALU.add)
            P.ts(posf[64:96, :], tmp[64:96, :], float(np.pi), ALU.is_gt, TWO_PI, ALU.mult)
            P.tt(tmp[64:96, :], tmp[64:96, :], posf[64:96, :], ALU.subtract)
            P.act(dst[64:96, :], tmp[64:96, :], AF.Sin, writes=[dst.sub("hi")])

    ones = K["ones"]
    cq = [P.tile([128, 2, TT], name=f"cq{i}") for i in range(2)]
    ckv = [P.tile([128, TT], name=f"ckv{i}") for i in range(2)]
    cqb = [P.tile([128, 2, TT], name=f"cqb{i}", dtype=BF16) for i in range(2)]
    ckvb = [P.tile([128, TT], name=f"ckvb{i}", dtype=BF16) for i in range(2)]
    kpe = [P.tile([96, TT], name=f"kpe{i}") for i in range(2)]
    sq = P.tile([128, 2, TT], name="msq")
    rs = P.tile([128, TT], name="mrs")
    raw = P.tile([96, TT], name="raw")
    nrm = P.tile([96, TT], name="nrm")
    rot = P.tile([96, TT], name="rot")
    QT = P.tile([96, 4, TT], name="QT", dtype=BF16)
    ps_a = P.pbank(0)
    ps_b = P.pbank(1)
    ps_c = P.pbank(2)

    def qk_finish(src_raw, gcolname, dst, tsl):
        P.act(sq[0:96, 0, :], src_raw, AF.Square)
        P.mm(ps_b[0:96, :], ones[0:96, 0:96], sq[0:96, 0, :])
        rstd_from_ps(P, rs[0:96, :], ps_b[0:96, :], 96, 1e-6)
        g = cv[0:96, CV[gcolname]:CV[gcolname] + 1]
        P.stt(nrm, src_raw, g, rs[0:96, :], ALU.mult, ALU.mult)
        P.mm(ps_c[0:96, :], K["ropeRT"], nrm)
        P.tt(rot, ps_c[0:96, :], rsn, ALU.mult)
        P.tt(nrm, nrm, rc, ALU.mult, eng="pool")
        P.tt(dst, nrm, rot, ALU.add)

    def load_norm(ti, want_q):
        tsl = slice(ti * TT, (ti + 1) * TT)
        i2 = ti % 2
        if want_q:
            P.dma(cq[i2], zT[G_CQ:G_CQ + 2, :, tsl].re("g p t -> p g t"))
            P.act(sq, cq[i2], AF.Square)
            P.mm(ps_a, ones, sq[:, 0, :], start=True, stop=False)
            P.mm(ps_a, ones, sq[:, 1, :], start=False, stop=True)
            rstd_from_ps(P, rs, ps_a, 256, 1e-6)
            for c in range(2):
                P.stt(cqb[i2][:, c, :], cq[i2][:, c, :], cv[:, CV["qng"] + c:CV["qng"] + c + 1], rs, ALU.mult, ALU.mult)
        else:
            P.dma(ckv[i2], zT[G_CKV, :, tsl])
            P.dma(kpe[i2][64:96, :], zT[G_KPE, 0:32, tsl])
            P.act(sq[:, 0, :], ckv[i2], AF.Square)
            P.mm(ps_a, ones, sq[:, 0, :])
            rstd_from_ps(P, rs, ps_a, 128, 1e-6)
            P.stt(ckvb[i2], ckv[i2], cv[:, CV["kvng"]:CV["kvng"] + 1], rs, ALU.mult, ALU.mult)
        return tsl, i2

    for ti in range(nt):
        rope_tile(ti)
        tsl, i2 = load_norm(ti, False)
        for h in range(4):
            P.mm(ps_b[0:64, :], wuk[:, h * 64:(h + 1) * 64], ckvb[i2])
            P.copy(raw[0:64, :], ps_b[0:64, :], eng="act", writes=[raw.sub("lo")])
            P.copy(raw[64:96, :], kpe[i2][64:96, :], eng="pool", writes=[raw.sub("hi")])
            qk_finish(raw, "qkk", KT[:, h, tsl].sub(h), tsl)
        for j in range(TT // 128):
            P.mm(ps_c[:, 0:256], ckvb[i2][:, j * 128:(j + 1) * 128], wuv)
            P.copy(VT[:, ti * (TT // 128) + j, :], ps_c[:, 0:256], eng="act")

    pt = [P.tile([128, TT], name=f"pt{i}", dtype=BF16) for i in range(3)]
    osb = P.tile([64, TT], name="osb")
    lsb = P.tile([64, TT], name="lsb")
    ps_s = [P.pbank(3), P.pbank(4)]
    ps_o = P.pbank(5)
    ps_l = P.pbank(6)
    scale = float(96 ** -0.5)
    for ti in range(nt):
        rope_tile(ti)
        tsl, i2 = load_norm(ti, True)
        for h in range(4):
            for c in range(2):
                P.mm(ps_b[0:96, :], wuq[:, c, h * 96:(h + 1) * 96], cqb[i2][:, c, :], start=(c == 0), stop=(c == 1))
            P.copy(raw, ps_b[0:96, :], eng="act")
            qk_finish(raw, "qkq", QT[:, h, :].sub(h), tsl)
        for h in range(4):
            nkc = 4 * (ti + 1)

            def c0_of(kc):
                j = kc - 4 * ti
                return 0 if j <= 0 else j * 128

            def score(kc):
                c0 = c0_of(kc)
                P.mm(ps_s[kc % 2][:, c0:TT], KT[:, h, kc * 128:(kc + 1) * 128].sub(h), QT[:, h, c0:TT].sub(h))
            score(0)
            for kc in range(nkc):
                if kc + 1 < nkc:
                    score(kc + 1)
                j = kc - 4 * ti
                c0 = c0_of(kc)
                pss = ps_s[kc % 2]
                p_t = pt[kc % 3]
                P.act(p_t[:, c0:TT], pss[:, c0:TT], AF.Exp, scale=scale)
                if j >= 0:
                    P.tt(p_t[:, c0:c0 + 128], p_t[:, c0:c0 + 128], K["att_mask"], ALU.mult, eng="pool")
                P.mm(ps_o[0:64, c0:TT], VT[:, kc, h * 64:(h + 1) * 64], p_t[:, c0:TT], start=(kc == 0), stop=(kc == nkc - 1))
                P.mm(ps_l[0:64, c0:TT], ones_b, p_t[:, c0:TT], start=(kc == 0), stop=(kc == nkc - 1))
            P.act(lsb, ps_l[0:64, :], AF.Ln)
            P.act(lsb, lsb, AF.Exp, scale=-1.0)
            P.tt(osb, ps_o[0:64, :], lsb, ALU.mult)
            P.dma(oT[h * 64:(h + 1) * 64, tsl].sub(h), osb, q="pool")
    P.release(mk)


def neumann_inv(P, C, A0, B0, bufs, ps1, ps2, ps3):
    id4 = C.k["id4"]
    TTt = bufs["TT"]
    P.tt(TTt, B0, id4, ALU.add)
    A = [A0, bufs["A1"]]
    B = [B0, bufs["B1"]]
    for k in range(1, 6):
        a_prev, a_new = A[(k - 1) % 2], A[k % 2]
        b_prev, b_new = B[(k - 1) % 2], B[k % 2]
        for h in range(4):
            hs = slice(h * 64, (h + 1) * 64)
            P.mm(ps1[0:64, hs], b_prev[:, hs], a_prev[:, hs])
        if k < 5:
            for h in range(4):
                hs = slice(h * 64, (h + 1) * 64)
                P.mm(ps2[0:64, hs], a_prev[:, hs], b_prev[:, hs])
        P.copy(a_new, ps1[0:64, 0:256], eng="act")
        if k < 5:
            P.copy(b_new, ps2[0:64, 0:256], eng="dve")
        for h in range(4):
            hs = slice(h * 64, (h + 1) * 64)
            P.mm(ps3[0:64, hs], a_new[:, hs], TTt[:, hs])
        P.tt(TTt, TTt, ps3[0:64, 0:256], ALU.add)
    return TTt


def phase_gdn(P, C, S, zT, oT, ttl=512, psbase=None, release=True):
    mk = P.mark()
    TT = ttl
    cv = C.cv
    K = C.k
    nt = S // TT
    NCH = TT // 64
    ones = K["ones"]
    ident = K["ident"]
    cvw = lambda seg, h, j: cv[0:64, CV["conv"] + (seg * 4 + h) * 4 + j:CV["conv"] + (seg * 4 + h) * 4 + j + 1]
    St = P.tile([64, 4, 64], name="gS")
    P.memset(St, 0.0)
    Sb = P.tile([64, 4, 64], name="gSb", dtype=BF16)
    P.copy(Sb, St, eng="pool")
    nA = P.tile([4, 1], name="nA")
    P.act(nA, cv[0:4, CV["alog"]:CV["alog"] + 1], AF.Exp)
    P.ts(nA, nA, -1.0, ALU.mult)
    def mkbuf(i):
        b = {}
        for nm in ["q", "k", "kb", "qd"]:
            b[nm] = P.tile([64, 4, TT], name=f"g{nm}{i}", dtype=BF16)
        b["k32"] = P.tile([64, 4, TT], name=f"gk32{i}")
        b["q32"] = P.tile([64, 4, TT], name=f"gq32{i}")
        b["ktm"] = P.tile([64, NCH, 4, 64], name=f"gktm{i}", dtype=BF16)
        b["bv"] = P.tile([64, NCH, 4, 64], name=f"gbv{i}")
        b["bg"] = P.tile([64, NCH, 12], name=f"gbg{i}")
        b["c2"] = P.tile([64, NCH, 4], name=f"gc2{i}")
        b["ngc"] = P.tile([64, NCH, 4], name=f"gngc{i}")
        b["dl"] = P.tile([64, 4, NCH], name=f"gdl{i}")
        return b
    TB = [mkbuf(0), mkbuf(1)]
    xin4 = [P.tile([64, TT + 3], name=f"gxin{i}") for i in range(4)]
    acc4 = [P.tile([64, TT], name=f"gacc{i}") for i in range(4)]
    sq4 = [P.tile([64, TT], name=f"gsq4{i}") for i in range(4)]
    rs4 = [P.tile([64, TT], name=f"grs4{i}") for i in range(4)]
    vfm = P.tile([64, 4, TT], name="gvfm")
    sq = P.tile([64, TT], name="gsq")
    rs = P.tile([64, TT], name="grs")
    bfm = P.tile([4, TT], name="gbfm")
    gfm = [P.tile([4, TT], name=f"ggfm{i}") for i in range(2)]
    efm = P.tile([4, TT], name="gefm")
    kdf = P.tile([4, TT], name="gkdf")
    gl4 = P.tile([4, NCH], name="ggl4")
    ob = [P.tile([64, 4, TT], name=f"gob{i}") for i in range(2)]
    gate = P.tile([64, TT], name="ggate")
    def mkch(i):
        d_ = {nm: P.tile([64, 256], name=f"gc_{nm}{i}") for nm in ["E", "F", "G1", "G2", "Gs"]}
        d_.update({nm: P.tile([64, 256], name=f"gc_{nm}{i}", dtype=BF16) for nm in ["A0", "B0", "A1", "B1", "TT", "Ain", "X", "vn"]})
        return d_
    CB = [mkch(0), mkch(1)]
    if psbase is None:
        psA, psB, psC, psD, psE, psF, psG, psH = [P.pbank(i) for i in range(8)]
    else:
        psA, psB, psC, psD, psE, psF, psG, psH = [P.pbank(psbase + j % 4) for j in range(8)]
    xk = 0
    for ti in range(nt):
        tb = TB[ti % 2]
        tsl = slice(ti * TT, (ti + 1) * TT)
        for seg, (g0, dst) in enumerate([(G_GQ, tb["q32"]), (G_GK, tb["k32"]), (G_GV, vfm)]):
            for h in range(4):
                x = xin4[h]
                a = acc4[h]
                if ti == 0:
                    P.memset(x[:, 0:3], 0.0, writes=[x.sub("halo")])
                    P.dma(x[:, 3:TT + 3].sub("body"), zT[g0 + h, 0:64, 0:TT])
                else:
                    P.dma(x, zT[g0 + h, 0:64, ti * TT - 3:(ti + 1) * TT])
                P.ts(a, x[:, 0:TT], cvw(seg, h, 0), ALU.mult)
                for j in range(1, 4):
                    P.stt(a, x[:, j:TT + j], cvw(seg, h, j), a, ALU.mult, ALU.add)
            for h in range(4):
                act_sigmoid(P, sq4[h], acc4[h])
            for h in range(4):
                if seg == 2:
                    P.tt(dst[:, h, :].sub(h), acc4[h], sq4[h], ALU.mult)
                else:
                    P.tt(acc4[h], acc4[h], sq4[h], ALU.mult)
            if seg < 2:
                pss_ = [psA, psB, psC, psD]
                for h in range(4):
                    P.act(sq4[h], acc4[h], AF.Square)
                for h in range(4):
                    P.mm(pss_[h][0:64, 0:TT], ones[0:64, 0:64], sq4[h])
                for h in range(4):
                    P.act(rs4[h], pss_[h][0:64, 0:TT], AF.Ln, scale=1.0, bias=1e-12)
                for h in range(4):
                    P.act(rs4[h], rs4[h], AF.Exp, scale=-0.5)
                for h in range(4):
                    P.stt(dst[:, h, :].sub(h), acc4[h], (0.125 if seg == 0 else 1.0), rs4[h], ALU.mult, ALU.mult)
        P.dma(bfm, zT[G_GB, 0:4, tsl])
        act_sigmoid(P, bfm, bfm)
        g0t = gfm[0]
        P.dma(g0t, zT[G_GA, 0:4, tsl])
        P.act(g0t, g0t, AF.Exp, bias=cv[0:4, CV["dtb"]:CV["dtb"] + 1])
        P.act(g0t, g0t, AF.Ln, bias=1.0)
        P.ts(g0t, g0t, nA[:, 0:1], ALU.mult)
        cur = 0
        for sh in (1, 2, 4, 8, 16, 32):
            src = gfm[cur].re("h (n c) -> h n c", c=64)
            dstt = gfm[1 - cur].re("h (n c) -> h n c", c=64)
            P.copy(dstt[:, :, 0:sh], src[:, :, 0:sh], eng="pool", writes=[gfm[1 - cur].sub("a")])
            P.tt(dstt[:, :, sh:64], src[:, :, sh:64], src[:, :, 0:64 - sh], ALU.add, writes=[gfm[1 - cur].sub("b")])
            cur = 1 - cur
        gc = gfm[cur]
        P.act(efm, gc, AF.Exp)
        gc3 = gc.re("h (n c) -> h n c", c=64)
        P.copy(gl4, gc3[:, :, 63])
        P.tt(kdf.re("h (n c) -> h n c", c=64), V(gl4.ap.unsqueeze(2).to_broadcast([4, NCH, 64]), gl4.key), gc3, ALU.subtract)
        P.act(kdf, kdf, AF.Exp)
        for h in range(4):
            P.mm(psA[0:64, h * NCH:(h + 1) * NCH], K["sel4"][:, h * 64:(h + 1) * 64], gl4)
        P.act(tb["dl"].re("p h n -> p (h n)"), psA[0:64, 0:4 * NCH], AF.Exp)
        P.copy(tb["k"], tb["k32"], eng="pool")
        P.copy(tb["q"], tb["q32"], eng="pool")
        for h in range(4):
            P.mm(psB[0:64, 0:TT], K["sel4"][:, h * 64:(h + 1) * 64], bfm)
            P.tt(tb["kb"][:, h, :].sub(h), tb["k32"][:, h, :].sub(h), psB[0:64, 0:TT], ALU.mult)
            P.mm(psC[0:64, 0:TT], K["sel4"][:, h * 64:(h + 1) * 64], efm)
            P.tt(tb["qd"][:, h, :].sub(h), tb["q32"][:, h, :].sub(h), psC[0:64, 0:TT], ALU.mult)
        for n in range(NCH):
            cs = slice(n * 64, (n + 1) * 64)
            for h in range(4):
                P.transpose(psD[0:64, h * 64:(h + 1) * 64], tb["k32"][:, h, cs].sub(h), ident[0:64, 0:64])
            P.copy(tb["ktm"][:, n, :, :].re("p h d -> p (h d)"), psD[0:64, 0:256], eng="act")
            for h in range(4):
                P.transpose(psE[0:64, h * 64:(h + 1) * 64], vfm[:, h, cs].sub(h), ident[0:64, 0:64])
            P.copy(tb["bv"][:, n, :, :].re("p h d -> p (h d)"), psE[0:64, 0:256], eng="dve")
            P.transpose(psF[0:64, 0:4], bfm[:, cs], ident[0:4, 0:4])
            P.transpose(psF[0:64, 4:8], gc[:, cs], ident[0:4, 0:4])
            P.transpose(psF[0:64, 8:12], kdf[:, cs], ident[0:4, 0:4])
            P.copy(tb["bg"][:, n, :], psF[0:64, 0:12], eng="act")
        bg = tb["bg"]
        P.ts(tb["ngc"], bg[:, :, 4:8], -1.0, ALU.mult)
        P.act(tb["c2"], bg[:, :, 4:8], AF.Exp)
        P.stt(tb["c2"], tb["c2"], -1.0, bg[:, :, 0:4], ALU.mult, ALU.mult)
        P.tt(tb["ktm"], tb["ktm"], V(bg.ap[:, :, 8:12].unsqueeze(3).to_broadcast([64, NCH, 4, 64]), bg.key), ALU.mult)
        P.tt(tb["bv"], tb["bv"], V(bg.ap[:, :, 0:4].unsqueeze(3).to_broadcast([64, NCH, 4, 64]), bg.key), ALU.mult)
        o_t = ob[ti % 2]

        def g_pre(n):
            cb = CB[n % 2]
            cs = slice(n * 64, (n + 1) * 64)
            gcn = V(bg.ap[:, n, 4:8].unsqueeze(2).to_broadcast([64, 4, 64]), bg.key)
            ngcn = V(tb["ngc"].ap[:, n, :].unsqueeze(2).to_broadcast([64, 4, 64]), tb["ngc"].key)
            E3 = cb["E"].re("p (h c) -> p h c", h=4)
            F3 = cb["F"].re("p (h c) -> p h c", h=4)
            P.tt(E3, K["id4"].re("p (h c) -> p h c", h=4), gcn, ALU.mult)
            P.tt(F3, K["neg4"].re("p (h c) -> p h c", h=4), ngcn, ALU.add)
            P.mm(psG[0:64, 0:256], ones[0:64, 0:64], cb["E"], start=True, stop=False)
            P.mm(psG[0:64, 0:256], ident[0:64, 0:64], cb["F"], start=False, stop=True)
            P.act(cb["G1"], psG[0:64, 0:256], AF.Exp)
            P.ts(cb["E"], cb["E"], -1.0, ALU.mult)
            P.tt(F3, K["neg4T"].re("p (h c) -> p h c", h=4), gcn, ALU.add)
            P.mm(psH[0:64, 0:256], ones[0:64, 0:64], cb["E"], start=True, stop=False)
            P.mm(psH[0:64, 0:256], ident[0:64, 0:64], cb["F"], start=False, stop=True)
            P.act(cb["G2"], psH[0:64, 0:256], AF.Exp)
            P.tt(cb["Gs"], cb["G1"], K["ms4"], ALU.mult, eng="pool")
            P.tt(cb["G2"], cb["G2"], K["ms4T"], ALU.mult, eng="pool")
            for h in range(4):
                hs = slice(h * 64, (h + 1) * 64)
                P.mm(psA[0:64, hs], tb["k"][:, h, cs].sub(h), tb["kb"][:, h, cs].sub(h))
                P.mm(psB[0:64, hs], tb["kb"][:, h, cs].sub(h), tb["k"][:, h, cs].sub(h))
                P.mm(psC[0:64, hs], tb["k"][:, h, cs].sub(h), tb["q"][:, h, cs].sub(h))
            P.stt(cb["B0"], psA[0:64, 0:256], -1.0, cb["Gs"], ALU.mult, ALU.mult)
            P.stt(cb["A0"], psB[0:64, 0:256], -1.0, cb["G2"], ALU.mult, ALU.mult)
            P.tt(cb["Ain"], psC[0:64, 0:256], cb["G1"], ALU.mult)
            return neumann_inv(P, C, cb["A0"], cb["B0"], cb, psA, psB, psC)

        def g_scan(n, TTm):
            cb = CB[n % 2]
            cs = slice(n * 64, (n + 1) * 64)
            for h in range(4):
                hs = slice(h * 64, (h + 1) * 64)
                P.mm(psD[0:64, hs], tb["k"][:, h, cs].sub(h), Sb[:, h, :])
            X3 = cb["X"].re("p (h v) -> p h v", h=4)
            c2n = V(tb["c2"].ap[:, n, :].unsqueeze(2).to_broadcast([64, 4, 64]), tb["c2"].key)
            P.tt(X3, psD[0:64, 0:256].re("p (h v) -> p h v", h=4), c2n, ALU.mult)
            P.tt(X3, X3, tb["bv"][:, n, :, :], ALU.add)
            for h in range(4):
                hs = slice(h * 64, (h + 1) * 64)
                P.mm(psE[0:64, hs], TTm[:, hs], cb["X"][:, hs])
            P.copy(cb["vn"], psE[0:64, 0:256], eng="act")
            for h in range(4):
                hs = slice(h * 64, (h + 1) * 64)
                P.mm(psF[0:64, hs], Sb[:, h, :], tb["qd"][:, h, cs].sub(h), start=True, stop=False)
                P.mm(psF[0:64, hs], cb["vn"][:, hs], cb["Ain"][:, hs], start=False, stop=True)
            P.copy(o_t[:, :, cs], psF[0:64, 0:256].re("p (h c) -> p h c", h=4), eng="act")
            for h in range(4):
                hs = slice(h * 64, (h + 1) * 64)
                P.mm(psG[0:64, hs], tb["ktm"][:, n, h, :], cb["vn"][:, hs])
            dln = V(tb["dl"].ap[:, :, n].unsqueeze(2).to_broadcast([64, 4, 64]), tb["dl"].key)
            P.tt(St, St, dln, ALU.mult)
            P.tt(St, St, psG[0:64, 0:256].re("p (h v) -> p h v", h=4), ALU.add)
            P.copy(Sb, St, eng="pool")

        tt_next = g_pre(0)
        for n in range(NCH):
            tt_cur = tt_next
            if n + 1 < NCH:
                tt_next = g_pre(n + 1)
            g_scan(n, tt_cur)
        for h in range(4):
            P.dma(gate, zT[G_GG + h, 0:64, tsl])
            act_sigmoid(P, rs, gate)
            P.tt(gate, gate, rs, ALU.mult)
            P.act(sq, o_t[:, h, :], AF.Square)
            P.mm(psH[0:64, 0:TT], ones[0:64, 0:64], sq)
            rstd_from_ps(P, rs, psH[0:64, 0:TT], 64.0, 1e-6)
            P.stt(rs, rs, cv[0:64, CV["gng"]:CV["gng"] + 1], gate, ALU.mult, ALU.mult)
            P.tt(sq, o_t[:, h, :], rs, ALU.mult)
            P.dma(oT[h * 64:(h + 1) * 64, tsl].sub(h), sq, q="pool")
    if release:
        P.release(mk)


def phase_rwkv(P, C, S, L, zT, oT, w_up_d, a_up_d, g_up_d, vfT, uT, v_down_d, v_up_d, ttl=512, psbase=None, release=True):
    mk = P.mark()
    TT = ttl
    cv = C.cv
    K = C.k
    nt = S // TT
    NCH = TT // 64
    ones = K["ones"]
    ident = K["ident"]
    col = lambda nm, h: cv[0:64, CV[nm] + h:CV[nm] + h + 1]
    w_up = P.tile([64, 256], name="r_wup"); P.dma(w_up, w_up_d)
    a_up = P.tile([64, 256], name="r_aup"); P.dma(a_up, a_up_d)
    g_up = P.tile([128, 256], name="r_gup"); P.dma(g_up, g_up_d)
    if L > 0:
        v_dn = P.tile([128, 8, 32], name="r_vdn"); P.dma(v_dn, v_down_d.re("(c p) n -> p c n", p=128))
        v_upt = P.tile([32, 256], name="r_vup"); P.dma(v_upt, v_up_d)
    ncv = P.tile([64, 12], name="r_ncv")
    P.ts(ncv[:, 0:4], cv[0:64, CV["w0"]:CV["w0"] + 4], -1.0, ALU.mult, writes=[ncv.sub(0)])
    P.ts(ncv[:, 4:8], cv[0:64, CV["a0"]:CV["a0"] + 4], -1.0, ALU.mult, writes=[ncv.sub(1)])
    P.ts(ncv[:, 8:12], cv[0:64, CV["vb"]:CV["vb"] + 4], -1.0, ALU.mult, writes=[ncv.sub(2)])
    oma = P.tile([64, 4], name="r_oma")
    P.ts(oma, cv[0:64, CV["ka"]:CV["ka"] + 4], -1.0, ALU.mult, 1.0, ALU.add)
    ST = P.tile([64, 4, 64], name="rST")
    P.memset(ST, 0.0)
    STb = P.tile([64, 4, 64], name="rSTb", dtype=BF16)
    P.copy(STb, ST, eng="pool")
    T4 = lambda nm: P.tile([64, 4, TT], name=nm)
    T4b = lambda nm: P.tile([64, 4, TT], name=nm, dtype=BF16)
    at, bt, kt, rt = T4b("r_at"), T4b("r_bt"), T4b("r_kt"), T4b("r_rt")
    bon, gate4 = T4("r_bon"), T4("r_gate")
    t0, t1, t2, t3, t4_, t5 = [T4(f"r_t{i}") for i in range(6)]
    y4 = t0
    bh_tm = P.tile([64, NCH, 4, 64], name="r_bhtm", dtype=BF16)
    kh_tm = P.tile([64, NCH, 4, 64], name="r_khtm", dtype=BF16)
    v_tm = P.tile([64, NCH, 4, 64], name="r_vtm", dtype=BF16)
    WC = P.tile([64, 4, NCH], name="r_WC")
    xin = [P.tile([128, TT + 1], name=f"r_xin{i}") for i in range(2)]
    dd = P.tile([128, TT], name="r_dd")
    lo_w = P.tile([64, TT], name="r_low")
    lo_a = P.tile([64, TT], name="r_loa")
    lo_g = P.tile([128, TT], name="r_log")
    sq = P.tile([64, TT], name="r_sq")
    rs = P.tile([64, TT], name="r_rs")
    if L > 0:
        uxb = [P.tile([128, TT + 1], name=f"r_ux{i}") for i in range(2)]
        xvb = [P.tile([128, TT], name=f"r_xv{i}") for i in range(2)]
        vl = P.tile([32, TT], name="r_vl")
        vf = rs

    def mkch(i):
        d_ = {nm: P.tile([64, 256], name=f"rc_{nm}{i}", dtype=BF16) for nm in ["A0", "B0", "A1", "B1", "TT", "Bak", "Brb", "Brk"]}
        d_["X"] = d_["A1"]
        d_["U"] = d_["B1"]
        return d_
    CB = [mkch(0), mkch(1)]
    if psbase is None:
        psA, psB, psC, psD, psE, psF, psG, psH = [P.pbank(i) for i in range(8)]
    else:
        psA, psB, psC, psD, psE, psF, psG, psH = [P.pbank(psbase + j % 4) for j in range(8)]
    xk = 0

    def shifted(g, rows, ti, dst):
        nonlocal xk
        x = xin[xk % 2]
        xk += 1
        if ti == 0:
            P.memset(x[0:rows, 0:1], 0.0, writes=[x.sub("halo")])
            P.dma(x[0:rows, 1:TT + 1].sub("body"), zT[g, 0:rows, 0:TT])
        else:
            P.dma(x[0:rows, :], zT[g, 0:rows, ti * TT - 1:(ti + 1) * TT])
        P.tt(dd[0:rows, :], x[0:rows, 0:TT], x[0:rows, 1:TT + 1], ALU.subtract)
        P.stt(dst, dd[0:rows, :], cv[0:rows, CV["mu"] + g:CV["mu"] + g + 1], x[0:rows, 1:TT + 1], ALU.mult, ALU.add)

    for ti in range(nt):
        tsl = slice(ti * TT, (ti + 1) * TT)
        r4, k4, v4, kk4, ic4, lw4 = t0, t1, t2, t3, t4_, t5
        shifted(G_WLO, 64, ti, lo_w)
        act_sigmoid(P, lo_w, lo_w, scale=2.0)
        P.ts(lo_w, lo_w, 2.0, ALU.mult, -1.0, ALU.add)
        shifted(G_ALO, 64, ti, lo_a)
        shifted(G_GLO, 128, ti, lo_g)
        act_sigmoid(P, lo_g, lo_g)
        if L > 0:
            uv = uT.re("(c p) t -> p c t", p=128)
            for c in range(8):
                ux = uxb[c % 2]
                xv = xvb[c % 2]
                if ti == 0:
                    P.memset(ux[:, 0:1], 0.0, writes=[ux.sub("halo")])
                    P.dma(ux[:, 1:TT + 1].sub("body"), uv[:, c, 0:TT])
                else:
                    P.dma(ux, uv[:, c, ti * TT - 1:(ti + 1) * TT])
                P.tt(xv, ux[:, 0:TT], ux[:, 1:TT + 1], ALU.subtract)
                P.stt(xv, xv, cv[:, CV["vmu"] + c:CV["vmu"] + c + 1], ux[:, 1:TT + 1], ALU.mult, ALU.add)
                P.mm(psH[0:32, 0:TT], v_dn[:, c, :], xv, start=(c == 0), stop=(c == 7))
            P.copy(vl, psH[0:32, 0:TT], eng="act")
        for h in range(4):
            hs = slice(h * 64, (h + 1) * 64)
            shifted(G_R + h, 64, ti, r4[:, h, :].sub(h))
            shifted(G_K + h, 64, ti, k4[:, h, :].sub(h))
            shifted(G_V + h, 64, ti, v4[:, h, :].sub(h))
            P.mm(psA[0:64, 0:TT], w_up[:, hs], lo_w)
            act_sigmoid(P, lw4[:, h, :].sub(h), psA[0:64, 0:TT], negbias=ncv[:, h:h + 1])
            P.mm(psB[0:64, 0:TT], a_up[:, hs], lo_a)
            act_sigmoid(P, ic4[:, h, :].sub(h), psB[0:64, 0:TT], negbias=ncv[:, 4 + h:5 + h])
            P.mm(psC[0:64, 0:TT], g_up[:, hs], lo_g)
            P.copy(gate4[:, h, :].sub(h), psC[0:64, 0:TT], eng="act")
            if L == 0:
                P.dma(vfT[h * 64:(h + 1) * 64, tsl].sub(h), v4[:, h, :].sub(h), q="pool")
            else:
                P.dma(vf, vfT[h * 64:(h + 1) * 64, tsl])
                P.mm(psD[0:64, 0:TT], v_upt[:, hs], vl)
                act_sigmoid(P, sq, psD[0:64, 0:TT], negbias=ncv[:, 8 + h:9 + h])
                P.tt(vf, vf, v4[:, h, :].sub(h), ALU.subtract)
                P.tt(vf, vf, sq, ALU.mult)
                P.tt(v4[:, h, :].sub(h), v4[:, h, :].sub(h), vf, ALU.add)
            P.ts(kk4[:, h, :].sub(h), k4[:, h, :].sub(h), col("kk", h), ALU.mult)
            P.act(sq, kk4[:, h, :].sub(h), AF.Square)
            P.mm(psE[0:64, 0:TT], ones[0:64, 0:64], sq)
            rstd_from_ps(P, rs, psE[0:64, 0:TT], 1.0, 1e-12)
            P.tt(kk4[:, h, :].sub(h), kk4[:, h, :].sub(h), rs, ALU.mult)
            P.ts(sq, ic4[:, h, :].sub(h), col("ka", h), ALU.mult, oma[:, h:h + 1], ALU.add)
            P.tt(k4[:, h, :].sub(h), k4[:, h, :].sub(h), sq, ALU.mult)
            P.stt(sq, r4[:, h, :].sub(h), col("rk", h), k4[:, h, :].sub(h), ALU.mult, ALU.mult)
            P.mm(psF[0:64, 0:TT], ones[0:64, 0:64], sq)
            P.tt(bon[:, h, :].sub(h), psF[0:64, 0:TT], v4[:, h, :].sub(h), ALU.mult)
        P.ts(lw4, lw4, float(-np.exp(-0.5)), ALU.mult)
        for n in range(NCH):
            cs = slice(n * 64, (n + 1) * 64)
            for h in range(4):
                P.transpose(psG[0:64, h * 64:(h + 1) * 64], v4[:, h, cs], ident[0:64, 0:64])
            P.copy(v_tm[:, n, :, :].re("p h d -> p (h d)"), psG[0:64, 0:256], eng="act")
        P.tt(ic4, ic4, kk4, ALU.mult)
        cb_ = [lw4, v4]
        cur = 0
        for sh in (1, 2, 4, 8, 16, 32):
            src = cb_[cur].re("p h (n c) -> p (h n) c", c=64)
            dstt = cb_[1 - cur].re("p h (n c) -> p (h n) c", c=64)
            P.copy(dstt[:, :, 0:sh], src[:, :, 0:sh], eng="pool", writes=[cb_[1 - cur].sub("a")])
            P.tt(dstt[:, :, sh:64], src[:, :, sh:64], src[:, :, 0:64 - sh], ALU.add, writes=[cb_[1 - cur].sub("b")])
            cur = 1 - cur
        assert cur == 0
        cl = lw4
        cl3 = cl.re("p h (n c) -> p (h n) c", c=64)
        e = v4
        e3 = e.re("p h (n c) -> p (h n) c", c=64)
        P.act(e, cl, AF.Exp)
        P.tt(rt, r4, e, ALU.mult)
        P.memset(at.re("p h (n c) -> p (h n) c", c=64)[:, :, 0:1], 1.0, writes=[at.sub("a")])
        P.copy(at.re("p h (n c) -> p (h n) c", c=64)[:, :, 1:64], e3[:, :, 0:63], eng="pool", writes=[at.sub("b")])
        P.stt(at, at, -1.0, kk4, ALU.mult, ALU.mult)
        P.act(e, cl, AF.Exp, scale=-1.0)
        P.tt(bt, ic4, e, ALU.mult)
        P.tt(kt, k4, e, ALU.mult)
        cl4 = cl.re("p h (n c) -> p h n c", c=64)
        P.copy(WC, cl4[:, :, :, 63])
        P.tt(e.re("p h (n c) -> p h n c", c=64), V(WC.ap.unsqueeze(3).to_broadcast([64, 4, NCH, 64]), WC.key), cl4, ALU.subtract)
        P.act(e, e, AF.Exp)
        P.act(WC, WC, AF.Exp)
        P.tt(ic4, ic4, e, ALU.mult)
        P.tt(k4, k4, e, ALU.mult)
        for n in range(NCH):
            cs = slice(n * 64, (n + 1) * 64)
            for h in range(4):
                P.transpose(psG[0:64, h * 64:(h + 1) * 64], ic4[:, h, cs], ident[0:64, 0:64])
            P.copy(bh_tm[:, n, :, :].re("p h d -> p (h d)"), psG[0:64, 0:256], eng="act")
            for h in range(4):
                P.transpose(psH[0:64, h * 64:(h + 1) * 64], k4[:, h, cs], ident[0:64, 0:64])
            P.copy(kh_tm[:, n, :, :].re("p h d -> p (h d)"), psH[0:64, 0:256], eng="dve")
        def r_pre(n):
            cb = CB[n % 2]
            cs = slice(n * 64, (n + 1) * 64)
            for h in range(4):
                hs = slice(h * 64, (h + 1) * 64)
                P.mm(psA[0:64, hs], bt[:, h, cs], at[:, h, cs])
                P.mm(psB[0:64, hs], at[:, h, cs], bt[:, h, cs])
                P.mm(psC[0:64, hs], kt[:, h, cs], at[:, h, cs])
                P.mm(psD[0:64, hs], bt[:, h, cs], rt[:, h, cs])
            P.tt(cb["B0"], psA[0:64, 0:256], K["ms4"], ALU.mult)
            P.tt(cb["A0"], psB[0:64, 0:256], K["ms4T"], ALU.mult)
            P.tt(cb["Bak"], psC[0:64, 0:256], K["ms4"], ALU.mult)
            P.tt(cb["Brb"], psD[0:64, 0:256], K["mi4"], ALU.mult)
            for h in range(4):
                hs = slice(h * 64, (h + 1) * 64)
                P.mm(psE[0:64, hs], kt[:, h, cs], rt[:, h, cs])
            P.tt(cb["Brk"], psE[0:64, 0:256], K["mi4"], ALU.mult)
            return neumann_inv(P, C, cb["A0"], cb["B0"], cb, psA, psB, psC)

        def r_scan(n, TTm):
            cb = CB[n % 2]
            cs = slice(n * 64, (n + 1) * 64)
            for h in range(4):
                hs = slice(h * 64, (h + 1) * 64)
                P.mm(psD[0:64, hs], at[:, h, cs], STb[:, h, :], start=True, stop=False)
                P.mm(psD[0:64, hs], cb["Bak"][:, hs], v_tm[:, n, h, :], start=False, stop=True)
            P.copy(cb["X"], psD[0:64, 0:256], eng="act")
            for h in range(4):
                hs = slice(h * 64, (h + 1) * 64)
                P.mm(psE[0:64, hs], TTm[:, hs], cb["X"][:, hs])
            P.copy(cb["U"], psE[0:64, 0:256], eng="act")
            for h in range(4):
                hs = slice(h * 64, (h + 1) * 64)
                P.mm(psF[0:64, hs], STb[:, h, :], rt[:, h, cs], start=True, stop=False)
                P.mm(psF[0:64, hs], cb["U"][:, hs], cb["Brb"][:, hs], start=False, stop=False)
                P.mm(psF[0:64, hs], v_tm[:, n, h, :], cb["Brk"][:, hs], start=False, stop=True)
            P.copy(y4[:, :, cs], psF[0:64, 0:256].re("p (h c) -> p h c", h=4), eng="act")
            for h in range(4):
                hs = slice(h * 64, (h + 1) * 64)
                P.mm(psG[0:64, hs], bh_tm[:, n, h, :], cb["U"][:, hs], start=True, stop=False)
                P.mm(psG[0:64, hs], kh_tm[:, n, h, :], v_tm[:, n, h, :], start=False, stop=True)
            wcn = V(WC.ap[:, :, n].unsqueeze(2).to_broadcast([64, 4, 64]), WC.key)
            P.tt(ST, ST, wcn, ALU.mult)
            P.tt(ST, ST, psG[0:64, 0:256].re("p (h v) -> p h v", h=4), ALU.add)
            P.copy(STb, ST, eng="pool")

        tt_next = r_pre(0)
        for n in range(NCH):
            tt_cur = tt_next
            if n + 1 < NCH:
                tt_next = r_pre(n + 1)
            r_scan(n, tt_cur)
        for h in range(4):
            yh = y4[:, h, :]
            P.mm(psH[0:64, 0:TT], ones[0:64, 0:64], yh)
            P.stt(yh, psH[0:64, 0:TT], float(-1.0 / 64), yh, ALU.mult, ALU.add)
            P.act(sq, yh, AF.Square)
            P.mm(psH[0:64, 0:TT], ones[0:64, 0:64], sq)
            rstd_from_ps(P, rs, psH[0:64, 0:TT], 64.0, 64e-5)
            P.stt(yh, yh, col("lng", h), rs, ALU.mult, ALU.mult)
            P.stt(yh, yh, col("lnb", h), bon[:, h, :].sub(h), ALU.add, ALU.add)
            P.tt(sq, yh, gate4[:, h, :].sub(h), ALU.mult)
            P.dma(oT[h * 64:(h + 1) * 64, tsl].sub(h), sq, q="pool")
    if release:
        P.release(mk)


def phase_merge(P, C, NT, hT, oT, gT, wbr_d, wout_d, h1T, dyn=None):
    mk = P.mark()
    nt = NT // TT
    wstg = [P.tile([128, 4, 1024], name=f"f_wstg{i}") for i in range(2)]
    wbr = []
    for br in range(3):
        t = P.tile([128, 4, 1024], name=f"wbr{br}", dtype=BF16)
        P.dma(wstg[br % 2], wbr_d[br].re("(c p) n -> p c n", p=128))
        P.copy(t, wstg[br % 2], eng=("pool" if br % 2 == 0 else "act"))
        wbr.append(t)
    wout = P.tile([128, 8, 1024], name="wout", dtype=BF16)
    wov = wout_d.re("(c p) n -> p c n", p=128)
    for hh in range(2):
        P.dma(wstg[(hh + 1) % 2], wov[:, hh * 4:(hh + 1) * 4, :])
        P.copy(wout[:, hh * 4:(hh + 1) * 4, :].sub(hh), wstg[(hh + 1) % 2], eng=("act" if hh == 0 else "pool"))
    h = P.tile([128, 8, TT], name="f_h")
    o = P.tile([128, 12, TT], name="f_o")
    ob_ = P.tile([128, 12, TT], name="f_ob", dtype=BF16)
    mg = P.tile([128, 8, TT], name="f_mg32") if dyn is not None else None
    mgb = P.tile([128, 8, TT], name="f_mg", dtype=BF16)
    o2 = P.tile([128, 6, TT], name="f_o2") if dyn is not None else None
    g3 = [P.tile([128, 3, TT], name=f"f_g3{i}") for i in range(2)]
    tmp = [P.tile([128, TT], name=f"f_tmp{i}") for i in range(2)]
    tmp2 = [P.tile([128, TT], name=f"f_tmpb{i}") for i in range(2)]
    ps = [P.pbank(i) for i in range(8)]
    hv = hT.re("(c p) t -> p c t", p=128)
    ov = oT.re("(c p) t -> p c t", p=128)
    gv = gT.re("(b c p) t -> p b c t", b=3, p=128)
    h1v = h1T.re("(c p) t -> p c t", p=128)
    for ti in range(nt):
        tsl = slice(ti * TT, (ti + 1) * TT)
        if dyn is not None:
            m0 = C.cv[:, CV["m0"]:CV["m0"] + 1]
            m1 = C.cv[:, CV["m1"]:CV["m1"] + 1]
            tsl2 = slice(dyn + ti * TT, dyn + (ti + 1) * TT)
            P.dma(h, hv[:, :, tsl])
            P.dma(mg, hv[:, :, tsl2])
            P.ts(h, h, m0, ALU.mult)
            P.stt(h, mg, m1, h, ALU.mult, ALU.add)
            for part in range(2):
                cs_ = slice(part * 6, (part + 1) * 6)
                P.dma(o[:, cs_, :].sub(part), ov[:, cs_, tsl])
                P.dma(o2, ov[:, cs_, tsl2])
                P.ts(o[:, cs_, :].sub(part), o[:, cs_, :].sub(part), m0, ALU.mult)
                P.stt(o[:, cs_, :].sub(part), o2, m1, o[:, cs_, :].sub(part), ALU.mult, ALU.add)
        else:
            P.dma(h, hv[:, :, tsl])
            P.dma(o, ov[:, :, tsl])
        P.copy(ob_[:, 0:6, :].sub(0), o[:, 0:6, :], eng="dve")
        P.copy(ob_[:, 6:12, :].sub(1), o[:, 6:12, :], eng="act")
        for n in range(8):
            ns = slice(n * 128, (n + 1) * 128)
            g = g3[n % 2]
            P.dma(g, gv[:, :, n, tsl])
            for br in range(3):
                pp = ps[(n % 2) * 3 + br]
                for k in range(4):
                    P.mm(pp, wbr[br][:, k, ns], ob_[:, br * 4 + k, :], start=(k == 0), stop=(k == 3))
            tm_ = tmp[n % 2]
            P.tt(tm_, ps[(n % 2) * 3 + 0], g[:, 0, :], ALU.mult)
            P.tt(tmp2[n % 2], ps[(n % 2) * 3 + 1], g[:, 1, :], ALU.mult)
            P.tt(tm_, tm_, tmp2[n % 2], ALU.add, eng="pool")
            P.tt(tmp2[n % 2], ps[(n % 2) * 3 + 2], g[:, 2, :], ALU.mult)
            P.tt(mgb[:, n, :].sub(n), tm_, tmp2[n % 2], ALU.add, eng="pool")
        for n in range(8):
            ns = slice(n * 128, (n + 1) * 128)
            pp = ps[6 + n % 2]
            for k in range(8):
                P.mm(pp, wout[:, k, ns], mgb[:, k, :], start=(k == 0), stop=(k == 7))
            P.tt(h[:, n, :].sub(n), h[:, n, :].sub(n), pp, ALU.add)
        P.dma(h1v[:, :, tsl], h, q="pool")
    P.release(mk)


def phase_ffn(P, C, NT, h1T, h2T, gcol, experts, FF, router_d=None):
    mk = P.mark()
    cv = C.cv
    K = C.k
    ones = K["ones"]
    ident = K["ident"]
    nt = NT // TT
    NF = FF // 128
    CB = 512
    blocks = [(c0, min(CB, FF - c0)) for c0 in range(0, FF, CB)]
    h = P.tile([128, 8, TT], name="m_h")
    u = P.tile([128, 8, TT], name="m_u", dtype=BF16)
    rs = P.tile([128, TT], name="m_rs")
    hid_raw = P.tile([128, NF * TT // 2], name="m_hid")
    hid = V(hid_raw.ap.bitcast(BF16).rearrange("p (f t) -> p f t", f=NF), hid_raw.key)
    u32 = V(hid_raw.ap[:, 0:8 * TT].rearrange("p (c t) -> p c t", c=8), hid_raw.key)
    sg = [P.tile([128, TT], name=f"m_sg{i}") for i in range(2)]
    wgb = [P.tile([128, 8, CB], name=f"m_wg{i}") for i in range(2)]
    wub = [P.tile([128, 8, CB], name=f"m_wu{i}") for i in range(2)]
    wgc = [P.tile([128, 8, CB], name=f"m_wgc{i}", dtype=BF16) for i in range(2)]
    wuc = [P.tile([128, 8, CB], name=f"m_wuc{i}", dtype=BF16) for i in range(2)]
    WDP = 4
    wdb = [P.tile([128, WDP, 512], name=f"m_wd{i}") for i in range(2)]
    wdc = [P.tile([128, WDP, 512], name=f"m_wdc{i}", dtype=BF16) for i in range(2)]
    dlist = [(half, f0, min(WDP, NF - f0)) for half in range(2) for f0 in range(0, NF, WDP)]
    ps = [P.pbank(i) for i in range(8)]
    hv = h1T.re("(c p) t -> p c t", p=128)
    h2v = h2T.re("(c p) t -> p c t", p=128)
    ne = len(experts)
    if router_d is not None:
        sel8 = P.tile([8, 1024], name="c_sel8")
        P.dma(sel8, C.sel8_d)
        rt_w = P.tile([128, 8, 8], name="m_rw")
        P.dma(rt_w, router_d.re("(c p) e -> p c e", p=128))
        lg = P.tile([8, TT], name="m_lg")
        ltm = P.tile([128, 4, 8], name="m_ltm")
        l2 = P.tile([128, 4, 8], name="m_l2")
        eq1 = P.tile([128, 4, 8], name="m_eq1")
        eq2 = P.tile([128, 4, 8], name="m_eq2")
        m1 = P.tile([128, 4], name="m_m1")
        m2 = P.tile([128, 4], name="m_m2")
        w1 = P.tile([128, 4], name="m_w1")
        w2 = P.tile([128, 4], name="m_w2")
        gwf = P.tile([8, TT], name="m_gwf")
        gwe = [P.tile([128, TT], name=f"m_gwe{i}") for i in range(2)]
    wk = 0
    dk = 0
    for ti in range(nt):
        tsl = slice(ti * TT, (ti + 1) * TT)
        P.dma(h, hv[:, :, tsl])
        P.act(u32, h, AF.Square)
        for c in range(8):
            P.mm(ps[7], ones, u32[:, c, :], start=(c == 0), stop=(c == 7))
        rstd_from_ps(P, rs, ps[7], D, 1e-6)
        if router_d is not None:
            for c in range(8):
                P.stt(u32[:, c, :], h[:, c, :], cv[:, gcol + c:gcol + c + 1], rs, ALU.mult, ALU.mult)
            P.copy(u, u32, eng="act")
            for c in range(8):
                P.mm(ps[6][0:8, :], rt_w[:, c, :], u32[:, c, :], start=(c == 0), stop=(c == 7))
        else:
            for c in range(8):
                P.stt(u[:, c, :], h[:, c, :], cv[:, gcol + c:gcol + c + 1], rs, ALU.mult, ALU.mult)
        if router_d is not None:
            P.copy(lg, ps[6][0:8, :], eng="act")
            for j in range(4):
                P.transpose(ps[5][:, j * 8:(j + 1) * 8], lg[:, j * 128:(j + 1) * 128], ident[0:8, 0:8])
            P.copy(ltm.re("p j e -> p (j e)"), ps[5][:, 0:32])
            bc = lambda t: V(t.ap.unsqueeze(2).to_broadcast([128, 4, 8]), t.key)
            P.op("dve", lambda e: e.reduce_max(_ap(m1), _ap(ltm), AX.X), [ltm], [m1])
            P.tt(eq1, ltm, bc(m1), ALU.is_equal)
            P.stt(l2, eq1, -1e30, ltm, ALU.mult, ALU.add)
            P.op("dve", lambda e: e.reduce_max(_ap(m2), _ap(l2), AX.X), [l2], [m2])
            P.tt(eq2, l2, bc(m2), ALU.is_equal)
            P.tt(w2, m2, m1, ALU.subtract)
            P.act(w2, w2, AF.Exp)
            P.ts(w1, w2, 1.0, ALU.add)
            P.recip(w1, w1)
            P.tt(w2, w2, w1, ALU.mult)
            P.tt(eq1, eq1, bc(w1), ALU.mult)
            P.tt(eq2, eq2, bc(w2), ALU.mult)
            P.tt(eq1, eq1, eq2, ALU.add)
            for j in range(4):
                P.transpose(ps[5][0:8, j * 128:(j + 1) * 128], eq1[:, j, :], ident)
            P.copy(gwf, ps[5][0:8, :], eng="act")
        work = [(e_, bi) for e_ in range(ne) for bi in range(len(blocks))]

        def prefetch(e_, bi, slot):
            wg_d, wu_d, _ = experts[e_]
            c0_, wdt = blocks[bi]
            wgv = wg_d.re("(c p) f -> p c f", p=128)
            wuv = wu_d.re("(c p) f -> p c f", p=128)
            P.dma(wgb[slot][:, :, 0:wdt], wgv[:, :, c0_:c0_ + wdt])
            P.dma(wub[slot][:, :, 0:wdt], wuv[:, :, c0_:c0_ + wdt], q="act")
            P.copy(wgc[slot][:, :, 0:wdt], wgb[slot][:, :, 0:wdt], eng="dve")
            P.copy(wuc[slot][:, :, 0:wdt], wub[slot][:, :, 0:wdt], eng="act")
        def wd_prefetch(e_, di, slot):
            half, f0, nf = dlist[di]
            wdv = experts[e_][2].re("(f p) n -> p f n", p=128)
            P.dma(wdb[slot][:, 0:nf, :], wdv[:, f0:f0 + nf, half * 512:(half + 1) * 512])
            P.copy(wdc[slot][:, 0:nf, :], wdb[slot][:, 0:nf, :], eng=("dve" if di % 2 == 0 else "act"))

        prefetch(work[0][0], work[0][1], wk % 2)
        for wi, (e_, bi) in enumerate(work):
            slot = wk % 2
            wk += 1
            if bi == len(blocks) - 1:
                wd_prefetch(e_, 0, dk % 2)
            if wi + 1 < len(work):
                prefetch(work[wi + 1][0], work[wi + 1][1], wk % 2)
            c0_, wdt = blocks[bi]
            wg_t, wu_t = wgc[slot], wuc[slot]
            if router_d is not None and bi == 0:
                gw_e = gwe[e_ % 2]
                P.mm(ps[3], sel8[:, e_ * 128:(e_ + 1) * 128], gwf)
                P.copy(gw_e, ps[3], eng="act")
            for j in range(wdt // 128):
                f = c0_ // 128 + j
                pg = ps[4 + f % 2]
                pu = ps[6 + f % 2]
                for c in range(8):
                    P.mm(pg, wg_t[:, c, j * 128:(j + 1) * 128], u[:, c, :], start=(c == 0), stop=(c == 7))
                for c in range(8):
                    P.mm(pu, wu_t[:, c, j * 128:(j + 1) * 128], u[:, c, :], start=(c == 0), stop=(c == 7))
                s_ = sg[f % 2]
                P.act(s_, pg, AF.Silu)
                if router_d is not None:
                    P.tt(s_, s_, gw_e, ALU.mult, eng="pool")
                P.tt(hid[:, f, :].sub(f), s_, pu, ALU.mult)
            if bi == len(blocks) - 1:
                for di in range(len(dlist)):
                    dslot = dk % 2
                    dk += 1
                    if di + 1 < len(dlist):
                        wd_prefetch(e_, di + 1, dk % 2)
                    half, f0, nf = dlist[di]
                    for j in range(nf):
                        f = f0 + j
                        for n4 in range(4):
                            P.mm(ps[n4], wdc[dslot][:, j, n4 * 128:(n4 + 1) * 128], hid[:, f, :].sub(f), start=(f == 0), stop=(f == NF - 1))
                    if f0 + nf == NF:
                        for n4 in range(4):
                            n = half * 4 + n4
                            P.tt(h[:, n, :].sub(n), h[:, n, :].sub(n), ps[n4], ALU.add)
        P.dma(h2v[:, :, tsl], h, q="pool")
    P.release(mk)


def phase_ple(P, C, NT, h2T, pT, proj_d, pgate_d, gcol, h3T):
    mk = P.mark()
    cv = C.cv
    ones = C.k["ones"]
    nt = NT // TT
    pstg = [P.tile([128, 4, 1024], name=f"p_stg{i}") for i in range(2)]
    proj = P.tile([128, 2, 1024], name="p_proj", dtype=BF16)
    P.dma(pstg[0][:, 0:2, :], proj_d.re("(c p) n -> p c n", p=128))
    P.copy(proj, pstg[0][:, 0:2, :], eng="pool")
    pg = P.tile([128, 8, 1024], name="p_gate", dtype=BF16)
    pgv = pgate_d.re("(c p) n -> p c n", p=128)
    for hh in range(2):
        P.dma(pstg[(hh + 1) % 2], pgv[:, hh * 4:(hh + 1) * 4, :])
        P.copy(pg[:, hh * 4:(hh + 1) * 4, :].sub(hh), pstg[(hh + 1) % 2], eng=("act" if hh == 0 else "pool"))
    h = P.tile([128, 8, TT], name="p_h")
    hb_ = P.tile([128, 8, TT], name="p_hb", dtype=BF16)
    pt = P.tile([128, 2, TT], name="p_p")
    ptb = P.tile([128, 2, TT], name="p_pb", dtype=BF16)
    er = P.tile([128, 8, TT], name="p_er")
    ho = P.tile([128, 8, TT], name="p_ho")
    sq = P.tile([128, TT], name="p_sq")
    rs = P.tile([128, TT], name="p_rs")
    gp = [P.tile([128, TT], name=f"p_gp{i}") for i in range(2)]
    ps = [P.pbank(i) for i in range(8)]
    hv = h2T.re("(c p) t -> p c t", p=128)
    pv = pT.re("(c p) t -> p c t", p=128)
    h3v = h3T.re("(c p) t -> p c t", p=128)
    for ti in range(nt):
        tsl = slice(ti * TT, (ti + 1) * TT)
        P.dma(h, hv[:, :, tsl])
        P.dma(pt, pv[:, :, tsl])
        P.copy(ptb, pt, eng="dve")
        P.copy(hb_, h, eng="act")
        for n in range(8):
            ns = slice(n * 128, (n + 1) * 128)
            pp = ps[n % 2]
            P.mm(pp, proj[:, 0, ns], ptb[:, 0, :], start=True, stop=False)
            P.mm(pp, proj[:, 1, ns], ptb[:, 1, :], start=False, stop=True)
            P.copy(er[:, n, :].sub(n), pp, eng="act")
            P.act(sq, pp, AF.Square)
            P.mm(ps[2], ones, sq, start=(n == 0), stop=(n == 7))
        rstd_from_ps(P, rs, ps[2], D, 1e-6)
        for n in range(8):
            ns = slice(n * 128, (n + 1) * 128)
            pp = ps[3 + n % 2]
            for k in range(8):
                P.mm(pp, pg[:, k, ns], hb_[:, k, :], start=(k == 0), stop=(k == 7))
            g = gp[n % 2]
            P.act(g, pp, AF.Sigmoid)
            P.stt(er[:, n, :].sub(n), er[:, n, :].sub(n), cv[:, gcol + n:gcol + n + 1], rs, ALU.mult, ALU.mult)
            P.tt(g, g, er[:, n, :].sub(n), ALU.mult)
            P.tt(ho[:, n, :].sub(n), h[:, n, :], g, ALU.add)
        P.dma(h3v[:, :, tsl], ho, q="pool")
    P.release(mk)


def own(hg, width=64):
    return slice(hg * 4 * width, (hg + 1) * 4 * width)


def col4(v):
    return np.ascontiguousarray(v.reshape(4, 64).T)


def col2(v):
    return np.ascontiguousarray(v.reshape(2, 128).T)


def mixer_host_inputs(inp, L, b, hg):
    f = np.float32
    w_in = inp["w_in"][L]
    o = own(hg)
    cols = np.concatenate([
        np.arange(0, 512)[o], np.arange(512, 1024)[o], np.arange(1024, 1536)[o],
        np.arange(1536, 1600), np.arange(1600, 1664), np.arange(1664, 1792),
        np.arange(1792, 2048), np.arange(2048, 2176), np.arange(2176, 2208),
        np.arange(2208, 2720)[o], np.arange(2720, 3232)[o], np.arange(3232, 3744)[o],
        np.arange(3760, 4272)[o],
        np.arange(3744, 3752)[hg * 4:(hg + 1) * 4], np.arange(3752, 3760)[hg * 4:(hg + 1) * 4]])
    assert len(cols) == NZ
    d = {}
    d["w_in_m"] = np.ascontiguousarray(w_in[:, cols])
    cv = np.zeros((128, NCV), f)

    def put(nm, arr):
        arr = np.asarray(arr, f)
        cv[:arr.shape[0], CV[nm]:CV[nm] + arr.shape[1]] = arr
    put("nmg", inp["norm_mix_g"][L].reshape(8, 128).T)
    mu = inp["rwkv_mu"][L]
    mucols = np.zeros((128, 15), f)
    rcols = cols[:1024]
    for g in range(15):
        seg = mu[rcols[ZOFF[g]:ZOFF[g + 1]]]
        mucols[:len(seg), g] = seg
    put("mu", mucols)
    put("w0", col4(inp["rwkv_w0"][L][o]))
    put("a0", col4(inp["rwkv_a0"][L][o]))
    put("kk", col4(inp["rwkv_k_k"][L][o]))
    put("ka", col4(inp["rwkv_k_a"][L][o]))
    put("rk", col4(inp["rwkv_r_k"][L].reshape(512)[o]))
    put("lng", col4(inp["rwkv_ln_g"][L][o]))
    put("lnb", col4(inp["rwkv_ln_b"][L][o]))
    if L > 0:
        put("vmu", inp["vres_mu"][L - 1].reshape(8, 128).T)
        put("vb", col4(inp["vres_b"][L - 1][o]))
    put("qng", inp["mla_q_norm_g"][L].reshape(2, 128).T)
    put("kvng", inp["mla_kv_norm_g"][L].reshape(128, 1))
    put("qkq", inp["mla_qk_norm_q"][L].reshape(96, 1))
    put("qkk", inp["mla_qk_norm_k"][L].reshape(96, 1))
    invf = (1.0 / (10000.0 ** (np.arange(0, 32, 2, dtype=f) / f(32)))).astype(f)
    iv = np.zeros((96, 1), f)
    iv[64:80, 0] = invf
    iv[80:96, 0] = invf
    put("invf", iv)
    put("ropec", np.full((128, 1), -np.pi, f))
    cw = inp["gdn_conv_w"][L]
    convc = np.zeros((128, 48), f)
    for seg in range(3):
        cc = cw[:, seg * 512:(seg + 1) * 512][:, o]
        for hh in range(4):
            for j in range(4):
                convc[:64, (seg * 4 + hh) * 4 + j] = cc[j, hh * 64:(hh + 1) * 64]
    put("conv", convc)
    put("alog", inp["gdn_a_log"][L][hg * 4:(hg + 1) * 4].reshape(4, 1))
    put("dtb", inp["gdn_dt_bias"][L][hg * 4:(hg + 1) * 4].reshape(4, 1))
    put("gng", inp["gdn_norm_g"][L].reshape(64, 1))
    d["cv"] = cv
    d["w_up"] = np.ascontiguousarray(inp["rwkv_w_up"][L][:, o])
    d["a_up"] = np.ascontiguousarray(inp["rwkv_a_up"][L][:, o])
    d["g_up"] = np.ascontiguousarray(inp["rwkv_g_up"][L][:, o])
    if L > 0:
        d["v_down"] = np.ascontiguousarray(inp["vres_down"][L - 1])
        d["v_up"] = np.ascontiguousarray(inp["vres_up"][L - 1][:, o])
    d["w_uq"] = np.ascontiguousarray(inp["mla_w_uq"][L][:, hg * 384:(hg + 1) * 384])
    ukv = inp["mla_w_ukv"][L].reshape(128, 8, 128)[:, hg * 4:(hg + 1) * 4, :]
    d["w_uk"] = np.ascontiguousarray(ukv[:, :, :64].reshape(128, 256))
    d["w_uv"] = np.ascontiguousarray(ukv[:, :, 64:].reshape(128, 256))
    d["pos"] = np.ascontiguousarray(inp["positions"][b:b + 1].astype(np.int32))
    for k_, v_ in consts_np().items():
        d["c_" + k_] = v_
    return d
from concourse.bass_utils import run_bass_kernel_spmd

B_, S_, NCORE = 4, 4096, 8
M_CONSTS = ["ident", "ones", "att_mask", "ropeRT", "sel4", "id4", "neg4", "neg4T", "ms4", "ms4T", "mi4"]
F_CONSTS = ["ident", "ones"]
_PROG_CACHE = {}


def build_mixer(S, L):
    P = Prog()
    C = Ctx()
    names = []

    def din(name, shape, dt=F32):
        names.append(name)
        return P.dview(P.dram(name, shape, dt, kind="ExternalInput"))
    hT = din("hT", [1024, S])
    w_d = din("w_in_m", [1024, NZ])
    cv_d = din("cv", [128, NCV])
    pos_d = din("pos", [1, S], I32)
    w_uq, w_uk, w_uv = din("w_uq", [256, 384]), din("w_uk", [128, 256]), din("w_uv", [128, 256])
    w_up, a_up, g_up = din("w_up", [64, 256]), din("a_up", [64, 256]), din("g_up", [128, 256])
    zT = P.dview(P.dram("zT", [NG, 128, S], F32, kind="Internal"))
    oT = P.dview(P.dram("oT", [768, S], F32, kind="ExternalOutput"))
    uT = v_down = v_up = None
    if L == 0:
        vfT = P.dview(P.dram("vfT_out", [256, S], F32, kind="ExternalOutput"))
    else:
        vfT = din("vfT_in", [256, S])
        uT = P.dview(P.dram("uT", [1024, S], F32, kind="Internal"))
        v_down, v_up = din("v_down", [1024, 32]), din("v_up", [32, 256])
    C.k = load_consts(P, M_CONSTS)
    names.extend(["c_" + c for c in M_CONSTS])
    C.cv = P.tile([128, NCV], name="cv")
    P.dma(C.cv, cv_d)
    groups = [(int(ZOFF[g]), int(ZG[g])) for g in range(NG)]
    phase_proj(P, C, S, hT, w_d, NZ, groups, zT, CV["nmg"], uT=uT)
    phase_rwkv(P, C, S, L, zT, V(oT.ap[0:256, :], "oT_r"), w_up, a_up, g_up, vfT, uT, v_down, v_up)
    phase_mla(P, C, S, zT, pos_d, w_uq, w_uk, w_uv, V(oT.ap[256:512, :], "oT_m"))
    phase_gdn(P, C, S, zT, V(oT.ap[512:768, :], "oT_g"))
    P.finalize()
    return P.nc, names


def build_token(NT, L):
    P = Prog()
    C = Ctx()
    names = []

    def din(name, shape, dt=F32):
        names.append(name)
        return P.dview(P.dram(name, shape, dt, kind="ExternalInput"))
    hT = din("hT", [1024, NT])
    oT = din("oT_all", [1536, NT])
    pT = din("pT", [256, NT])
    cv_d = din("cv", [128, NCV])
    w_g = din("w_gate", [1024, 3072])
    wbr = [din(f"w_br{i}", [512, 1024]) for i in range(3)]
    wout = din("w_out", [1024, 1024])
    proj = din("ple_proj", [256, 1024])
    pgate = din("ple_gate", [1024, 1024])
    if L % 2 == 0:
        experts = [(din("ffn_wg", [1024, 2816]), din("ffn_wu", [1024, 2816]), din("ffn_wd", [2816, 1024]))]
        FF = 2816
        router = None
    else:
        wg_all = din("moe_wg", [8, 1024, 3584])
        wu_all = din("moe_wu", [8, 1024, 3584])
        wd_all = din("moe_wd", [8, 3584, 1024])
        experts = [(V(wg_all.ap[e], wg_all.key), V(wu_all.ap[e], wu_all.key), V(wd_all.ap[e], wd_all.key)) for e in range(8)]
        FF = 3584
        router = din("moe_router", [1024, 8])
    gT = P.dview(P.dram("gT", [3072, NT], F32, kind="Internal"))
    h1T = P.dview(P.dram("h1T", [1024, NT], F32, kind="Internal"))
    h2T = P.dview(P.dram("h2T", [1024, NT], F32, kind="Internal"))
    h3T = P.dview(P.dram("h3T", [1024, NT], F32, kind="ExternalOutput"))
    C.k = load_consts(P, F_CONSTS)
    names.extend(["c_" + c for c in F_CONSTS])
    C.sel8_d = din("c_sel8", [8, 1024])
    C.cv = P.tile([128, NCV], name="cv")
    P.dma(C.cv, cv_d)
    groups = [(g * 128, 128) for g in range(24)]
    phase_proj(P, C, NT, hT, w_g, 3072, groups, gT, CV["nmg"], func=AF.Sigmoid, nbuf=1, zflat=True)
    phase_merge(P, C, NT, hT, oT, gT, wbr, wout, h1T)
    phase_ffn(P, C, NT, h1T, h2T, CV["nfg"], experts, FF, router)
    phase_ple(P, C, NT, h2T, pT, proj, pgate, CV["png"], h3T)
    P.finalize()
    return P.nc, names


def token_host_inputs(inp, L, half=0):
    f = np.float32
    d = {}
    cv = np.zeros((128, NCV), f)
    cv[:, CV["m0"]] = 1.0 if half == 0 else 0.0
    cv[:, CV["m1"]] = 1.0 if half == 1 else 0.0
    cv[:, CV["nmg"]:CV["nmg"] + 8] = inp["norm_mix_g"][L].reshape(8, 128).T
    cv[:, CV["nfg"]:CV["nfg"] + 8] = inp["norm_ffn_g"][L].reshape(8, 128).T
    cv[:, CV["png"]:CV["png"] + 8] = inp["ple_norm_g"][L].reshape(8, 128).T
    d["cv"] = cv
    d["w_gate"] = np.ascontiguousarray(inp["w_in"][L][:, 4272:7344])
    d["w_br0"] = inp["w_br_rwkv"][L]
    d["w_br1"] = inp["w_br_mla"][L]
    d["w_br2"] = inp["w_br_gdn"][L]
    d["w_out"] = inp["w_out"][L]
    d["ple_proj"] = inp["ple_proj"][L]
    d["ple_gate"] = inp["ple_gate"][L]
    if L % 2 == 0:
        d["ffn_wg"], d["ffn_wu"], d["ffn_wd"] = inp["ffn_wg"][L // 2], inp["ffn_wu"][L // 2], inp["ffn_wd"][L // 2]
    else:
        d["moe_wg"], d["moe_wu"], d["moe_wd"] = inp["moe_wg"][L // 2], inp["moe_wu"][L // 2], inp["moe_wd"][L // 2]
        d["moe_router"] = inp["moe_router"][L // 2]
    cs = consts_np()
    for c in F_CONSTS + ["sel8"]:
        d["c_" + c] = cs[c]
    return d


def kernel(**inputs):
    inp = {k: np.asarray(v) for k, v in inputs.items()}
    x = inp["x"].astype(np.float32)
    Bn, S, Dm = x.shape
    NT = S // 2
    hT = [np.ascontiguousarray(x[b].T) for b in range(Bn)]
    vf = [None] * NCORE
    for L in range(2):
        key = ("M", S, L)
        if key not in _PROG_CACHE:
            _PROG_CACHE[key] = build_mixer(S, L)
        nc, names = _PROG_CACHE[key]
        in_maps = []
        for core in range(NCORE):
            b, hg = core // 2, core % 2
            d = mixer_host_inputs(inp, L, b, hg)
            d["hT"] = hT[b]
            if L > 0:
                d["vfT_in"] = vf[core]
            in_maps.append({n: np.ascontiguousarray(d[n]) for n in names})
        res = run_bass_kernel_spmd(nc, in_maps, core_ids=list(range(NCORE)))
        oTs = [r["oT"] for r in res.results]
        if L == 0:
            vf = [r["vfT_out"] for r in res.results]
        key = ("F", NT, L)
        if key not in _PROG_CACHE:
            _PROG_CACHE[key] = build_token(NT, L)
        nc, names = _PROG_CACHE[key]
        th = token_host_inputs(inp, L)
        in_maps = []
        for core in range(NCORE):
            b, half = core // 2, core % 2
            tsl = slice(half * NT, (half + 1) * NT)
            d = dict(th)
            d["hT"] = hT[b][:, tsl]
            o0, o1 = oTs[2 * b], oTs[2 * b + 1]
            d["oT_all"] = np.concatenate([o0[0:256, tsl], o1[0:256, tsl], o0[256:512, tsl], o1[256:512, tsl],
                                          o0[512:768, tsl], o1[512:768, tsl]], axis=0)
            d["pT"] = inp["p"][L, b, tsl, :].T
            in_maps.append({n: np.ascontiguousarray(d[n]) for n in names})
        res = run_bass_kernel_spmd(nc, in_maps, core_ids=list(range(NCORE)))
        for b in range(Bn):
            hT[b] = np.concatenate([res.results[2 * b]["h3T"], res.results[2 * b + 1]["h3T"]], axis=1)
    out = np.stack([hT[b].T for b in range(Bn)], axis=0)
    return np.ascontiguousarray(out.astype(np.float32))


ALL_M_KEYS = ["w_in_m", "cv", "w_uq", "w_uk", "w_uv", "w_up", "a_up", "g_up"]


def build_fused(S):
    NTH = S // 2
    P = Prog()
    C = Ctx()
    names = []

    def din(name, shape, dt=F32):
        names.append(name)
        return P.dview(P.dram(name, shape, dt, kind="ExternalInput"))
    hT0 = din("hT0", [1024, S])
    pos_d = din("pos", [1, S], I32)
    pT = [din("pT0", [256, S]), din("pT1", [256, NTH])]
    C.sel8_d = din("c_sel8", [8, 1024])
    mi = {}
    for L in range(2):
        for hg in range(2):
            pre = f"m{L}{hg}_"
            d = {"w_in_m": din(pre + "w_in_m", [1024, NZ]), "cv": din(pre + "cv", [128, NCV]),
                 "w_uq": din(pre + "w_uq", [256, 384]), "w_uk": din(pre + "w_uk", [128, 256]), "w_uv": din(pre + "w_uv", [128, 256]),
                 "w_up": din(pre + "w_up", [64, 256]), "a_up": din(pre + "a_up", [64, 256]), "g_up": din(pre + "g_up", [128, 256])}
            if L > 0:
                d["v_down"] = din(pre + "v_down", [1024, 32])
                d["v_up"] = din(pre + "v_up", [32, 256])
            mi[(L, hg)] = d
    ti_ = {}
    for L in range(2):
        pre = f"t{L}_"
        d = {"cv": din(pre + "cv", [128, NCV]), "w_gate": din(pre + "w_gate", [1024, 3072]),
             "wbr": [din(pre + f"w_br{i}", [512, 1024]) for i in range(3)], "w_out": din(pre + "w_out", [1024, 1024]),
             "ple_proj": din(pre + "ple_proj", [256, 1024]), "ple_gate": din(pre + "ple_gate", [1024, 1024])}
        if L % 2 == 0:
            d["experts"] = [(din(pre + "ffn_wg", [1024, 2816]), din(pre + "ffn_wu", [1024, 2816]), din(pre + "ffn_wd", [2816, 1024]))]
            d["FF"] = 2816
            d["router"] = None
        else:
            wg_all = din(pre + "moe_wg", [8, 1024, 3584])
            wu_all = din(pre + "moe_wu", [8, 1024, 3584])
            wd_all = din(pre + "moe_wd", [8, 3584, 1024])
            d["experts"] = [(V(wg_all.ap[e], wg_all.key), V(wu_all.ap[e], wu_all.key), V(wd_all.ap[e], wd_all.key)) for e in range(8)]
            d["FF"] = 3584
            d["router"] = din(pre + "moe_router", [1024, 8])
        ti_[L] = d
    zT = P.dview(P.dram("zT", [NG, 128, S], F32, kind="Internal"))
    uT = P.dview(P.dram("uT", [1024, S], F32, kind="Internal"))
    oTa = P.dview(P.dram("oT_all", [1536, S], F32, kind="Internal"))
    vfT = P.dview(P.dram("vfT", [512, S], F32, kind="Internal"))
    gT = P.dview(P.dram("gT", [3072, S], F32, kind="Internal"))
    h1T = P.dview(P.dram("h1T", [1024, S], F32, kind="Internal"))
    h2T = P.dview(P.dram("h2T", [1024, S], F32, kind="Internal"))
    hT1 = P.dview(P.dram("hT1", [1024, S], F32, kind="Internal"))
    h3T = P.dview(P.dram("h3T", [1024, NTH], F32, kind="ExternalOutput"))
    C.k = load_consts(P, M_CONSTS)
    names.extend(["c_" + c for c in M_CONSTS])
    C.cv = P.tile([128, NCV], name="cv")
    groups = [(int(ZOFF[g]), int(ZG[g])) for g in range(NG)]
    ggroups = [(g * 128, 128) for g in range(24)]
    hin = hT0
    for L in range(2):
        for hg in range(2):
            d = mi[(L, hg)]
            P.dma(C.cv, d["cv"])
            phase_proj(P, C, S, hin, d["w_in_m"], NZ, groups, zT, CV["nmg"], uT=(uT if L > 0 else None))
            sub = lambda br: V(oTa.ap[br * 512 + hg * 256:br * 512 + (hg + 1) * 256, :], f"oT_{br}_{hg}")
            vfv = V(vfT.ap[hg * 256:(hg + 1) * 256, :], f"vfT_{hg}")
            mk_ = P.mark()
            o_r, o_g = sub(0), sub(2)
            P.run_interleaved([
                lambda: phase_rwkv(P, C, S, L, zT, o_r, d["w_up"], d["a_up"], d["g_up"], vfv,
                                   (uT if L > 0 else None), d.get("v_down"), d.get("v_up"), ttl=256, psbase=0, release=False),
                lambda: phase_gdn(P, C, S, zT, o_g, ttl=256, psbase=4, release=False)])
            P.release(mk_)
            phase_mla(P, C, S, zT, pos_d, d["w_uq"], d["w_uk"], d["w_uv"], sub(1))
        t = ti_[L]
        P.dma(C.cv, t["cv"])
        if L == 0:
            NT, dyn, hout = S, None, hT1
        else:
            NT, dyn, hout = NTH, NTH, h3T
        gv = V(gT.ap[:, 0:NT], gT.key)
        h1v = V(h1T.ap[:, 0:NT], h1T.key)
        h2v = V(h2T.ap[:, 0:NT], h2T.key)
        phase_proj(P, C, NT, hin, t["w_gate"], 3072, ggroups, gv, CV["nmg"], func=AF.Sigmoid, nbuf=1, zflat=True, dyn=dyn)
        phase_merge(P, C, NT, hin, oTa, gv, t["wbr"], t["w_out"], h1v, dyn=dyn)
        phase_ffn(P, C, NT, h1v, h2v, CV["nfg"], t["experts"], t["FF"], t["router"])
        phase_ple(P, C, NT, h2v, pT[L], t["ple_proj"], t["ple_gate"], CV["png"], hout)
        hin = hT1
    P.finalize()
    return P.nc, names


def kernel_unfused(**inputs):
    return _kernel_unfused(**inputs)


_kernel_unfused = kernel


def kernel(**inputs):
    inp = {k: np.asarray(v) for k, v in inputs.items()}
    x = inp["x"].astype(np.float32)
    Bn, S, Dm = x.shape
    NTH = S // 2
    key = ("FUSED", S)
    if key not in _PROG_CACHE:
        _PROG_CACHE[key] = build_fused(S)
    nc, names = _PROG_CACHE[key]
    cs = consts_np()
    shared = {"c_" + k: v for k, v in cs.items()}
    tok = []
    for L in range(2):
        th = token_host_inputs(inp, L)
        tok.append({f"t{L}_" + k: v for k, v in th.items() if not k.startswith("c_")})
    in_maps = []
    for core in range(NCORE):
        b, half = core // 2, core % 2
        d = dict(shared)
        d["hT0"] = x[b].T
        d["pos"] = inp["positions"][b:b + 1].astype(np.int32)
        d["pT0"] = inp["p"][0, b].T
        d["pT1"] = inp["p"][1, b, half * NTH:(half + 1) * NTH, :].T
        for L in range(2):
            for hg in range(2):
                md = mixer_host_inputs(inp, L, b, hg)
                for k, v in md.items():
                    if not k.startswith("c_") and k != "pos":
                        d[f"m{L}{hg}_" + k] = v
            d.update(tok[L])
            cvt = tok[L][f"t{L}_cv"].copy()
            cvt[:, CV["m0"]] = 1.0 if half == 0 else 0.0
            cvt[:, CV["m1"]] = 1.0 if half == 1 else 0.0
            d[f"t{L}_cv"] = cvt
        in_maps.append({n: np.ascontiguousarray(d[n]) for n in names})
    res = run_bass_kernel_spmd(nc, in_maps, core_ids=list(range(NCORE)))
    out = np.empty((Bn, S, Dm), np.float32)
    for core in range(NCORE):
        b, half = core // 2, core % 2
        out[b, half * NTH:(half + 1) * NTH, :] = res.results[core]["h3T"].T
    return out
```
